# Optimizing a Trainium2 kernel written in Bass

```python
import math
import jax
import jax.numpy as jnp
from jax import lax
import numpy as np

D_MODEL = 1024
BATCH = 16
SEQ = 2048
DEPTH = 4

GROUP_HEADS = 4
HEAD_DIM = 64
GROUP_WIDTH = GROUP_HEADS * HEAD_DIM
N_GROUPS = 4
D_MIX = N_GROUPS * GROUP_WIDTH
IN_SIZES = (GROUP_WIDTH, GROUP_WIDTH, GROUP_WIDTH, GROUP_WIDTH, GROUP_HEADS, GROUP_HEADS, GROUP_WIDTH, GROUP_WIDTH, GROUP_WIDTH, GROUP_WIDTH, GROUP_WIDTH, GROUP_WIDTH, GROUP_WIDTH, GROUP_WIDTH, GROUP_WIDTH, GROUP_WIDTH)
N_IN = sum(IN_SIZES)
MLSTM_F_OFFSET = 4 * GROUP_WIDTH + GROUP_HEADS
MLSTM_CHUNK = 64
HGRN_CHUNK = 64
SCONV_WIDTH = 3
MOBA_BLOCK = 256
MOBA_TOPK = 3
MOBA_QCHUNK = 32
REL_BUCKETS = 32
REL_MAX_DIST = 128
N_MEM = 256
CROSS_HEADS = 4
CROSS_HEAD_DIM = 128
CROSS_WIDTH = CROSS_HEADS * CROSS_HEAD_DIM
D_FF = 2816
FFN_CONV_WIDTH = 3
RMS_EPS = 1e-6
NEG_BIG = -1e30

kernel_name = "hybrid_parallel_group_trunk"


def rmsnorm(x, gain):
    x32 = x.astype(jnp.float32)
    y = x32 * lax.rsqrt(jnp.mean(x32 * x32, axis=-1, keepdims=True) + RMS_EPS)
    return (y * gain.astype(jnp.float32)).astype(x.dtype)


def head_rmsnorm(h, gain):
    b, nh, s, d = h.shape
    h32 = h.astype(jnp.float32).transpose(0, 2, 1, 3)
    h32 = h32 * lax.rsqrt(jnp.mean(h32 * h32, axis=-1, keepdims=True) + RMS_EPS)
    return h32.reshape(b, s, nh * d) * gain.astype(jnp.float32)


def causal_dwconv(x, w):
    width, ch = w.shape
    return lax.conv_general_dilated(x, w.astype(x.dtype)[:, None, :], window_strides=(1,), padding=[(width - 1, 0)], dimension_numbers=('NWC', 'WIO', 'NWC'), feature_group_count=ch)


def to_heads(a, n_heads):
    b, s, _ = a.shape
    return a.reshape(b, s, n_heads, -1).transpose(0, 2, 1, 3)


def _chunks(a, size):
    b, h, s = a.shape[:3]
    return jnp.moveaxis(a.reshape(b, h, s // size, size, *a.shape[3:]), 2, 0)


def _unchunk(a):
    nc, b, h, l, d = a.shape
    return jnp.moveaxis(a, 0, 2).reshape(b, h, nc * l, d)


def rel_bucket(dist):
    n = jnp.maximum(dist, 0)
    exact = REL_BUCKETS // 2
    nf = jnp.maximum(n, 1).astype(jnp.float32)
    large = exact + (jnp.log(nf / exact) / math.log(REL_MAX_DIST / exact) * (REL_BUCKETS - exact)).astype(jnp.int32)
    large = jnp.minimum(large, REL_BUCKETS - 1)
    return jnp.where(n < exact, n, large)


def mlstm_chunkwise(q, k, v, i_pre, f_pre):
    b, nh, s, d = q.shape
    f32 = jnp.float32
    q = q.astype(f32)
    k = k.astype(f32) * (d ** -0.5)
    v = v.astype(f32)
    log_i = i_pre.astype(f32)
    log_f = jax.nn.log_sigmoid(f_pre.astype(f32))
    causal = jnp.tril(jnp.ones((MLSTM_CHUNK, MLSTM_CHUNK), dtype=bool))

    def step(carry, xs):
        c_mat, n_vec, m_prev = carry
        qc, kc, vc, lic, lfc = xs
        g = jnp.cumsum(lfc, axis=-1)
        dlog = jnp.where(causal, g[..., :, None] - g[..., None, :] + lic[..., None, :], NEG_BIG)
        a = g + m_prev[..., None]
        m_t = jnp.maximum(a, jnp.max(dlog, axis=-1))
        w = jnp.exp(dlog - m_t[..., None]) * jnp.einsum('bhtd,bhsd->bhts', qc, kc)
        inter = jnp.exp(a - m_t)
        num = jnp.einsum('bhts,bhsd->bhtd', w, vc) + inter[..., None] * jnp.einsum('bhvk,bhtk->bhtv', c_mat, qc)
        den = jnp.sum(w, axis=-1) + inter * jnp.einsum('bhk,bhtk->bht', n_vec, qc)
        h = num / jnp.maximum(jnp.abs(den), jnp.exp(-m_t))[..., None]
        g_last = g[..., -1]
        u = g_last[..., None] - g + lic
        m_new = jnp.maximum(g_last + m_prev, jnp.max(u, axis=-1))
        ws = jnp.exp(u - m_new[..., None])
        decay = jnp.exp(g_last + m_prev - m_new)
        c_new = decay[..., None, None] * c_mat + jnp.einsum('bhs,bhsv,bhsk->bhvk', ws, vc, kc)
        n_new = decay[..., None] * n_vec + jnp.einsum('bhs,bhsk->bhk', ws, kc)
        return (c_new, n_new, m_new), h

    init = (jnp.zeros((b, nh, d, d), f32), jnp.zeros((b, nh, d), f32), jnp.zeros((b, nh), f32))
    _, hs = lax.scan(step, init, tuple(_chunks(a, MLSTM_CHUNK) for a in (q, k, v, log_i, log_f)))
    return _unchunk(hs)


def hgrn2_chunkwise(q, k, log_f, v):
    b, nh, s, dk = q.shape
    dv = v.shape[-1]
    causal = jnp.tril(jnp.ones((HGRN_CHUNK, HGRN_CHUNK), dtype=bool))[:, :, None]

    def step(state, xs):
        qc, kc, lfc, vc = xs
        g = jnp.cumsum(lfc, axis=2)
        rel = jnp.exp(jnp.where(causal, g[:, :, :, None, :] - g[:, :, None, :, :], NEG_BIG))
        attn = jnp.einsum('bhtk,bhtsk,bhsk->bhts', qc, rel, kc)
        out = jnp.einsum('bhts,bhsv->bhtv', attn, vc) + jnp.einsum('bhtk,bhkv->bhtv', qc * jnp.exp(g), state)
        g_last = g[:, :, -1:, :]
        new_state = jnp.exp(g_last[:, :, 0, :])[..., None] * state + jnp.einsum('bhsk,bhsv->bhkv', kc * jnp.exp(g_last - g), vc)
        return new_state, out

    init = jnp.zeros((b, nh, dk, dv), jnp.float32)
    _, outs = lax.scan(step, init, tuple(_chunks(a, HGRN_CHUNK) for a in (q, k, log_f, v)))
    return _unchunk(outs)


def moba_attention(q, k, v, rel_bias):
    b, nh, s, d = q.shape
    f32 = jnp.float32
    nb = -(-s // MOBA_BLOCK)
    s_pad = nb * MOBA_BLOCK
    padw = ((0, 0), (0, 0), (0, s_pad - s), (0, 0))
    q, k, v = jnp.pad(q, padw), jnp.pad(k, padw), jnp.pad(v, padw)
    kb = k.reshape(b, nh, nb, MOBA_BLOCK, d)
    vb = v.reshape(b, nh, nb, MOBA_BLOCK, d)
    n_sel = min(MOBA_TOPK, nb - 1)
    scale = d ** -0.5
    bias_t = rel_bias.astype(f32).T
    blk_of = jnp.arange(s_pad, dtype=jnp.int32) // MOBA_BLOCK
    if n_sel > 0:
        kmean = jnp.mean(kb.astype(f32), axis=3)
        gate = jnp.einsum('bhsd,bhnd->bhsn', q.astype(f32), kmean)
        past = jnp.arange(nb, dtype=jnp.int32)[None, :] < blk_of[:, None]
        gate = jnp.where(past, gate, NEG_BIG)
        _, sel = lax.top_k(gate, n_sel)
        sel = sel.astype(jnp.int32)
        sel_valid = sel < blk_of[:, None]
    b_ix = jnp.arange(b)[:, None, None, None]
    h_ix = jnp.arange(nh)[None, :, None, None]
    key_off = jnp.arange(MOBA_BLOCK, dtype=jnp.int32)

    def attend_chunk(c):
        start = c * MOBA_QCHUNK
        qc = lax.dynamic_slice_in_dim(q, start, MOBA_QCHUNK, axis=2)
        t = start + jnp.arange(MOBA_QCHUNK, dtype=jnp.int32)
        j = start // MOBA_BLOCK
        k_own = lax.dynamic_index_in_dim(kb, j, axis=2, keepdims=False)
        v_own = lax.dynamic_index_in_dim(vb, j, axis=2, keepdims=False)
        dist_own = t[:, None] - (j * MOBA_BLOCK + key_off)[None, :]
        s_own = jnp.einsum('bhqd,bhkd->bhqk', qc, k_own).astype(f32) * scale + bias_t[:, rel_bucket(dist_own)][None]
        s_own = jnp.where(dist_own >= 0, s_own, NEG_BIG)
        if n_sel == 0:
            p = jax.nn.softmax(s_own, axis=-1)
            out = jnp.einsum('bhqk,bhkd->bhqd', p, v_own.astype(f32))
        else:
            sel_c = lax.dynamic_slice_in_dim(sel, start, MOBA_QCHUNK, axis=2)
            valid_c = lax.dynamic_slice_in_dim(sel_valid, start, MOBA_QCHUNK, axis=2)
            k_sel = kb[b_ix, h_ix, sel_c]
            v_sel = vb[b_ix, h_ix, sel_c]
            dist = t[None, None, :, None, None] - (sel_c[..., None] * MOBA_BLOCK + key_off)
            bias = bias_t[h_ix[..., None], rel_bucket(dist)]
            s_past = jnp.einsum('bhqd,bhqnkd->bhqnk', qc, k_sel).astype(f32) * scale + bias
            s_past = jnp.where(valid_c[..., None], s_past, NEG_BIG)
            n_past = n_sel * MOBA_BLOCK
            p = jax.nn.softmax(jnp.concatenate([s_past.reshape(b, nh, MOBA_QCHUNK, n_past), s_own], axis=-1), axis=-1)
            p_past = p[..., :n_past].reshape(b, nh, MOBA_QCHUNK, n_sel, MOBA_BLOCK)
            out = jnp.einsum('bhqnk,bhqnkd->bhqd', p_past, v_sel.astype(f32)) + jnp.einsum('bhqk,bhkd->bhqd', p[..., n_past:], v_own.astype(f32))
        return out.astype(q.dtype)

    outs = lax.map(attend_chunk, jnp.arange(s_pad // MOBA_QCHUNK, dtype=jnp.int32))
    return _unchunk(outs)[:, :, :s]


def token_mixer(xn, w_in, b_in, mlstm_norm, sconv_w, rel_bias, hgrn_lb, hgrn_norm, w_out):
    b, s, _ = xn.shape
    f32 = jnp.float32
    dt = xn.dtype
    proj = xn @ w_in + b_in
    splits = np.cumsum(IN_SIZES)[:-1].tolist()
    (m_q, m_k, m_v, m_o, m_i, m_f, c_b, c_c, c_h, a_q, a_k, a_v, h_q, h_f, h_i, h_g) = jnp.split(proj, splits, axis=-1)
    h_m = mlstm_chunkwise(to_heads(m_q, GROUP_HEADS), to_heads(m_k, GROUP_HEADS), to_heads(m_v, GROUP_HEADS), m_i.transpose(0, 2, 1), m_f.transpose(0, 2, 1))
    y_m = head_rmsnorm(h_m, mlstm_norm) * jax.nn.sigmoid(m_o.astype(f32))
    y_c = c_b * causal_dwconv(c_c * c_h, sconv_w)
    y_a = moba_attention(to_heads(a_q, GROUP_HEADS), to_heads(a_k, GROUP_HEADS), to_heads(a_v, GROUP_HEADS), rel_bias)
    y_a = y_a.transpose(0, 2, 1, 3).reshape(b, s, GROUP_WIDTH)
    lb = hgrn_lb.astype(f32).reshape(1, GROUP_HEADS, 1, HEAD_DIM)
    z = to_heads(h_f, GROUP_HEADS).astype(f32)
    f_gate = lb + (1.0 - lb) * jax.nn.sigmoid(z)
    log_f = jnp.log(f_gate)
    k_h = 1.0 - f_gate
    q_h = jax.nn.silu(to_heads(h_q, GROUP_HEADS).astype(f32))
    o_h = hgrn2_chunkwise(q_h, k_h, log_f, to_heads(h_i, GROUP_HEADS).astype(f32))
    y_h = head_rmsnorm(o_h, hgrn_norm) * jax.nn.silu(h_g.astype(f32))
    y = jnp.concatenate([y_m.astype(dt), y_c.astype(dt), y_a.astype(dt), y_h.astype(dt)], axis=-1)
    return y @ w_out


def memory_cross_attention(xn, memn, wq, wk, wv, wo):
    b, s, _ = xn.shape
    m = memn.shape[1]
    q = (xn @ wq).reshape(b, s, CROSS_HEADS, CROSS_HEAD_DIM)
    k = (memn @ wk).reshape(b, m, CROSS_HEADS, CROSS_HEAD_DIM)
    v = (memn @ wv).reshape(b, m, CROSS_HEADS, CROSS_HEAD_DIM)
    logits = jnp.einsum('bshd,bmhd->bhsm', q, k).astype(jnp.float32) * (CROSS_HEAD_DIM ** -0.5)
    p = jax.nn.softmax(logits, axis=-1)
    o = jnp.einsum('bhsm,bmhd->bshd', p, v.astype(jnp.float32)).astype(xn.dtype).reshape(b, s, CROSS_WIDTH)
    return o @ wo


def conv_glu_ffn(xn, w_up, conv_w, conv_b, w_down):
    gu = xn @ w_up
    gate, up = jnp.split(gu, 2, axis=-1)
    gate = causal_dwconv(gate, conv_w) + conv_b.astype(gate.dtype)
    return (jax.nn.silu(gate) * up) @ w_down


def setup_inputs(seed: int = 0) -> dict:
    key = jax.random.key(seed)
    ks = jax.random.split(key, 26)
    f32 = jnp.float32

    def nrm(k, shape, scale):
        return scale * jax.random.normal(k, shape, f32)

    def gain(k, shape):
        return 1.0 + 0.05 * jax.random.normal(k, shape, f32)

    b_in = nrm(ks[3], (DEPTH, N_IN), 0.01)
    b_in = b_in.at[:, MLSTM_F_OFFSET:MLSTM_F_OFFSET + GROUP_HEADS].add(jnp.linspace(3.0, 6.0, GROUP_HEADS))
    return {
        'x': nrm(ks[0], (BATCH, SEQ, D_MODEL), 1.0),
        'mem': nrm(ks[1], (BATCH, N_MEM, D_MODEL), 1.0),
        'w_in': nrm(ks[2], (DEPTH, D_MODEL, N_IN), D_MODEL ** -0.5),
        'b_in': b_in,
        'mlstm_norm': gain(ks[4], (DEPTH, GROUP_WIDTH)),
        'sconv_w': nrm(ks[5], (DEPTH, SCONV_WIDTH, GROUP_WIDTH), SCONV_WIDTH ** -0.5),
        'rel_bias': nrm(ks[6], (REL_BUCKETS, GROUP_HEADS), 0.5),
        'hgrn_lb_logits': nrm(ks[7], (DEPTH, GROUP_WIDTH), 0.5),
        'hgrn_norm': gain(ks[8], (DEPTH, GROUP_WIDTH)),
        'w_mix_out': nrm(ks[9], (DEPTH, D_MIX, D_MODEL), D_MIX ** -0.5),
        'norm_mix_pre': gain(ks[10], (DEPTH, D_MODEL)),
        'norm_mix_post': gain(ks[11], (DEPTH, D_MODEL)),
        'mem_norm': gain(ks[12], (DEPTH, D_MODEL)),
        'w_cq': nrm(ks[13], (DEPTH, D_MODEL, CROSS_WIDTH), D_MODEL ** -0.5),
        'w_ck': nrm(ks[14], (DEPTH, D_MODEL, CROSS_WIDTH), D_MODEL ** -0.5),
        'w_cv': nrm(ks[15], (DEPTH, D_MODEL, CROSS_WIDTH), D_MODEL ** -0.5),
        'w_co': nrm(ks[16], (DEPTH, CROSS_WIDTH, D_MODEL), CROSS_WIDTH ** -0.5),
        'norm_cross_pre': gain(ks[17], (DEPTH, D_MODEL)),
        'norm_cross_post': gain(ks[18], (DEPTH, D_MODEL)),
        'w_ffn_in': nrm(ks[19], (DEPTH, D_MODEL, 2 * D_FF), D_MODEL ** -0.5),
        'ffn_conv_w': nrm(ks[20], (DEPTH, FFN_CONV_WIDTH, D_FF), FFN_CONV_WIDTH ** -0.5),
        'ffn_conv_b': nrm(ks[21], (DEPTH, D_FF), 0.01),
        'w_ffn_out': nrm(ks[22], (DEPTH, D_FF, D_MODEL), D_FF ** -0.5),
        'norm_ffn_pre': gain(ks[23], (DEPTH, D_MODEL)),
        'norm_ffn_post': gain(ks[24], (DEPTH, D_MODEL)),
    }


def reference(x, mem, w_in, b_in, mlstm_norm, sconv_w, rel_bias, hgrn_lb_logits, hgrn_norm, w_mix_out, norm_mix_pre, norm_mix_post, mem_norm, w_cq, w_ck, w_cv, w_co, norm_cross_pre, norm_cross_post, w_ffn_in, ffn_conv_w, ffn_conv_b, w_ffn_out, norm_ffn_pre, norm_ffn_post):
    lb_soft = jax.nn.softmax(hgrn_lb_logits.astype(jnp.float32), axis=0)
    lb_all = jnp.cumsum(lb_soft, axis=0) - lb_soft[0]
    for l in range(DEPTH):
        h = token_mixer(rmsnorm(x, norm_mix_pre[l]), w_in[l], b_in[l], mlstm_norm[l], sconv_w[l], rel_bias, lb_all[l], hgrn_norm[l], w_mix_out[l])
        x = x + rmsnorm(h, norm_mix_post[l])
        h = memory_cross_attention(rmsnorm(x, norm_cross_pre[l]), rmsnorm(mem, mem_norm[l]), w_cq[l], w_ck[l], w_cv[l], w_co[l])
        x = x + rmsnorm(h, norm_cross_post[l])
        h = conv_glu_ffn(rmsnorm(x, norm_ffn_pre[l]), w_ffn_in[l], ffn_conv_w[l], ffn_conv_b[l], w_ffn_out[l])
        x = x + rmsnorm(h, norm_ffn_post[l])
    return x
```

```python
import contextlib
import math
import numpy as np
import concourse.bass as bass
import concourse.mybir as mybir
from concourse.bass_utils import run_bass_kernel_spmd

F32 = mybir.dt.float32
BF16 = mybir.dt.bfloat16
AF = mybir.ActivationFunctionType
ALU = mybir.AluOpType
AX = mybir.AxisListType

D = 1024
SEQ = 2048
NSEQ = 2
DEPTH = 4
TT = 512
NT = SEQ // TT
KC = 8
DFF = 2816
NHC = DFF // 128
NMEM = 256
EPS = 1e-6
NEG = -30000.0


class Buf:
    __slots__ = ("w", "r")

    def __init__(self):
        self.w = None
        self.r = []


class Sched:
    NDMA = 8

    def __init__(self, nc, es):
        self.nc = nc
        self.engs = {"pe": nc.tensor, "act": nc.scalar, "dve": nc.vector,
                     "pool": nc.gpsimd, "sp": nc.sync}
        self.sems = {}
        for k in ("pe", "act", "dve", "pool"):
            self.sems[("e", k)] = es.enter_context(nc.semaphore("prog_" + k))
        for k in ("sp", "pool", "act"):
            for i in range(self.NDMA):
                self.sems[("d", k, i)] = es.enter_context(nc.semaphore("dma_%s_%d" % (k, i)))
        self.cnt = {k: 0 for k in self.engs}
        self.dcnt = {k: 0 for k in self.engs}
        self.waited = {k: {} for k in self.engs}
        self.nops = 0

    def _deps(self, eng, reads, writes):
        need = {}

        def add(tok):
            if tok is None:
                return
            k, v = tok
            if need.get(k, 0) < v:
                need[k] = v
        for b in reads:
            add(b.w)
        for b in writes:
            add(b.w)
            for t in b.r:
                add(t)
        wd = self.waited[eng]
        e = self.engs[eng]
        for k, v in need.items():
            if k == ("e", "pe") and eng == "pe":
                continue
            if wd.get(k, 0) >= v:
                continue
            wd[k] = v
            e.wait_ge(self.sems[k], v)

    def _commit(self, tok, reads, writes):
        for b in reads:
            b.r.append(tok)
            if len(b.r) > 64:
                m = {}
                for k, v in b.r:
                    if m.get(k, 0) < v:
                        m[k] = v
                b.r = list(m.items())
        for b in writes:
            b.w = tok
            b.r = []

    def op(self, eng, fn, reads=(), writes=()):
        self._deps(eng, reads, writes)
        self.cnt[eng] += 1
        k = ("e", eng)
        fn(self.engs[eng]).then_inc(self.sems[k], 1)
        tok = (k, self.cnt[eng])
        self._commit(tok, reads, writes)
        self.nops += 1
        return tok

    def dma(self, eng, out, in_, reads=(), writes=(), **kw):
        j = self.dcnt[eng]
        self.dcnt[eng] += 1
        sk = ("d", eng, j % self.NDMA)
        val = 16 * (j // self.NDMA + 1)
        self._deps(eng, reads, writes)
        if j >= self.NDMA:
            prev = 16 * (j // self.NDMA)
            wd = self.waited[eng]
            if wd.get(sk, 0) < prev:
                wd[sk] = prev
                self.engs[eng].wait_ge(self.sems[sk], prev)
        self.engs[eng].dma_start(out=out, in_=in_, **kw).then_inc(self.sems[sk], 16)
        tok = (sk, val)
        self._commit(tok, reads, writes)
        self.nops += 1
        return tok

    def finish(self, eng, bufs):
        need = {}
        for b in bufs:
            if b.w is not None:
                k, v = b.w
                need[k] = max(need.get(k, 0), v)
        for k, v in need.items():
            self.engs[eng].wait_ge(self.sems[k], v)


class T:
    _n = [0]

    def __init__(self, es, nc, name, shape, dtype, psum=False):
        T._n[0] += 1
        name = "t%d_%s" % (T._n[0], name)
        if psum:
            self.t = es.enter_context(nc.psum_tensor(name, shape, dtype))
        else:
            self.t = es.enter_context(nc.sbuf_tensor(name, shape, dtype))
        self.b = Buf()
        self.b.r = list(T.grave.items())
        es.callback(self._retire)

    grave = {}

    def _retire(self):
        g = T.grave
        toks = list(self.b.r)
        if self.b.w is not None:
            toks.append(self.b.w)
        for k, v in toks:
            if g.get(k, 0) < v:
                g[k] = v

    def __getitem__(self, k):
        return self.t[k]


def arr_w(w, ocw):
    K, N = w.shape
    a = w.reshape(K // 128, 128, N // ocw, ocw)
    return np.ascontiguousarray(a.transpose(2, 1, 0, 3))


def to_T(x):
    a = x.reshape(x.shape[0] // TT, TT, KC, 128)
    return np.ascontiguousarray(a.transpose(0, 3, 2, 1))


def from_T(a):
    return np.ascontiguousarray(a.transpose(0, 3, 2, 1)).reshape(-1, KC * 128)


def colT(v, w=128):
    return np.ascontiguousarray(v.reshape(-1, w).T)


def host_prepare(inp, depth):
    sh = {}
    L = depth
    wup = []
    for l in range(L):
        w = inp["w_ffn_in"][l]
        g = arr_w(w[:, :DFF], 128)
        u = arr_w(w[:, DFF:], 128)
        wup.append(np.concatenate([g, u], axis=3))
    sh["w_up"] = np.stack(wup)
    wd = []
    for l in range(L):
        w = inp["w_ffn_out"][l]
        a = w.reshape(NHC, 128, 8, 128)
        wd.append(np.ascontiguousarray(a.transpose(2, 1, 0, 3)))
    sh["w_down"] = np.stack(wd)
    sh["ffn_cw"] = np.stack([np.stack([colT(inp["ffn_conv_w"][l][j]) for j in range(3)] + [colT(inp["ffn_conv_b"][l])], axis=1)
                             for l in range(L)])
    names = ["norm_mix_pre", "norm_mix_post", "norm_cross_pre", "norm_cross_post", "norm_ffn_pre", "norm_ffn_post", "mem_norm"]
    sh["gains"] = np.stack([np.stack([colT(inp[n][l]) for n in names], axis=1) for l in range(L)])
    sh["w_cq"] = np.stack([arr_w(inp["w_cq"][l], 128) for l in range(L)])
    sh["w_ck"] = np.stack([arr_w(inp["w_ck"][l], 128) for l in range(L)])
    sh["w_cv"] = np.stack([arr_w(inp["w_cv"][l], 512)[0] for l in range(L)])
    sh["w_co"] = np.stack([np.ascontiguousarray(inp["w_co"][l].reshape(4, 128, 8, 128).transpose(2, 1, 0, 3)) for l in range(L)])
    sh["ident"] = np.eye(128, dtype=np.float32)
    return sh


class Prog:
    def __init__(self, cfg):
        self.cfg = cfg
        self.depth = cfg.get("depth", DEPTH)
        self.nseq = cfg.get("nseq", NSEQ)

    def dram_in(self, name, shape, dt=F32):
        return self.nc.dram_tensor(name, list(shape), dt, kind="ExternalInput").ap()

    def build(self, shapes):
        cfg = self.cfg
        nc = self.nc = bass.Bass("TRN2", target_bir_lowering=False)
        L = self.depth
        self.din = {k: self.dram_in(k, v) for k, v in shapes.items()}
        self.outT = nc.dram_tensor("outT", [self.nseq, NT, 128, KC, TT], F32, kind="ExternalOutput").ap()
        self.dbg = {}
        es = self.es = contextlib.ExitStack()
        with es:
            S = self.S = Sched(nc, es)
            T.grave = {}
            self.x_buf = [[Buf() for _ in range(NT)] for _ in range(self.nseq)]
            self.alloc_static()
            self.load_consts()
            for s in range(self.nseq):
                for l in range(L):
                    first = (l == 0)
                    src_first = first
                    if cfg.get("mix", True):
                        self.mixer_sublayer(s, l, src_first)
                        src_first = False
                    if cfg.get("cross", True):
                        self.cross_sublayer(s, l, src_first)
                        src_first = False
                    if cfg.get("ffn", True):
                        self.ffn_sublayer(s, l, src_first)
                        src_first = False
            S.finish("sp", [b for row in self.x_buf for b in row] + list(self.dbg_bufs))
        return nc

    def dump(self, name, t, ap, shape, dt=F32):
        if not self.cfg.get("dump", False) or name in self.dbg:
            return
        d = self.nc.dram_tensor("dbg_" + name, list(shape), dt, kind="ExternalOutput").ap()
        b = Buf()
        self.S.dma("sp", d, ap, reads=[t.b], writes=[b])
        self.dbg[name] = d
        self.dbg_bufs.append(b)

    def alloc_static(self):
        nc, es = self.nc, self.es
        L = self.depth
        self.dbg_bufs = []
        self.ones = T(es, nc, "ones", [128, 128], BF16)
        self.ident = T(es, nc, "ident", [128, 128], BF16)
        self.gains = T(es, nc, "gains", [128, L * 7 * 8], F32)
        self.ffn_cw = T(es, nc, "ffn_cw", [128, L * 4 * NHC], F32)
        self.ps = [T(es, nc, "ps%d" % i, [128, 512], F32, psum=True) for i in range(8)]
        self.wb = [T(es, nc, "wb%d" % i, [128, 6144], BF16) for i in range(2)]
        self.xs = [T(es, nc, "xs%d" % i, [128, KC * TT], F32) for i in range(1)]
        self.hout = T(es, nc, "hout", [128, KC * TT], F32)
        self.sq = T(es, nc, "sq", [128, KC * TT], BF16)
        self.rs = [T(es, nc, "rs%d" % i, [128, TT], F32) for i in range(2)]
        self.xn = [T(es, nc, "xn%d" % i, [128, KC * TT], BF16) for i in range(NT)]
        self.epsb = T(es, nc, "epsb", [128, 1], F32)
        self.wrot = 0

    def load_consts(self):
        S = self.S
        L = self.depth
        S.op("dve", lambda e: e.memset(self.ones[:], 1.0), writes=[self.ones.b])
        S.op("dve", lambda e: e.memset(self.epsb[:], EPS), writes=[self.epsb.b])
        S.dma("pool", self.ident[:], self.din["ident"], writes=[self.ident.b])
        S.dma("sp", self.gains[:].rearrange("p (l f) -> p l f", l=L), self.din["gains"].rearrange("l p a c -> p l (a c)"), writes=[self.gains.b])
        S.dma("sp", self.ffn_cw[:].rearrange("p (l f) -> p l f", l=L), self.din["ffn_cw"].rearrange("l p a c -> p l (a c)"), writes=[self.ffn_cw.b])

    def gain_col(self, l, which, c):
        i = (l * 7 + which) * 8 + c
        return self.gains[:, i:i + 1]

    def next_wb(self):
        w = self.wb[self.wrot % len(self.wb)]
        self.wrot += 1
        return w

    def x_src(self, s, tt, first):
        return self.din["xT"][s, tt] if first else self.outT[s, tt]

    def load_x(self, s, tt, first, dst):
        S = self.S
        S.dma("sp", dst[:].rearrange("p (c t) -> p c t", c=KC), self.x_src(s, tt, first),
              reads=[self.x_buf[s][tt]], writes=[dst.b])

    def rms_T(self, src, ncols, nchunks, dim, psA, rs_out):
        S = self.S
        n = nchunks * ncols
        S.op("act", lambda e: e.activation(out=self.sq[:, :n], in_=src[:, :n], func=AF.Square),
             reads=[src.b], writes=[self.sq.b])
        for c in range(nchunks):
            S.op("pe", lambda e, c=c: e.matmul(psA[:, :ncols], lhsT=self.ones[:], rhs=self.sq[:, c * ncols:(c + 1) * ncols],
                                               start=(c == 0), stop=(c == nchunks - 1)),
                 reads=[self.ones.b, self.sq.b], writes=[psA.b])
        S.op("act", lambda e: e.activation(out=rs_out[:, :ncols], in_=psA[:, :ncols], func=AF.Sqrt, scale=1.0 / dim, bias=self.epsb[:, 0:1]),
             reads=[psA.b, self.epsb.b], writes=[rs_out.b])
        S.op("dve", lambda e: e.reciprocal(out=rs_out[:, :ncols], in_=rs_out[:, :ncols]),
             reads=[rs_out.b], writes=[rs_out.b])

    def norm_tile(self, s, tt, first, l, which):
        S = self.S
        xs = self.xs[0]
        self.load_x(s, tt, first, xs)
        rs = self.rs[tt % 2]
        self.rms_T(xs, TT, KC, D, self.ps[7], rs)
        xn = self.xn[tt]
        for c in range(KC):
            S.op("dve", lambda e, c=c: e.scalar_tensor_tensor(out=xn[:, c * TT:(c + 1) * TT], in0=xs[:, c * TT:(c + 1) * TT],
                                                                scalar=self.gain_col(l, which, c), in1=rs[:, :TT],
                                                                op0=ALU.mult, op1=ALU.mult),
                 reads=[xs.b, rs.b, self.gains.b], writes=[xn.b])

    def residual_tile(self, s, tt, first, l, which):
        S = self.S
        hout = self.hout
        rs = self.rs[tt % 2]
        self.rms_T(hout, TT, KC, D, self.ps[7], rs)
        xs = self.xs[0]
        self.load_x(s, tt, first, xs)
        for c in range(KC):
            sl = slice(c * TT, (c + 1) * TT)
            S.op("dve", lambda e, sl=sl, c=c: e.scalar_tensor_tensor(out=hout[:, sl], in0=hout[:, sl], scalar=self.gain_col(l, which, c),
                                                                      in1=rs[:, :TT], op0=ALU.mult, op1=ALU.mult),
                 reads=[hout.b, rs.b, self.gains.b], writes=[hout.b])
        S.op("pool", lambda e: e.tensor_tensor(out=xs[:], in0=xs[:], in1=hout[:], op=ALU.add),
             reads=[xs.b, hout.b], writes=[xs.b])
        S.dma("sp", self.outT[s, tt], xs[:].rearrange("p (c t) -> p c t", c=KC), reads=[xs.b], writes=[self.x_buf[s][tt]])

    def ffn_sublayer(self, s, l, first):
        S, nc = self.S, self.nc
        HT = 1024
        NTH = HT // TT
        with contextlib.ExitStack() as es:
            hT = T(es, nc, "ffn_hT", [128, NHC * HT], BF16)
            gsb = T(es, nc, "ffn_g", [128, 2 + HT], F32)
            usb = T(es, nc, "ffn_u", [128, HT], F32)
            a1 = T(es, nc, "ffn_a1", [128, HT], F32)
            halo = T(es, nc, "ffn_halo", [128, NHC * 2], F32)
            S.op("dve", lambda e: e.memset(halo[:], 0.0), writes=[halo.b])
            for half in range(2):
                for k in range(NTH):
                    self.norm_tile(s, half * NTH + k, first, l, 4)
                for g in range(NHC // 2):
                    wt = self.next_wb()
                    S.dma("pool", wt[:, :2 * 8 * 256].rearrange("p (g k m) -> p g k m", g=2, k=8),
                          self.din["w_up"][l, 2 * g:2 * g + 2].rearrange("g p k m -> p g k m"), writes=[wt.b])
                    for ci in range(2):
                        c = 2 * g + ci
                        S.op("act", lambda e, c=c: e.copy(out=gsb[:, 0:2], in_=halo[:, 2 * c:2 * c + 2]),
                             reads=[halo.b], writes=[gsb.b])
                        for k in range(NTH):
                            tt = half * NTH + k
                            pg = self.ps[(2 * k) % 4]
                            pu = self.ps[(2 * k + 1) % 4]
                            for which, pp in ((0, pg), (1, pu)):
                                for kc in range(KC):
                                    off = (ci * 8 + kc) * 256 + which * 128
                                    S.op("pe", lambda e, pp=pp, off=off, kc=kc, tt=tt: e.matmul(
                                        pp[:, :], lhsT=wt[:, off:off + 128], rhs=self.xn[tt][:, kc * TT:(kc + 1) * TT],
                                        start=(kc == 0), stop=(kc == KC - 1)),
                                        reads=[wt.b, self.xn[tt].b], writes=[pp.b])
                            S.op("act", lambda e, k=k, pg=pg: e.copy(out=gsb[:, 2 + k * TT:2 + (k + 1) * TT], in_=pg[:, :]),
                                 reads=[pg.b], writes=[gsb.b])
                            S.op("act", lambda e, k=k, pu=pu: e.copy(out=usb[:, k * TT:(k + 1) * TT], in_=pu[:, :]),
                                 reads=[pu.b], writes=[usb.b])
                        cw = lambda j, c=c: self.ffn_cw[:, (l * 4 + j) * NHC + c:(l * 4 + j) * NHC + c + 1]
                        S.op("dve", lambda e, cw=cw: e.tensor_scalar(out=a1[:], in0=gsb[:, 2:2 + HT], scalar1=cw(2), scalar2=cw(3),
                                                                      op0=ALU.mult, op1=ALU.add),
                             reads=[gsb.b, self.ffn_cw.b], writes=[a1.b])
                        S.op("dve", lambda e, cw=cw: e.scalar_tensor_tensor(out=a1[:], in0=gsb[:, 1:1 + HT], scalar=cw(1), in1=a1[:],
                                                                             op0=ALU.mult, op1=ALU.add),
                             reads=[gsb.b, a1.b, self.ffn_cw.b], writes=[a1.b])
                        S.op("dve", lambda e, cw=cw: e.scalar_tensor_tensor(out=a1[:], in0=gsb[:, 0:HT], scalar=cw(0), in1=a1[:],
                                                                             op0=ALU.mult, op1=ALU.add),
                             reads=[gsb.b, a1.b, self.ffn_cw.b], writes=[a1.b])
                        S.op("act", lambda e, c=c: e.copy(out=halo[:, 2 * c:2 * c + 2], in_=gsb[:, HT:HT + 2]),
                             reads=[gsb.b], writes=[halo.b])
                        S.op("act", lambda e: e.activation(out=a1[:], in_=a1[:], func=AF.Silu), reads=[a1.b], writes=[a1.b])
                        S.op("dve", lambda e, c=c: e.tensor_tensor(out=hT[:, c * HT:(c + 1) * HT], in0=a1[:], in1=usb[:], op=ALU.mult),
                             reads=[a1.b, usb.b], writes=[hT.b])
                houts = [self.hout, None]
                with contextlib.ExitStack() as es2:
                    hout2 = T(es2, nc, "ffn_hout2", [128, KC * TT], F32)
                    hs = [self.hout, hout2]
                    for o in range(KC):
                        wt = self.next_wb()
                        S.dma("pool", wt[:, :NHC * 128].rearrange("p (c m) -> p c m", c=NHC), self.din["w_down"][l, o], writes=[wt.b])
                        for k in range(NTH):
                            pp = self.ps[4 + (o * NTH + k) % 3]
                            for c in range(NHC):
                                S.op("pe", lambda e, pp=pp, c=c, k=k: e.matmul(
                                    pp[:, :], lhsT=wt[:, c * 128:(c + 1) * 128], rhs=hT[:, c * HT + k * TT:c * HT + (k + 1) * TT],
                                    start=(c == 0), stop=(c == NHC - 1)),
                                    reads=[wt.b, hT.b], writes=[pp.b])
                            S.op("act", lambda e, pp=pp, o=o, k=k: e.copy(out=hs[k][:, o * TT:(o + 1) * TT], in_=pp[:, :]),
                                 reads=[pp.b], writes=[hs[k].b])
                    for k in range(NTH):
                        if k == 1:
                            S.op("pool", lambda e: e.tensor_copy(out=self.hout[:], in_=hout2[:]), reads=[hout2.b], writes=[self.hout.b])
                        self.residual_tile(s, half * NTH + k, first, l, 5)

    def cross_sublayer(self, s, l, first):
        S, nc = self.S, self.nc
        with contextlib.ExitStack() as es:
            memf = T(es, nc, "c_memf", [128, KC * NMEM], F32)
            memn = T(es, nc, "c_memn", [128, KC * NMEM], BF16)
            kT = T(es, nc, "c_kT", [128, 4 * NMEM], BF16)
            vtok = T(es, nc, "c_vtok", [128, 2 * 512], BF16)
            wq = T(es, nc, "c_wq", [128, 4 * 8 * 128], BF16)
            wo = T(es, nc, "c_wo", [128, 8 * 4 * 128], BF16)
            qT = T(es, nc, "c_qT", [128, 4 * TT], BF16)
            oT = T(es, nc, "c_oT", [128, 4 * TT], BF16)
            ex = [T(es, nc, "c_ex%d" % i, [128, TT], BF16) for i in range(2)]
            rden = T(es, nc, "c_rden", [128, TT], F32)
            S.dma("pool", wq[:].rearrange("p (o k m) -> p o k m", o=4, k=8), self.din["w_cq"][l].rearrange("o p k m -> p o k m"), writes=[wq.b])
            S.dma("pool", wo[:].rearrange("p (o h m) -> p o h m", o=8, h=4), self.din["w_co"][l].rearrange("o p h m -> p o h m"), writes=[wo.b])
            wk = self.next_wb()
            S.dma("pool", wk[:, :4096].rearrange("p (o k m) -> p o k m", o=4, k=8), self.din["w_ck"][l].rearrange("o p k m -> p o k m"), writes=[wk.b])
            wv = self.next_wb()
            S.dma("pool", wv[:, :4096].rearrange("p (k m) -> p k m", k=8), self.din["w_cv"][l], writes=[wv.b])
            S.dma("sp", memf[:].rearrange("p (c t) -> p c t", c=KC), self.din["memT"][s], writes=[memf.b])
            rs = self.rs[0]
            self.rms_T(memf, NMEM, KC, D, self.ps[7], rs)
            for c in range(KC):
                S.op("dve", lambda e, c=c: e.scalar_tensor_tensor(out=memn[:, c * NMEM:(c + 1) * NMEM], in0=memf[:, c * NMEM:(c + 1) * NMEM],
                                                                    scalar=self.gain_col(l, 6, c), in1=rs[:, :NMEM], op0=ALU.mult, op1=ALU.mult),
                     reads=[memf.b, rs.b, self.gains.b], writes=[memn.b])
            for h in range(4):
                pp = self.ps[h % 2]
                for kc in range(KC):
                    S.op("pe", lambda e, pp=pp, h=h, kc=kc: e.matmul(pp[:, :NMEM], lhsT=wk[:, (h * 8 + kc) * 128:(h * 8 + kc + 1) * 128],
                                                                      rhs=memn[:, kc * NMEM:(kc + 1) * NMEM], start=(kc == 0), stop=(kc == KC - 1)),
                         reads=[wk.b, memn.b], writes=[pp.b])
                S.op("act", lambda e, pp=pp, h=h: e.copy(out=kT[:, h * NMEM:(h + 1) * NMEM], in_=pp[:, :NMEM]), reads=[pp.b], writes=[kT.b])
            for mc in range(2):
                pp = self.ps[2 + mc]
                for kc in range(KC):
                    S.op("pe", lambda e, pp=pp, mc=mc, kc=kc: e.matmul(pp[:, :], lhsT=memn[:, kc * NMEM + mc * 128:kc * NMEM + (mc + 1) * 128],
                                                                        rhs=wv[:, kc * 512:(kc + 1) * 512], start=(kc == 0), stop=(kc == KC - 1)),
                         reads=[wv.b, memn.b], writes=[pp.b])
                S.op("act", lambda e, pp=pp, mc=mc: e.copy(out=vtok[:, mc * 512:(mc + 1) * 512], in_=pp[:, :]), reads=[pp.b], writes=[vtok.b])
            scale = 128 ** -0.5
            for tt in range(NT):
                self.norm_tile(s, tt, first, l, 2)
                xn = self.xn[tt]
                for h in range(4):
                    pp = self.ps[h % 2]
                    for kc in range(KC):
                        S.op("pe", lambda e, pp=pp, h=h, kc=kc: e.matmul(pp[:, :], lhsT=wq[:, (h * 8 + kc) * 128:(h * 8 + kc + 1) * 128],
                                                                          rhs=xn[:, kc * TT:(kc + 1) * TT], start=(kc == 0), stop=(kc == KC - 1)),
                             reads=[wq.b, xn.b], writes=[pp.b])
                    S.op("act", lambda e, pp=pp, h=h: e.copy(out=qT[:, h * TT:(h + 1) * TT], in_=pp[:, :]), reads=[pp.b], writes=[qT.b])
                for h in range(4):
                    po = self.ps[4]
                    pd = self.ps[5]
                    for mc in range(2):
                        pss = self.ps[2 + mc]
                        S.op("pe", lambda e, pss=pss, h=h, mc=mc: e.matmul(pss[:, :], lhsT=kT[:, h * NMEM + mc * 128:h * NMEM + (mc + 1) * 128],
                                                                            rhs=qT[:, h * TT:(h + 1) * TT], start=True, stop=True),
                             reads=[kT.b, qT.b], writes=[pss.b])
                        S.op("act", lambda e, pss=pss, mc=mc: e.activation(out=ex[mc][:], in_=pss[:, :], func=AF.Exp, scale=scale),
                             reads=[pss.b], writes=[ex[mc].b])
                    for mc in range(2):
                        S.op("pe", lambda e, h=h, mc=mc: e.matmul(po[:, :], lhsT=vtok[:, mc * 512 + h * 128:mc * 512 + (h + 1) * 128], rhs=ex[mc][:],
                                                                   start=(mc == 0), stop=(mc == 1)),
                             reads=[vtok.b, ex[mc].b], writes=[po.b])
                    for mc in range(2):
                        S.op("pe", lambda e, mc=mc: e.matmul(pd[:, :], lhsT=self.ones[:], rhs=ex[mc][:], start=(mc == 0), stop=(mc == 1)),
                             reads=[self.ones.b, ex[mc].b], writes=[pd.b])
                    S.op("dve", lambda e: e.reciprocal(out=rden[:], in_=pd[:, :]), reads=[pd.b], writes=[rden.b])
                    S.op("dve", lambda e, h=h: e.tensor_tensor(out=oT[:, h * TT:(h + 1) * TT], in0=po[:, :], in1=rden[:], op=ALU.mult),
                         reads=[po.b, rden.b], writes=[oT.b])
                for o in range(KC):
                    pp = self.ps[o % 2]
                    for h in range(4):
                        S.op("pe", lambda e, pp=pp, o=o, h=h: e.matmul(pp[:, :], lhsT=wo[:, (o * 4 + h) * 128:(o * 4 + h + 1) * 128],
                                                                        rhs=oT[:, h * TT:(h + 1) * TT], start=(h == 0), stop=(h == 3)),
                             reads=[wo.b, oT.b], writes=[pp.b])
                    S.op("act", lambda e, pp=pp, o=o: e.copy(out=self.hout[:, o * TT:(o + 1) * TT], in_=pp[:, :]), reads=[pp.b], writes=[self.hout.b])
                self.residual_tile(s, tt, first, l, 3)

    def mixer_sublayer(self, s, l, first):
        raise NotImplementedError


OFF = dict(m_q=0, m_k=256, m_v=512, m_o=768, m_i=1024, m_f=1028, c_b=1032, c_c=1288, c_h=1544,
           a_q=1800, a_k=2056, a_v=2312, h_q=2568, h_f=2824, h_i=3080, h_g=3336)
LN8 = math.log(8.0)
BIGRAW = 240000.0


def rel_bucket_np(dist):
    n = np.maximum(dist, 0)
    exact = 16
    nf = np.maximum(n, 1).astype(np.float32)
    large = exact + (np.log(nf / exact) / math.log(128 / exact) * (32 - exact)).astype(np.int32)
    large = np.minimum(large, 31)
    return np.where(n < exact, n, large)


def host_prepare_mixer(inp, depth, sh):
    L = depth
    colsA, colsC, colsD = [], [], []
    for h in range(4):
        colsA += list(range(OFF["m_q"] + 64 * h, OFF["m_q"] + 64 * h + 64))
        colsA += list(range(OFF["m_k"] + 64 * h, OFF["m_k"] + 64 * h + 64))
        colsA += [OFF["m_i"] + h] * 64
        colsA += [OFF["m_f"] + h] * 64
        colsA += list(range(OFF["m_o"] + 64 * h, OFF["m_o"] + 64 * h + 64))
        colsC += list(range(OFF["a_q"] + 64 * h, OFF["a_q"] + 64 * h + 64))
        colsC += list(range(OFF["a_k"] + 64 * h, OFF["a_k"] + 64 * h + 64))
        colsD += list(range(OFF["h_q"] + 64 * h, OFF["h_q"] + 64 * h + 64))
        colsD += list(range(OFF["h_f"] + 64 * h, OFF["h_f"] + 64 * h + 64))
        colsD += list(range(OFF["h_g"] + 64 * h, OFF["h_g"] + 64 * h + 64))
    colsB = list(range(OFF["c_b"], OFF["c_b"] + 768))
    w_in, b_in = inp["w_in"], inp["b_in"]
    sh["wA_T"] = np.stack([arr_w(w_in[l][:, colsA], 64) for l in range(L)])
    sh["wC_T"] = np.stack([arr_w(w_in[l][:, colsC], 64) for l in range(L)])
    sh["wD_T"] = np.stack([arr_w(w_in[l][:, colsD], 64) for l in range(L)])
    sh["wB_T"] = np.stack([arr_w(w_in[l][:, colsB], 128) for l in range(L)])
    vcols = [OFF["m_v"], OFF["a_v"], OFF["h_i"]]
    sh["w_v"] = np.stack([np.stack([arr_w(w_in[l][:, o:o + 256], 256)[0] for o in vcols]) for l in range(L)])
    sh["b_v"] = np.stack([np.stack([b_in[l][o:o + 256] for o in vcols]) for l in range(L)])
    sh["pA"] = np.stack([colT(b_in[l][colsA], 64) for l in range(L)])
    sh["pC"] = np.stack([colT(b_in[l][colsC], 64) for l in range(L)])
    sh["pD"] = np.stack([colT(b_in[l][colsD], 64) for l in range(L)])
    sh["pB"] = np.stack([colT(b_in[l][colsB], 128) for l in range(L)])
    sh["scw"] = np.stack([np.stack([colT(inp["sconv_w"][l][j], 128) for j in range(3)], axis=1) for l in range(L)])
    sh["hn"] = np.stack([np.stack([colT(inp["mlstm_norm"][l], 64), colT(inp["hgrn_norm"][l], 64)], axis=1) for l in range(L)])
    lg = inp["hgrn_lb_logits"]
    sh["lbl"] = np.ascontiguousarray(lg.reshape(4, 4, 64).transpose(2, 1, 0))
    sh["w_mo"] = np.stack([arr_w(inp["w_mix_out"][l], 128) for l in range(L)])
    m8 = np.tile(np.triu(np.ones((64, 64), np.float32)), (1, 8))
    sh["mask8"] = m8
    x = np.arange(1152)
    dist = x - 511
    oh = np.zeros((33, 1152), np.float32)
    bk = rel_bucket_np(dist)
    for i in range(1152):
        if dist[i] >= 0:
            oh[bk[i], i] = 1.0
        else:
            oh[32, i] = 1.0
    sh["oh"] = oh
    ra = np.zeros((33, 4), np.float32)
    ra[:32] = inp["rel_bias"]
    ra[32] = NEG
    sh["rel_aug"] = ra
    sh["b31"] = np.ascontiguousarray(inp["rel_bias"][31:32, :])
    pairs = [(n, m) for n in range(8) for m in range(8) if m != n]
    Pm = np.zeros((8, 56), np.float32)
    Agg = np.zeros((56, 8), np.float32)
    for i, (n, m) in enumerate(pairs):
        Pm[m, i] += 1.0
        Pm[n, i] -= 1.0
        Agg[i, n] = 1.0
    sh["Pm"] = Pm
    sh["Agg"] = Agg
    seln = np.zeros((8, 8, 128), np.float32)
    for n in range(8):
        seln[n, n, :] = 1.0
    sh["seln"] = seln.reshape(8, 1024)
    pastm = np.zeros((8, 4, 2), np.float32)
    validc = np.zeros((8, 4, 2), np.float32)
    ownm1 = np.zeros((8, 4, 2), np.float32)
    for n in range(8):
        for tt in range(4):
            for j in range(2):
                b = 2 * tt + j
                pastm[n, tt, j] = 0.0 if n < b else -1e9
                validc[n, tt, j] = 1.0 if n < b else 0.0
                ownm1[n, tt, j] = (1.0 if n == b else 0.0) - 1.0
    sh["mobac"] = np.stack([pastm, validc, ownm1], axis=1).reshape(8, 24)
    return sh


class V:
    def __init__(self, ap_fn):
        self.f = ap_fn
        self.b = Buf()

    def __getitem__(self, k):
        return self.f()[k]


class ProgM(Prog):
    def alloc_static(self):
        super().alloc_static()
        nc, es, L = self.nc, self.es, self.depth
        self.ident32 = T(es, nc, "ident32", [128, 128], F32)
        self.mask8 = T(es, nc, "mask8", [64, 512], F32)
        self.onesf = T(es, nc, "onesf", [64, 512], F32)
        self.oneb = T(es, nc, "oneb", [128, 1], F32)
        self.pA = T(es, nc, "pA", [64, L * 20], F32)
        self.pC = T(es, nc, "pC", [64, L * 8], F32)
        self.pD = T(es, nc, "pD", [64, L * 12], F32)
        self.pB = T(es, nc, "pB", [128, L * 6], F32)
        self.scw = T(es, nc, "scw", [128, L * 6], F32)
        self.hn = T(es, nc, "hn", [64, L * 8], F32)
        self.lbe = T(es, nc, "lbe", [64, 16], F32)
        self.lb = T(es, nc, "lb", [64, 16], F32)
        self.omlb = T(es, nc, "omlb", [64, 16], F32)
        self.lbs = T(es, nc, "lbs", [64, 4], F32)
        self.rel_aug = T(es, nc, "rel_aug", [33, 4], F32)
        self.b31 = T(es, nc, "b31", [128, 4], F32)
        self.Pm = T(es, nc, "Pm", [8, 56], F32)
        self.Agg = T(es, nc, "Agg", [56, 8], BF16)
        self.seln = T(es, nc, "seln", [8, 1024], BF16)
        self.mobac = T(es, nc, "mobac", [8, 24], F32)
        self.tbd = nc.dram_tensor("tbd", [4, 128, 1152], F32, kind="Internal")
        self.tbd_b = Buf()

    def load_consts(self):
        super().load_consts()
        S, L, din = self.S, self.depth, self.din
        S.dma("sp", self.ident32[:], din["ident"], writes=[self.ident32.b])
        S.dma("sp", self.mask8[:], din["mask8"], writes=[self.mask8.b])
        S.op("dve", lambda e: e.memset(self.onesf[:], 1.0), writes=[self.onesf.b])
        S.op("dve", lambda e: e.memset(self.oneb[:], 1.0), writes=[self.oneb.b])
        for nm, t, w in (("pA", self.pA, 20), ("pC", self.pC, 8), ("pD", self.pD, 12), ("pB", self.pB, 6)):
            S.dma("sp", t[:].rearrange("p (l f) -> p l f", l=L), din[nm].rearrange("l p f -> p l f"), writes=[t.b])
        S.dma("sp", self.scw[:].rearrange("p (l f) -> p l f", l=L), din["scw"].rearrange("l p a c -> p l (a c)"), writes=[self.scw.b])
        S.dma("sp", self.hn[:].rearrange("p (l f) -> p l f", l=L), din["hn"].rearrange("l p a c -> p l (a c)"), writes=[self.hn.b])
        S.dma("sp", self.lbe[:], din["lbl"].rearrange("p h l -> p (h l)"), writes=[self.lbe.b])
        S.dma("sp", self.rel_aug[:], din["rel_aug"], writes=[self.rel_aug.b])
        S.dma("sp", self.b31[:], din["b31"].partition_broadcast(128).rearrange("p a b -> p (a b)"), writes=[self.b31.b])
        S.dma("sp", self.Pm[:], din["Pm"], writes=[self.Pm.b])
        S.dma("pool", self.Agg[:], din["Agg"], writes=[self.Agg.b])
        S.dma("pool", self.seln[:], din["seln"], writes=[self.seln.b])
        S.dma("sp", self.mobac[:], din["mobac"], writes=[self.mobac.b])
        pa3 = self.pA[:].rearrange("p (g j) -> p g j", j=5)
        S.op("dve", lambda e: e.tensor_scalar(out=pa3[:, :, 2:3], in0=pa3[:, :, 2:3], scalar1=-LN8, scalar2=None, op0=ALU.add),
             reads=[self.pA.b], writes=[self.pA.b])
        S.op("dve", lambda e: e.tensor_scalar(out=pa3[:, :, 3:5], in0=pa3[:, :, 3:5], scalar1=-1.0, scalar2=None, op0=ALU.mult),
             reads=[self.pA.b], writes=[self.pA.b])
        lbe3 = self.lbe[:].rearrange("p (h l) -> p h l", l=4)
        S.op("act", lambda e: e.activation(out=self.lbe[:], in_=self.lbe[:], func=AF.Exp), reads=[self.lbe.b], writes=[self.lbe.b])
        S.op("dve", lambda e: e.reduce_sum(out=self.lbs[:], in_=lbe3, axis=AX.X), reads=[self.lbe.b], writes=[self.lbs.b])
        S.op("dve", lambda e: e.reciprocal(out=self.lbs[:], in_=self.lbs[:]), reads=[self.lbs.b], writes=[self.lbs.b])
        for h in range(4):
            S.op("dve", lambda e, h=h: e.tensor_scalar(out=self.lbe[:, h * 4:h * 4 + 4], in0=self.lbe[:, h * 4:h * 4 + 4],
                                                       scalar1=self.lbs[:, h:h + 1], scalar2=None, op0=ALU.mult),
                 reads=[self.lbe.b, self.lbs.b], writes=[self.lbe.b])
        lb3 = self.lb[:].rearrange("p (h l) -> p h l", l=4)
        S.op("dve", lambda e: e.memset(self.lb[:], 0.0), writes=[self.lb.b])
        for li in range(1, 4):
            S.op("dve", lambda e, li=li: e.tensor_tensor(out=lb3[:, :, li:li + 1], in0=lb3[:, :, li - 1:li], in1=lbe3[:, :, li:li + 1], op=ALU.add),
                 reads=[self.lb.b, self.lbe.b], writes=[self.lb.b])
        S.op("dve", lambda e: e.tensor_scalar(out=self.omlb[:], in0=self.lb[:], scalar1=-1.0, scalar2=1.0, op0=ALU.mult, op1=ALU.add),
             reads=[self.lb.b], writes=[self.omlb.b])
        nc = self.nc
        with contextlib.ExitStack() as es2:
            oh = T(es2, nc, "mboh", [33, 1152], F32)
            tbv = T(es2, nc, "mbtbv", [4, 1152], F32)
            S.dma("sp", oh[:], self.din["oh"], writes=[oh.b])
            for j in range(3):
                pp = self.ps[j % 2]
                S.op("pe", lambda e: e.matmul(pp[:4, :384], lhsT=self.rel_aug[:, :], rhs=oh[:, j * 384:(j + 1) * 384], start=True, stop=True),
                     reads=[self.rel_aug.b, oh.b], writes=[pp.b])
                S.op("act", lambda e: e.copy(out=tbv[:, j * 384:(j + 1) * 384], in_=pp[:4, :384]), reads=[pp.b], writes=[tbv.b])
            S.dma("sp", self.tbd.ap(), tbv[:].rearrange("p (a x) -> p a x", a=1).broadcast_to([4, 128, 1152]), reads=[tbv.b], writes=[self.tbd_b])

    def proj64(self, pp, W, woff, xn):
        S = self.S
        for kc in range(KC):
            S.op("pe", lambda e, kc=kc: e.matmul(pp[:64, :TT], lhsT=W[:, woff + kc * 64:woff + (kc + 1) * 64],
                                                 rhs=xn[:, kc * TT:(kc + 1) * TT], start=(kc == 0), stop=(kc == KC - 1)),
                 reads=[W.b, xn.b], writes=[pp.b])

    def y_store(self, yT, yb, chunk, h, tt):
        self.S.dma("sp", yT[(h % 2) * 64:(h % 2) * 64 + 64, chunk * SEQ + tt * TT:chunk * SEQ + (tt + 1) * TT], yb[:64, :TT],
                   reads=[yb.b], writes=[yT.b])

    def mixer_sublayer(self, s, l, first):
        S, nc, cfg = self.S, self.nc, self.cfg
        with contextlib.ExitStack() as es:
            yT = T(es, nc, "yT", [128, 8 * SEQ], BF16)
            self.yT = yT
            groups = cfg.get("groups", "ABCD")
            if groups != "ABCD":
                S.op("pool", lambda e: e.memset(yT[:], 0.0), writes=[yT.b])
            for tt in range(NT):
                self.norm_tile(s, tt, first, l, 0)
            if "B" in groups:
                self.group_B(s, l, yT)
            if "A" in groups:
                self.group_gla(s, l, yT, "A")
            if "D" in groups:
                self.group_gla(s, l, yT, "D")
            if "C" in groups:
                self.group_C(s, l, yT)
            if cfg.get("dbg_y", False) and s == 0 and l == 0:
                d = nc.dram_tensor("dbg_y", [128, 8 * SEQ], BF16, kind="ExternalOutput").ap()
                b = Buf()
                S.dma("sp", d, yT[:], reads=[yT.b], writes=[b])
                self.dbg_bufs.append(b)
            with contextlib.ExitStack() as es2:
                wo = T(es2, nc, "w_mo", [128, 8 * 8 * 128], BF16)
                S.dma("pool", wo[:].rearrange("p (o k m) -> p o k m", o=8, k=8), self.din["w_mo"][l].rearrange("o p k m -> p o k m"), writes=[wo.b])
                for tt in range(NT):
                    for o in range(KC):
                        pp = self.ps[o % 2]
                        for kc in range(KC):
                            S.op("pe", lambda e, pp=pp, o=o, kc=kc, tt=tt: e.matmul(
                                pp[:, :], lhsT=wo[:, (o * 8 + kc) * 128:(o * 8 + kc + 1) * 128],
                                rhs=yT[:, kc * SEQ + tt * TT:kc * SEQ + (tt + 1) * TT], start=(kc == 0), stop=(kc == KC - 1)),
                                reads=[wo.b, yT.b], writes=[pp.b])
                        S.op("act", lambda e, pp=pp, o=o: e.copy(out=self.hout[:, o * TT:(o + 1) * TT], in_=pp[:, :]),
                             reads=[pp.b], writes=[self.hout.b])
                    self.residual_tile(s, tt, first, l, 1)

    def group_B(self, s, l, yT):
        S, nc = self.S, self.nc
        with contextlib.ExitStack() as es:
            wB = T(es, nc, "wB", [128, 6 * 8 * 128], BF16)
            S.dma("pool", wB[:].rearrange("p (o k m) -> p o k m", o=6, k=8), self.din["wB_T"][l].rearrange("o p k m -> p o k m"), writes=[wB.b])
            u = T(es, nc, "scu", [128, 2 + SEQ], F32)
            cbs = T(es, nc, "sccb", [128, SEQ], F32)
            a = T(es, nc, "sca", [128, SEQ], F32)
            ccs = T(es, nc, "sccc", [128, TT], F32)
            bcol = lambda oc: self.pB[:, l * 6 + oc:l * 6 + oc + 1]
            wcol = lambda j, c: self.scw[:, l * 6 + j * 2 + c:l * 6 + j * 2 + c + 1]
            for j in range(2):
                S.op("dve", lambda e: e.memset(u[:, 0:2], 0.0), writes=[u.b])
                for tt in range(NT):
                    xn = self.xn[tt]
                    pcb, pcc, pch = self.ps[0], self.ps[1], self.ps[2]
                    for oc, pp in ((0 + j, pcb), (2 + j, pcc), (4 + j, pch)):
                        for kc in range(KC):
                            S.op("pe", lambda e, oc=oc, pp=pp, kc=kc: e.matmul(pp[:, :], lhsT=wB[:, (oc * 8 + kc) * 128:(oc * 8 + kc + 1) * 128],
                                                                                rhs=xn[:, kc * TT:(kc + 1) * TT], start=(kc == 0), stop=(kc == KC - 1)),
                                 reads=[wB.b, xn.b], writes=[pp.b])
                    S.op("act", lambda e: e.activation(out=ccs[:], in_=pcc[:, :], func=AF.Identity, bias=bcol(2 + j)),
                         reads=[pcc.b, self.pB.b], writes=[ccs.b])
                    S.op("dve", lambda e, tt=tt: e.scalar_tensor_tensor(out=u[:, 2 + tt * TT:2 + (tt + 1) * TT], in0=pch[:, :], scalar=bcol(4 + j),
                                                                          in1=ccs[:], op0=ALU.add, op1=ALU.mult),
                         reads=[pch.b, ccs.b, self.pB.b], writes=[u.b])
                    if self.cfg.get("dump"):
                        S.op("act", lambda e, tt=tt: e.copy(out=a[:, tt * TT:(tt + 1) * TT], in_=pcb[:, :]), reads=[pcb.b], writes=[a.b])
                    S.op("act", lambda e, tt=tt: e.activation(out=cbs[:, tt * TT:(tt + 1) * TT], in_=pcb[:, :], func=AF.Identity, bias=bcol(0 + j)),
                         reads=[pcb.b, self.pB.b], writes=[cbs.b])
                self.dump("cbs", cbs, cbs[:], [128, SEQ])
                self.dump("araw", a, a[:], [128, SEQ])
                self.dump("wB", wB, wB[:], [128, 6144], BF16)
                self.dump("u", u, u[:], [128, 2 + SEQ])
                self.dump("xn0", self.xn[0], self.xn[0][:], [128, KC * TT], BF16)
                S.op("dve", lambda e: e.tensor_scalar(out=a[:], in0=u[:, 2:2 + SEQ], scalar1=wcol(2, j), scalar2=None, op0=ALU.mult),
                     reads=[u.b, self.scw.b], writes=[a.b])
                S.op("dve", lambda e: e.scalar_tensor_tensor(out=a[:], in0=u[:, 1:1 + SEQ], scalar=wcol(1, j), in1=a[:], op0=ALU.mult, op1=ALU.add),
                     reads=[u.b, a.b, self.scw.b], writes=[a.b])
                S.op("dve", lambda e: e.scalar_tensor_tensor(out=a[:], in0=u[:, 0:SEQ], scalar=wcol(0, j), in1=a[:], op0=ALU.mult, op1=ALU.add),
                     reads=[u.b, a.b, self.scw.b], writes=[a.b])
                S.op("dve", lambda e: e.tensor_tensor(out=yT[:, (2 + j) * SEQ:(3 + j) * SEQ], in0=a[:], in1=cbs[:], op=ALU.mult),
                     reads=[a.b, cbs.b], writes=[yT.b])

    def group_gla(self, s, l, yT, mode):
        S, nc = self.S, self.nc
        isA = mode == "A"
        nT = 5 if isA else 3
        vidx = 0 if isA else 2
        vw = 128 if isA else 64
        ych0 = 0 if isA else 6
        pb = self.pA if isA else self.pD
        with contextlib.ExitStack() as es:
            W = T(es, nc, "glaW", [128, 4 * nT * 8 * 64], BF16)
            Wv = T(es, nc, "glaWv", [128, 8 * 256], BF16)
            bvb = T(es, nc, "glabv", [64, 256], F32)
            vt = T(es, nc, "glavt", [64, 8 * 4 * vw], BF16)
            Qs = [T(es, nc, "glaQs%d" % h, [64, TT], BF16) for h in range(4)]
            Am = [T(es, nc, "glaAm%d" % h, [64, TT], BF16) for h in range(4)]
            KeT = [T(es, nc, "glaKeT%d" % h, [64, TT], BF16) for h in range(4)]
            og = [T(es, nc, "glaog%d" % h, [64, TT], F32) for h in range(4)]
            Ks = T(es, nc, "glaKs", [64, TT], BF16)
            yb = T(es, nc, "glayb", [64, TT], BF16)
            dec = T(es, nc, "gladec", [64, 32], F32)
            S32 = [T(es, nc, "glaS32_%d" % h, [64, vw], F32) for h in range(4)]
            Sbf = [T(es, nc, "glaSbf_%d" % h, [64, vw], BF16) for h in range(4)]
            gc = T(es, nc, "glagc", [64, TT + 8], F32)
            rsh = T(es, nc, "glarsh", [64, TT], F32)
            xs = self.xs[0]
            sl = [V(lambda i=i: xs[0:64, i * TT:(i + 1) * TT]) for i in range(8)]
            t_q, t_k, t_e, t_f, t_gn, t_x, t_ke, t_hh = sl
            psU = [self.ps[i] for i in (6, 0, 1, 2)]
            wsrc = self.din["wA_T" if isA else "wD_T"][l]
            S.dma("pool", W[:].rearrange("p (o k m) -> p o k m", o=4 * nT, k=8), wsrc.rearrange("o p k m -> p o k m"), writes=[W.b])
            S.dma("pool", Wv[:].rearrange("p (k m) -> p k m", k=8), self.din["w_v"][l, vidx], writes=[Wv.b])
            S.dma("sp", bvb[:], self.din["b_v"][l, vidx:vidx + 1, :].partition_broadcast(64).rearrange("p a b -> p (a b)"), writes=[bvb.b])
            S.op("dve", lambda e: e.memset(gc[:, 0:1], 0.0), reads=[xs.b], writes=[gc.b] + [v.b for v in sl])
            S.op("dve", lambda e: e.memset(vt[:], 1.0), writes=[vt.b])
            for h in range(4):
                S.op("dve", lambda e, h=h: e.memset(S32[h][:], 0.0), writes=[S32[h].b])
                S.op("dve", lambda e, h=h: e.memset(Sbf[h][:], 0.0), writes=[Sbf[h].b])
            vt4 = vt[:].rearrange("p (b h w) -> p b h w", b=8, h=4)
            for tt in range(NT):
                xn = self.xn[tt]
                for b in range(8):
                    pv = self.ps[2 + b % 2]
                    for kc in range(KC):
                        S.op("pe", lambda e, kc=kc: e.matmul(pv[:64, :256], lhsT=xn[:, kc * TT + b * 64:kc * TT + (b + 1) * 64],
                                                             rhs=Wv[:, kc * 256:(kc + 1) * 256], start=(kc == 0), stop=(kc == KC - 1)),
                             reads=[xn.b, Wv.b], writes=[pv.b])
                    S.op("dve", lambda e: e.tensor_tensor(out=vt4[:, b, :, 0:64], in0=pv[:64, :256].rearrange("p (h w) -> p h w", h=4),
                                                          in1=bvb[:].rearrange("p (h w) -> p h w", h=4), op=ALU.add),
                         reads=[pv.b, bvb.b], writes=[vt.b])
                for h in range(4):
                    bc = lambda j: pb[:, (l * 4 + h) * nT + j:(l * 4 + h) * nT + j + 1]
                    wo = lambda j: (h * nT + j) * 8 * 64
                    p0, p1 = self.ps[0], self.ps[1]
                    if isA:
                        self.proj64(p0, W, wo(0), xn)
                        S.op("act", lambda e: e.activation(out=t_q[:, :], in_=p0[:64, :TT], func=AF.Identity, bias=bc(0)), reads=[p0.b, pb.b], writes=[t_q.b])
                        self.proj64(p1, W, wo(1), xn)
                        S.op("act", lambda e: e.activation(out=t_k[:, :], in_=p1[:64, :TT], func=AF.Identity, bias=bc(1)), reads=[p1.b, pb.b], writes=[t_k.b])
                        self.proj64(p0, W, wo(2), xn)
                        S.op("act", lambda e: e.activation(out=t_e[:, :], in_=p0[:64, :TT], func=AF.Exp, bias=bc(2)), reads=[p0.b, pb.b], writes=[t_e.b])
                        self.proj64(p1, W, wo(3), xn)
                        S.op("act", lambda e: e.activation(out=t_f[:, :], in_=p1[:64, :TT], func=AF.Exp, bias=bc(3), scale=-1.0), reads=[p1.b, pb.b], writes=[t_f.b])
                        S.op("dve", lambda e: e.tensor_tensor(out=t_k[:, :], in0=t_k[:, :], in1=t_e[:, :], op=ALU.mult), reads=[t_k.b, t_e.b], writes=[t_k.b])
                        self.proj64(p0, W, wo(4), xn)
                        S.op("act", lambda e: e.activation(out=t_e[:, :], in_=p0[:64, :TT], func=AF.Exp, bias=bc(4), scale=-1.0), reads=[p0.b, pb.b], writes=[t_e.b])
                        S.op("dve", lambda e: e.tensor_scalar(out=t_e[:, :], in0=t_e[:, :], scalar1=1.0, scalar2=None, op0=ALU.add), reads=[t_e.b], writes=[t_e.b])
                        S.op("dve", lambda e: e.reciprocal(out=og[h][:], in_=t_e[:, :]), reads=[t_e.b], writes=[og[h].b])
                        S.op("act", lambda e: e.activation(out=t_f[:, :], in_=t_f[:, :], func=AF.Ln, bias=self.oneb[0:64, 0:1]), reads=[t_f.b, self.oneb.b], writes=[t_f.b])
                        S.op("dve", lambda e: e.tensor_tensor_scan(out=gc[:, 1:TT + 1], data0=self.onesf[:, :], data1=t_f[:, :], initial=0.0, op0=ALU.mult, op1=ALU.add),
                             reads=[self.onesf.b, t_f.b], writes=[gc.b])
                    else:
                        lbi = h * 4 + l
                        self.proj64(p0, W, wo(0), xn)
                        S.op("act", lambda e: e.activation(out=t_q[:, :], in_=p0[:64, :TT], func=AF.Silu, bias=bc(0)), reads=[p0.b, pb.b], writes=[t_q.b])
                        self.proj64(p1, W, wo(1), xn)
                        S.op("act", lambda e: e.activation(out=t_f[:, :], in_=p1[:64, :TT], func=AF.Sigmoid, bias=bc(1)), reads=[p1.b, pb.b], writes=[t_f.b])
                        S.op("dve", lambda e: e.tensor_scalar(out=t_f[:, :], in0=t_f[:, :], scalar1=self.omlb[:, lbi:lbi + 1], scalar2=self.lb[:, lbi:lbi + 1],
                                                              op0=ALU.mult, op1=ALU.add), reads=[t_f.b, self.omlb.b, self.lb.b], writes=[t_f.b])
                        S.op("dve", lambda e: e.tensor_scalar(out=t_k[:, :], in0=t_f[:, :], scalar1=-1.0, scalar2=1.0, op0=ALU.mult, op1=ALU.add),
                             reads=[t_f.b], writes=[t_k.b])
                        S.op("act", lambda e: e.activation(out=t_f[:, :], in_=t_f[:, :], func=AF.Ln), reads=[t_f.b], writes=[t_f.b])
                        S.op("dve", lambda e: e.tensor_tensor_scan(out=gc[:, 1:TT + 1], data0=self.onesf[:, :], data1=t_f[:, :], initial=0.0, op0=ALU.mult, op1=ALU.subtract),
                             reads=[self.onesf.b, t_f.b], writes=[gc.b])
                        self.proj64(p0, W, wo(2), xn)
                        S.op("act", lambda e: e.activation(out=og[h][:], in_=p0[:64, :TT], func=AF.Silu, bias=bc(2)), reads=[p0.b, pb.b], writes=[og[h].b])
                    g3 = lambda ap: ap.rearrange("p (b t) -> p b t", t=64)
                    gn3 = g3(t_gn[:, :])
                    S.op("dve", lambda e: e.tensor_tensor(out=gn3, in0=g3(gc[:, 1:TT + 1]), in1=g3(gc[:, 0:TT])[:, :, 0:1].broadcast_to([64, 8, 64]), op=ALU.subtract),
                         reads=[gc.b], writes=[t_gn.b])
                    S.op("act", lambda e: e.activation(out=t_x[:, :], in_=t_gn[:, :], func=AF.Exp, scale=-1.0), reads=[t_gn.b], writes=[t_x.b])
                    S.op("dve", lambda e: e.tensor_tensor(out=Qs[h][:], in0=t_q[:, :], in1=t_x[:, :], op=ALU.mult), reads=[t_q.b, t_x.b], writes=[Qs[h].b])
                    S.op("act", lambda e: e.activation(out=t_x[:, :], in_=t_gn[:, :], func=AF.Exp), reads=[t_gn.b], writes=[t_x.b])
                    S.op("dve", lambda e: e.tensor_tensor(out=Ks[:], in0=t_k[:, :], in1=t_x[:, :], op=ALU.mult), reads=[t_k.b, t_x.b], writes=[Ks.b])
                    S.op("act", lambda e: e.activation(out=dec[:, h * 8:h * 8 + 8], in_=t_gn[:, 63:TT:64], func=AF.Exp, scale=-1.0), reads=[t_gn.b], writes=[dec.b])
                    S.op("dve", lambda e: e.tensor_tensor(out=g3(t_x[:, :]), in0=gn3, in1=gn3[:, :, 63:64].broadcast_to([64, 8, 64]), op=ALU.subtract),
                         reads=[t_gn.b], writes=[t_x.b])
                    S.op("act", lambda e: e.activation(out=t_x[:, :], in_=t_x[:, :], func=AF.Exp), reads=[t_x.b], writes=[t_x.b])
                    S.op("dve", lambda e: e.tensor_tensor(out=t_ke[:, :], in0=t_k[:, :], in1=t_x[:, :], op=ALU.mult), reads=[t_k.b, t_x.b], writes=[t_ke.b])
                    pa = self.ps[3]
                    for b in range(8):
                        S.op("pe", lambda e, b=b: e.matmul(pa[:64, b * 64:(b + 1) * 64], lhsT=Ks[:, b * 64:(b + 1) * 64], rhs=Qs[h][:, b * 64:(b + 1) * 64],
                                                           start=True, stop=True), reads=[Ks.b, Qs[h].b], writes=[pa.b])
                    S.op("dve", lambda e: e.tensor_tensor(out=Am[h][:], in0=pa[:64, :TT], in1=self.mask8[:, :], op=ALU.mult),
                         reads=[pa.b, self.mask8.b], writes=[Am[h].b])
                    ptr = self.ps[7]
                    for b in range(8):
                        S.op("pe", lambda e, b=b: e.transpose(out=ptr[:64, b * 64:(b + 1) * 64], in_=t_ke[:, b * 64:(b + 1) * 64], identity=self.ident32[0:64, 0:64]),
                             reads=[t_ke.b, self.ident32.b], writes=[ptr.b])
                    S.op("act", lambda e: e.copy(out=KeT[h][:], in_=ptr[:64, :TT]), reads=[ptr.b], writes=[KeT[h].b])
                nd = self.hout
                for b in range(8):
                    pnd = self.ps[4 + b % 2]
                    for h in range(4):
                        vb = (b * 4 + h) * vw
                        bs = slice(b * 64, (b + 1) * 64)
                        S.op("pe", lambda e: e.matmul(pnd[:64, h * 64:(h + 1) * 64], lhsT=vt[:, vb:vb + 64], rhs=Am[h][:, bs], start=True, stop=False),
                             reads=[vt.b, Am[h].b], writes=[pnd.b])
                        S.op("pe", lambda e: e.matmul(pnd[:64, h * 64:(h + 1) * 64], lhsT=Sbf[h][:, 0:64], rhs=Qs[h][:, bs], start=False, stop=True),
                             reads=[Sbf[h].b, Qs[h].b], writes=[pnd.b])
                        if isA:
                            S.op("pe", lambda e: e.matmul(pnd[:64, 256 + h * 64:256 + (h + 1) * 64], lhsT=vt[:, vb + 64:vb + 128], rhs=Am[h][:, bs], start=True, stop=False),
                                 reads=[vt.b, Am[h].b], writes=[pnd.b])
                            S.op("pe", lambda e: e.matmul(pnd[:64, 256 + h * 64:256 + (h + 1) * 64], lhsT=Sbf[h][:, 64:128], rhs=Qs[h][:, bs], start=False, stop=True),
                                 reads=[Sbf[h].b, Qs[h].b], writes=[pnd.b])
                        S.op("pe", lambda e: e.matmul(psU[h][:64, 0:vw], lhsT=KeT[h][:, bs], rhs=vt[:, vb:vb + vw], start=True, stop=True),
                             reads=[KeT[h].b, vt.b], writes=[psU[h].b])
                        S.op("dve", lambda e: e.scalar_tensor_tensor(out=S32[h][:], in0=S32[h][:], scalar=dec[:, h * 8 + b:h * 8 + b + 1], in1=psU[h][:64, 0:vw],
                                                                      op0=ALU.mult, op1=ALU.add), reads=[S32[h].b, dec.b, psU[h].b], writes=[S32[h].b])
                        S.op("act", lambda e: e.copy(out=Sbf[h][:], in_=S32[h][:]), reads=[S32[h].b], writes=[Sbf[h].b])
                    ncl = TT if isA else 256
                    S.op("act", lambda e: e.copy(out=nd[0:64, b * TT:b * TT + ncl], in_=pnd[:64, :ncl]), reads=[pnd.b], writes=[nd.b])
                nd3 = nd[0:64, :].rearrange("p (b x) -> p b x", b=8)
                for h in range(4):
                    numv = nd3[:, :, h * 64:(h + 1) * 64]
                    hh3 = t_hh[:, :].rearrange("p (b t) -> p b t", t=64)
                    if isA:
                        denv = nd3[:, :, 256 + h * 64:256 + (h + 1) * 64]
                        S.op("dve", lambda e: e.scalar_tensor_tensor(out=hh3, in0=denv, scalar=-1.0, in1=denv, op0=ALU.mult, op1=ALU.max), reads=[nd.b], writes=[t_hh.b])
                        S.op("dve", lambda e: e.tensor_scalar(out=t_hh[:, :], in0=t_hh[:, :], scalar1=1.0, scalar2=None, op0=ALU.max), reads=[t_hh.b], writes=[t_hh.b])
                        S.op("dve", lambda e: e.reciprocal(out=t_hh[:, :], in_=t_hh[:, :]), reads=[t_hh.b], writes=[t_hh.b])
                        S.op("dve", lambda e: e.tensor_tensor(out=hh3, in0=numv, in1=hh3, op=ALU.mult), reads=[nd.b, t_hh.b], writes=[t_hh.b])
                    else:
                        S.op("dve", lambda e: e.tensor_copy(out=hh3, in_=numv), reads=[nd.b], writes=[t_hh.b])
                    sq = self.sq
                    S.op("act", lambda e: e.activation(out=sq[0:64, :TT], in_=t_hh[:, :], func=AF.Square), reads=[t_hh.b], writes=[sq.b])
                    pss = self.ps[7]
                    S.op("pe", lambda e: e.matmul(pss[:64, :TT], lhsT=self.ones[0:64, 0:64], rhs=sq[0:64, :TT], start=True, stop=True),
                         reads=[self.ones.b, sq.b], writes=[pss.b])
                    S.op("act", lambda e: e.activation(out=rsh[:], in_=pss[:64, :TT], func=AF.Sqrt, scale=1.0 / 64, bias=self.epsb[0:64, 0:1]),
                         reads=[pss.b, self.epsb.b], writes=[rsh.b])
                    S.op("dve", lambda e: e.reciprocal(out=rsh[:], in_=rsh[:]), reads=[rsh.b], writes=[rsh.b])
                    gi = (l * 2 + (0 if isA else 1)) * 4 + h
                    S.op("dve", lambda e: e.scalar_tensor_tensor(out=t_hh[:, :], in0=t_hh[:, :], scalar=self.hn[:, gi:gi + 1], in1=rsh[:], op0=ALU.mult, op1=ALU.mult),
                         reads=[t_hh.b, self.hn.b, rsh.b], writes=[t_hh.b])
                    S.op("dve", lambda e: e.tensor_tensor(out=yb[:], in0=t_hh[:, :], in1=og[h][:], op=ALU.mult), reads=[t_hh.b, og[h].b], writes=[yb.b])
                    self.y_store(yT, yb, ych0 + h // 2, h, tt)
            S.op("dve", lambda e: e.memset(gc[:, 0:1], 0.0), reads=[v.b for v in sl], writes=[xs.b, gc.b])

    def group_C(self, s, l, yT):
        S, nc = self.S, self.nc
        scale = 0.125
        with contextlib.ExitStack() as es:
            W = T(es, nc, "mbW", [128, 8 * 8 * 64], BF16)
            Wv = T(es, nc, "mbWv", [128, 8 * 256], BF16)
            bvb = T(es, nc, "mbbv", [128, 256], F32)
            kT = [T(es, nc, "mbkT%d" % h, [64, SEQ], BF16) for h in range(4)]
            vtok = T(es, nc, "mbvtok", [128, 16 * 256], BF16)
            TBr = [T(es, nc, "mbTB%d" % h, [128, 1024], F32) for h in range(2)]
            q32 = T(es, nc, "mbq32", [64, TT], F32)
            qT = T(es, nc, "mbqT", [64, TT], BF16)
            k32 = T(es, nc, "mbk32", [64, TT], F32)
            kmean = T(es, nc, "mbkmean", [64, 32], F32)
            gm = T(es, nc, "mbgm", [8, TT], F32)
            gt = T(es, nc, "mbgt", [56, TT], BF16)
            nm = T(es, nc, "mbnm", [8, TT], BF16)
            tmp8 = T(es, nc, "mbtmp8", [8, TT], F32)
            tmpS = [self.hout, self.xs[0]]
            ex = [T(es, nc, "mbex%d" % i, [128, TT], BF16) for i in range(2)]
            rden = T(es, nc, "mbrden", [64, TT], F32)
            yb = T(es, nc, "mbyb", [64, TT], BF16)
            S.dma("pool", W[:].rearrange("p (o k m) -> p o k m", o=8, k=8), self.din["wC_T"][l].rearrange("o p k m -> p o k m"), writes=[W.b])
            S.dma("pool", Wv[:].rearrange("p (k m) -> p k m", k=8), self.din["w_v"][l, 1], writes=[Wv.b])
            S.dma("sp", bvb[:], self.din["b_v"][l, 1:2, :].partition_broadcast(128).rearrange("p a b -> p (a b)"), writes=[bvb.b])
            S.op("dve", lambda e: e.memset(kmean[:], 0.0), writes=[kmean.b])
            mc3 = lambda which, tt: self.mobac[:, (which * 4 + tt) * 2:(which * 4 + tt) * 2 + 2].rearrange("p (a b) -> p a b", b=1).broadcast_to([8, 2, 256])
            v3 = lambda ap: ap.rearrange("p (a b) -> p a b", b=256)
            for tt in range(NT):
                xn = self.xn[tt]
                for h in range(4):
                    pp = self.ps[h % 2]
                    self.proj64(pp, W, (h * 2 + 1) * 512, xn)
                    S.op("act", lambda e: e.activation(out=k32[:], in_=pp[:64, :TT], func=AF.Identity, bias=self.pC[:, l * 8 + h * 2 + 1:l * 8 + h * 2 + 2]),
                         reads=[pp.b, self.pC.b], writes=[k32.b])
                    S.op("act", lambda e: e.copy(out=kT[h][:, tt * TT:(tt + 1) * TT], in_=k32[:]), reads=[k32.b], writes=[kT[h].b])
                    S.op("dve", lambda e: e.reduce_sum(out=kmean[:, h * 8 + 2 * tt:h * 8 + 2 * tt + 2], in_=v3(k32[:]), axis=AX.X),
                         reads=[k32.b], writes=[kmean.b])
                for j in range(4):
                    pv = self.ps[2 + j % 2]
                    for kc in range(KC):
                        S.op("pe", lambda e, kc=kc: e.matmul(pv[:, :256], lhsT=xn[:, kc * TT + j * 128:kc * TT + (j + 1) * 128],
                                                             rhs=Wv[:, kc * 256:(kc + 1) * 256], start=(kc == 0), stop=(kc == KC - 1)),
                             reads=[xn.b, Wv.b], writes=[pv.b])
                    S.op("dve", lambda e: e.tensor_tensor(out=vtok[:, (tt * 4 + j) * 256:(tt * 4 + j + 1) * 256], in0=pv[:, :256], in1=bvb[:], op=ALU.add),
                         reads=[pv.b, bvb.b], writes=[vtok.b])
                for h in range(4):
                    pq = self.ps[1]
                    self.proj64(pq, W, (h * 2) * 512, xn)
                    S.op("act", lambda e: e.activation(out=q32[:], in_=pq[:64, :TT], func=AF.Identity, bias=self.pC[:, l * 8 + h * 2:l * 8 + h * 2 + 1]),
                         reads=[pq.b, self.pC.b], writes=[q32.b])
                    S.op("act", lambda e: e.copy(out=qT[:], in_=q32[:]), reads=[q32.b], writes=[qT.b])
                    pg, pdm = self.ps[4], self.ps[5]
                    S.op("pe", lambda e: e.matmul(pg[:8, :TT], lhsT=kmean[:, h * 8:h * 8 + 8], rhs=q32[:], start=True, stop=True),
                         reads=[kmean.b, q32.b], writes=[pg.b])
                    S.op("dve", lambda e: e.tensor_tensor(out=v3(gm[:]), in0=v3(pg[:8, :TT]), in1=mc3(0, tt), op=ALU.add),
                         reads=[pg.b, self.mobac.b], writes=[gm.b])
                    S.op("pe", lambda e: e.matmul(pdm[:56, :TT], lhsT=self.Pm[:, :], rhs=gm[:], start=True, stop=True),
                         reads=[self.Pm.b, gm.b], writes=[pdm.b])
                    S.op("dve", lambda e: e.tensor_single_scalar(out=gt[:], in_=pdm[:56, :TT], scalar=0.0, op=ALU.is_gt), reads=[pdm.b], writes=[gt.b])
                    S.op("pe", lambda e: e.matmul(pg[:8, :TT], lhsT=self.Agg[:, :], rhs=gt[:], start=True, stop=True),
                         reads=[self.Agg.b, gt.b], writes=[pg.b])
                    S.op("dve", lambda e: e.scalar_tensor_tensor(out=v3(tmp8[:]), in0=v3(pg[:8, :TT]), scalar=2.5, in1=mc3(1, tt), op0=ALU.is_lt, op1=ALU.mult),
                         reads=[pg.b, self.mobac.b], writes=[tmp8.b])
                    S.op("dve", lambda e: e.tensor_tensor(out=v3(tmp8[:]), in0=v3(tmp8[:]), in1=mc3(2, tt), op=ALU.add),
                         reads=[tmp8.b, self.mobac.b], writes=[tmp8.b])
                    S.op("dve", lambda e: e.tensor_scalar(out=nm[:], in0=tmp8[:], scalar1=BIGRAW, scalar2=None, op0=ALU.mult), reads=[tmp8.b], writes=[nm.b])
                    TBh = TBr[h % 2]
                    S.dma("sp", TBh[:], bass.AP(self.tbd, h * 128 * 1152 + 127, [[1151, 128], [1, 1024]]), reads=[self.tbd_b], writes=[TBh.b])
                    po, pd = self.ps[6], self.ps[7]
                    nj = 4 * tt + 4
                    for j in range(nj):
                        pS = self.ps[2 + j % 2]
                        S.op("pe", lambda e: e.matmul(pS[:, :TT], lhsT=kT[h][:, j * 128:(j + 1) * 128], rhs=qT[:], start=True, stop=False),
                             reads=[kT[h].b, qT.b], writes=[pS.b])
                        S.op("pe", lambda e: e.matmul(pS[:, :TT], lhsT=self.seln[:, (j // 2) * 128:(j // 2 + 1) * 128], rhs=nm[:], start=False, stop=True),
                             reads=[self.seln.b, nm.b], writes=[pS.b])
                        o = tt * 512 - j * 128
                        exj = ex[j % 2]
                        if o <= 128:
                            tS = tmpS[j % 2]
                            S.op("dve", lambda e: e.scalar_tensor_tensor(out=tS[:, :TT], in0=pS[:, :TT], scalar=scale, in1=TBh[:, o + 384:o + 384 + 512],
                                                                          op0=ALU.mult, op1=ALU.add), reads=[pS.b, TBh.b], writes=[tS.b])
                            S.op("act", lambda e: e.activation(out=exj[:], in_=tS[:, :TT], func=AF.Exp), reads=[tS.b], writes=[exj.b])
                        else:
                            S.op("act", lambda e: e.activation(out=exj[:], in_=pS[:, :TT], func=AF.Exp, scale=scale, bias=self.b31[:, h:h + 1]),
                                 reads=[pS.b, self.b31.b], writes=[exj.b])
                        S.op("pe", lambda e: e.matmul(po[:64, :TT], lhsT=vtok[:, j * 256 + h * 64:j * 256 + (h + 1) * 64], rhs=exj[:], start=(j == 0), stop=(j == nj - 1)),
                             reads=[vtok.b, exj.b], writes=[po.b])
                        S.op("pe", lambda e: e.matmul(pd[:64, :TT], lhsT=self.ones[:, 0:64], rhs=exj[:], start=(j == 0), stop=(j == nj - 1)),
                             reads=[self.ones.b, exj.b], writes=[pd.b])
                    S.op("dve", lambda e: e.reciprocal(out=rden[:], in_=pd[:64, :TT]), reads=[pd.b], writes=[rden.b])
                    S.op("dve", lambda e: e.tensor_tensor(out=yb[:], in0=po[:64, :TT], in1=rden[:], op=ALU.mult), reads=[po.b, rden.b], writes=[yb.b])
                    self.y_store(yT, yb, 4 + h // 2, h, tt)


N_CORES = 8


def kernel(**inputs):
    inp = {k: np.asarray(v) for k, v in inputs.items()}
    B = inp["x"].shape[0]
    nseq = B // N_CORES
    sh = host_prepare(inp, DEPTH)
    sh = host_prepare_mixer(inp, DEPTH, sh)
    in_maps = []
    for c in range(N_CORES):
        core = dict(sh)
        core["xT"] = np.stack([to_T(inp["x"][c * nseq + b]) for b in range(nseq)])
        core["memT"] = np.stack([np.ascontiguousarray(inp["mem"][c * nseq + b].reshape(NMEM, 8, 128).transpose(2, 1, 0))
                                 for b in range(nseq)])
        in_maps.append(core)
    P = ProgM(dict(depth=DEPTH, nseq=nseq))
    nc = P.build({k: v.shape for k, v in in_maps[0].items()})
    res = run_bass_kernel_spmd(nc, in_maps, core_ids=list(range(N_CORES)))
    out = np.empty((B, SEQ, D), np.float32)
    for c in range(N_CORES):
        o = np.asarray(res.results[c]["outT"])
        for b in range(nseq):
            out[c * nseq + b] = from_T(o[b])
    return out
```

```python
import contextlib
import math
import numpy as np
import concourse.bass as bass
import concourse.mybir as mybir
from concourse.bass_utils import run_bass_kernel_spmd

F32 = mybir.dt.float32
BF16 = mybir.dt.bfloat16
AF = mybir.ActivationFunctionType
ALU = mybir.AluOpType
AX = mybir.AxisListType

D = 1024
SEQ = 2048
NSEQ = 2
DEPTH = 4
TT = 512
NT = SEQ // TT
KC = 8
DFF = 2816
NHC = DFF // 128
NMEM = 256
EPS = 1e-6
NEG = -30000.0


class Buf:
    __slots__ = ("w", "r")

    def __init__(self):
        self.w = None
        self.r = []


class Sched:
    NDMA = 8

    def __init__(self, nc, es):
        self.nc = nc
        self.engs = {"pe": nc.tensor, "act": nc.scalar, "dve": nc.vector,
                     "pool": nc.gpsimd, "sp": nc.sync}
        self.sems = {}
        for k in ("pe", "act", "dve", "pool"):
            self.sems[("e", k)] = es.enter_context(nc.semaphore("prog_" + k))
        for k in ("sp", "pool", "act"):
            for i in range(self.NDMA):
                self.sems[("d", k, i)] = es.enter_context(nc.semaphore("dma_%s_%d" % (k, i)))
        self.cnt = {k: 0 for k in self.engs}
        self.dcnt = {k: 0 for k in self.engs}
        self.waited = {k: {} for k in self.engs}
        self.nops = 0

    def _deps(self, eng, reads, writes):
        need = {}

        def add(tok):
            if tok is None:
                return
            k, v = tok
            if need.get(k, 0) < v:
                need[k] = v
        for b in reads:
            add(b.w)
        for b in writes:
            add(b.w)
            for t in b.r:
                add(t)
        wd = self.waited[eng]
        e = self.engs[eng]
        for k, v in need.items():
            if k == ("e", "pe") and eng == "pe":
                continue
            if wd.get(k, 0) >= v:
                continue
            wd[k] = v
            e.wait_ge(self.sems[k], v)

    def _commit(self, tok, reads, writes):
        for b in reads:
            b.r.append(tok)
            if len(b.r) > 64:
                m = {}
                for k, v in b.r:
                    if m.get(k, 0) < v:
                        m[k] = v
                b.r = list(m.items())
        for b in writes:
            b.w = tok
            b.r = []

    def op(self, eng, fn, reads=(), writes=()):
        self._deps(eng, reads, writes)
        self.cnt[eng] += 1
        k = ("e", eng)
        fn(self.engs[eng]).then_inc(self.sems[k], 1)
        tok = (k, self.cnt[eng])
        self._commit(tok, reads, writes)
        self.nops += 1
        return tok

    def dma(self, eng, out, in_, reads=(), writes=(), **kw):
        j = self.dcnt[eng]
        self.dcnt[eng] += 1
        sk = ("d", eng, j % self.NDMA)
        val = 16 * (j // self.NDMA + 1)
        self._deps(eng, reads, writes)
        if j >= self.NDMA:
            prev = 16 * (j // self.NDMA)
            wd = self.waited[eng]
            if wd.get(sk, 0) < prev:
                wd[sk] = prev
                self.engs[eng].wait_ge(self.sems[sk], prev)
        self.engs[eng].dma_start(out=out, in_=in_, **kw).then_inc(self.sems[sk], 16)
        tok = (sk, val)
        self._commit(tok, reads, writes)
        self.nops += 1
        return tok

    def finish(self, eng, bufs):
        need = {}
        for b in bufs:
            if b.w is not None:
                k, v = b.w
                need[k] = max(need.get(k, 0), v)
        for k, v in need.items():
            self.engs[eng].wait_ge(self.sems[k], v)


class T:
    _n = [0]

    def __init__(self, es, nc, name, shape, dtype, psum=False):
        T._n[0] += 1
        name = "t%d_%s" % (T._n[0], name)
        if psum:
            self.t = es.enter_context(nc.psum_tensor(name, shape, dtype))
        else:
            self.t = es.enter_context(nc.sbuf_tensor(name, shape, dtype))
        self.b = Buf()
        self.b.r = list(T.grave.items())
        es.callback(self._retire)

    grave = {}

    def _retire(self):
        g = T.grave
        toks = list(self.b.r)
        if self.b.w is not None:
            toks.append(self.b.w)
        for k, v in toks:
            if g.get(k, 0) < v:
                g[k] = v

    def __getitem__(self, k):
        return self.t[k]


def arr_w(w, ocw):
    K, N = w.shape
    a = w.reshape(K // 128, 128, N // ocw, ocw)
    return np.ascontiguousarray(a.transpose(2, 1, 0, 3))


def to_T(x):
    a = x.reshape(x.shape[0] // TT, TT, KC, 128)
    return np.ascontiguousarray(a.transpose(0, 3, 2, 1))


def from_T(a):
    return np.ascontiguousarray(a.transpose(0, 3, 2, 1)).reshape(-1, KC * 128)


def colT(v, w=128):
    return np.ascontiguousarray(v.reshape(-1, w).T)


def host_prepare(inp, depth):
    sh = {}
    L = depth
    wup = []
    for l in range(L):
        w = inp["w_ffn_in"][l]
        g = arr_w(w[:, :DFF], 128)
        u = arr_w(w[:, DFF:], 128)
        wup.append(np.concatenate([g, u], axis=3))
    sh["w_up"] = np.stack(wup)
    wd = []
    for l in range(L):
        w = inp["w_ffn_out"][l]
        a = w.reshape(NHC, 128, 8, 128)
        wd.append(np.ascontiguousarray(a.transpose(2, 1, 0, 3)))
    sh["w_down"] = np.stack(wd)
    sh["ffn_cw"] = np.stack([np.stack([colT(inp["ffn_conv_w"][l][j]) for j in range(3)] + [colT(inp["ffn_conv_b"][l])], axis=1)
                             for l in range(L)])
    names = ["norm_mix_pre", "norm_mix_post", "norm_cross_pre", "norm_cross_post", "norm_ffn_pre", "norm_ffn_post", "mem_norm"]
    sh["gains"] = np.stack([np.stack([colT(inp[n][l]) for n in names], axis=1) for l in range(L)])
    sh["w_cq"] = np.stack([arr_w(inp["w_cq"][l], 128) for l in range(L)])
    sh["w_ck"] = np.stack([arr_w(inp["w_ck"][l], 128) for l in range(L)])
    sh["w_cv"] = np.stack([arr_w(inp["w_cv"][l], 512)[0] for l in range(L)])
    sh["w_co"] = np.stack([np.ascontiguousarray(inp["w_co"][l].reshape(4, 128, 8, 128).transpose(2, 1, 0, 3)) for l in range(L)])
    sh["ident"] = np.eye(128, dtype=np.float32)
    return sh


class Prog:
    def __init__(self, cfg):
        self.cfg = cfg
        self.depth = cfg.get("depth", DEPTH)
        self.nseq = cfg.get("nseq", NSEQ)

    def dram_in(self, name, shape, dt=F32):
        return self.nc.dram_tensor(name, list(shape), dt, kind="ExternalInput").ap()

    def build(self, shapes):
        cfg = self.cfg
        nc = self.nc = bass.Bass("TRN2", target_bir_lowering=False)
        L = self.depth
        self.din = {k: self.dram_in(k, v) for k, v in shapes.items()}
        self.outT = nc.dram_tensor("outT", [self.nseq, NT, 128, KC, TT], F32, kind="ExternalOutput").ap()
        self.dbg = {}
        es = self.es = contextlib.ExitStack()
        with es:
            S = self.S = Sched(nc, es)
            T.grave = {}
            self.x_buf = [[Buf() for _ in range(NT)] for _ in range(self.nseq)]
            self.alloc_static()
            self.load_consts()
            for s in range(self.nseq):
                for l in range(L):
                    first = (l == 0)
                    src_first = first
                    if cfg.get("mix", True):
                        self.mixer_sublayer(s, l, src_first)
                        src_first = False
                    if cfg.get("cross", True):
                        self.cross_sublayer(s, l, src_first)
                        src_first = False
                    if cfg.get("ffn", True):
                        self.ffn_sublayer(s, l, src_first)
                        src_first = False
            S.finish("sp", [b for row in self.x_buf for b in row] + list(self.dbg_bufs))
        return nc

    def dump(self, name, t, ap, shape, dt=F32):
        if not self.cfg.get("dump", False) or name in self.dbg:
            return
        d = self.nc.dram_tensor("dbg_" + name, list(shape), dt, kind="ExternalOutput").ap()
        b = Buf()
        self.S.dma("sp", d, ap, reads=[t.b], writes=[b])
        self.dbg[name] = d
        self.dbg_bufs.append(b)

    def alloc_static(self):
        nc, es = self.nc, self.es
        L = self.depth
        self.dbg_bufs = []
        self.ones = T(es, nc, "ones", [128, 128], BF16)
        self.ident = T(es, nc, "ident", [128, 128], BF16)
        self.gains = T(es, nc, "gains", [128, L * 7 * 8], F32)
        self.ffn_cw = T(es, nc, "ffn_cw", [128, L * 4 * NHC], F32)
        self.ps = [T(es, nc, "ps%d" % i, [128, 512], F32, psum=True) for i in range(8)]
        self.wb = [T(es, nc, "wb%d" % i, [128, 6144], BF16) for i in range(2)]
        self.xs = [T(es, nc, "xs%d" % i, [128, KC * TT], F32) for i in range(1)]
        self.hout = T(es, nc, "hout", [128, KC * TT], F32)
        self.sq = T(es, nc, "sq", [128, KC * TT], BF16)
        self.rs = [T(es, nc, "rs%d" % i, [128, TT], F32) for i in range(2)]
        self.xn = [T(es, nc, "xn%d" % i, [128, KC * TT], BF16) for i in range(NT)]
        self.epsb = T(es, nc, "epsb", [128, 1], F32)
        self.wrot = 0

    def load_consts(self):
        S = self.S
        L = self.depth
        S.op("dve", lambda e: e.memset(self.ones[:], 1.0), writes=[self.ones.b])
        S.op("dve", lambda e: e.memset(self.epsb[:], EPS), writes=[self.epsb.b])
        S.dma("pool", self.ident[:], self.din["ident"], writes=[self.ident.b])
        S.dma("sp", self.gains[:].rearrange("p (l f) -> p l f", l=L), self.din["gains"].rearrange("l p a c -> p l (a c)"), writes=[self.gains.b])
        S.dma("sp", self.ffn_cw[:].rearrange("p (l f) -> p l f", l=L), self.din["ffn_cw"].rearrange("l p a c -> p l (a c)"), writes=[self.ffn_cw.b])

    def gain_col(self, l, which, c):
        i = (l * 7 + which) * 8 + c
        return self.gains[:, i:i + 1]

    def next_wb(self):
        w = self.wb[self.wrot % len(self.wb)]
        self.wrot += 1
        return w

    def x_src(self, s, tt, first):
        return self.din["xT"][s, tt] if first else self.outT[s, tt]

    def load_x(self, s, tt, first, dst):
        S = self.S
        S.dma("sp", dst[:].rearrange("p (c t) -> p c t", c=KC), self.x_src(s, tt, first),
              reads=[self.x_buf[s][tt]], writes=[dst.b])

    def rms_T(self, src, ncols, nchunks, dim, psA, rs_out):
        S = self.S
        n = nchunks * ncols
        S.op("act", lambda e: e.activation(out=self.sq[:, :n], in_=src[:, :n], func=AF.Square),
             reads=[src.b], writes=[self.sq.b])
        for c in range(nchunks):
            S.op("pe", lambda e, c=c: e.matmul(psA[:, :ncols], lhsT=self.ones[:], rhs=self.sq[:, c * ncols:(c + 1) * ncols],
                                               start=(c == 0), stop=(c == nchunks - 1)),
                 reads=[self.ones.b, self.sq.b], writes=[psA.b])
        S.op("act", lambda e: e.activation(out=rs_out[:, :ncols], in_=psA[:, :ncols], func=AF.Ln, scale=1.0 / dim, bias=self.epsb[:, 0:1]),
             reads=[psA.b, self.epsb.b], writes=[rs_out.b])
        S.op("act", lambda e: e.activation(out=rs_out[:, :ncols], in_=rs_out[:, :ncols], func=AF.Exp, scale=-0.5),
             reads=[rs_out.b], writes=[rs_out.b])

    def norm_tile(self, s, tt, first, l, which, stage=None):
        S = self.S
        xs = stage if stage is not None else self.xs[0]
        self.load_x(s, tt, first, xs)
        rs = self.rs[tt % 2]
        self.rms_T(xs, TT, KC, D, self.ps[7], rs)
        xn = self.xn[tt]
        for c in range(KC):
            S.op("dve", lambda e, c=c: e.scalar_tensor_tensor(out=xn[:, c * TT:(c + 1) * TT], in0=xs[:, c * TT:(c + 1) * TT],
                                                                scalar=self.gain_col(l, which, c), in1=rs[:, :TT],
                                                                op0=ALU.mult, op1=ALU.mult),
                 reads=[xs.b, rs.b, self.gains.b], writes=[xn.b])

    def residual_tile(self, s, tt, first, l, which):
        S = self.S
        hout = self.hout
        rs = self.rs[tt % 2]
        self.rms_T(hout, TT, KC, D, self.ps[7], rs)
        xs = self.xs[0]
        self.load_x(s, tt, first, xs)
        for c in range(KC):
            sl = slice(c * TT, (c + 1) * TT)
            S.op("dve", lambda e, sl=sl, c=c: e.scalar_tensor_tensor(out=hout[:, sl], in0=hout[:, sl], scalar=self.gain_col(l, which, c),
                                                                      in1=rs[:, :TT], op0=ALU.mult, op1=ALU.mult),
                 reads=[hout.b, rs.b, self.gains.b], writes=[hout.b])
            S.op("pool", lambda e, sl=sl: e.tensor_tensor(out=xs[:, sl], in0=xs[:, sl], in1=hout[:, sl], op=ALU.add),
                 reads=[xs.b, hout.b], writes=[xs.b])
        S.dma("sp", self.outT[s, tt], xs[:].rearrange("p (c t) -> p c t", c=KC), reads=[xs.b], writes=[self.x_buf[s][tt]])

    def ffn_sublayer(self, s, l, first):
        S, nc = self.S, self.nc
        HT = 1024
        NTH = HT // TT
        with contextlib.ExitStack() as es:
            hT = T(es, nc, "ffn_hT", [128, NHC * HT], BF16)
            gsb = T(es, nc, "ffn_g", [128, 2 + HT], F32)
            usb = T(es, nc, "ffn_u", [128, HT], F32)
            a1 = T(es, nc, "ffn_a1", [128, HT], F32)
            halo = T(es, nc, "ffn_halo", [128, NHC * 2], F32)
            S.op("dve", lambda e: e.memset(halo[:], 0.0), writes=[halo.b])
            for half in range(2):
                for k in range(NTH):
                    self.norm_tile(s, half * NTH + k, first, l, 4, stage=(self.xs[0] if k % 2 == 0 else self.hout))
                for g in range(NHC // 2):
                    wt = self.next_wb()
                    S.dma("pool", wt[:, :2 * 8 * 256].rearrange("p (g k m) -> p g k m", g=2, k=8),
                          self.din["w_up"][l, 2 * g:2 * g + 2].rearrange("g p k m -> p g k m"), writes=[wt.b])
                    for ci in range(2):
                        c = 2 * g + ci
                        S.op("act", lambda e, c=c: e.copy(out=gsb[:, 0:2], in_=halo[:, 2 * c:2 * c + 2]),
                             reads=[halo.b], writes=[gsb.b])
                        for k in range(NTH):
                            tt = half * NTH + k
                            pg = self.ps[(2 * k) % 4]
                            pu = self.ps[(2 * k + 1) % 4]
                            for which, pp in ((0, pg), (1, pu)):
                                for kc in range(KC):
                                    off = (ci * 8 + kc) * 256 + which * 128
                                    S.op("pe", lambda e, pp=pp, off=off, kc=kc, tt=tt: e.matmul(
                                        pp[:, :], lhsT=wt[:, off:off + 128], rhs=self.xn[tt][:, kc * TT:(kc + 1) * TT],
                                        start=(kc == 0), stop=(kc == KC - 1)),
                                        reads=[wt.b, self.xn[tt].b], writes=[pp.b])
                            S.op("act", lambda e, k=k, pg=pg: e.copy(out=gsb[:, 2 + k * TT:2 + (k + 1) * TT], in_=pg[:, :]),
                                 reads=[pg.b], writes=[gsb.b])
                            S.op("act", lambda e, k=k, pu=pu: e.copy(out=usb[:, k * TT:(k + 1) * TT], in_=pu[:, :]),
                                 reads=[pu.b], writes=[usb.b])
                        cw = lambda j, c=c: self.ffn_cw[:, (l * 4 + j) * NHC + c:(l * 4 + j) * NHC + c + 1]
                        S.op("dve", lambda e, cw=cw: e.tensor_scalar(out=a1[:], in0=gsb[:, 2:2 + HT], scalar1=cw(2), scalar2=cw(3),
                                                                      op0=ALU.mult, op1=ALU.add),
                             reads=[gsb.b, self.ffn_cw.b], writes=[a1.b])
                        S.op("dve", lambda e, cw=cw: e.scalar_tensor_tensor(out=a1[:], in0=gsb[:, 1:1 + HT], scalar=cw(1), in1=a1[:],
                                                                             op0=ALU.mult, op1=ALU.add),
                             reads=[gsb.b, a1.b, self.ffn_cw.b], writes=[a1.b])
                        S.op("dve", lambda e, cw=cw: e.scalar_tensor_tensor(out=a1[:], in0=gsb[:, 0:HT], scalar=cw(0), in1=a1[:],
                                                                             op0=ALU.mult, op1=ALU.add),
                             reads=[gsb.b, a1.b, self.ffn_cw.b], writes=[a1.b])
                        S.op("act", lambda e, c=c: e.copy(out=halo[:, 2 * c:2 * c + 2], in_=gsb[:, HT:HT + 2]),
                             reads=[gsb.b], writes=[halo.b])
                        S.op("act", lambda e: e.activation(out=a1[:], in_=a1[:], func=AF.Silu), reads=[a1.b], writes=[a1.b])
                        S.op("dve", lambda e, c=c: e.tensor_tensor(out=hT[:, c * HT:(c + 1) * HT], in0=a1[:], in1=usb[:], op=ALU.mult),
                             reads=[a1.b, usb.b], writes=[hT.b])
                houts = [self.hout, None]
                with contextlib.ExitStack() as es2:
                    hout2 = T(es2, nc, "ffn_hout2", [128, KC * TT], F32)
                    hs = [self.hout, hout2]
                    for o in range(KC):
                        wt = self.next_wb()
                        S.dma("pool", wt[:, :NHC * 128].rearrange("p (c m) -> p c m", c=NHC), self.din["w_down"][l, o], writes=[wt.b])
                        for k in range(NTH):
                            pp = self.ps[4 + (o * NTH + k) % 3]
                            for c in range(NHC):
                                S.op("pe", lambda e, pp=pp, c=c, k=k: e.matmul(
                                    pp[:, :], lhsT=wt[:, c * 128:(c + 1) * 128], rhs=hT[:, c * HT + k * TT:c * HT + (k + 1) * TT],
                                    start=(c == 0), stop=(c == NHC - 1)),
                                    reads=[wt.b, hT.b], writes=[pp.b])
                            S.op("act", lambda e, pp=pp, o=o, k=k: e.copy(out=hs[k][:, o * TT:(o + 1) * TT], in_=pp[:, :]),
                                 reads=[pp.b], writes=[hs[k].b])
                    for k in range(NTH):
                        if k == 1:
                            S.op("pool", lambda e: e.tensor_copy(out=self.hout[:], in_=hout2[:]), reads=[hout2.b], writes=[self.hout.b])
                        self.residual_tile(s, half * NTH + k, first, l, 5)

    def cross_sublayer(self, s, l, first):
        S, nc = self.S, self.nc
        with contextlib.ExitStack() as es:
            memf = T(es, nc, "c_memf", [128, KC * NMEM], F32)
            memn = T(es, nc, "c_memn", [128, KC * NMEM], BF16)
            kT = T(es, nc, "c_kT", [128, 4 * NMEM], BF16)
            vtok = T(es, nc, "c_vtok", [128, 2 * 512], BF16)
            wq = T(es, nc, "c_wq", [128, 4 * 8 * 128], BF16)
            wo = T(es, nc, "c_wo", [128, 8 * 4 * 128], BF16)
            qT = T(es, nc, "c_qT", [128, 4 * TT], BF16)
            oT = T(es, nc, "c_oT", [128, 4 * TT], BF16)
            ex = [T(es, nc, "c_ex%d" % i, [128, TT], BF16) for i in range(2)]
            rden = T(es, nc, "c_rden", [128, TT], F32)
            S.dma("pool", wq[:].rearrange("p (o k m) -> p o k m", o=4, k=8), self.din["w_cq"][l].rearrange("o p k m -> p o k m"), writes=[wq.b])
            S.dma("pool", wo[:].rearrange("p (o h m) -> p o h m", o=8, h=4), self.din["w_co"][l].rearrange("o p h m -> p o h m"), writes=[wo.b])
            wk = self.next_wb()
            S.dma("pool", wk[:, :4096].rearrange("p (o k m) -> p o k m", o=4, k=8), self.din["w_ck"][l].rearrange("o p k m -> p o k m"), writes=[wk.b])
            wv = self.next_wb()
            S.dma("pool", wv[:, :4096].rearrange("p (k m) -> p k m", k=8), self.din["w_cv"][l], writes=[wv.b])
            S.dma("sp", memf[:].rearrange("p (c t) -> p c t", c=KC), self.din["memT"][s], writes=[memf.b])
            rs = self.rs[0]
            self.rms_T(memf, NMEM, KC, D, self.ps[7], rs)
            for c in range(KC):
                S.op("dve", lambda e, c=c: e.scalar_tensor_tensor(out=memn[:, c * NMEM:(c + 1) * NMEM], in0=memf[:, c * NMEM:(c + 1) * NMEM],
                                                                    scalar=self.gain_col(l, 6, c), in1=rs[:, :NMEM], op0=ALU.mult, op1=ALU.mult),
                     reads=[memf.b, rs.b, self.gains.b], writes=[memn.b])
            for h in range(4):
                pp = self.ps[h % 2]
                for kc in range(KC):
                    S.op("pe", lambda e, pp=pp, h=h, kc=kc: e.matmul(pp[:, :NMEM], lhsT=wk[:, (h * 8 + kc) * 128:(h * 8 + kc + 1) * 128],
                                                                      rhs=memn[:, kc * NMEM:(kc + 1) * NMEM], start=(kc == 0), stop=(kc == KC - 1)),
                         reads=[wk.b, memn.b], writes=[pp.b])
                S.op("act", lambda e, pp=pp, h=h: e.copy(out=kT[:, h * NMEM:(h + 1) * NMEM], in_=pp[:, :NMEM]), reads=[pp.b], writes=[kT.b])
            for mc in range(2):
                pp = self.ps[2 + mc]
                for kc in range(KC):
                    S.op("pe", lambda e, pp=pp, mc=mc, kc=kc: e.matmul(pp[:, :], lhsT=memn[:, kc * NMEM + mc * 128:kc * NMEM + (mc + 1) * 128],
                                                                        rhs=wv[:, kc * 512:(kc + 1) * 512], start=(kc == 0), stop=(kc == KC - 1)),
                         reads=[wv.b, memn.b], writes=[pp.b])
                S.op("act", lambda e, pp=pp, mc=mc: e.copy(out=vtok[:, mc * 512:(mc + 1) * 512], in_=pp[:, :]), reads=[pp.b], writes=[vtok.b])
            scale = 128 ** -0.5
            for tt in range(NT):
                self.norm_tile(s, tt, first, l, 2)
                xn = self.xn[tt]
                for h in range(4):
                    pp = self.ps[h % 2]
                    for kc in range(KC):
                        S.op("pe", lambda e, pp=pp, h=h, kc=kc: e.matmul(pp[:, :], lhsT=wq[:, (h * 8 + kc) * 128:(h * 8 + kc + 1) * 128],
                                                                          rhs=xn[:, kc * TT:(kc + 1) * TT], start=(kc == 0), stop=(kc == KC - 1)),
                             reads=[wq.b, xn.b], writes=[pp.b])
                    S.op("act", lambda e, pp=pp, h=h: e.copy(out=qT[:, h * TT:(h + 1) * TT], in_=pp[:, :]), reads=[pp.b], writes=[qT.b])
                for h in range(4):
                    po = self.ps[4]
                    pd = self.ps[5]
                    for mc in range(2):
                        pss = self.ps[2 + mc]
                        S.op("pe", lambda e, pss=pss, h=h, mc=mc: e.matmul(pss[:, :], lhsT=kT[:, h * NMEM + mc * 128:h * NMEM + (mc + 1) * 128],
                                                                            rhs=qT[:, h * TT:(h + 1) * TT], start=True, stop=True),
                             reads=[kT.b, qT.b], writes=[pss.b])
                        S.op("act", lambda e, pss=pss, mc=mc: e.activation(out=ex[mc][:], in_=pss[:, :], func=AF.Exp, scale=scale),
                             reads=[pss.b], writes=[ex[mc].b])
                    for mc in range(2):
                        S.op("pe", lambda e, h=h, mc=mc: e.matmul(po[:, :], lhsT=vtok[:, mc * 512 + h * 128:mc * 512 + (h + 1) * 128], rhs=ex[mc][:],
                                                                   start=(mc == 0), stop=(mc == 1)),
                             reads=[vtok.b, ex[mc].b], writes=[po.b])
                    for mc in range(2):
                        S.op("pe", lambda e, mc=mc: e.matmul(pd[:, :], lhsT=self.ones[:], rhs=ex[mc][:], start=(mc == 0), stop=(mc == 1)),
                             reads=[self.ones.b, ex[mc].b], writes=[pd.b])
                    S.op("act", lambda e: e.activation(out=rden[:], in_=pd[:, :], func=AF.Ln), reads=[pd.b], writes=[rden.b])
                    S.op("act", lambda e: e.activation(out=rden[:], in_=rden[:], func=AF.Exp, scale=-1.0), reads=[rden.b], writes=[rden.b])
                    S.op("dve", lambda e, h=h: e.tensor_tensor(out=oT[:, h * TT:(h + 1) * TT], in0=po[:, :], in1=rden[:], op=ALU.mult),
                         reads=[po.b, rden.b], writes=[oT.b])
                for o in range(KC):
                    pp = self.ps[o % 2]
                    for h in range(4):
                        S.op("pe", lambda e, pp=pp, o=o, h=h: e.matmul(pp[:, :], lhsT=wo[:, (o * 4 + h) * 128:(o * 4 + h + 1) * 128],
                                                                        rhs=oT[:, h * TT:(h + 1) * TT], start=(h == 0), stop=(h == 3)),
                             reads=[wo.b, oT.b], writes=[pp.b])
                    S.op("act", lambda e, pp=pp, o=o: e.copy(out=self.hout[:, o * TT:(o + 1) * TT], in_=pp[:, :]), reads=[pp.b], writes=[self.hout.b])
                self.residual_tile(s, tt, first, l, 3)

    def mixer_sublayer(self, s, l, first):
        raise NotImplementedError


OFF = dict(m_q=0, m_k=256, m_v=512, m_o=768, m_i=1024, m_f=1028, c_b=1032, c_c=1288, c_h=1544,
           a_q=1800, a_k=2056, a_v=2312, h_q=2568, h_f=2824, h_i=3080, h_g=3336)
LN8 = math.log(8.0)
BIGRAW = 240000.0


def rel_bucket_np(dist):
    n = np.maximum(dist, 0)
    exact = 16
    nf = np.maximum(n, 1).astype(np.float32)
    large = exact + (np.log(nf / exact) / math.log(128 / exact) * (32 - exact)).astype(np.int32)
    large = np.minimum(large, 31)
    return np.where(n < exact, n, large)


def host_prepare_mixer(inp, depth, sh):
    L = depth
    colsA, colsC, colsD = [], [], []
    for h in range(4):
        colsA += list(range(OFF["m_q"] + 64 * h, OFF["m_q"] + 64 * h + 64))
        colsA += list(range(OFF["m_k"] + 64 * h, OFF["m_k"] + 64 * h + 64))
        colsA += [OFF["m_i"] + h] * 64
        colsA += [OFF["m_f"] + h] * 64
        colsA += list(range(OFF["m_o"] + 64 * h, OFF["m_o"] + 64 * h + 64))
        colsC += list(range(OFF["a_q"] + 64 * h, OFF["a_q"] + 64 * h + 64))
        colsC += list(range(OFF["a_k"] + 64 * h, OFF["a_k"] + 64 * h + 64))
        colsD += list(range(OFF["h_q"] + 64 * h, OFF["h_q"] + 64 * h + 64))
        colsD += list(range(OFF["h_f"] + 64 * h, OFF["h_f"] + 64 * h + 64))
        colsD += list(range(OFF["h_g"] + 64 * h, OFF["h_g"] + 64 * h + 64))
    colsB = list(range(OFF["c_b"], OFF["c_b"] + 768))
    w_in, b_in = inp["w_in"], inp["b_in"]
    sh["wA_T"] = np.stack([arr_w(w_in[l][:, colsA], 64) for l in range(L)])
    sh["wC_T"] = np.stack([arr_w(w_in[l][:, colsC], 64) for l in range(L)])
    sh["wD_T"] = np.stack([arr_w(w_in[l][:, colsD], 64) for l in range(L)])
    sh["wB_T"] = np.stack([arr_w(w_in[l][:, colsB], 128) for l in range(L)])
    vcols = [OFF["m_v"], OFF["a_v"], OFF["h_i"]]
    sh["w_v"] = np.stack([np.stack([arr_w(w_in[l][:, o:o + 256], 256)[0] for o in vcols]) for l in range(L)])
    sh["b_v"] = np.stack([np.stack([b_in[l][o:o + 256] for o in vcols]) for l in range(L)])
    sh["pA"] = np.stack([colT(b_in[l][colsA], 64) for l in range(L)])
    sh["pC"] = np.stack([colT(b_in[l][colsC], 64) for l in range(L)])
    sh["pD"] = np.stack([colT(b_in[l][colsD], 64) for l in range(L)])
    sh["pB"] = np.stack([colT(b_in[l][colsB], 128) for l in range(L)])
    sh["scw"] = np.stack([np.stack([colT(inp["sconv_w"][l][j], 128) for j in range(3)], axis=1) for l in range(L)])
    sh["hn"] = np.stack([np.stack([colT(inp["mlstm_norm"][l], 64), colT(inp["hgrn_norm"][l], 64)], axis=1) for l in range(L)])
    lg = inp["hgrn_lb_logits"]
    sh["lbl"] = np.ascontiguousarray(lg.reshape(4, 4, 64).transpose(2, 1, 0))
    sh["w_mo"] = np.stack([arr_w(inp["w_mix_out"][l], 128) for l in range(L)])
    m8 = np.tile(np.triu(np.ones((64, 64), np.float32)), (1, 8))
    sh["mask8"] = m8
    x = np.arange(1152)
    dist = x - 511
    oh = np.zeros((33, 1152), np.float32)
    bk = rel_bucket_np(dist)
    for i in range(1152):
        if dist[i] >= 0:
            oh[bk[i], i] = 1.0
        else:
            oh[32, i] = 1.0
    sh["oh"] = oh
    ra = np.zeros((33, 4), np.float32)
    ra[:32] = inp["rel_bias"]
    ra[32] = NEG
    sh["rel_aug"] = ra
    sh["b31"] = np.ascontiguousarray(inp["rel_bias"][31:32, :])
    pairs = [(n, m) for n in range(8) for m in range(8) if m != n]
    Pm = np.zeros((8, 56), np.float32)
    Agg = np.zeros((56, 8), np.float32)
    for i, (n, m) in enumerate(pairs):
        Pm[m, i] += 1.0
        Pm[n, i] -= 1.0
        Agg[i, n] = 1.0
    sh["Pm"] = Pm
    sh["Agg"] = Agg
    seln = np.zeros((8, 8, 128), np.float32)
    for n in range(8):
        seln[n, n, :] = 1.0
    sh["seln"] = seln.reshape(8, 1024)
    pastm = np.zeros((8, 4, 2), np.float32)
    validc = np.zeros((8, 4, 2), np.float32)
    ownm1 = np.zeros((8, 4, 2), np.float32)
    for n in range(8):
        for tt in range(4):
            for j in range(2):
                b = 2 * tt + j
                pastm[n, tt, j] = 0.0 if n < b else -1e9
                validc[n, tt, j] = 1.0 if n < b else 0.0
                ownm1[n, tt, j] = (1.0 if n == b else 0.0) - 1.0
    sh["mobac"] = np.stack([pastm, validc, ownm1], axis=1).reshape(8, 24)
    return sh


class V:
    def __init__(self, ap_fn):
        self.f = ap_fn
        self.b = Buf()

    def __getitem__(self, k):
        return self.f()[k]


class ProgM(Prog):
    def alloc_static(self):
        super().alloc_static()
        nc, es, L = self.nc, self.es, self.depth
        self.ident32 = T(es, nc, "ident32", [128, 128], F32)
        self.mask8 = T(es, nc, "mask8", [64, 512], F32)
        self.onesf = T(es, nc, "onesf", [64, 512], F32)
        self.oneb = T(es, nc, "oneb", [128, 1], F32)
        self.pA = T(es, nc, "pA", [64, L * 20], F32)
        self.pC = T(es, nc, "pC", [64, L * 8], F32)
        self.pD = T(es, nc, "pD", [64, L * 12], F32)
        self.pB = T(es, nc, "pB", [128, L * 6], F32)
        self.scw = T(es, nc, "scw", [128, L * 6], F32)
        self.hn = T(es, nc, "hn", [64, L * 8], F32)
        self.lbe = T(es, nc, "lbe", [64, 16], F32)
        self.lb = T(es, nc, "lb", [64, 16], F32)
        self.omlb = T(es, nc, "omlb", [64, 16], F32)
        self.lbs = T(es, nc, "lbs", [64, 4], F32)
        self.rel_aug = T(es, nc, "rel_aug", [33, 4], F32)
        self.b31 = T(es, nc, "b31", [128, 4], F32)
        self.Pm = T(es, nc, "Pm", [8, 56], F32)
        self.Agg = T(es, nc, "Agg", [56, 8], BF16)
        self.seln = T(es, nc, "seln", [8, 1024], BF16)
        self.mobac = T(es, nc, "mobac", [8, 24], F32)
        self.tbd = nc.dram_tensor("tbd", [4, 128, 1152], F32, kind="Internal")
        self.tbd_b = Buf()

    def load_consts(self):
        super().load_consts()
        S, L, din = self.S, self.depth, self.din
        S.dma("sp", self.ident32[:], din["ident"], writes=[self.ident32.b])
        S.dma("sp", self.mask8[:], din["mask8"], writes=[self.mask8.b])
        S.op("dve", lambda e: e.memset(self.onesf[:], 1.0), writes=[self.onesf.b])
        S.op("dve", lambda e: e.memset(self.oneb[:], 1.0), writes=[self.oneb.b])
        for nm, t, w in (("pA", self.pA, 20), ("pC", self.pC, 8), ("pD", self.pD, 12), ("pB", self.pB, 6)):
            S.dma("sp", t[:].rearrange("p (l f) -> p l f", l=L), din[nm].rearrange("l p f -> p l f"), writes=[t.b])
        S.dma("sp", self.scw[:].rearrange("p (l f) -> p l f", l=L), din["scw"].rearrange("l p a c -> p l (a c)"), writes=[self.scw.b])
        S.dma("sp", self.hn[:].rearrange("p (l f) -> p l f", l=L), din["hn"].rearrange("l p a c -> p l (a c)"), writes=[self.hn.b])
        S.dma("sp", self.lbe[:], din["lbl"].rearrange("p h l -> p (h l)"), writes=[self.lbe.b])
        S.dma("sp", self.rel_aug[:], din["rel_aug"], writes=[self.rel_aug.b])
        S.dma("sp", self.b31[:], din["b31"].partition_broadcast(128).rearrange("p a b -> p (a b)"), writes=[self.b31.b])
        S.dma("sp", self.Pm[:], din["Pm"], writes=[self.Pm.b])
        S.dma("pool", self.Agg[:], din["Agg"], writes=[self.Agg.b])
        S.dma("pool", self.seln[:], din["seln"], writes=[self.seln.b])
        S.dma("sp", self.mobac[:], din["mobac"], writes=[self.mobac.b])
        pa3 = self.pA[:].rearrange("p (g j) -> p g j", j=5)
        S.op("dve", lambda e: e.tensor_scalar(out=pa3[:, :, 2:3], in0=pa3[:, :, 2:3], scalar1=-LN8, scalar2=None, op0=ALU.add),
             reads=[self.pA.b], writes=[self.pA.b])
        S.op("dve", lambda e: e.tensor_scalar(out=pa3[:, :, 3:5], in0=pa3[:, :, 3:5], scalar1=-1.0, scalar2=None, op0=ALU.mult),
             reads=[self.pA.b], writes=[self.pA.b])
        lbe3 = self.lbe[:].rearrange("p (h l) -> p h l", l=4)
        S.op("act", lambda e: e.activation(out=self.lbe[:], in_=self.lbe[:], func=AF.Exp), reads=[self.lbe.b], writes=[self.lbe.b])
        S.op("dve", lambda e: e.reduce_sum(out=self.lbs[:], in_=lbe3, axis=AX.X), reads=[self.lbe.b], writes=[self.lbs.b])
        S.op("dve", lambda e: e.reciprocal(out=self.lbs[:], in_=self.lbs[:]), reads=[self.lbs.b], writes=[self.lbs.b])
        for h in range(4):
            S.op("dve", lambda e, h=h: e.tensor_scalar(out=self.lbe[:, h * 4:h * 4 + 4], in0=self.lbe[:, h * 4:h * 4 + 4],
                                                       scalar1=self.lbs[:, h:h + 1], scalar2=None, op0=ALU.mult),
                 reads=[self.lbe.b, self.lbs.b], writes=[self.lbe.b])
        lb3 = self.lb[:].rearrange("p (h l) -> p h l", l=4)
        S.op("dve", lambda e: e.memset(self.lb[:], 0.0), writes=[self.lb.b])
        for li in range(1, 4):
            S.op("dve", lambda e, li=li: e.tensor_tensor(out=lb3[:, :, li:li + 1], in0=lb3[:, :, li - 1:li], in1=lbe3[:, :, li:li + 1], op=ALU.add),
                 reads=[self.lb.b, self.lbe.b], writes=[self.lb.b])
        S.op("dve", lambda e: e.tensor_scalar(out=self.omlb[:], in0=self.lb[:], scalar1=-1.0, scalar2=1.0, op0=ALU.mult, op1=ALU.add),
             reads=[self.lb.b], writes=[self.omlb.b])
        nc = self.nc
        with contextlib.ExitStack() as es2:
            oh = T(es2, nc, "mboh", [33, 1152], F32)
            tbv = T(es2, nc, "mbtbv", [4, 1152], F32)
            S.dma("sp", oh[:], self.din["oh"], writes=[oh.b])
            for j in range(3):
                pp = self.ps[j % 2]
                S.op("pe", lambda e: e.matmul(pp[:4, :384], lhsT=self.rel_aug[:, :], rhs=oh[:, j * 384:(j + 1) * 384], start=True, stop=True),
                     reads=[self.rel_aug.b, oh.b], writes=[pp.b])
                S.op("act", lambda e: e.copy(out=tbv[:, j * 384:(j + 1) * 384], in_=pp[:4, :384]), reads=[pp.b], writes=[tbv.b])
            S.dma("sp", self.tbd.ap(), tbv[:].rearrange("p (a x) -> p a x", a=1).broadcast_to([4, 128, 1152]), reads=[tbv.b], writes=[self.tbd_b])

    def proj64(self, pp, W, woff, xn):
        S = self.S
        for kc in range(KC):
            S.op("pe", lambda e, kc=kc: e.matmul(pp[:64, :TT], lhsT=W[:, woff + kc * 64:woff + (kc + 1) * 64],
                                                 rhs=xn[:, kc * TT:(kc + 1) * TT], start=(kc == 0), stop=(kc == KC - 1)),
                 reads=[W.b, xn.b], writes=[pp.b])

    def y_store(self, yT, yb, chunk, h, tt):
        self.S.dma("sp", yT[(h % 2) * 64:(h % 2) * 64 + 64, chunk * SEQ + tt * TT:chunk * SEQ + (tt + 1) * TT], yb[:64, :TT],
                   reads=[yb.b], writes=[yT.b])

    def mixer_sublayer(self, s, l, first):
        S, nc, cfg = self.S, self.nc, self.cfg
        with contextlib.ExitStack() as es:
            yT = T(es, nc, "yT", [128, 8 * SEQ], BF16)
            self.yT = yT
            groups = cfg.get("groups", "ABCD")
            if groups != "ABCD":
                S.op("pool", lambda e: e.memset(yT[:], 0.0), writes=[yT.b])
            for tt in range(NT):
                self.norm_tile(s, tt, first, l, 0, stage=(self.xs[0] if tt % 2 == 0 else self.hout))
            if "B" in groups:
                self.group_B(s, l, yT)
            if "A" in groups:
                self.group_gla(s, l, yT, "A")
            if "D" in groups:
                self.group_gla(s, l, yT, "D")
            if "C" in groups:
                self.group_C(s, l, yT)
            if cfg.get("dbg_y", False) and s == 0 and l == 0:
                d = nc.dram_tensor("dbg_y", [128, 8 * SEQ], BF16, kind="ExternalOutput").ap()
                b = Buf()
                S.dma("sp", d, yT[:], reads=[yT.b], writes=[b])
                self.dbg_bufs.append(b)
            with contextlib.ExitStack() as es2:
                wo = T(es2, nc, "w_mo", [128, 8 * 8 * 128], BF16)
                S.dma("pool", wo[:].rearrange("p (o k m) -> p o k m", o=8, k=8), self.din["w_mo"][l].rearrange("o p k m -> p o k m"), writes=[wo.b])
                for tt in range(NT):
                    for o in range(KC):
                        pp = self.ps[o % 2]
                        for kc in range(KC):
                            S.op("pe", lambda e, pp=pp, o=o, kc=kc, tt=tt: e.matmul(
                                pp[:, :], lhsT=wo[:, (o * 8 + kc) * 128:(o * 8 + kc + 1) * 128],
                                rhs=yT[:, kc * SEQ + tt * TT:kc * SEQ + (tt + 1) * TT], start=(kc == 0), stop=(kc == KC - 1)),
                                reads=[wo.b, yT.b], writes=[pp.b])
                        S.op("act", lambda e, pp=pp, o=o: e.copy(out=self.hout[:, o * TT:(o + 1) * TT], in_=pp[:, :]),
                             reads=[pp.b], writes=[self.hout.b])
                    self.residual_tile(s, tt, first, l, 1)

    def group_B(self, s, l, yT):
        S, nc = self.S, self.nc
        with contextlib.ExitStack() as es:
            wB = T(es, nc, "wB", [128, 6 * 8 * 128], BF16)
            S.dma("pool", wB[:].rearrange("p (o k m) -> p o k m", o=6, k=8), self.din["wB_T"][l].rearrange("o p k m -> p o k m"), writes=[wB.b])
            u = T(es, nc, "scu", [128, 2 + SEQ], F32)
            cbs = T(es, nc, "sccb", [128, SEQ], F32)
            a = T(es, nc, "sca", [128, SEQ], F32)
            ccs = T(es, nc, "sccc", [128, TT], F32)
            bcol = lambda oc: self.pB[:, l * 6 + oc:l * 6 + oc + 1]
            wcol = lambda j, c: self.scw[:, l * 6 + j * 2 + c:l * 6 + j * 2 + c + 1]
            for j in range(2):
                S.op("dve", lambda e: e.memset(u[:, 0:2], 0.0), writes=[u.b])
                for tt in range(NT):
                    xn = self.xn[tt]
                    pcb, pcc, pch = self.ps[0], self.ps[1], self.ps[2]
                    for oc, pp in ((0 + j, pcb), (2 + j, pcc), (4 + j, pch)):
                        for kc in range(KC):
                            S.op("pe", lambda e, oc=oc, pp=pp, kc=kc: e.matmul(pp[:, :], lhsT=wB[:, (oc * 8 + kc) * 128:(oc * 8 + kc + 1) * 128],
                                                                                rhs=xn[:, kc * TT:(kc + 1) * TT], start=(kc == 0), stop=(kc == KC - 1)),
                                 reads=[wB.b, xn.b], writes=[pp.b])
                    S.op("act", lambda e: e.activation(out=ccs[:], in_=pcc[:, :], func=AF.Identity, bias=bcol(2 + j)),
                         reads=[pcc.b, self.pB.b], writes=[ccs.b])
                    S.op("dve", lambda e, tt=tt: e.scalar_tensor_tensor(out=u[:, 2 + tt * TT:2 + (tt + 1) * TT], in0=pch[:, :], scalar=bcol(4 + j),
                                                                          in1=ccs[:], op0=ALU.add, op1=ALU.mult),
                         reads=[pch.b, ccs.b, self.pB.b], writes=[u.b])
                    if self.cfg.get("dump"):
                        S.op("act", lambda e, tt=tt: e.copy(out=a[:, tt * TT:(tt + 1) * TT], in_=pcb[:, :]), reads=[pcb.b], writes=[a.b])
                    S.op("act", lambda e, tt=tt: e.activation(out=cbs[:, tt * TT:(tt + 1) * TT], in_=pcb[:, :], func=AF.Identity, bias=bcol(0 + j)),
                         reads=[pcb.b, self.pB.b], writes=[cbs.b])
                self.dump("cbs", cbs, cbs[:], [128, SEQ])
                self.dump("araw", a, a[:], [128, SEQ])
                self.dump("wB", wB, wB[:], [128, 6144], BF16)
                self.dump("u", u, u[:], [128, 2 + SEQ])
                self.dump("xn0", self.xn[0], self.xn[0][:], [128, KC * TT], BF16)
                S.op("dve", lambda e: e.tensor_scalar(out=a[:], in0=u[:, 2:2 + SEQ], scalar1=wcol(2, j), scalar2=None, op0=ALU.mult),
                     reads=[u.b, self.scw.b], writes=[a.b])
                S.op("dve", lambda e: e.scalar_tensor_tensor(out=a[:], in0=u[:, 1:1 + SEQ], scalar=wcol(1, j), in1=a[:], op0=ALU.mult, op1=ALU.add),
                     reads=[u.b, a.b, self.scw.b], writes=[a.b])
                S.op("dve", lambda e: e.scalar_tensor_tensor(out=a[:], in0=u[:, 0:SEQ], scalar=wcol(0, j), in1=a[:], op0=ALU.mult, op1=ALU.add),
                     reads=[u.b, a.b, self.scw.b], writes=[a.b])
                S.op("dve", lambda e: e.tensor_tensor(out=yT[:, (2 + j) * SEQ:(3 + j) * SEQ], in0=a[:], in1=cbs[:], op=ALU.mult),
                     reads=[a.b, cbs.b], writes=[yT.b])

    @staticmethod
    def run_interleaved(gens):
        gens = list(gens)
        while gens:
            nxt = []
            for g in gens:
                try:
                    next(g)
                    nxt.append(g)
                except StopIteration:
                    pass
            gens = nxt

    def group_gla(self, s, l, yT, mode):
        S, nc = self.S, self.nc
        isA = mode == "A"
        nT = 5 if isA else 3
        vidx = 0 if isA else 2
        vw = 128 if isA else 64
        ych0 = 0 if isA else 6
        pb = self.pA if isA else self.pD
        with contextlib.ExitStack() as es:
            W = T(es, nc, "glaW", [128, 4 * nT * 8 * 64], BF16)
            Wv = T(es, nc, "glaWv", [128, 8 * 256], BF16)
            bvb = T(es, nc, "glabv", [64, 256], F32)
            vt = T(es, nc, "glavt", [64, 8 * 4 * vw], BF16)
            Qs = [T(es, nc, "glaQs%d" % h, [64, TT], BF16) for h in range(4)]
            Am = [T(es, nc, "glaAm%d" % h, [64, TT], BF16) for h in range(4)]
            KeT = [T(es, nc, "glaKeT%d" % h, [64, TT], BF16) for h in range(4)]
            og = [T(es, nc, "glaog%d" % h, [64, TT], F32) for h in range(4)]
            dec = [T(es, nc, "gladec%d" % h, [64, 8], F32) for h in range(4)]
            Ks = [T(es, nc, "glaKs%d" % i, [64, TT], BF16) for i in range(2)]
            gcs = [T(es, nc, "glagc%d" % i, [64, TT + 8], F32) for i in range(2)]
            yb = [T(es, nc, "glayb%d" % i, [64, TT], BF16) for i in range(2)]
            S32 = [T(es, nc, "glaS32_%d" % h, [64, vw], F32) for h in range(4)]
            Sbf = [T(es, nc, "glaSbf_%d" % h, [64, vw], BF16) for h in range(4)]
            xs = self.xs[0]
            wbf = [self.wb[i] for i in range(2)]
            lane_slots = [
                [V(lambda i=i: xs[0:64, i * TT:(i + 1) * TT]) for i in range(7)],
                [V(lambda i=i: wbf[0][0:64, :].bitcast(F32)[:, i * TT:(i + 1) * TT]) for i in range(6)]
                + [V(lambda: wbf[1][0:64, :].bitcast(F32)[:, 0:TT])],
            ]
            sqv = [V(lambda h=h: self.sq[0:64, h * TT:(h + 1) * TT]) for h in range(4)]
            psU = [self.ps[i] for i in (6, 0, 1, 2)]
            wsrc = self.din["wA_T" if isA else "wD_T"][l]
            S.dma("pool", W[:].rearrange("p (o k m) -> p o k m", o=4 * nT, k=8), wsrc.rearrange("o p k m -> p o k m"), writes=[W.b])
            S.dma("pool", Wv[:].rearrange("p (k m) -> p k m", k=8), self.din["w_v"][l, vidx], writes=[Wv.b])
            S.dma("sp", bvb[:], self.din["b_v"][l, vidx:vidx + 1, :].partition_broadcast(64).rearrange("p a b -> p (a b)"), writes=[bvb.b])
            S.op("dve", lambda e: e.memset(gcs[0][:, 0:1], 0.0), reads=[xs.b], writes=[gcs[0].b] + [v.b for v in lane_slots[0]])
            S.op("dve", lambda e: e.memset(gcs[1][:, 0:1], 0.0), reads=[wbf[0].b, wbf[1].b], writes=[gcs[1].b] + [v.b for v in lane_slots[1]])
            S.op("dve", lambda e: e.memset(vt[:], 1.0), reads=[self.sq.b], writes=[vt.b] + [v.b for v in sqv])
            for h in range(4):
                S.op("dve", lambda e, h=h: e.memset(S32[h][:], 0.0), writes=[S32[h].b])
                S.op("dve", lambda e, h=h: e.memset(Sbf[h][:], 0.0), writes=[Sbf[h].b])
            vt4 = vt[:].rearrange("p (b h w) -> p b h w", b=8, h=4)
            g3 = lambda ap: ap.rearrange("p (b t) -> p b t", t=64)

            def vtok(tt):
                xn = self.xn[tt]
                for b in range(8):
                    pv = self.ps[2 + b % 2]
                    for kc in range(KC):
                        S.op("pe", lambda e, kc=kc: e.matmul(pv[:64, :256], lhsT=xn[:, kc * TT + b * 64:kc * TT + (b + 1) * 64],
                                                             rhs=Wv[:, kc * 256:(kc + 1) * 256], start=(kc == 0), stop=(kc == KC - 1)),
                             reads=[xn.b, Wv.b], writes=[pv.b])
                    S.op("dve", lambda e: e.tensor_tensor(out=vt4[:, b, :, 0:64], in0=pv[:64, :256].rearrange("p (h w) -> p h w", h=4),
                                                          in1=bvb[:].rearrange("p (h w) -> p h w", h=4), op=ALU.add),
                         reads=[pv.b, bvb.b], writes=[vt.b])

            def prep(tt, h, lane):
                xn = self.xn[tt]
                t_q, t_k, t_e, t_f, t_gn, t_x, t_ke = lane_slots[lane]
                gc = gcs[lane]
                ks = Ks[lane]
                p0, p1 = (self.ps[0], self.ps[1]) if lane == 0 else (self.ps[2], self.ps[3])
                pa = self.ps[4 + 2 * lane]
                ptr = self.ps[5 + 2 * lane]
                bc = lambda j: pb[:, (l * 4 + h) * nT + j:(l * 4 + h) * nT + j + 1]
                wo = lambda j: (h * nT + j) * 8 * 64
                if isA:
                    self.proj64(p0, W, wo(0), xn)
                    S.op("act", lambda e: e.activation(out=t_q[:, :], in_=p0[:64, :TT], func=AF.Identity, bias=bc(0)), reads=[p0.b, pb.b], writes=[t_q.b])
                    yield
                    self.proj64(p1, W, wo(1), xn)
                    S.op("act", lambda e: e.activation(out=t_k[:, :], in_=p1[:64, :TT], func=AF.Identity, bias=bc(1)), reads=[p1.b, pb.b], writes=[t_k.b])
                    yield
                    self.proj64(p0, W, wo(2), xn)
                    S.op("act", lambda e: e.activation(out=t_e[:, :], in_=p0[:64, :TT], func=AF.Exp, bias=bc(2)), reads=[p0.b, pb.b], writes=[t_e.b])
                    yield
                    self.proj64(p1, W, wo(3), xn)
                    S.op("act", lambda e: e.activation(out=t_f[:, :], in_=p1[:64, :TT], func=AF.Exp, bias=bc(3), scale=-1.0), reads=[p1.b, pb.b], writes=[t_f.b])
                    S.op("pool", lambda e: e.tensor_tensor(out=t_k[:, :], in0=t_k[:, :], in1=t_e[:, :], op=ALU.mult), reads=[t_k.b, t_e.b], writes=[t_k.b])
                    yield
                    self.proj64(p0, W, wo(4), xn)
                    S.op("act", lambda e: e.activation(out=t_e[:, :], in_=p0[:64, :TT], func=AF.Exp, bias=bc(4), scale=-1.0), reads=[p0.b, pb.b], writes=[t_e.b])
                    yield
                    S.op("act", lambda e: e.activation(out=t_f[:, :], in_=t_f[:, :], func=AF.Ln, bias=self.oneb[0:64, 0:1]), reads=[t_f.b, self.oneb.b], writes=[t_f.b])
                    S.op("act", lambda e: e.activation(out=t_e[:, :], in_=t_e[:, :], func=AF.Ln, bias=self.oneb[0:64, 0:1]), reads=[t_e.b, self.oneb.b], writes=[t_e.b])
                    yield
                    S.op("act", lambda e: e.activation(out=og[h][:], in_=t_e[:, :], func=AF.Exp, scale=-1.0), reads=[t_e.b], writes=[og[h].b])
                    S.op("dve", lambda e: e.tensor_tensor_scan(out=gc[:, 1:TT + 1], data0=self.onesf[:, :], data1=t_f[:, :], initial=0.0, op0=ALU.mult, op1=ALU.add),
                         reads=[self.onesf.b, t_f.b], writes=[gc.b])
                    yield
                else:
                    lbi = h * 4 + l
                    self.proj64(p0, W, wo(0), xn)
                    S.op("act", lambda e: e.activation(out=t_q[:, :], in_=p0[:64, :TT], func=AF.Silu, bias=bc(0)), reads=[p0.b, pb.b], writes=[t_q.b])
                    yield
                    self.proj64(p1, W, wo(1), xn)
                    S.op("act", lambda e: e.activation(out=t_f[:, :], in_=p1[:64, :TT], func=AF.Sigmoid, bias=bc(1)), reads=[p1.b, pb.b], writes=[t_f.b])
                    yield
                    self.proj64(p0, W, wo(2), xn)
                    S.op("act", lambda e: e.activation(out=og[h][:], in_=p0[:64, :TT], func=AF.Silu, bias=bc(2)), reads=[p0.b, pb.b], writes=[og[h].b])
                    S.op("dve", lambda e: e.tensor_scalar(out=t_f[:, :], in0=t_f[:, :], scalar1=self.omlb[:, lbi:lbi + 1], scalar2=self.lb[:, lbi:lbi + 1],
                                                          op0=ALU.mult, op1=ALU.add), reads=[t_f.b, self.omlb.b, self.lb.b], writes=[t_f.b])
                    yield
                    S.op("pool", lambda e: e.tensor_scalar(out=t_k[:, :], in0=t_f[:, :], scalar1=-1.0, scalar2=1.0, op0=ALU.mult, op1=ALU.add),
                         reads=[t_f.b], writes=[t_k.b])
                    S.op("act", lambda e: e.activation(out=t_e[:, :], in_=t_f[:, :], func=AF.Ln), reads=[t_f.b], writes=[t_e.b])
                    yield
                    S.op("dve", lambda e: e.tensor_tensor_scan(out=gc[:, 1:TT + 1], data0=self.onesf[:, :], data1=t_e[:, :], initial=0.0, op0=ALU.mult, op1=ALU.subtract),
                         reads=[self.onesf.b, t_e.b], writes=[gc.b])
                    yield
                gn3 = g3(t_gn[:, :])
                S.op("dve", lambda e: e.tensor_tensor(out=gn3, in0=g3(gc[:, 1:TT + 1]), in1=g3(gc[:, 0:TT])[:, :, 0:1].broadcast_to([64, 8, 64]), op=ALU.subtract),
                     reads=[gc.b], writes=[t_gn.b])
                yield
                S.op("act", lambda e: e.activation(out=t_x[:, :], in_=t_gn[:, :], func=AF.Exp, scale=-1.0), reads=[t_gn.b], writes=[t_x.b])
                S.op("pool", lambda e: e.tensor_tensor(out=g3(t_f[:, :]), in0=gn3, in1=gn3[:, :, 63:64].broadcast_to([64, 8, 64]), op=ALU.subtract),
                     reads=[t_gn.b], writes=[t_f.b])
                yield
                S.op("dve", lambda e: e.tensor_tensor(out=Qs[h][:], in0=t_q[:, :], in1=t_x[:, :], op=ALU.mult), reads=[t_q.b, t_x.b], writes=[Qs[h].b])
                S.op("act", lambda e: e.activation(out=t_e[:, :], in_=t_gn[:, :], func=AF.Exp), reads=[t_gn.b], writes=[t_e.b])
                yield
                S.op("dve", lambda e: e.tensor_tensor(out=ks[:], in0=t_k[:, :], in1=t_e[:, :], op=ALU.mult), reads=[t_k.b, t_e.b], writes=[ks.b])
                S.op("act", lambda e: e.activation(out=t_f[:, :], in_=t_f[:, :], func=AF.Exp), reads=[t_f.b], writes=[t_f.b])
                yield
                for b in range(8):
                    S.op("pe", lambda e, b=b: e.matmul(pa[:64, b * 64:(b + 1) * 64], lhsT=ks[:, b * 64:(b + 1) * 64], rhs=Qs[h][:, b * 64:(b + 1) * 64],
                                                       start=True, stop=True), reads=[ks.b, Qs[h].b], writes=[pa.b])
                S.op("act", lambda e: e.activation(out=dec[h][:, :], in_=t_gn[:, 63:TT:64], func=AF.Exp, scale=-1.0), reads=[t_gn.b], writes=[dec[h].b])
                S.op("pool", lambda e: e.tensor_tensor(out=t_ke[:, :], in0=t_k[:, :], in1=t_f[:, :], op=ALU.mult), reads=[t_k.b, t_f.b], writes=[t_ke.b])
                yield
                S.op("dve", lambda e: e.tensor_tensor(out=Am[h][:], in0=pa[:64, :TT], in1=self.mask8[:, :], op=ALU.mult),
                     reads=[pa.b, self.mask8.b], writes=[Am[h].b])
                for b in range(8):
                    S.op("pe", lambda e, b=b: e.transpose(out=ptr[:64, b * 64:(b + 1) * 64], in_=t_ke[:, b * 64:(b + 1) * 64], identity=self.ident32[0:64, 0:64]),
                         reads=[t_ke.b, self.ident32.b], writes=[ptr.b])
                yield
                S.op("act", lambda e: e.copy(out=KeT[h][:], in_=ptr[:64, :TT]), reads=[ptr.b], writes=[KeT[h].b])
                yield

            nd = self.hout

            def blocks(tt):
                for b in range(8):
                    pnd = self.ps[4 + b % 2]
                    for h in range(4):
                        vb = (b * 4 + h) * vw
                        bs = slice(b * 64, (b + 1) * 64)
                        S.op("pe", lambda e: e.matmul(pnd[:64, h * 64:(h + 1) * 64], lhsT=vt[:, vb:vb + 64], rhs=Am[h][:, bs], start=True, stop=False),
                             reads=[vt.b, Am[h].b], writes=[pnd.b])
                        S.op("pe", lambda e: e.matmul(pnd[:64, h * 64:(h + 1) * 64], lhsT=Sbf[h][:, 0:64], rhs=Qs[h][:, bs], start=False, stop=True),
                             reads=[Sbf[h].b, Qs[h].b], writes=[pnd.b])
                        if isA:
                            S.op("pe", lambda e: e.matmul(pnd[:64, 256 + h * 64:256 + (h + 1) * 64], lhsT=vt[:, vb + 64:vb + 128], rhs=Am[h][:, bs], start=True, stop=False),
                                 reads=[vt.b, Am[h].b], writes=[pnd.b])
                            S.op("pe", lambda e: e.matmul(pnd[:64, 256 + h * 64:256 + (h + 1) * 64], lhsT=Sbf[h][:, 64:128], rhs=Qs[h][:, bs], start=False, stop=True),
                                 reads=[Sbf[h].b, Qs[h].b], writes=[pnd.b])
                        S.op("pe", lambda e: e.matmul(psU[h][:64, 0:vw], lhsT=KeT[h][:, bs], rhs=vt[:, vb:vb + vw], start=True, stop=True),
                             reads=[KeT[h].b, vt.b], writes=[psU[h].b])
                        S.op("dve", lambda e: e.scalar_tensor_tensor(out=S32[h][:], in0=S32[h][:], scalar=dec[h][:, b:b + 1], in1=psU[h][:64, 0:vw],
                                                                      op0=ALU.mult, op1=ALU.add), reads=[S32[h].b, dec[h].b, psU[h].b], writes=[S32[h].b])
                        S.op("act", lambda e: e.copy(out=Sbf[h][:], in_=S32[h][:]), reads=[S32[h].b], writes=[Sbf[h].b])
                    ncl = TT if isA else 256
                    S.op("act", lambda e: e.copy(out=nd[0:64, b * TT:b * TT + ncl], in_=pnd[:64, :ncl]), reads=[pnd.b], writes=[nd.b])

            nd3 = nd[0:64, :].rearrange("p (b x) -> p b x", b=8)

            def outputs(tt, h):
                lane = h % 2
                t_hh = lane_slots[lane][(h // 2) * 2]
                rsh = lane_slots[lane][(h // 2) * 2 + 1]
                ybh = yb[lane]
                sq = sqv[h]
                pss = self.ps[h]
                numv = nd3[:, :, h * 64:(h + 1) * 64]
                hh3 = g3(t_hh[:, :])
                if isA:
                    denv = nd3[:, :, 256 + h * 64:256 + (h + 1) * 64]
                    S.op("dve", lambda e: e.scalar_tensor_tensor(out=hh3, in0=denv, scalar=-1.0, in1=denv, op0=ALU.mult, op1=ALU.max), reads=[nd.b], writes=[t_hh.b])
                    yield
                    S.op("dve", lambda e: e.tensor_scalar(out=t_hh[:, :], in0=t_hh[:, :], scalar1=1.0, scalar2=None, op0=ALU.max), reads=[t_hh.b], writes=[t_hh.b])
                    yield
                    S.op("act", lambda e: e.activation(out=t_hh[:, :], in_=t_hh[:, :], func=AF.Ln), reads=[t_hh.b], writes=[t_hh.b])
                    yield
                    S.op("act", lambda e: e.activation(out=t_hh[:, :], in_=t_hh[:, :], func=AF.Exp, scale=-1.0), reads=[t_hh.b], writes=[t_hh.b])
                    yield
                    S.op("dve", lambda e: e.tensor_tensor(out=hh3, in0=numv, in1=hh3, op=ALU.mult), reads=[nd.b, t_hh.b], writes=[t_hh.b])
                    yield
                else:
                    S.op("act", lambda e: e.copy(out=hh3, in_=numv), reads=[nd.b], writes=[t_hh.b])
                    yield
                S.op("act", lambda e: e.activation(out=sq[:, :], in_=t_hh[:, :], func=AF.Square), reads=[t_hh.b], writes=[sq.b])
                yield
                S.op("pe", lambda e: e.matmul(pss[:64, :TT], lhsT=self.ones[0:64, 0:64], rhs=sq[:, :], start=True, stop=True),
                     reads=[self.ones.b, sq.b], writes=[pss.b])
                yield
                S.op("act", lambda e: e.activation(out=rsh[:, :], in_=pss[:64, :TT], func=AF.Ln, scale=1.0 / 64, bias=self.epsb[0:64, 0:1]),
                     reads=[pss.b, self.epsb.b], writes=[rsh.b])
                yield
                S.op("act", lambda e: e.activation(out=rsh[:, :], in_=rsh[:, :], func=AF.Exp, scale=-0.5), reads=[rsh.b], writes=[rsh.b])
                yield
                gi = (l * 2 + (0 if isA else 1)) * 4 + h
                S.op("dve", lambda e: e.scalar_tensor_tensor(out=t_hh[:, :], in0=t_hh[:, :], scalar=self.hn[:, gi:gi + 1], in1=rsh[:, :], op0=ALU.mult, op1=ALU.mult),
                     reads=[t_hh.b, self.hn.b, rsh.b], writes=[t_hh.b])
                yield
                S.op("dve", lambda e: e.tensor_tensor(out=ybh[:], in0=t_hh[:, :], in1=og[h][:], op=ALU.mult), reads=[t_hh.b, og[h].b], writes=[ybh.b])
                self.y_store(yT, ybh, ych0 + h // 2, h, tt)
                yield

            vtok(0)
            for tt in range(NT):
                self.run_interleaved([prep(tt, 0, 0), prep(tt, 1, 1)])
                self.run_interleaved([prep(tt, 2, 0), prep(tt, 3, 1)])
                blocks(tt)
                if tt + 1 < NT:
                    vtok(tt + 1)
                self.run_interleaved([outputs(tt, h) for h in range(4)])
            S.op("dve", lambda e: e.memset(gcs[0][:, 0:1], 0.0), reads=[v.b for v in lane_slots[0]], writes=[xs.b, gcs[0].b])
            S.op("dve", lambda e: e.memset(gcs[1][:, 0:1], 0.0), reads=[v.b for v in lane_slots[1]], writes=[wbf[0].b, wbf[1].b, gcs[1].b])
            S.op("dve", lambda e: e.memset(gcs[0][:, 0:1], 0.0), reads=[v.b for v in sqv], writes=[self.sq.b, gcs[0].b])

    def group_C(self, s, l, yT):
        S, nc = self.S, self.nc
        scale = 0.125
        with contextlib.ExitStack() as es:
            W = T(es, nc, "mbW", [128, 8 * 8 * 64], BF16)
            Wv = T(es, nc, "mbWv", [128, 8 * 256], BF16)
            bvb = T(es, nc, "mbbv", [128, 256], F32)
            kT = [T(es, nc, "mbkT%d" % h, [64, SEQ], BF16) for h in range(4)]
            vtok = T(es, nc, "mbvtok", [128, 16 * 256], BF16)
            TBr = [T(es, nc, "mbTB%d" % h, [128, 1024], F32) for h in range(2)]
            q32 = T(es, nc, "mbq32", [64, TT], F32)
            qT = T(es, nc, "mbqT", [64, TT], BF16)
            k32 = T(es, nc, "mbk32", [64, TT], F32)
            kmean = T(es, nc, "mbkmean", [64, 32], F32)
            gm = T(es, nc, "mbgm", [8, TT], F32)
            gt = T(es, nc, "mbgt", [56, TT], BF16)
            nm = T(es, nc, "mbnm", [8, TT], BF16)
            tmp8 = T(es, nc, "mbtmp8", [8, TT], F32)
            tmpS = [self.hout, self.xs[0]]
            ex = [T(es, nc, "mbex%d" % i, [128, TT], BF16) for i in range(2)]
            rden = T(es, nc, "mbrden", [64, TT], F32)
            yb = T(es, nc, "mbyb", [64, TT], BF16)
            S.dma("pool", W[:].rearrange("p (o k m) -> p o k m", o=8, k=8), self.din["wC_T"][l].rearrange("o p k m -> p o k m"), writes=[W.b])
            S.dma("pool", Wv[:].rearrange("p (k m) -> p k m", k=8), self.din["w_v"][l, 1], writes=[Wv.b])
            S.dma("sp", bvb[:], self.din["b_v"][l, 1:2, :].partition_broadcast(128).rearrange("p a b -> p (a b)"), writes=[bvb.b])
            S.op("dve", lambda e: e.memset(kmean[:], 0.0), writes=[kmean.b])
            mc3 = lambda which, tt: self.mobac[:, (which * 4 + tt) * 2:(which * 4 + tt) * 2 + 2].rearrange("p (a b) -> p a b", b=1).broadcast_to([8, 2, 256])
            v3 = lambda ap: ap.rearrange("p (a b) -> p a b", b=256)
            for tt in range(NT):
                xn = self.xn[tt]
                for h in range(4):
                    pp = self.ps[h % 2]
                    self.proj64(pp, W, (h * 2 + 1) * 512, xn)
                    S.op("act", lambda e: e.activation(out=k32[:], in_=pp[:64, :TT], func=AF.Identity, bias=self.pC[:, l * 8 + h * 2 + 1:l * 8 + h * 2 + 2]),
                         reads=[pp.b, self.pC.b], writes=[k32.b])
                    S.op("act", lambda e: e.copy(out=kT[h][:, tt * TT:(tt + 1) * TT], in_=k32[:]), reads=[k32.b], writes=[kT[h].b])
                    S.op("dve", lambda e: e.reduce_sum(out=kmean[:, h * 8 + 2 * tt:h * 8 + 2 * tt + 2], in_=v3(k32[:]), axis=AX.X),
                         reads=[k32.b], writes=[kmean.b])
                for j in range(4):
                    pv = self.ps[2 + j % 2]
                    for kc in range(KC):
                        S.op("pe", lambda e, kc=kc: e.matmul(pv[:, :256], lhsT=xn[:, kc * TT + j * 128:kc * TT + (j + 1) * 128],
                                                             rhs=Wv[:, kc * 256:(kc + 1) * 256], start=(kc == 0), stop=(kc == KC - 1)),
                             reads=[xn.b, Wv.b], writes=[pv.b])
                    S.op("dve", lambda e: e.tensor_tensor(out=vtok[:, (tt * 4 + j) * 256:(tt * 4 + j + 1) * 256], in0=pv[:, :256], in1=bvb[:], op=ALU.add),
                         reads=[pv.b, bvb.b], writes=[vtok.b])
                for h in range(4):
                    pq = self.ps[1]
                    self.proj64(pq, W, (h * 2) * 512, xn)
                    S.op("act", lambda e: e.activation(out=q32[:], in_=pq[:64, :TT], func=AF.Identity, bias=self.pC[:, l * 8 + h * 2:l * 8 + h * 2 + 1]),
                         reads=[pq.b, self.pC.b], writes=[q32.b])
                    S.op("act", lambda e: e.copy(out=qT[:], in_=q32[:]), reads=[q32.b], writes=[qT.b])
                    pg, pdm = self.ps[4], self.ps[5]
                    S.op("pe", lambda e: e.matmul(pg[:8, :TT], lhsT=kmean[:, h * 8:h * 8 + 8], rhs=q32[:], start=True, stop=True),
                         reads=[kmean.b, q32.b], writes=[pg.b])
                    S.op("dve", lambda e: e.tensor_tensor(out=v3(gm[:]), in0=v3(pg[:8, :TT]), in1=mc3(0, tt), op=ALU.add),
                         reads=[pg.b, self.mobac.b], writes=[gm.b])
                    S.op("pe", lambda e: e.matmul(pdm[:56, :TT], lhsT=self.Pm[:, :], rhs=gm[:], start=True, stop=True),
                         reads=[self.Pm.b, gm.b], writes=[pdm.b])
                    S.op("dve", lambda e: e.tensor_single_scalar(out=gt[:], in_=pdm[:56, :TT], scalar=0.0, op=ALU.is_gt), reads=[pdm.b], writes=[gt.b])
                    S.op("pe", lambda e: e.matmul(pg[:8, :TT], lhsT=self.Agg[:, :], rhs=gt[:], start=True, stop=True),
                         reads=[self.Agg.b, gt.b], writes=[pg.b])
                    S.op("dve", lambda e: e.scalar_tensor_tensor(out=v3(tmp8[:]), in0=v3(pg[:8, :TT]), scalar=2.5, in1=mc3(1, tt), op0=ALU.is_lt, op1=ALU.mult),
                         reads=[pg.b, self.mobac.b], writes=[tmp8.b])
                    S.op("dve", lambda e: e.tensor_tensor(out=v3(tmp8[:]), in0=v3(tmp8[:]), in1=mc3(2, tt), op=ALU.add),
                         reads=[tmp8.b, self.mobac.b], writes=[tmp8.b])
                    S.op("dve", lambda e: e.tensor_scalar(out=nm[:], in0=tmp8[:], scalar1=BIGRAW, scalar2=None, op0=ALU.mult), reads=[tmp8.b], writes=[nm.b])
                    TBh = TBr[h % 2]
                    S.dma("sp", TBh[:], bass.AP(self.tbd, h * 128 * 1152 + 127, [[1151, 128], [1, 1024]]), reads=[self.tbd_b], writes=[TBh.b])
                    po, pd = self.ps[6], self.ps[7]
                    nj = 4 * tt + 4
                    for j in range(nj):
                        pS = self.ps[2 + j % 2]
                        S.op("pe", lambda e: e.matmul(pS[:, :TT], lhsT=kT[h][:, j * 128:(j + 1) * 128], rhs=qT[:], start=True, stop=False),
                             reads=[kT[h].b, qT.b], writes=[pS.b])
                        S.op("pe", lambda e: e.matmul(pS[:, :TT], lhsT=self.seln[:, (j // 2) * 128:(j // 2 + 1) * 128], rhs=nm[:], start=False, stop=True),
                             reads=[self.seln.b, nm.b], writes=[pS.b])
                        o = tt * 512 - j * 128
                        exj = ex[j % 2]
                        if o <= 128:
                            tS = tmpS[j % 2]
                            S.op("dve", lambda e: e.scalar_tensor_tensor(out=tS[:, :TT], in0=pS[:, :TT], scalar=scale, in1=TBh[:, o + 384:o + 384 + 512],
                                                                          op0=ALU.mult, op1=ALU.add), reads=[pS.b, TBh.b], writes=[tS.b])
                            S.op("act", lambda e: e.activation(out=exj[:], in_=tS[:, :TT], func=AF.Exp), reads=[tS.b], writes=[exj.b])
                        else:
                            S.op("act", lambda e: e.activation(out=exj[:], in_=pS[:, :TT], func=AF.Exp, scale=scale, bias=self.b31[:, h:h + 1]),
                                 reads=[pS.b, self.b31.b], writes=[exj.b])
                        S.op("pe", lambda e: e.matmul(po[:64, :TT], lhsT=vtok[:, j * 256 + h * 64:j * 256 + (h + 1) * 64], rhs=exj[:], start=(j == 0), stop=(j == nj - 1)),
                             reads=[vtok.b, exj.b], writes=[po.b])
                        S.op("pe", lambda e: e.matmul(pd[:64, :TT], lhsT=self.ones[:, 0:64], rhs=exj[:], start=(j == 0), stop=(j == nj - 1)),
                             reads=[self.ones.b, exj.b], writes=[pd.b])
                    S.op("act", lambda e: e.activation(out=rden[:], in_=pd[:64, :TT], func=AF.Ln), reads=[pd.b], writes=[rden.b])
                    S.op("act", lambda e: e.activation(out=rden[:], in_=rden[:], func=AF.Exp, scale=-1.0), reads=[rden.b], writes=[rden.b])
                    S.op("dve", lambda e: e.tensor_tensor(out=yb[:], in0=po[:64, :TT], in1=rden[:], op=ALU.mult), reads=[po.b, rden.b], writes=[yb.b])
                    self.y_store(yT, yb, 4 + h // 2, h, tt)


N_CORES = 8


def kernel(**inputs):
    inp = {k: np.asarray(v) for k, v in inputs.items()}
    B = inp["x"].shape[0]
    nseq = B // N_CORES
    sh = host_prepare(inp, DEPTH)
    sh = host_prepare_mixer(inp, DEPTH, sh)
    in_maps = []
    for c in range(N_CORES):
        core = dict(sh)
        core["xT"] = np.stack([to_T(inp["x"][c * nseq + b]) for b in range(nseq)])
        core["memT"] = np.stack([np.ascontiguousarray(inp["mem"][c * nseq + b].reshape(NMEM, 8, 128).transpose(2, 1, 0))
                                 for b in range(nseq)])
        in_maps.append(core)
    P = ProgM(dict(depth=DEPTH, nseq=nseq))
    nc = P.build({k: v.shape for k, v in in_maps[0].items()})
    res = run_bass_kernel_spmd(nc, in_maps, core_ids=list(range(N_CORES)))
    out = np.empty((B, SEQ, D), np.float32)
    for c in range(N_CORES):
        o = np.asarray(res.results[c]["outT"])
        for b in range(nseq):
            out[c * nseq + b] = from_T(o[b])
    return out
```

```python
import contextlib
import math
import numpy as np
import concourse.bass as bass
import concourse.mybir as mybir
from concourse.bass_utils import run_bass_kernel_spmd

F32 = mybir.dt.float32
BF16 = mybir.dt.bfloat16
AF = mybir.ActivationFunctionType
ALU = mybir.AluOpType
AX = mybir.AxisListType

D = 1024
SEQ = 2048
NSEQ = 2
DEPTH = 4
TT = 512
NT = SEQ // TT
KC = 8
DFF = 2816
NHC = DFF // 128
NMEM = 256
EPS = 1e-6
NEG = -30000.0


class Buf:
    __slots__ = ("w", "r")

    def __init__(self):
        self.w = None
        self.r = []


class Sched:
    NDMA = 8

    def __init__(self, nc, es):
        self.nc = nc
        self.engs = {"pe": nc.tensor, "act": nc.scalar, "dve": nc.vector,
                     "pool": nc.gpsimd, "sp": nc.sync}
        self.sems = {}
        for k in ("pe", "act", "dve", "pool"):
            self.sems[("e", k)] = es.enter_context(nc.semaphore("prog_" + k))
        for k in ("sp", "pool", "act"):
            for i in range(self.NDMA):
                self.sems[("d", k, i)] = es.enter_context(nc.semaphore("dma_%s_%d" % (k, i)))
        self.cnt = {k: 0 for k in self.engs}
        self.dcnt = {k: 0 for k in self.engs}
        self.waited = {k: {} for k in self.engs}
        self.nops = 0

    def _deps(self, eng, reads, writes):
        need = {}

        def add(tok):
            if tok is None:
                return
            k, v = tok
            if need.get(k, 0) < v:
                need[k] = v
        for b in reads:
            add(b.w)
        for b in writes:
            add(b.w)
            for t in b.r:
                add(t)
        wd = self.waited[eng]
        e = self.engs[eng]
        for k, v in need.items():
            if k == ("e", "pe") and eng == "pe":
                continue
            if wd.get(k, 0) >= v:
                continue
            wd[k] = v
            e.wait_ge(self.sems[k], v)

    def _commit(self, tok, reads, writes):
        for b in reads:
            b.r.append(tok)
            if len(b.r) > 64:
                m = {}
                for k, v in b.r:
                    if m.get(k, 0) < v:
                        m[k] = v
                b.r = list(m.items())
        for b in writes:
            b.w = tok
            b.r = []

    def op(self, eng, fn, reads=(), writes=()):
        self._deps(eng, reads, writes)
        self.cnt[eng] += 1
        k = ("e", eng)
        fn(self.engs[eng]).then_inc(self.sems[k], 1)
        tok = (k, self.cnt[eng])
        self._commit(tok, reads, writes)
        self.nops += 1
        return tok

    def dma(self, eng, out, in_, reads=(), writes=(), **kw):
        j = self.dcnt[eng]
        self.dcnt[eng] += 1
        sk = ("d", eng, j % self.NDMA)
        val = 16 * (j // self.NDMA + 1)
        self._deps(eng, reads, writes)
        if j >= self.NDMA:
            prev = 16 * (j // self.NDMA)
            wd = self.waited[eng]
            if wd.get(sk, 0) < prev:
                wd[sk] = prev
                self.engs[eng].wait_ge(self.sems[sk], prev)
        self.engs[eng].dma_start(out=out, in_=in_, **kw).then_inc(self.sems[sk], 16)
        tok = (sk, val)
        self._commit(tok, reads, writes)
        self.nops += 1
        return tok

    def finish(self, eng, bufs):
        need = {}
        for b in bufs:
            if b.w is not None:
                k, v = b.w
                need[k] = max(need.get(k, 0), v)
        for k, v in need.items():
            self.engs[eng].wait_ge(self.sems[k], v)


class T:
    _n = [0]

    def __init__(self, es, nc, name, shape, dtype, psum=False):
        T._n[0] += 1
        name = "t%d_%s" % (T._n[0], name)
        if psum:
            self.t = es.enter_context(nc.psum_tensor(name, shape, dtype))
        else:
            self.t = es.enter_context(nc.sbuf_tensor(name, shape, dtype))
        self.b = Buf()
        self.b.r = list(T.grave.items())
        es.callback(self._retire)

    grave = {}

    def _retire(self):
        g = T.grave
        toks = list(self.b.r)
        if self.b.w is not None:
            toks.append(self.b.w)
        for k, v in toks:
            if g.get(k, 0) < v:
                g[k] = v

    def __getitem__(self, k):
        return self.t[k]


def arr_w(w, ocw):
    K, N = w.shape
    a = w.reshape(K // 128, 128, N // ocw, ocw)
    return np.ascontiguousarray(a.transpose(2, 1, 0, 3))


def to_T(x):
    a = x.reshape(x.shape[0] // TT, TT, KC, 128)
    return np.ascontiguousarray(a.transpose(0, 3, 2, 1))


def from_T(a):
    return np.ascontiguousarray(a.transpose(0, 3, 2, 1)).reshape(-1, KC * 128)


def colT(v, w=128):
    return np.ascontiguousarray(v.reshape(-1, w).T)


def host_prepare(inp, depth):
    sh = {}
    L = depth
    wup = []
    for l in range(L):
        w = inp["w_ffn_in"][l]
        g = arr_w(w[:, :DFF], 128)
        u = arr_w(w[:, DFF:], 128)
        wup.append(np.concatenate([g, u], axis=3))
    sh["w_up"] = np.stack(wup)
    wd = []
    for l in range(L):
        w = inp["w_ffn_out"][l]
        a = w.reshape(NHC, 128, 8, 128)
        wd.append(np.ascontiguousarray(a.transpose(2, 1, 0, 3)))
    sh["w_down"] = np.stack(wd)
    sh["ffn_cw"] = np.stack([np.stack([colT(inp["ffn_conv_w"][l][j]) for j in range(3)] + [colT(inp["ffn_conv_b"][l])], axis=1)
                             for l in range(L)])
    names = ["norm_mix_pre", "norm_mix_post", "norm_cross_pre", "norm_cross_post", "norm_ffn_pre", "norm_ffn_post", "mem_norm"]
    sh["gains"] = np.stack([np.stack([colT(inp[n][l]) for n in names], axis=1) for l in range(L)])
    sh["w_cq"] = np.stack([arr_w(inp["w_cq"][l], 128) for l in range(L)])
    sh["w_ck"] = np.stack([arr_w(inp["w_ck"][l], 128) for l in range(L)])
    sh["w_cv"] = np.stack([arr_w(inp["w_cv"][l], 512)[0] for l in range(L)])
    sh["w_co"] = np.stack([np.ascontiguousarray(inp["w_co"][l].reshape(4, 128, 8, 128).transpose(2, 1, 0, 3)) for l in range(L)])
    sh["ident"] = np.eye(128, dtype=np.float32)
    return sh


class Prog:
    def __init__(self, cfg):
        self.cfg = cfg
        self.depth = cfg.get("depth", DEPTH)
        self.nseq = cfg.get("nseq", NSEQ)

    def dram_in(self, name, shape, dt=F32):
        return self.nc.dram_tensor(name, list(shape), dt, kind="ExternalInput").ap()

    def build(self, shapes):
        cfg = self.cfg
        nc = self.nc = bass.Bass("TRN2", target_bir_lowering=False)
        L = self.depth
        self.din = {k: self.dram_in(k, v) for k, v in shapes.items()}
        self.outT = nc.dram_tensor("outT", [self.nseq, NT, 128, KC, TT], F32, kind="ExternalOutput").ap()
        self.dbg = {}
        es = self.es = contextlib.ExitStack()
        with es:
            S = self.S = Sched(nc, es)
            T.grave = {}
            self.x_buf = [[Buf() for _ in range(NT)] for _ in range(self.nseq)]
            self.alloc_static()
            self.load_consts()
            for s in range(self.nseq):
                for l in range(L):
                    first = (l == 0)
                    src_first = first
                    if cfg.get("mix", True):
                        self.mixer_sublayer(s, l, src_first)
                        src_first = False
                    if cfg.get("cross", True):
                        self.cross_sublayer(s, l, src_first)
                        src_first = False
                    if cfg.get("ffn", True):
                        self.ffn_sublayer(s, l, src_first)
                        src_first = False
            S.finish("sp", [b for row in self.x_buf for b in row] + list(self.dbg_bufs))
        return nc

    def dump(self, name, t, ap, shape, dt=F32):
        if not self.cfg.get("dump", False) or name in self.dbg:
            return
        d = self.nc.dram_tensor("dbg_" + name, list(shape), dt, kind="ExternalOutput").ap()
        b = Buf()
        self.S.dma("sp", d, ap, reads=[t.b], writes=[b])
        self.dbg[name] = d
        self.dbg_bufs.append(b)

    def alloc_static(self):
        nc, es = self.nc, self.es
        L = self.depth
        self.dbg_bufs = []
        self.ones = T(es, nc, "ones", [128, 128], BF16)
        self.ident = T(es, nc, "ident", [128, 128], BF16)
        self.gains = T(es, nc, "gains", [128, L * 7 * 8], F32)
        self.ffn_cw = T(es, nc, "ffn_cw", [128, L * 4 * NHC], F32)
        self.ps = [T(es, nc, "ps%d" % i, [128, 512], F32, psum=True) for i in range(8)]
        self.wb = [T(es, nc, "wb%d" % i, [128, 6144], BF16) for i in range(2)]
        self.xs = [T(es, nc, "xs%d" % i, [128, KC * TT], F32) for i in range(1)]
        self.hout = T(es, nc, "hout", [128, KC * TT], F32)
        self.sq = T(es, nc, "sq", [128, KC * TT], BF16)
        self.rs = [T(es, nc, "rs%d" % i, [128, TT], F32) for i in range(2)]
        self.xn = [T(es, nc, "xn%d" % i, [128, KC * TT], BF16) for i in range(NT)]
        self.epsb = T(es, nc, "epsb", [128, 1], F32)
        self.wrot = 0

    def load_consts(self):
        S = self.S
        L = self.depth
        S.op("dve", lambda e: e.memset(self.ones[:], 1.0), writes=[self.ones.b])
        S.op("dve", lambda e: e.memset(self.epsb[:], EPS), writes=[self.epsb.b])
        S.dma("pool", self.ident[:], self.din["ident"], writes=[self.ident.b])
        S.dma("sp", self.gains[:].rearrange("p (l f) -> p l f", l=L), self.din["gains"].rearrange("l p a c -> p l (a c)"), writes=[self.gains.b])
        S.dma("sp", self.ffn_cw[:].rearrange("p (l f) -> p l f", l=L), self.din["ffn_cw"].rearrange("l p a c -> p l (a c)"), writes=[self.ffn_cw.b])

    def gain_col(self, l, which, c):
        i = (l * 7 + which) * 8 + c
        return self.gains[:, i:i + 1]

    def next_wb(self):
        w = self.wb[self.wrot % len(self.wb)]
        self.wrot += 1
        return w

    def x_src(self, s, tt, first):
        return self.din["xT"][s, tt] if first else self.outT[s, tt]

    def load_x(self, s, tt, first, dst):
        S = self.S
        S.dma("sp", dst[:].rearrange("p (c t) -> p c t", c=KC), self.x_src(s, tt, first),
              reads=[self.x_buf[s][tt]], writes=[dst.b])

    def rms_T(self, src, ncols, nchunks, dim, psA, rs_out):
        S = self.S
        n = nchunks * ncols
        S.op("act", lambda e: e.activation(out=self.sq[:, :n], in_=src[:, :n], func=AF.Square),
             reads=[src.b], writes=[self.sq.b])
        for c in range(nchunks):
            S.op("pe", lambda e, c=c: e.matmul(psA[:, :ncols], lhsT=self.ones[:], rhs=self.sq[:, c * ncols:(c + 1) * ncols],
                                               start=(c == 0), stop=(c == nchunks - 1)),
                 reads=[self.ones.b, self.sq.b], writes=[psA.b])
        S.op("act", lambda e: e.activation(out=rs_out[:, :ncols], in_=psA[:, :ncols], func=AF.Ln, scale=1.0 / dim, bias=self.epsb[:, 0:1]),
             reads=[psA.b, self.epsb.b], writes=[rs_out.b])
        S.op("act", lambda e: e.activation(out=rs_out[:, :ncols], in_=rs_out[:, :ncols], func=AF.Exp, scale=-0.5),
             reads=[rs_out.b], writes=[rs_out.b])

    def norm_tile(self, s, tt, first, l, which, stage=None):
        S = self.S
        xs = stage if stage is not None else self.xs[0]
        self.load_x(s, tt, first, xs)
        rs = self.rs[tt % 2]
        self.rms_T(xs, TT, KC, D, self.ps[7], rs)
        xn = self.xn[tt]
        for c in range(KC):
            S.op("dve", lambda e, c=c: e.scalar_tensor_tensor(out=xn[:, c * TT:(c + 1) * TT], in0=xs[:, c * TT:(c + 1) * TT],
                                                                scalar=self.gain_col(l, which, c), in1=rs[:, :TT],
                                                                op0=ALU.mult, op1=ALU.mult),
                 reads=[xs.b, rs.b, self.gains.b], writes=[xn.b])

    def residual_tile(self, s, tt, first, l, which, hout=None, xs=None):
        S = self.S
        hout = hout if hout is not None else self.hout
        rs = self.rs[tt % 2]
        self.rms_T(hout, TT, KC, D, self.ps[7], rs)
        xs = xs if xs is not None else self.xs[0]
        self.load_x(s, tt, first, xs)
        for c in range(KC):
            sl = slice(c * TT, (c + 1) * TT)
            S.op("dve", lambda e, sl=sl, c=c: e.scalar_tensor_tensor(out=hout[:, sl], in0=hout[:, sl], scalar=self.gain_col(l, which, c),
                                                                      in1=rs[:, :TT], op0=ALU.mult, op1=ALU.mult),
                 reads=[hout.b, rs.b, self.gains.b], writes=[hout.b])
            S.op("pool", lambda e, sl=sl: e.tensor_tensor(out=xs[:, sl], in0=xs[:, sl], in1=hout[:, sl], op=ALU.add),
                 reads=[xs.b, hout.b], writes=[xs.b])
        S.dma("sp", self.outT[s, tt], xs[:].rearrange("p (c t) -> p c t", c=KC), reads=[xs.b], writes=[self.x_buf[s][tt]])

    def ffn_sublayer(self, s, l, first):
        S, nc = self.S, self.nc
        HT = 1024
        NTH = HT // TT
        with contextlib.ExitStack() as es:
            hT = T(es, nc, "ffn_hT", [128, NHC * HT], BF16)
            gsb = T(es, nc, "ffn_g", [128, 2 + HT], F32)
            usb = T(es, nc, "ffn_u", [128, HT], F32)
            a1 = T(es, nc, "ffn_a1", [128, HT], F32)
            halo = T(es, nc, "ffn_halo", [128, NHC * 2], F32)
            S.op("dve", lambda e: e.memset(halo[:], 0.0), writes=[halo.b])
            for tt in range(NT):
                self.norm_tile(s, tt, first, l, 4, stage=(self.xs[0] if tt % 2 == 0 else self.hout))
            for half in range(2):
                for g in range(NHC // 2):
                    wt = self.next_wb()
                    S.dma("pool", wt[:, :2 * 8 * 256].rearrange("p (g k m) -> p g k m", g=2, k=8),
                          self.din["w_up"][l, 2 * g:2 * g + 2].rearrange("g p k m -> p g k m"), writes=[wt.b])
                    for ci in range(2):
                        c = 2 * g + ci
                        S.op("act", lambda e, c=c: e.copy(out=gsb[:, 0:2], in_=halo[:, 2 * c:2 * c + 2]),
                             reads=[halo.b], writes=[gsb.b])
                        for k in range(NTH):
                            tt = half * NTH + k
                            pg = self.ps[(2 * k) % 4]
                            pu = self.ps[(2 * k + 1) % 4]
                            for which, pp in ((0, pg), (1, pu)):
                                for kc in range(KC):
                                    off = (ci * 8 + kc) * 256 + which * 128
                                    S.op("pe", lambda e, pp=pp, off=off, kc=kc, tt=tt: e.matmul(
                                        pp[:, :], lhsT=wt[:, off:off + 128], rhs=self.xn[tt][:, kc * TT:(kc + 1) * TT],
                                        start=(kc == 0), stop=(kc == KC - 1)),
                                        reads=[wt.b, self.xn[tt].b], writes=[pp.b])
                            S.op("act", lambda e, k=k, pg=pg: e.copy(out=gsb[:, 2 + k * TT:2 + (k + 1) * TT], in_=pg[:, :]),
                                 reads=[pg.b], writes=[gsb.b])
                            S.op("act", lambda e, k=k, pu=pu: e.copy(out=usb[:, k * TT:(k + 1) * TT], in_=pu[:, :]),
                                 reads=[pu.b], writes=[usb.b])
                        cw = lambda j, c=c: self.ffn_cw[:, (l * 4 + j) * NHC + c:(l * 4 + j) * NHC + c + 1]
                        S.op("dve", lambda e, cw=cw: e.tensor_scalar(out=a1[:], in0=gsb[:, 2:2 + HT], scalar1=cw(2), scalar2=cw(3),
                                                                      op0=ALU.mult, op1=ALU.add),
                             reads=[gsb.b, self.ffn_cw.b], writes=[a1.b])
                        S.op("dve", lambda e, cw=cw: e.scalar_tensor_tensor(out=a1[:], in0=gsb[:, 1:1 + HT], scalar=cw(1), in1=a1[:],
                                                                             op0=ALU.mult, op1=ALU.add),
                             reads=[gsb.b, a1.b, self.ffn_cw.b], writes=[a1.b])
                        S.op("dve", lambda e, cw=cw: e.scalar_tensor_tensor(out=a1[:], in0=gsb[:, 0:HT], scalar=cw(0), in1=a1[:],
                                                                             op0=ALU.mult, op1=ALU.add),
                             reads=[gsb.b, a1.b, self.ffn_cw.b], writes=[a1.b])
                        S.op("act", lambda e, c=c: e.copy(out=halo[:, 2 * c:2 * c + 2], in_=gsb[:, HT:HT + 2]),
                             reads=[gsb.b], writes=[halo.b])
                        S.op("act", lambda e: e.activation(out=a1[:], in_=a1[:], func=AF.Silu), reads=[a1.b], writes=[a1.b])
                        S.op("dve", lambda e, c=c: e.tensor_tensor(out=hT[:, c * HT:(c + 1) * HT], in0=a1[:], in1=usb[:], op=ALU.mult),
                             reads=[a1.b, usb.b], writes=[hT.b])
                houts = [self.hout, None]
                with contextlib.ExitStack() as es2:
                    hout2 = T(es2, nc, "ffn_hout2", [128, KC * TT], F32)
                    hs = [self.hout, hout2]
                    for o in range(KC):
                        wt = self.next_wb()
                        S.dma("pool", wt[:, :NHC * 128].rearrange("p (c m) -> p c m", c=NHC), self.din["w_down"][l, o], writes=[wt.b])
                        for k in range(NTH):
                            pp = self.ps[4 + (o * NTH + k) % 3]
                            for c in range(NHC):
                                S.op("pe", lambda e, pp=pp, c=c, k=k: e.matmul(
                                    pp[:, :], lhsT=wt[:, c * 128:(c + 1) * 128], rhs=hT[:, c * HT + k * TT:c * HT + (k + 1) * TT],
                                    start=(c == 0), stop=(c == NHC - 1)),
                                    reads=[wt.b, hT.b], writes=[pp.b])
                            S.op("act", lambda e, pp=pp, o=o, k=k: e.copy(out=hs[k][:, o * TT:(o + 1) * TT], in_=pp[:, :]),
                                 reads=[pp.b], writes=[hs[k].b])
                    for k in range(NTH):
                        if k == 1:
                            S.op("pool", lambda e: e.tensor_copy(out=self.hout[:], in_=hout2[:]), reads=[hout2.b], writes=[self.hout.b])
                        self.residual_tile(s, half * NTH + k, first, l, 5)

    def cross_sublayer(self, s, l, first):
        S, nc = self.S, self.nc
        with contextlib.ExitStack() as es:
            memf = T(es, nc, "c_memf", [128, KC * NMEM], F32)
            memn = T(es, nc, "c_memn", [128, KC * NMEM], BF16)
            kT = T(es, nc, "c_kT", [128, 4 * NMEM], BF16)
            vtok = T(es, nc, "c_vtok", [128, 2 * 512], BF16)
            wq = T(es, nc, "c_wq", [128, 4 * 8 * 128], BF16)
            wo = T(es, nc, "c_wo", [128, 8 * 4 * 128], BF16)
            qT = T(es, nc, "c_qT", [128, 4 * TT], BF16)
            oT = T(es, nc, "c_oT", [128, 4 * TT], BF16)
            ex = [T(es, nc, "c_ex%d" % i, [128, TT], BF16) for i in range(4)]
            hout2 = T(es, nc, "c_hout2", [128, KC * TT], F32)
            houts = [self.hout, hout2]
            rden = T(es, nc, "c_rden", [128, TT], F32)
            S.dma("pool", wq[:].rearrange("p (o k m) -> p o k m", o=4, k=8), self.din["w_cq"][l].rearrange("o p k m -> p o k m"), writes=[wq.b])
            S.dma("pool", wo[:].rearrange("p (o h m) -> p o h m", o=8, h=4), self.din["w_co"][l].rearrange("o p h m -> p o h m"), writes=[wo.b])
            wk = self.next_wb()
            S.dma("pool", wk[:, :4096].rearrange("p (o k m) -> p o k m", o=4, k=8), self.din["w_ck"][l].rearrange("o p k m -> p o k m"), writes=[wk.b])
            wv = self.next_wb()
            S.dma("pool", wv[:, :4096].rearrange("p (k m) -> p k m", k=8), self.din["w_cv"][l], writes=[wv.b])
            S.dma("sp", memf[:].rearrange("p (c t) -> p c t", c=KC), self.din["memT"][s], writes=[memf.b])
            rs = self.rs[0]
            self.rms_T(memf, NMEM, KC, D, self.ps[7], rs)
            for c in range(KC):
                S.op("dve", lambda e, c=c: e.scalar_tensor_tensor(out=memn[:, c * NMEM:(c + 1) * NMEM], in0=memf[:, c * NMEM:(c + 1) * NMEM],
                                                                    scalar=self.gain_col(l, 6, c), in1=rs[:, :NMEM], op0=ALU.mult, op1=ALU.mult),
                     reads=[memf.b, rs.b, self.gains.b], writes=[memn.b])
            for h in range(4):
                pp = self.ps[h % 2]
                for kc in range(KC):
                    S.op("pe", lambda e, pp=pp, h=h, kc=kc: e.matmul(pp[:, :NMEM], lhsT=wk[:, (h * 8 + kc) * 128:(h * 8 + kc + 1) * 128],
                                                                      rhs=memn[:, kc * NMEM:(kc + 1) * NMEM], start=(kc == 0), stop=(kc == KC - 1)),
                         reads=[wk.b, memn.b], writes=[pp.b])
                S.op("act", lambda e, pp=pp, h=h: e.copy(out=kT[:, h * NMEM:(h + 1) * NMEM], in_=pp[:, :NMEM]), reads=[pp.b], writes=[kT.b])
            for mc in range(2):
                pp = self.ps[2 + mc]
                for kc in range(KC):
                    S.op("pe", lambda e, pp=pp, mc=mc, kc=kc: e.matmul(pp[:, :], lhsT=memn[:, kc * NMEM + mc * 128:kc * NMEM + (mc + 1) * 128],
                                                                        rhs=wv[:, kc * 512:(kc + 1) * 512], start=(kc == 0), stop=(kc == KC - 1)),
                         reads=[wv.b, memn.b], writes=[pp.b])
                S.op("act", lambda e, pp=pp, mc=mc: e.copy(out=vtok[:, mc * 512:(mc + 1) * 512], in_=pp[:, :]), reads=[pp.b], writes=[vtok.b])
            scale = 128 ** -0.5
            xs2 = T(es, nc, "c_xs2", [128, KC * TT], F32)
            self.norm_tile(s, 0, first, l, 2, stage=xs2)
            for tt in range(NT):
                xn = self.xn[tt]
                for h in range(4):
                    pp = self.ps[h % 2]
                    for kc in range(KC):
                        S.op("pe", lambda e, pp=pp, h=h, kc=kc: e.matmul(pp[:, :], lhsT=wq[:, (h * 8 + kc) * 128:(h * 8 + kc + 1) * 128],
                                                                          rhs=xn[:, kc * TT:(kc + 1) * TT], start=(kc == 0), stop=(kc == KC - 1)),
                             reads=[wq.b, xn.b], writes=[pp.b])
                    S.op("act", lambda e, pp=pp, h=h: e.copy(out=qT[:, h * TT:(h + 1) * TT], in_=pp[:, :]), reads=[pp.b], writes=[qT.b])
                if tt + 1 < NT:
                    self.norm_tile(s, tt + 1, first, l, 2, stage=xs2)
                units = [(h, mc) for h in range(4) for mc in range(2)]
                pob = [(self.ps[4], self.ps[5]), (self.ps[6], self.ps[7])]

                def c1(u):
                    h, mc = units[u]
                    pss = self.ps[2 + u % 2]
                    S.op("pe", lambda e: e.matmul(pss[:, :], lhsT=kT[:, h * NMEM + mc * 128:h * NMEM + (mc + 1) * 128],
                                                  rhs=qT[:, h * TT:(h + 1) * TT], start=True, stop=True),
                         reads=[kT.b, qT.b], writes=[pss.b])
                    S.op("act", lambda e: e.activation(out=ex[u % 4][:], in_=pss[:, :], func=AF.Exp, scale=scale),
                         reads=[pss.b], writes=[ex[u % 4].b])

                def c2(u):
                    h, mc = units[u]
                    po, pd = pob[h % 2]
                    S.op("pe", lambda e: e.matmul(po[:, :], lhsT=vtok[:, mc * 512 + h * 128:mc * 512 + (h + 1) * 128], rhs=ex[u % 4][:],
                                                  start=(mc == 0), stop=(mc == 1)),
                         reads=[vtok.b, ex[u % 4].b], writes=[po.b])
                    S.op("pe", lambda e: e.matmul(pd[:, :], lhsT=self.ones[:], rhs=ex[u % 4][:], start=(mc == 0), stop=(mc == 1)),
                         reads=[self.ones.b, ex[u % 4].b], writes=[pd.b])
                    if mc == 1:
                        S.op("act", lambda e: e.activation(out=rden[:], in_=pd[:, :], func=AF.Ln), reads=[pd.b], writes=[rden.b])
                        S.op("act", lambda e: e.activation(out=rden[:], in_=rden[:], func=AF.Exp, scale=-1.0), reads=[rden.b], writes=[rden.b])
                        S.op("dve", lambda e: e.tensor_tensor(out=oT[:, h * TT:(h + 1) * TT], in0=po[:, :], in1=rden[:], op=ALU.mult),
                             reads=[po.b, rden.b], writes=[oT.b])

                LA = 2
                for u in range(len(units) + LA):
                    if u < len(units):
                        c1(u)
                    if u - LA >= 0:
                        c2(u - LA)
                for o in range(KC):
                    pp = self.ps[o % 2]
                    for h in range(4):
                        S.op("pe", lambda e, pp=pp, o=o, h=h: e.matmul(pp[:, :], lhsT=wo[:, (o * 4 + h) * 128:(o * 4 + h + 1) * 128],
                                                                        rhs=oT[:, h * TT:(h + 1) * TT], start=(h == 0), stop=(h == 3)),
                             reads=[wo.b, oT.b], writes=[pp.b])
                    S.op("act", lambda e, pp=pp, o=o: e.copy(out=houts[tt % 2][:, o * TT:(o + 1) * TT], in_=pp[:, :]), reads=[pp.b], writes=[houts[tt % 2].b])
                self.residual_tile(s, tt, first, l, 3, hout=houts[tt % 2])

    def mixer_sublayer(self, s, l, first):
        raise NotImplementedError


OFF = dict(m_q=0, m_k=256, m_v=512, m_o=768, m_i=1024, m_f=1028, c_b=1032, c_c=1288, c_h=1544,
           a_q=1800, a_k=2056, a_v=2312, h_q=2568, h_f=2824, h_i=3080, h_g=3336)
LN8 = math.log(8.0)
BIGRAW = 240000.0


def rel_bucket_np(dist):
    n = np.maximum(dist, 0)
    exact = 16
    nf = np.maximum(n, 1).astype(np.float32)
    large = exact + (np.log(nf / exact) / math.log(128 / exact) * (32 - exact)).astype(np.int32)
    large = np.minimum(large, 31)
    return np.where(n < exact, n, large)


def host_prepare_mixer(inp, depth, sh):
    L = depth
    colsA, colsC, colsD = [], [], []
    for h in range(4):
        colsA += list(range(OFF["m_q"] + 64 * h, OFF["m_q"] + 64 * h + 64))
        colsA += list(range(OFF["m_k"] + 64 * h, OFF["m_k"] + 64 * h + 64))
        colsA += [OFF["m_i"] + h] * 64
        colsA += [OFF["m_f"] + h] * 64
        colsA += list(range(OFF["m_o"] + 64 * h, OFF["m_o"] + 64 * h + 64))
        colsC += list(range(OFF["a_q"] + 64 * h, OFF["a_q"] + 64 * h + 64))
        colsC += list(range(OFF["a_k"] + 64 * h, OFF["a_k"] + 64 * h + 64))
        colsD += list(range(OFF["h_q"] + 64 * h, OFF["h_q"] + 64 * h + 64))
        colsD += list(range(OFF["h_f"] + 64 * h, OFF["h_f"] + 64 * h + 64))
        colsD += list(range(OFF["h_g"] + 64 * h, OFF["h_g"] + 64 * h + 64))
    colsB = list(range(OFF["c_b"], OFF["c_b"] + 768))
    w_in, b_in = inp["w_in"], inp["b_in"]
    sh["wA_T"] = np.stack([arr_w(w_in[l][:, colsA], 64) for l in range(L)])
    sh["wC_T"] = np.stack([arr_w(w_in[l][:, colsC], 64) for l in range(L)])
    sh["wD_T"] = np.stack([arr_w(w_in[l][:, colsD], 64) for l in range(L)])
    sh["wB_T"] = np.stack([arr_w(w_in[l][:, colsB], 128) for l in range(L)])
    vcols = [OFF["m_v"], OFF["a_v"], OFF["h_i"]]
    sh["w_v"] = np.stack([np.stack([arr_w(w_in[l][:, o:o + 256], 256)[0] for o in vcols]) for l in range(L)])
    sh["b_v"] = np.stack([np.stack([b_in[l][o:o + 256] for o in vcols]) for l in range(L)])
    sh["pA"] = np.stack([colT(b_in[l][colsA], 64) for l in range(L)])
    sh["pC"] = np.stack([colT(b_in[l][colsC], 64) for l in range(L)])
    sh["pD"] = np.stack([colT(b_in[l][colsD], 64) for l in range(L)])
    sh["pB"] = np.stack([colT(b_in[l][colsB], 128) for l in range(L)])
    sh["scw"] = np.stack([np.stack([colT(inp["sconv_w"][l][j], 128) for j in range(3)], axis=1) for l in range(L)])
    sh["hn"] = np.stack([np.stack([colT(inp["mlstm_norm"][l], 64), colT(inp["hgrn_norm"][l], 64)], axis=1) for l in range(L)])
    lg = inp["hgrn_lb_logits"]
    sh["lbl"] = np.ascontiguousarray(lg.reshape(4, 4, 64).transpose(2, 1, 0))
    sh["w_mo"] = np.stack([arr_w(inp["w_mix_out"][l], 128) for l in range(L)])
    m8 = np.tile(np.triu(np.ones((64, 64), np.float32)), (1, 8))
    sh["mask8"] = m8
    x = np.arange(1152)
    dist = x - 511
    oh = np.zeros((33, 1152), np.float32)
    bk = rel_bucket_np(dist)
    for i in range(1152):
        if dist[i] >= 0:
            oh[bk[i], i] = 1.0
        else:
            oh[32, i] = 1.0
    sh["oh"] = oh
    ra = np.zeros((33, 4), np.float32)
    ra[:32] = inp["rel_bias"]
    ra[32] = NEG
    sh["rel_aug"] = ra
    sh["b31"] = np.ascontiguousarray(inp["rel_bias"][31:32, :])
    pairs = [(n, m) for n in range(8) for m in range(8) if m != n]
    Pm = np.zeros((8, 56), np.float32)
    Agg = np.zeros((56, 8), np.float32)
    for i, (n, m) in enumerate(pairs):
        Pm[m, i] += 1.0
        Pm[n, i] -= 1.0
        Agg[i, n] = 1.0
    sh["Pm"] = Pm
    sh["Agg"] = Agg
    seln = np.zeros((8, 8, 128), np.float32)
    for n in range(8):
        seln[n, n, :] = 1.0
    sh["seln"] = seln.reshape(8, 1024)
    pastm = np.zeros((8, 4, 2), np.float32)
    validc = np.zeros((8, 4, 2), np.float32)
    ownm1 = np.zeros((8, 4, 2), np.float32)
    for n in range(8):
        for tt in range(4):
            for j in range(2):
                b = 2 * tt + j
                pastm[n, tt, j] = 0.0 if n < b else -1e9
                validc[n, tt, j] = 1.0 if n < b else 0.0
                ownm1[n, tt, j] = (1.0 if n == b else 0.0) - 1.0
    sh["mobac"] = np.stack([pastm, validc, ownm1], axis=1).reshape(8, 24)
    kind = np.zeros((8, SEQ), np.float32)
    for n in range(8):
        kind[n, n * 256:(n + 1) * 256] = 1.0
    sh["kind"] = kind
    return sh


class V:
    def __init__(self, ap_fn):
        self.f = ap_fn
        self.b = Buf()

    def __getitem__(self, k):
        return self.f()[k]


class ProgM(Prog):
    def alloc_static(self):
        super().alloc_static()
        nc, es, L = self.nc, self.es, self.depth
        self.ident32 = T(es, nc, "ident32", [128, 128], F32)
        self.mask8 = T(es, nc, "mask8", [64, 512], F32)
        self.onesf = T(es, nc, "onesf", [64, 512], F32)
        self.oneb = T(es, nc, "oneb", [128, 1], F32)
        self.pA = T(es, nc, "pA", [64, L * 20], F32)
        self.pC = T(es, nc, "pC", [64, L * 8], F32)
        self.pD = T(es, nc, "pD", [64, L * 12], F32)
        self.pB = T(es, nc, "pB", [128, L * 6], F32)
        self.scw = T(es, nc, "scw", [128, L * 6], F32)
        self.hn = T(es, nc, "hn", [64, L * 8], F32)
        self.lbe = T(es, nc, "lbe", [64, 16], F32)
        self.lb = T(es, nc, "lb", [64, 16], F32)
        self.omlb = T(es, nc, "omlb", [64, 16], F32)
        self.lbs = T(es, nc, "lbs", [64, 4], F32)
        self.rel_aug = T(es, nc, "rel_aug", [33, 4], F32)
        self.b31 = T(es, nc, "b31", [128, 4], F32)
        self.Pm = T(es, nc, "Pm", [8, 56], F32)
        self.Agg = T(es, nc, "Agg", [56, 8], BF16)
        self.seln = T(es, nc, "seln", [8, 1024], BF16)
        self.mobac = T(es, nc, "mobac", [8, 24], F32)
        self.tbd = nc.dram_tensor("tbd", [4, 128, 1152], F32, kind="Internal")
        self.tbd_b = Buf()

    def load_consts(self):
        super().load_consts()
        S, L, din = self.S, self.depth, self.din
        S.dma("sp", self.ident32[:], din["ident"], writes=[self.ident32.b])
        S.dma("sp", self.mask8[:], din["mask8"], writes=[self.mask8.b])
        S.op("dve", lambda e: e.memset(self.onesf[:], 1.0), writes=[self.onesf.b])
        S.op("dve", lambda e: e.memset(self.oneb[:], 1.0), writes=[self.oneb.b])
        for nm, t, w in (("pA", self.pA, 20), ("pC", self.pC, 8), ("pD", self.pD, 12), ("pB", self.pB, 6)):
            S.dma("sp", t[:].rearrange("p (l f) -> p l f", l=L), din[nm].rearrange("l p f -> p l f"), writes=[t.b])
        S.dma("sp", self.scw[:].rearrange("p (l f) -> p l f", l=L), din["scw"].rearrange("l p a c -> p l (a c)"), writes=[self.scw.b])
        S.dma("sp", self.hn[:].rearrange("p (l f) -> p l f", l=L), din["hn"].rearrange("l p a c -> p l (a c)"), writes=[self.hn.b])
        S.dma("sp", self.lbe[:], din["lbl"].rearrange("p h l -> p (h l)"), writes=[self.lbe.b])
        S.dma("sp", self.rel_aug[:], din["rel_aug"], writes=[self.rel_aug.b])
        S.dma("sp", self.b31[:], din["b31"].partition_broadcast(128).rearrange("p a b -> p (a b)"), writes=[self.b31.b])
        S.dma("sp", self.Pm[:], din["Pm"], writes=[self.Pm.b])
        S.dma("pool", self.Agg[:], din["Agg"], writes=[self.Agg.b])
        S.dma("pool", self.seln[:], din["seln"], writes=[self.seln.b])
        S.dma("sp", self.mobac[:], din["mobac"], writes=[self.mobac.b])
        pa3 = self.pA[:].rearrange("p (g j) -> p g j", j=5)
        S.op("dve", lambda e: e.tensor_scalar(out=pa3[:, :, 2:3], in0=pa3[:, :, 2:3], scalar1=-LN8, scalar2=None, op0=ALU.add),
             reads=[self.pA.b], writes=[self.pA.b])
        S.op("dve", lambda e: e.tensor_scalar(out=pa3[:, :, 3:5], in0=pa3[:, :, 3:5], scalar1=-1.0, scalar2=None, op0=ALU.mult),
             reads=[self.pA.b], writes=[self.pA.b])
        lbe3 = self.lbe[:].rearrange("p (h l) -> p h l", l=4)
        S.op("act", lambda e: e.activation(out=self.lbe[:], in_=self.lbe[:], func=AF.Exp), reads=[self.lbe.b], writes=[self.lbe.b])
        S.op("dve", lambda e: e.reduce_sum(out=self.lbs[:], in_=lbe3, axis=AX.X), reads=[self.lbe.b], writes=[self.lbs.b])
        S.op("dve", lambda e: e.reciprocal(out=self.lbs[:], in_=self.lbs[:]), reads=[self.lbs.b], writes=[self.lbs.b])
        for h in range(4):
            S.op("dve", lambda e, h=h: e.tensor_scalar(out=self.lbe[:, h * 4:h * 4 + 4], in0=self.lbe[:, h * 4:h * 4 + 4],
                                                       scalar1=self.lbs[:, h:h + 1], scalar2=None, op0=ALU.mult),
                 reads=[self.lbe.b, self.lbs.b], writes=[self.lbe.b])
        lb3 = self.lb[:].rearrange("p (h l) -> p h l", l=4)
        S.op("dve", lambda e: e.memset(self.lb[:], 0.0), writes=[self.lb.b])
        for li in range(1, 4):
            S.op("dve", lambda e, li=li: e.tensor_tensor(out=lb3[:, :, li:li + 1], in0=lb3[:, :, li - 1:li], in1=lbe3[:, :, li:li + 1], op=ALU.add),
                 reads=[self.lb.b, self.lbe.b], writes=[self.lb.b])
        S.op("dve", lambda e: e.tensor_scalar(out=self.omlb[:], in0=self.lb[:], scalar1=-1.0, scalar2=1.0, op0=ALU.mult, op1=ALU.add),
             reads=[self.lb.b], writes=[self.omlb.b])
        nc = self.nc
        with contextlib.ExitStack() as es2:
            oh = T(es2, nc, "mboh", [33, 1152], F32)
            tbv = T(es2, nc, "mbtbv", [4, 1152], F32)
            S.dma("sp", oh[:], self.din["oh"], writes=[oh.b])
            for j in range(3):
                pp = self.ps[j % 2]
                S.op("pe", lambda e: e.matmul(pp[:4, :384], lhsT=self.rel_aug[:, :], rhs=oh[:, j * 384:(j + 1) * 384], start=True, stop=True),
                     reads=[self.rel_aug.b, oh.b], writes=[pp.b])
                S.op("act", lambda e: e.copy(out=tbv[:, j * 384:(j + 1) * 384], in_=pp[:4, :384]), reads=[pp.b], writes=[tbv.b])
            S.dma("sp", self.tbd.ap(), tbv[:].rearrange("p (a x) -> p a x", a=1).broadcast_to([4, 128, 1152]), reads=[tbv.b], writes=[self.tbd_b])

    def proj64(self, pp, W, woff, xn):
        S = self.S
        for kc in range(KC):
            S.op("pe", lambda e, kc=kc: e.matmul(pp[:64, :TT], lhsT=W[:, woff + kc * 64:woff + (kc + 1) * 64],
                                                 rhs=xn[:, kc * TT:(kc + 1) * TT], start=(kc == 0), stop=(kc == KC - 1)),
                 reads=[W.b, xn.b], writes=[pp.b])

    def y_store(self, yT, yb, chunk, h, tt):
        self.S.dma("sp", yT[(h % 2) * 64:(h % 2) * 64 + 64, chunk * SEQ + tt * TT:chunk * SEQ + (tt + 1) * TT], yb[:64, :TT],
                   reads=[yb.b], writes=[yT.b])

    def mixer_sublayer(self, s, l, first):
        S, nc, cfg = self.S, self.nc, self.cfg
        with contextlib.ExitStack() as es:
            yT = T(es, nc, "yT", [128, 8 * SEQ], BF16)
            self.yT = yT
            groups = cfg.get("groups", "ABCD")
            if groups != "ABCD":
                S.op("pool", lambda e: e.memset(yT[:], 0.0), writes=[yT.b])
            for tt in range(NT):
                self.norm_tile(s, tt, first, l, 0, stage=(self.xs[0] if tt % 2 == 0 else self.hout))
            if "B" in groups:
                self.group_B(s, l, yT)
            if "A" in groups:
                self.group_gla(s, l, yT, "A")
            if "D" in groups:
                self.group_gla(s, l, yT, "D")
            if "C" in groups:
                self.group_C(s, l, yT)
            if cfg.get("dbg_y", False) and s == 0 and l == 0:
                d = nc.dram_tensor("dbg_y", [128, 8 * SEQ], BF16, kind="ExternalOutput").ap()
                b = Buf()
                S.dma("sp", d, yT[:], reads=[yT.b], writes=[b])
                self.dbg_bufs.append(b)
            with contextlib.ExitStack() as es2:
                wo = T(es2, nc, "w_mo", [128, 8 * 8 * 128], BF16)
                S.dma("pool", wo[:].rearrange("p (o k m) -> p o k m", o=8, k=8), self.din["w_mo"][l].rearrange("o p k m -> p o k m"), writes=[wo.b])
                hout2 = T(es2, nc, "mo_hout2", [128, KC * TT], F32)
                houts = [self.hout, hout2]
                for tt in range(NT):
                    ho = houts[tt % 2]
                    for o in range(KC):
                        pp = self.ps[o % 4]
                        for kc in range(KC):
                            S.op("pe", lambda e, pp=pp, o=o, kc=kc, tt=tt: e.matmul(
                                pp[:, :], lhsT=wo[:, (o * 8 + kc) * 128:(o * 8 + kc + 1) * 128],
                                rhs=yT[:, kc * SEQ + tt * TT:kc * SEQ + (tt + 1) * TT], start=(kc == 0), stop=(kc == KC - 1)),
                                reads=[wo.b, yT.b], writes=[pp.b])
                        S.op("act", lambda e, pp=pp, o=o: e.copy(out=ho[:, o * TT:(o + 1) * TT], in_=pp[:, :]),
                             reads=[pp.b], writes=[ho.b])
                    self.residual_tile(s, tt, first, l, 1, hout=ho)

    def group_B(self, s, l, yT):
        S, nc = self.S, self.nc
        with contextlib.ExitStack() as es:
            wB = T(es, nc, "wB", [128, 6 * 8 * 128], BF16)
            S.dma("pool", wB[:].rearrange("p (o k m) -> p o k m", o=6, k=8), self.din["wB_T"][l].rearrange("o p k m -> p o k m"), writes=[wB.b])
            u = T(es, nc, "scu", [128, 2 + SEQ], F32)
            cbs = T(es, nc, "sccb", [128, SEQ], F32)
            a = T(es, nc, "sca", [128, SEQ], F32)
            ccs = T(es, nc, "sccc", [128, TT], F32)
            bcol = lambda oc: self.pB[:, l * 6 + oc:l * 6 + oc + 1]
            wcol = lambda j, c: self.scw[:, l * 6 + j * 2 + c:l * 6 + j * 2 + c + 1]
            for j in range(2):
                S.op("dve", lambda e: e.memset(u[:, 0:2], 0.0), writes=[u.b])
                for tt in range(NT):
                    xn = self.xn[tt]
                    pcb, pcc, pch = self.ps[0], self.ps[1], self.ps[2]
                    for oc, pp in ((0 + j, pcb), (2 + j, pcc), (4 + j, pch)):
                        for kc in range(KC):
                            S.op("pe", lambda e, oc=oc, pp=pp, kc=kc: e.matmul(pp[:, :], lhsT=wB[:, (oc * 8 + kc) * 128:(oc * 8 + kc + 1) * 128],
                                                                                rhs=xn[:, kc * TT:(kc + 1) * TT], start=(kc == 0), stop=(kc == KC - 1)),
                                 reads=[wB.b, xn.b], writes=[pp.b])
                    S.op("act", lambda e: e.activation(out=ccs[:], in_=pcc[:, :], func=AF.Identity, bias=bcol(2 + j)),
                         reads=[pcc.b, self.pB.b], writes=[ccs.b])
                    S.op("dve", lambda e, tt=tt: e.scalar_tensor_tensor(out=u[:, 2 + tt * TT:2 + (tt + 1) * TT], in0=pch[:, :], scalar=bcol(4 + j),
                                                                          in1=ccs[:], op0=ALU.add, op1=ALU.mult),
                         reads=[pch.b, ccs.b, self.pB.b], writes=[u.b])
                    if self.cfg.get("dump"):
                        S.op("act", lambda e, tt=tt: e.copy(out=a[:, tt * TT:(tt + 1) * TT], in_=pcb[:, :]), reads=[pcb.b], writes=[a.b])
                    S.op("act", lambda e, tt=tt: e.activation(out=cbs[:, tt * TT:(tt + 1) * TT], in_=pcb[:, :], func=AF.Identity, bias=bcol(0 + j)),
                         reads=[pcb.b, self.pB.b], writes=[cbs.b])
                self.dump("cbs", cbs, cbs[:], [128, SEQ])
                self.dump("araw", a, a[:], [128, SEQ])
                self.dump("wB", wB, wB[:], [128, 6144], BF16)
                self.dump("u", u, u[:], [128, 2 + SEQ])
                self.dump("xn0", self.xn[0], self.xn[0][:], [128, KC * TT], BF16)
                S.op("dve", lambda e: e.tensor_scalar(out=a[:], in0=u[:, 2:2 + SEQ], scalar1=wcol(2, j), scalar2=None, op0=ALU.mult),
                     reads=[u.b, self.scw.b], writes=[a.b])
                S.op("dve", lambda e: e.scalar_tensor_tensor(out=a[:], in0=u[:, 1:1 + SEQ], scalar=wcol(1, j), in1=a[:], op0=ALU.mult, op1=ALU.add),
                     reads=[u.b, a.b, self.scw.b], writes=[a.b])
                S.op("dve", lambda e: e.scalar_tensor_tensor(out=a[:], in0=u[:, 0:SEQ], scalar=wcol(0, j), in1=a[:], op0=ALU.mult, op1=ALU.add),
                     reads=[u.b, a.b, self.scw.b], writes=[a.b])
                S.op("dve", lambda e: e.tensor_tensor(out=yT[:, (2 + j) * SEQ:(3 + j) * SEQ], in0=a[:], in1=cbs[:], op=ALU.mult),
                     reads=[a.b, cbs.b], writes=[yT.b])

    @staticmethod
    def run_interleaved(gens):
        gens = list(gens)
        while gens:
            nxt = []
            for g in gens:
                try:
                    next(g)
                    nxt.append(g)
                except StopIteration:
                    pass
            gens = nxt

    def group_gla(self, s, l, yT, mode):
        S, nc = self.S, self.nc
        isA = mode == "A"
        nT = 5 if isA else 3
        vidx = 0 if isA else 2
        vw = 128 if isA else 64
        ych0 = 0 if isA else 6
        pb = self.pA if isA else self.pD
        with contextlib.ExitStack() as es:
            W = T(es, nc, "glaW", [128, 4 * nT * 8 * 64], BF16)
            Wv = T(es, nc, "glaWv", [128, 8 * 256], BF16)
            bvb = T(es, nc, "glabv", [64, 256], F32)
            vt = T(es, nc, "glavt", [64, 8 * 4 * vw], BF16)
            Qs = [T(es, nc, "glaQs%d" % h, [64, TT], BF16) for h in range(4)]
            Am = [T(es, nc, "glaAm%d" % h, [64, TT], BF16) for h in range(4)]
            KeT = [T(es, nc, "glaKeT%d" % h, [64, TT], BF16) for h in range(4)]
            og = [T(es, nc, "glaog%d" % h, [64, TT], F32) for h in range(4)]
            dec = [T(es, nc, "gladec%d" % h, [64, 8], F32) for h in range(4)]
            Ks = [T(es, nc, "glaKs%d" % i, [64, TT], BF16) for i in range(2)]
            gcs = [T(es, nc, "glagc%d" % i, [64, TT + 8], F32) for i in range(2)]
            yb = [T(es, nc, "glayb%d" % i, [64, TT], BF16) for i in range(2)]
            S32 = [T(es, nc, "glaS32_%d" % h, [64, vw], F32) for h in range(4)]
            Sbf = [T(es, nc, "glaSbf_%d" % h, [64, vw], BF16) for h in range(4)]
            xs = self.xs[0]
            wbf = [self.wb[i] for i in range(2)]
            lane_slots = [
                [V(lambda i=i: xs[0:64, i * TT:(i + 1) * TT]) for i in range(7)],
                [V(lambda i=i: wbf[0][0:64, :].bitcast(F32)[:, i * TT:(i + 1) * TT]) for i in range(6)]
                + [V(lambda: wbf[1][0:64, :].bitcast(F32)[:, 0:TT])],
            ]
            sqv = [V(lambda h=h: self.sq[0:64, h * TT:(h + 1) * TT]) for h in range(4)]
            psU = [self.ps[i] for i in (6, 0, 1, 2)]
            wsrc = self.din["wA_T" if isA else "wD_T"][l]
            S.dma("pool", W[:].rearrange("p (o k m) -> p o k m", o=4 * nT, k=8), wsrc.rearrange("o p k m -> p o k m"), writes=[W.b])
            S.dma("pool", Wv[:].rearrange("p (k m) -> p k m", k=8), self.din["w_v"][l, vidx], writes=[Wv.b])
            S.dma("sp", bvb[:], self.din["b_v"][l, vidx:vidx + 1, :].partition_broadcast(64).rearrange("p a b -> p (a b)"), writes=[bvb.b])
            S.op("dve", lambda e: e.memset(gcs[0][:, 0:1], 0.0), reads=[xs.b], writes=[gcs[0].b] + [v.b for v in lane_slots[0]])
            S.op("dve", lambda e: e.memset(gcs[1][:, 0:1], 0.0), reads=[wbf[0].b, wbf[1].b], writes=[gcs[1].b] + [v.b for v in lane_slots[1]])
            S.op("dve", lambda e: e.memset(vt[:], 1.0), reads=[self.sq.b], writes=[vt.b] + [v.b for v in sqv])
            for h in range(4):
                S.op("dve", lambda e, h=h: e.memset(S32[h][:], 0.0), writes=[S32[h].b])
                S.op("dve", lambda e, h=h: e.memset(Sbf[h][:], 0.0), writes=[Sbf[h].b])
            vt4 = vt[:].rearrange("p (b h w) -> p b h w", b=8, h=4)
            g3 = lambda ap: ap.rearrange("p (b t) -> p b t", t=64)

            def vtok(tt):
                xn = self.xn[tt]
                for b in range(8):
                    pv = self.ps[2 + b % 2]
                    for kc in range(KC):
                        S.op("pe", lambda e, kc=kc: e.matmul(pv[:64, :256], lhsT=xn[:, kc * TT + b * 64:kc * TT + (b + 1) * 64],
                                                             rhs=Wv[:, kc * 256:(kc + 1) * 256], start=(kc == 0), stop=(kc == KC - 1)),
                             reads=[xn.b, Wv.b], writes=[pv.b])
                    S.op("dve", lambda e: e.tensor_tensor(out=vt4[:, b, :, 0:64], in0=pv[:64, :256].rearrange("p (h w) -> p h w", h=4),
                                                          in1=bvb[:].rearrange("p (h w) -> p h w", h=4), op=ALU.add),
                         reads=[pv.b, bvb.b], writes=[vt.b])

            def prep(tt, h, lane):
                xn = self.xn[tt]
                t_q, t_k, t_e, t_f, t_gn, t_x, t_ke = lane_slots[lane]
                gc = gcs[lane]
                ks = Ks[lane]
                p0, p1 = (self.ps[0], self.ps[1]) if lane == 0 else (self.ps[2], self.ps[3])
                pa = self.ps[4 + 2 * lane]
                ptr = self.ps[5 + 2 * lane]
                bc = lambda j: pb[:, (l * 4 + h) * nT + j:(l * 4 + h) * nT + j + 1]
                wo = lambda j: (h * nT + j) * 8 * 64
                if isA:
                    self.proj64(p0, W, wo(0), xn)
                    S.op("act", lambda e: e.activation(out=t_q[:, :], in_=p0[:64, :TT], func=AF.Identity, bias=bc(0)), reads=[p0.b, pb.b], writes=[t_q.b])
                    yield
                    self.proj64(p1, W, wo(1), xn)
                    S.op("act", lambda e: e.activation(out=t_k[:, :], in_=p1[:64, :TT], func=AF.Identity, bias=bc(1)), reads=[p1.b, pb.b], writes=[t_k.b])
                    yield
                    self.proj64(p0, W, wo(2), xn)
                    S.op("act", lambda e: e.activation(out=t_e[:, :], in_=p0[:64, :TT], func=AF.Exp, bias=bc(2)), reads=[p0.b, pb.b], writes=[t_e.b])
                    yield
                    self.proj64(p1, W, wo(3), xn)
                    S.op("act", lambda e: e.activation(out=t_f[:, :], in_=p1[:64, :TT], func=AF.Exp, bias=bc(3), scale=-1.0), reads=[p1.b, pb.b], writes=[t_f.b])
                    S.op("pool", lambda e: e.tensor_tensor(out=t_k[:, :], in0=t_k[:, :], in1=t_e[:, :], op=ALU.mult), reads=[t_k.b, t_e.b], writes=[t_k.b])
                    yield
                    self.proj64(p0, W, wo(4), xn)
                    S.op("act", lambda e: e.activation(out=t_e[:, :], in_=p0[:64, :TT], func=AF.Exp, bias=bc(4), scale=-1.0), reads=[p0.b, pb.b], writes=[t_e.b])
                    yield
                    S.op("act", lambda e: e.activation(out=t_f[:, :], in_=t_f[:, :], func=AF.Ln, bias=self.oneb[0:64, 0:1]), reads=[t_f.b, self.oneb.b], writes=[t_f.b])
                    S.op("act", lambda e: e.activation(out=t_e[:, :], in_=t_e[:, :], func=AF.Ln, bias=self.oneb[0:64, 0:1]), reads=[t_e.b, self.oneb.b], writes=[t_e.b])
                    yield
                    S.op("act", lambda e: e.activation(out=og[h][:], in_=t_e[:, :], func=AF.Exp, scale=-1.0), reads=[t_e.b], writes=[og[h].b])
                    S.op("dve", lambda e: e.tensor_tensor_scan(out=gc[:, 1:TT + 1], data0=self.onesf[:, :], data1=t_f[:, :], initial=0.0, op0=ALU.mult, op1=ALU.add),
                         reads=[self.onesf.b, t_f.b], writes=[gc.b])
                    yield
                else:
                    lbi = h * 4 + l
                    self.proj64(p0, W, wo(0), xn)
                    S.op("act", lambda e: e.activation(out=t_q[:, :], in_=p0[:64, :TT], func=AF.Silu, bias=bc(0)), reads=[p0.b, pb.b], writes=[t_q.b])
                    yield
                    self.proj64(p1, W, wo(1), xn)
                    S.op("act", lambda e: e.activation(out=t_f[:, :], in_=p1[:64, :TT], func=AF.Sigmoid, bias=bc(1)), reads=[p1.b, pb.b], writes=[t_f.b])
                    yield
                    self.proj64(p0, W, wo(2), xn)
                    S.op("act", lambda e: e.activation(out=og[h][:], in_=p0[:64, :TT], func=AF.Silu, bias=bc(2)), reads=[p0.b, pb.b], writes=[og[h].b])
                    S.op("dve", lambda e: e.tensor_scalar(out=t_f[:, :], in0=t_f[:, :], scalar1=self.omlb[:, lbi:lbi + 1], scalar2=self.lb[:, lbi:lbi + 1],
                                                          op0=ALU.mult, op1=ALU.add), reads=[t_f.b, self.omlb.b, self.lb.b], writes=[t_f.b])
                    yield
                    S.op("pool", lambda e: e.tensor_scalar(out=t_k[:, :], in0=t_f[:, :], scalar1=-1.0, scalar2=1.0, op0=ALU.mult, op1=ALU.add),
                         reads=[t_f.b], writes=[t_k.b])
                    S.op("act", lambda e: e.activation(out=t_e[:, :], in_=t_f[:, :], func=AF.Ln), reads=[t_f.b], writes=[t_e.b])
                    yield
                    S.op("dve", lambda e: e.tensor_tensor_scan(out=gc[:, 1:TT + 1], data0=self.onesf[:, :], data1=t_e[:, :], initial=0.0, op0=ALU.mult, op1=ALU.subtract),
                         reads=[self.onesf.b, t_e.b], writes=[gc.b])
                    yield
                gn3 = g3(t_gn[:, :])
                S.op("dve", lambda e: e.tensor_tensor(out=gn3, in0=g3(gc[:, 1:TT + 1]), in1=g3(gc[:, 0:TT])[:, :, 0:1].broadcast_to([64, 8, 64]), op=ALU.subtract),
                     reads=[gc.b], writes=[t_gn.b])
                yield
                S.op("act", lambda e: e.activation(out=t_x[:, :], in_=t_gn[:, :], func=AF.Exp, scale=-1.0), reads=[t_gn.b], writes=[t_x.b])
                S.op("pool", lambda e: e.tensor_tensor(out=g3(t_f[:, :]), in0=gn3, in1=gn3[:, :, 63:64].broadcast_to([64, 8, 64]), op=ALU.subtract),
                     reads=[t_gn.b], writes=[t_f.b])
                yield
                S.op("dve", lambda e: e.tensor_tensor(out=Qs[h][:], in0=t_q[:, :], in1=t_x[:, :], op=ALU.mult), reads=[t_q.b, t_x.b], writes=[Qs[h].b])
                S.op("act", lambda e: e.activation(out=t_e[:, :], in_=t_gn[:, :], func=AF.Exp), reads=[t_gn.b], writes=[t_e.b])
                yield
                S.op("dve", lambda e: e.tensor_tensor(out=ks[:], in0=t_k[:, :], in1=t_e[:, :], op=ALU.mult), reads=[t_k.b, t_e.b], writes=[ks.b])
                S.op("act", lambda e: e.activation(out=t_f[:, :], in_=t_f[:, :], func=AF.Exp), reads=[t_f.b], writes=[t_f.b])
                yield
                for b in range(8):
                    S.op("pe", lambda e, b=b: e.matmul(pa[:64, b * 64:(b + 1) * 64], lhsT=ks[:, b * 64:(b + 1) * 64], rhs=Qs[h][:, b * 64:(b + 1) * 64],
                                                       start=True, stop=True), reads=[ks.b, Qs[h].b], writes=[pa.b])
                S.op("act", lambda e: e.activation(out=dec[h][:, :], in_=t_gn[:, 63:TT:64], func=AF.Exp, scale=-1.0), reads=[t_gn.b], writes=[dec[h].b])
                S.op("pool", lambda e: e.tensor_tensor(out=t_ke[:, :], in0=t_k[:, :], in1=t_f[:, :], op=ALU.mult), reads=[t_k.b, t_f.b], writes=[t_ke.b])
                yield
                S.op("dve", lambda e: e.tensor_tensor(out=Am[h][:], in0=pa[:64, :TT], in1=self.mask8[:, :], op=ALU.mult),
                     reads=[pa.b, self.mask8.b], writes=[Am[h].b])
                for b in range(8):
                    S.op("pe", lambda e, b=b: e.transpose(out=ptr[:64, b * 64:(b + 1) * 64], in_=t_ke[:, b * 64:(b + 1) * 64], identity=self.ident32[0:64, 0:64]),
                         reads=[t_ke.b, self.ident32.b], writes=[ptr.b])
                yield
                S.op("act", lambda e: e.copy(out=KeT[h][:], in_=ptr[:64, :TT]), reads=[ptr.b], writes=[KeT[h].b])
                yield

            nd = self.hout

            def blocks(tt):
                for b in range(8):
                    pnd = self.ps[4 + b % 2]
                    for h in range(4):
                        vb = (b * 4 + h) * vw
                        bs = slice(b * 64, (b + 1) * 64)
                        S.op("pe", lambda e: e.matmul(pnd[:64, h * 64:(h + 1) * 64], lhsT=vt[:, vb:vb + 64], rhs=Am[h][:, bs], start=True, stop=False),
                             reads=[vt.b, Am[h].b], writes=[pnd.b])
                        S.op("pe", lambda e: e.matmul(pnd[:64, h * 64:(h + 1) * 64], lhsT=Sbf[h][:, 0:64], rhs=Qs[h][:, bs], start=False, stop=True),
                             reads=[Sbf[h].b, Qs[h].b], writes=[pnd.b])
                        if isA:
                            S.op("pe", lambda e: e.matmul(pnd[:64, 256 + h * 64:256 + (h + 1) * 64], lhsT=vt[:, vb + 64:vb + 128], rhs=Am[h][:, bs], start=True, stop=False),
                                 reads=[vt.b, Am[h].b], writes=[pnd.b])
                            S.op("pe", lambda e: e.matmul(pnd[:64, 256 + h * 64:256 + (h + 1) * 64], lhsT=Sbf[h][:, 64:128], rhs=Qs[h][:, bs], start=False, stop=True),
                                 reads=[Sbf[h].b, Qs[h].b], writes=[pnd.b])
                        S.op("pe", lambda e: e.matmul(psU[h][:64, 0:vw], lhsT=KeT[h][:, bs], rhs=vt[:, vb:vb + vw], start=True, stop=True),
                             reads=[KeT[h].b, vt.b], writes=[psU[h].b])
                        S.op("dve", lambda e: e.scalar_tensor_tensor(out=S32[h][:], in0=S32[h][:], scalar=dec[h][:, b:b + 1], in1=psU[h][:64, 0:vw],
                                                                      op0=ALU.mult, op1=ALU.add), reads=[S32[h].b, dec[h].b, psU[h].b], writes=[S32[h].b])
                        S.op("act", lambda e: e.copy(out=Sbf[h][:], in_=S32[h][:]), reads=[S32[h].b], writes=[Sbf[h].b])
                    ncl = TT if isA else 256
                    S.op("act", lambda e: e.copy(out=nd[0:64, b * TT:b * TT + ncl], in_=pnd[:64, :ncl]), reads=[pnd.b], writes=[nd.b])

            nd3 = nd[0:64, :].rearrange("p (b x) -> p b x", b=8)

            def outputs(tt, h):
                lane = h % 2
                t_hh = lane_slots[lane][(h // 2) * 2]
                rsh = lane_slots[lane][(h // 2) * 2 + 1]
                ybh = yb[lane]
                sq = sqv[h]
                pss = self.ps[h]
                numv = nd3[:, :, h * 64:(h + 1) * 64]
                hh3 = g3(t_hh[:, :])
                if isA:
                    denv = nd3[:, :, 256 + h * 64:256 + (h + 1) * 64]
                    S.op("dve", lambda e: e.scalar_tensor_tensor(out=hh3, in0=denv, scalar=-1.0, in1=denv, op0=ALU.mult, op1=ALU.max), reads=[nd.b], writes=[t_hh.b])
                    yield
                    S.op("dve", lambda e: e.tensor_scalar(out=t_hh[:, :], in0=t_hh[:, :], scalar1=1.0, scalar2=None, op0=ALU.max), reads=[t_hh.b], writes=[t_hh.b])
                    yield
                    S.op("act", lambda e: e.activation(out=t_hh[:, :], in_=t_hh[:, :], func=AF.Ln), reads=[t_hh.b], writes=[t_hh.b])
                    yield
                    S.op("act", lambda e: e.activation(out=t_hh[:, :], in_=t_hh[:, :], func=AF.Exp, scale=-1.0), reads=[t_hh.b], writes=[t_hh.b])
                    yield
                    S.op("dve", lambda e: e.tensor_tensor(out=hh3, in0=numv, in1=hh3, op=ALU.mult), reads=[nd.b, t_hh.b], writes=[t_hh.b])
                    yield
                else:
                    S.op("act", lambda e: e.copy(out=hh3, in_=numv), reads=[nd.b], writes=[t_hh.b])
                    yield
                S.op("act", lambda e: e.activation(out=sq[:, :], in_=t_hh[:, :], func=AF.Square), reads=[t_hh.b], writes=[sq.b])
                yield
                S.op("pe", lambda e: e.matmul(pss[:64, :TT], lhsT=self.ones[0:64, 0:64], rhs=sq[:, :], start=True, stop=True),
                     reads=[self.ones.b, sq.b], writes=[pss.b])
                yield
                S.op("act", lambda e: e.activation(out=rsh[:, :], in_=pss[:64, :TT], func=AF.Ln, scale=1.0 / 64, bias=self.epsb[0:64, 0:1]),
                     reads=[pss.b, self.epsb.b], writes=[rsh.b])
                yield
                S.op("act", lambda e: e.activation(out=rsh[:, :], in_=rsh[:, :], func=AF.Exp, scale=-0.5), reads=[rsh.b], writes=[rsh.b])
                yield
                gi = (l * 2 + (0 if isA else 1)) * 4 + h
                S.op("dve", lambda e: e.scalar_tensor_tensor(out=t_hh[:, :], in0=t_hh[:, :], scalar=self.hn[:, gi:gi + 1], in1=rsh[:, :], op0=ALU.mult, op1=ALU.mult),
                     reads=[t_hh.b, self.hn.b, rsh.b], writes=[t_hh.b])
                yield
                S.op("dve", lambda e: e.tensor_tensor(out=ybh[:], in0=t_hh[:, :], in1=og[h][:], op=ALU.mult), reads=[t_hh.b, og[h].b], writes=[ybh.b])
                self.y_store(yT, ybh, ych0 + h // 2, h, tt)
                yield

            vtok(0)
            for tt in range(NT):
                self.run_interleaved([prep(tt, 0, 0), prep(tt, 1, 1)])
                self.run_interleaved([prep(tt, 2, 0), prep(tt, 3, 1)])
                blocks(tt)
                if tt + 1 < NT:
                    vtok(tt + 1)
                self.run_interleaved([outputs(tt, h) for h in range(4)])
            S.op("dve", lambda e: e.memset(gcs[0][:, 0:1], 0.0), reads=[v.b for v in lane_slots[0]], writes=[xs.b, gcs[0].b])
            S.op("dve", lambda e: e.memset(gcs[1][:, 0:1], 0.0), reads=[v.b for v in lane_slots[1]], writes=[wbf[0].b, wbf[1].b, gcs[1].b])
            S.op("dve", lambda e: e.memset(gcs[0][:, 0:1], 0.0), reads=[v.b for v in sqv], writes=[self.sq.b, gcs[0].b])

    def group_C(self, s, l, yT):
        S, nc = self.S, self.nc
        scale = 0.125
        with contextlib.ExitStack() as es:
            W = T(es, nc, "mbW", [128, 8 * 8 * 64], BF16)
            Wv = T(es, nc, "mbWv", [128, 8 * 256], BF16)
            bvb = T(es, nc, "mbbv", [128, 256], F32)
            kT = [T(es, nc, "mbkT%d" % h, [128, SEQ], BF16) for h in range(4)]
            vtok = T(es, nc, "mbvtok", [128, 16 * 4 * 65], BF16)
            vt5 = vtok[:].rearrange("p (c h w) -> p c h w", c=16, h=4)
            rrowb = T(es, nc, "mbrrowb", [65, TT], BF16)
            TBr = [T(es, nc, "mbTB%d" % h, [128, 1024], F32) for h in range(2)]
            q32 = [T(es, nc, "mbq32_%d" % i, [64, TT], F32) for i in range(2)]
            qT = [T(es, nc, "mbqT%d" % i, [128, TT], BF16) for i in range(2)]
            k32 = q32[0]
            kmean = T(es, nc, "mbkmean", [64, 32], F32)
            gm0 = T(es, nc, "mbgm", [8, TT], F32)
            gm = [gm0, gm0]
            gt = [T(es, nc, "mbgt%d" % i, [56, TT], BF16) for i in range(2)]
            nm = [T(es, nc, "mbnm%d" % i, [8, TT], BF16) for i in range(2)]
            tmp80 = gm0
            tmp8 = [tmp80, tmp80]
            tmpS = [self.hout, self.xs[0]]
            ex = [T(es, nc, "mbex%d" % i, [128, TT], BF16) for i in range(3)]
            rden = T(es, nc, "mbrden", [65, TT], F32)
            rrow = rden
            yb = T(es, nc, "mbyb", [64, TT], BF16)
            S.dma("pool", W[:].rearrange("p (o k m) -> p o k m", o=8, k=8), self.din["wC_T"][l].rearrange("o p k m -> p o k m"), writes=[W.b])
            S.dma("pool", Wv[:].rearrange("p (k m) -> p k m", k=8), self.din["w_v"][l, 1], writes=[Wv.b])
            S.dma("sp", bvb[:], self.din["b_v"][l, 1:2, :].partition_broadcast(128).rearrange("p a b -> p (a b)"), writes=[bvb.b])
            S.op("dve", lambda e: e.memset(kmean[:], 0.0), writes=[kmean.b])
            S.op("pool", lambda e: e.memset(vtok[:], 1.0), writes=[vtok.b])
            for h in range(4):
                S.op("pool", lambda e, h=h: e.memset(kT[h][64:128, :], 0.0), writes=[kT[h].b])
                S.dma("pool", kT[h][64:72, :], self.din["kind"], reads=[kT[h].b], writes=[kT[h].b])
            for i in range(2):
                S.op("pool", lambda e, i=i: e.memset(qT[i][64:128, :], 0.0), writes=[qT[i].b])
            mc3 = lambda which, tt: self.mobac[:, (which * 4 + tt) * 2:(which * 4 + tt) * 2 + 2].rearrange("p (a b) -> p a b", b=1).broadcast_to([8, 2, 256])
            v3 = lambda ap: ap.rearrange("p (a b) -> p a b", b=256)

            def head_prep(tt, h):
                xn = self.xn[tt]
                i = h % 2
                pq = self.ps[1]
                self.proj64(pq, W, (h * 2) * 512, xn)
                S.op("act", lambda e: e.activation(out=q32[i][:], in_=pq[:64, :TT], func=AF.Identity, bias=self.pC[:, l * 8 + h * 2:l * 8 + h * 2 + 1]),
                     reads=[pq.b, self.pC.b], writes=[q32[i].b])
                S.op("act", lambda e: e.copy(out=qT[i][0:64, :], in_=q32[i][:]), reads=[q32[i].b], writes=[qT[i].b])
                pg, pdm = self.ps[4], self.ps[5]
                S.op("pe", lambda e: e.matmul(pg[:8, :TT], lhsT=kmean[:, h * 8:h * 8 + 8], rhs=q32[i][:], start=True, stop=True),
                     reads=[kmean.b, q32[i].b], writes=[pg.b])
                S.op("dve", lambda e: e.tensor_tensor(out=v3(gm[i][:]), in0=v3(pg[:8, :TT]), in1=mc3(0, tt), op=ALU.add),
                     reads=[pg.b, self.mobac.b], writes=[gm[i].b])
                S.op("pe", lambda e: e.matmul(pdm[:56, :TT], lhsT=self.Pm[:, :], rhs=gm[i][:], start=True, stop=True),
                     reads=[self.Pm.b, gm[i].b], writes=[pdm.b])
                S.op("dve", lambda e: e.tensor_single_scalar(out=gt[i][:], in_=pdm[:56, :TT], scalar=0.0, op=ALU.is_gt), reads=[pdm.b], writes=[gt[i].b])
                S.op("pe", lambda e: e.matmul(pg[:8, :TT], lhsT=self.Agg[:, :], rhs=gt[i][:], start=True, stop=True),
                     reads=[self.Agg.b, gt[i].b], writes=[pg.b])
                S.op("dve", lambda e: e.scalar_tensor_tensor(out=v3(tmp8[i][:]), in0=v3(pg[:8, :TT]), scalar=2.5, in1=mc3(1, tt), op0=ALU.is_lt, op1=ALU.mult),
                     reads=[pg.b, self.mobac.b], writes=[tmp8[i].b])
                S.op("dve", lambda e: e.tensor_tensor(out=v3(tmp8[i][:]), in0=v3(tmp8[i][:]), in1=mc3(2, tt), op=ALU.add),
                     reads=[tmp8[i].b, self.mobac.b], writes=[tmp8[i].b])
                S.op("dve", lambda e: e.tensor_scalar(out=nm[i][:], in0=tmp8[i][:], scalar1=BIGRAW, scalar2=None, op0=ALU.mult), reads=[tmp8[i].b], writes=[nm[i].b])
                S.dma("sp", qT[i][64:72, :], nm[i][:], reads=[nm[i].b, qT[i].b], writes=[qT[i].b])
                TBh = TBr[i]
                S.dma("sp", TBh[:], bass.AP(self.tbd, h * 128 * 1152 + 127, [[1151, 128], [1, 1024]]), reads=[self.tbd_b], writes=[TBh.b])

            def head_attn(tt, h):
                i = h % 2
                TBh = TBr[i]
                po, pd = self.ps[6], self.ps[7]
                nj = 4 * tt + 4
                pSr = [self.ps[2], self.ps[3], self.ps[0]]
                def st1(j):
                    pS = pSr[j % 3]
                    S.op("pe", lambda e: e.matmul(pS[:, :TT], lhsT=kT[h][:, j * 128:(j + 1) * 128], rhs=qT[i][:, :], start=True, stop=True),
                         reads=[kT[h].b, qT[i].b], writes=[pS.b])
                    o = tt * 512 - j * 128
                    exj = ex[j % 3]
                    if o <= 128:
                        tS = tmpS[j % 2]
                        S.op("dve", lambda e: e.scalar_tensor_tensor(out=tS[:, :TT], in0=pS[:, :TT], scalar=scale, in1=TBh[:, o + 384:o + 384 + 512],
                                                                      op0=ALU.mult, op1=ALU.add), reads=[pS.b, TBh.b], writes=[tS.b])
                        S.op("act", lambda e: e.activation(out=exj[:], in_=tS[:, :TT], func=AF.Exp), reads=[tS.b], writes=[exj.b])
                    else:
                        S.op("act", lambda e: e.activation(out=exj[:], in_=pS[:, :TT], func=AF.Exp, scale=scale, bias=self.b31[:, h:h + 1]),
                             reads=[pS.b, self.b31.b], writes=[exj.b])

                def st2(j):
                    exj = ex[j % 3]
                    S.op("pe", lambda e: e.matmul(po[:65, :TT], lhsT=vt5[:, j, h, :], rhs=exj[:], start=(j == 0), stop=(j == nj - 1)),
                         reads=[vtok.b, exj.b], writes=[po.b])

                LA = 2
                for j in range(nj + LA):
                    if j < nj:
                        st1(j)
                    if j - LA >= 0:
                        st2(j - LA)
                S.op("act", lambda e: e.activation(out=rrow[64:65, :], in_=po[64:65, :TT], func=AF.Ln), reads=[po.b], writes=[rrow.b])
                S.op("act", lambda e: e.activation(out=rrowb[64:65, :], in_=rrow[64:65, :], func=AF.Exp, scale=-1.0), reads=[rrow.b], writes=[rrowb.b])
                S.op("pe", lambda e: e.matmul(pd[:64, :TT], lhsT=self.ones[64:65, 0:64], rhs=rrowb[64:65, :], start=True, stop=True),
                     reads=[self.ones.b, rrowb.b], writes=[pd.b])
                S.op("act", lambda e: e.copy(out=rden[0:64, :], in_=pd[:64, :TT]), reads=[pd.b], writes=[rden.b])
                S.op("dve", lambda e: e.tensor_tensor(out=yb[:], in0=po[:64, :TT], in1=rden[0:64, :], op=ALU.mult), reads=[po.b, rden.b], writes=[yb.b])
                self.y_store(yT, yb, 4 + h // 2, h, tt)

            for tt in range(NT):
                xn = self.xn[tt]
                for h in range(4):
                    pp = self.ps[h % 2]
                    self.proj64(pp, W, (h * 2 + 1) * 512, xn)
                    S.op("act", lambda e: e.activation(out=k32[:], in_=pp[:64, :TT], func=AF.Identity, bias=self.pC[:, l * 8 + h * 2 + 1:l * 8 + h * 2 + 2]),
                         reads=[pp.b, self.pC.b], writes=[k32.b])
                    S.op("act", lambda e: e.copy(out=kT[h][0:64, tt * TT:(tt + 1) * TT], in_=k32[:]), reads=[k32.b], writes=[kT[h].b])
                    S.op("dve", lambda e: e.reduce_sum(out=kmean[:, h * 8 + 2 * tt:h * 8 + 2 * tt + 2], in_=v3(k32[:]), axis=AX.X),
                         reads=[k32.b], writes=[kmean.b])
                for j in range(4):
                    pv = self.ps[2 + j % 2]
                    for kc in range(KC):
                        S.op("pe", lambda e, kc=kc: e.matmul(pv[:, :256], lhsT=xn[:, kc * TT + j * 128:kc * TT + (j + 1) * 128],
                                                             rhs=Wv[:, kc * 256:(kc + 1) * 256], start=(kc == 0), stop=(kc == KC - 1)),
                             reads=[xn.b, Wv.b], writes=[pv.b])
                    S.op("dve", lambda e: e.tensor_tensor(out=vt5[:, tt * 4 + j, :, 0:64], in0=pv[:, :256].rearrange("p (h w) -> p h w", h=4),
                                                          in1=bvb[:].rearrange("p (h w) -> p h w", h=4), op=ALU.add),
                         reads=[pv.b, bvb.b], writes=[vtok.b])
                head_prep(tt, 0)
                head_prep(tt, 1)
                head_attn(tt, 0)
                head_prep(tt, 2)
                head_attn(tt, 1)
                head_prep(tt, 3)
                head_attn(tt, 2)
                head_attn(tt, 3)


N_CORES = 8


def kernel(**inputs):
    inp = {k: np.asarray(v) for k, v in inputs.items()}
    B = inp["x"].shape[0]
    nseq = B // N_CORES
    sh = host_prepare(inp, DEPTH)
    sh = host_prepare_mixer(inp, DEPTH, sh)
    in_maps = []
    for c in range(N_CORES):
        core = dict(sh)
        core["xT"] = np.stack([to_T(inp["x"][c * nseq + b]) for b in range(nseq)])
        core["memT"] = np.stack([np.ascontiguousarray(inp["mem"][c * nseq + b].reshape(NMEM, 8, 128).transpose(2, 1, 0))
                                 for b in range(nseq)])
        in_maps.append(core)
    P = ProgM(dict(depth=DEPTH, nseq=nseq))
    nc = P.build({k: v.shape for k, v in in_maps[0].items()})
    res = run_bass_kernel_spmd(nc, in_maps, core_ids=list(range(N_CORES)))
    out = np.empty((B, SEQ, D), np.float32)
    for c in range(N_CORES):
        o = np.asarray(res.results[c]["outT"])
        for b in range(nseq):
            out[c * nseq + b] = from_T(o[b])
    return out
```

```python
import contextlib
import math
import numpy as np
import concourse.bass as bass
import concourse.mybir as mybir
from concourse.bass_utils import run_bass_kernel_spmd

F32 = mybir.dt.float32
BF16 = mybir.dt.bfloat16
AF = mybir.ActivationFunctionType
ALU = mybir.AluOpType
AX = mybir.AxisListType

D = 1024
SEQ = 2048
NSEQ = 2
DEPTH = 4
TT = 512
NT = SEQ // TT
KC = 8
DFF = 2816
NHC = DFF // 128
NMEM = 256
EPS = 1e-6
NEG = -30000.0


class Buf:
    __slots__ = ("w", "r")

    def __init__(self):
        self.w = None
        self.r = []


class Sched:
    NDMA = 8

    def __init__(self, nc, es):
        self.nc = nc
        self.engs = {"pe": nc.tensor, "act": nc.scalar, "dve": nc.vector,
                     "pool": nc.gpsimd, "sp": nc.sync}
        self.sems = {}
        for k in ("pe", "act", "dve", "pool"):
            self.sems[("e", k)] = es.enter_context(nc.semaphore("prog_" + k))
        for k in ("sp", "pool", "act"):
            for i in range(self.NDMA):
                self.sems[("d", k, i)] = es.enter_context(nc.semaphore("dma_%s_%d" % (k, i)))
        self.cnt = {k: 0 for k in self.engs}
        self.dcnt = {k: 0 for k in self.engs}
        self.waited = {k: {} for k in self.engs}
        self.nops = 0

    def _deps(self, eng, reads, writes):
        need = {}

        def add(tok):
            if tok is None:
                return
            k, v = tok
            if need.get(k, 0) < v:
                need[k] = v
        for b in reads:
            add(b.w)
        for b in writes:
            add(b.w)
            for t in b.r:
                add(t)
        wd = self.waited[eng]
        e = self.engs[eng]
        for k, v in need.items():
            if k == ("e", "pe") and eng == "pe":
                continue
            if wd.get(k, 0) >= v:
                continue
            wd[k] = v
            e.wait_ge(self.sems[k], v)

    def _commit(self, tok, reads, writes):
        for b in reads:
            b.r.append(tok)
            if len(b.r) > 64:
                m = {}
                for k, v in b.r:
                    if m.get(k, 0) < v:
                        m[k] = v
                b.r = list(m.items())
        for b in writes:
            b.w = tok
            b.r = []

    def op(self, eng, fn, reads=(), writes=()):
        self._deps(eng, reads, writes)
        self.cnt[eng] += 1
        k = ("e", eng)
        fn(self.engs[eng]).then_inc(self.sems[k], 1)
        tok = (k, self.cnt[eng])
        self._commit(tok, reads, writes)
        self.nops += 1
        return tok

    def dma(self, eng, out, in_, reads=(), writes=(), **kw):
        j = self.dcnt[eng]
        self.dcnt[eng] += 1
        sk = ("d", eng, j % self.NDMA)
        val = 16 * (j // self.NDMA + 1)
        self._deps(eng, reads, writes)
        if j >= self.NDMA:
            prev = 16 * (j // self.NDMA)
            wd = self.waited[eng]
            if wd.get(sk, 0) < prev:
                wd[sk] = prev
                self.engs[eng].wait_ge(self.sems[sk], prev)
        self.engs[eng].dma_start(out=out, in_=in_, **kw).then_inc(self.sems[sk], 16)
        tok = (sk, val)
        self._commit(tok, reads, writes)
        self.nops += 1
        return tok

    def finish(self, eng, bufs):
        need = {}
        for b in bufs:
            if b.w is not None:
                k, v = b.w
                need[k] = max(need.get(k, 0), v)
        for k, v in need.items():
            self.engs[eng].wait_ge(self.sems[k], v)


class T:
    _n = [0]

    def __init__(self, es, nc, name, shape, dtype, psum=False):
        T._n[0] += 1
        name = "t%d_%s" % (T._n[0], name)
        if psum:
            self.t = es.enter_context(nc.psum_tensor(name, shape, dtype))
        else:
            self.t = es.enter_context(nc.sbuf_tensor(name, shape, dtype))
        self.b = Buf()
        self.b.r = list(T.grave.items())
        es.callback(self._retire)

    grave = {}

    def _retire(self):
        g = T.grave
        toks = list(self.b.r)
        if self.b.w is not None:
            toks.append(self.b.w)
        for k, v in toks:
            if g.get(k, 0) < v:
                g[k] = v

    def __getitem__(self, k):
        return self.t[k]


def arr_w(w, ocw):
    K, N = w.shape
    a = w.reshape(K // 128, 128, N // ocw, ocw)
    return np.ascontiguousarray(a.transpose(2, 1, 0, 3))


def to_T(x):
    a = x.reshape(x.shape[0] // TT, TT, KC, 128)
    return np.ascontiguousarray(a.transpose(0, 3, 2, 1))


def from_T(a):
    return np.ascontiguousarray(a.transpose(0, 3, 2, 1)).reshape(-1, KC * 128)


def colT(v, w=128):
    return np.ascontiguousarray(v.reshape(-1, w).T)


def host_prepare(inp, depth):
    sh = {}
    L = depth
    wup = []
    for l in range(L):
        w = inp["w_ffn_in"][l]
        g = arr_w(w[:, :DFF], 128)
        u = arr_w(w[:, DFF:], 128)
        wup.append(np.concatenate([g, u], axis=3))
    sh["w_up"] = np.stack(wup)
    wd = []
    for l in range(L):
        w = inp["w_ffn_out"][l]
        a = w.reshape(NHC, 128, 8, 128)
        wd.append(np.ascontiguousarray(a.transpose(2, 1, 0, 3)))
    sh["w_down"] = np.stack(wd)
    sh["ffn_cw"] = np.stack([np.stack([colT(inp["ffn_conv_w"][l][j]) for j in range(3)] + [colT(inp["ffn_conv_b"][l])], axis=1)
                             for l in range(L)])
    names = ["norm_mix_pre", "norm_mix_post", "norm_cross_pre", "norm_cross_post", "norm_ffn_pre", "norm_ffn_post", "mem_norm"]
    sh["gains"] = np.stack([np.stack([colT(inp[n][l]) for n in names], axis=1) for l in range(L)])
    sh["w_cq"] = np.stack([arr_w(inp["w_cq"][l], 128) for l in range(L)])
    sh["w_ck"] = np.stack([arr_w(inp["w_ck"][l], 128) for l in range(L)])
    sh["w_cv"] = np.stack([arr_w(inp["w_cv"][l], 512)[0] for l in range(L)])
    sh["w_co"] = np.stack([np.ascontiguousarray(inp["w_co"][l].reshape(4, 128, 8, 128).transpose(2, 1, 0, 3)) for l in range(L)])
    sh["ident"] = np.eye(128, dtype=np.float32)
    return sh


class Prog:
    def __init__(self, cfg):
        self.cfg = cfg
        self.depth = cfg.get("depth", DEPTH)
        self.nseq = cfg.get("nseq", NSEQ)

    def dram_in(self, name, shape, dt=F32):
        return self.nc.dram_tensor(name, list(shape), dt, kind="ExternalInput").ap()

    def build(self, shapes):
        cfg = self.cfg
        nc = self.nc = bass.Bass("TRN2", target_bir_lowering=False)
        L = self.depth
        self.din = {k: self.dram_in(k, v) for k, v in shapes.items()}
        self.outT = nc.dram_tensor("outT", [self.nseq, NT, 128, KC, TT], F32, kind="ExternalOutput").ap()
        self.dbg = {}
        es = self.es = contextlib.ExitStack()
        with es:
            S = self.S = Sched(nc, es)
            T.grave = {}
            self.x_buf = [[Buf() for _ in range(NT)] for _ in range(self.nseq)]
            self.alloc_static()
            self.load_consts()
            seq_list = []
            for l in range(L):
                if cfg.get("mix", True):
                    seq_list.append((l, 0, 1))
                if cfg.get("cross", True):
                    seq_list.append((l, 2, 3))
                if cfg.get("ffn", True):
                    seq_list.append((l, 4, 5))
            self.next_of = {}
            if cfg.get("chain_norm", True):
                for i in range(len(seq_list) - 1):
                    self.next_of[(seq_list[i][0], seq_list[i][2])] = (seq_list[i + 1][0], seq_list[i + 1][1])
            self.norm_done = set()
            for s in range(self.nseq):
                for l in range(L):
                    first = (l == 0)
                    src_first = first
                    if cfg.get("mix", True):
                        self.mixer_sublayer(s, l, src_first)
                        src_first = False
                    if cfg.get("cross", True):
                        self.cross_sublayer(s, l, src_first)
                        src_first = False
                    if cfg.get("ffn", True):
                        self.ffn_sublayer(s, l, src_first)
                        src_first = False
            S.finish("sp", [b for row in self.x_buf for b in row] + list(self.dbg_bufs))
        return nc

    def dump(self, name, t, ap, shape, dt=F32):
        if not self.cfg.get("dump", False) or name in self.dbg:
            return
        d = self.nc.dram_tensor("dbg_" + name, list(shape), dt, kind="ExternalOutput").ap()
        b = Buf()
        self.S.dma("sp", d, ap, reads=[t.b], writes=[b])
        self.dbg[name] = d
        self.dbg_bufs.append(b)

    def alloc_static(self):
        nc, es = self.nc, self.es
        L = self.depth
        self.dbg_bufs = []
        self.ones = T(es, nc, "ones", [128, 128], BF16)
        self.ident = T(es, nc, "ident", [128, 128], BF16)
        self.gains = T(es, nc, "gains", [128, L * 7 * 8], F32)
        self.ffn_cw = T(es, nc, "ffn_cw", [128, L * 4 * NHC], F32)
        self.ps = [T(es, nc, "ps%d" % i, [128, 512], F32, psum=True) for i in range(8)]
        self.wb = [T(es, nc, "wb%d" % i, [128, 6144], BF16) for i in range(2)]
        self.xs = [T(es, nc, "xs%d" % i, [128, KC * TT], F32) for i in range(1)]
        self.hout = T(es, nc, "hout", [128, KC * TT], F32)
        self.sq = T(es, nc, "sq", [128, KC * TT], BF16)
        self.rs = [T(es, nc, "rs%d" % i, [128, TT], F32) for i in range(2)]
        self.xn = [T(es, nc, "xn%d" % i, [128, KC * TT], BF16) for i in range(NT)]
        self.epsb = T(es, nc, "epsb", [128, 1], F32)
        self.wrot = 0

    def load_consts(self):
        S = self.S
        L = self.depth
        S.op("dve", lambda e: e.memset(self.ones[:], 1.0), writes=[self.ones.b])
        S.op("dve", lambda e: e.memset(self.epsb[:], EPS), writes=[self.epsb.b])
        S.dma("pool", self.ident[:], self.din["ident"], writes=[self.ident.b])
        S.dma("sp", self.gains[:].rearrange("p (l f) -> p l f", l=L), self.din["gains"].rearrange("l p a c -> p l (a c)"), writes=[self.gains.b])
        S.dma("sp", self.ffn_cw[:].rearrange("p (l f) -> p l f", l=L), self.din["ffn_cw"].rearrange("l p a c -> p l (a c)"), writes=[self.ffn_cw.b])

    def gain_col(self, l, which, c):
        i = (l * 7 + which) * 8 + c
        return self.gains[:, i:i + 1]

    def next_wb(self):
        w = self.wb[self.wrot % len(self.wb)]
        self.wrot += 1
        return w

    def x_src(self, s, tt, first):
        return self.din["xT"][s, tt] if first else self.outT[s, tt]

    def load_x(self, s, tt, first, dst):
        S = self.S
        S.dma("sp", dst[:].rearrange("p (c t) -> p c t", c=KC), self.x_src(s, tt, first),
              reads=[self.x_buf[s][tt]], writes=[dst.b])

    def rms_T(self, src, ncols, nchunks, dim, psA, rs_out):
        S = self.S
        n = nchunks * ncols
        S.op("act", lambda e: e.activation(out=self.sq[:, :n], in_=src[:, :n], func=AF.Square),
             reads=[src.b], writes=[self.sq.b])
        for c in range(nchunks):
            S.op("pe", lambda e, c=c: e.matmul(psA[:, :ncols], lhsT=self.ones[:], rhs=self.sq[:, c * ncols:(c + 1) * ncols],
                                               start=(c == 0), stop=(c == nchunks - 1)),
                 reads=[self.ones.b, self.sq.b], writes=[psA.b])
        S.op("act", lambda e: e.activation(out=rs_out[:, :ncols], in_=psA[:, :ncols], func=AF.Ln, scale=1.0 / dim, bias=self.epsb[:, 0:1]),
             reads=[psA.b, self.epsb.b], writes=[rs_out.b])
        S.op("act", lambda e: e.activation(out=rs_out[:, :ncols], in_=rs_out[:, :ncols], func=AF.Exp, scale=-0.5),
             reads=[rs_out.b], writes=[rs_out.b])

    def norm_tile(self, s, tt, first, l, which, stage=None):
        S = self.S
        if (s, l, which, tt) in self.norm_done:
            return
        xs = stage if stage is not None else self.xs[0]
        self.load_x(s, tt, first, xs)
        rs = self.rs[tt % 2]
        self.rms_T(xs, TT, KC, D, self.ps[7], rs)
        xn = self.xn[tt]
        for c in range(KC):
            S.op("dve", lambda e, c=c: e.scalar_tensor_tensor(out=xn[:, c * TT:(c + 1) * TT], in0=xs[:, c * TT:(c + 1) * TT],
                                                                scalar=self.gain_col(l, which, c), in1=rs[:, :TT],
                                                                op0=ALU.mult, op1=ALU.mult),
                 reads=[xs.b, rs.b, self.gains.b], writes=[xn.b])

    def residual_gen(self, s, tt, first, l, which, hout=None, xs=None):
        S = self.S
        hout = hout if hout is not None else self.hout
        rs = self.rs[tt % 2]
        xs = xs if xs is not None else self.xs[0]
        for _ in self.rms_gen(hout, TT, KC, D, self.ps[7], rs):
            yield
        self.load_x(s, tt, first, xs)
        for c in range(KC):
            sl = slice(c * TT, (c + 1) * TT)
            S.op("dve", lambda e, sl=sl, c=c: e.scalar_tensor_tensor(out=hout[:, sl], in0=hout[:, sl], scalar=self.gain_col(l, which, c),
                                                                      in1=rs[:, :TT], op0=ALU.mult, op1=ALU.mult),
                 reads=[hout.b, rs.b, self.gains.b], writes=[hout.b])
            S.op("dve", lambda e, sl=sl: e.tensor_tensor(out=xs[:, sl], in0=xs[:, sl], in1=hout[:, sl], op=ALU.add),
                 reads=[xs.b, hout.b], writes=[xs.b])
        S.dma("sp", self.outT[s, tt], xs[:].rearrange("p (c t) -> p c t", c=KC), reads=[xs.b], writes=[self.x_buf[s][tt]])
        nxt = self.next_of.get((l, which))
        if nxt is not None:
            l2, w2 = nxt
            rs2 = self.rs[(tt + 1) % 2]
            for _ in self.rms_gen(xs, TT, KC, D, self.ps[7], rs2):
                yield
            xn = self.xn[tt]
            for c in range(KC):
                S.op("dve", lambda e, c=c: e.scalar_tensor_tensor(out=xn[:, c * TT:(c + 1) * TT], in0=xs[:, c * TT:(c + 1) * TT],
                                                                    scalar=self.gain_col(l2, w2, c), in1=rs2[:, :TT],
                                                                    op0=ALU.mult, op1=ALU.mult),
                     reads=[xs.b, rs2.b, self.gains.b], writes=[xn.b])
            self.norm_done.add((s, l2, w2, tt))

    def residual_tile(self, *a, **kw):
        for _ in self.residual_gen(*a, **kw):
            pass

    def rms_gen(self, src, ncols, nchunks, dim, psA, rs_out):
        S = self.S
        n = nchunks * ncols
        S.op("act", lambda e: e.activation(out=self.sq[:, :n], in_=src[:, :n], func=AF.Square),
             reads=[src.b], writes=[self.sq.b])
        yield
        for c in range(nchunks):
            S.op("pe", lambda e, c=c: e.matmul(psA[:, :ncols], lhsT=self.ones[:], rhs=self.sq[:, c * ncols:(c + 1) * ncols],
                                               start=(c == 0), stop=(c == nchunks - 1)),
                 reads=[self.ones.b, self.sq.b], writes=[psA.b])
        S.op("act", lambda e: e.activation(out=rs_out[:, :ncols], in_=psA[:, :ncols], func=AF.Ln, scale=1.0 / dim, bias=self.epsb[:, 0:1]),
             reads=[psA.b, self.epsb.b], writes=[rs_out.b])
        S.op("act", lambda e: e.activation(out=rs_out[:, :ncols], in_=rs_out[:, :ncols], func=AF.Exp, scale=-0.5),
             reads=[rs_out.b], writes=[rs_out.b])

    def pend(self, gen):
        if not hasattr(self, "pending"):
            self.pending = []
        self.pending.append(gen)
        self.pump()

    def pump(self):
        for g in list(getattr(self, "pending", [])):
            try:
                next(g)
            except StopIteration:
                self.pending.remove(g)

    def drain(self):
        while getattr(self, "pending", []):
            self.pump()

    def ffn_sublayer(self, s, l, first):
        S, nc = self.S, self.nc
        HT = 1024
        NTH = HT // TT
        with contextlib.ExitStack() as es:
            hT = T(es, nc, "ffn_hT", [128, NHC * HT], BF16)
            gsb = T(es, nc, "ffn_g", [128, 2 + HT], F32)
            usb = T(es, nc, "ffn_u", [128, HT], F32)
            a1 = T(es, nc, "ffn_a1", [128, HT], F32)
            halo = T(es, nc, "ffn_halo", [128, NHC * 2], F32)
            S.op("dve", lambda e: e.memset(halo[:], 0.0), writes=[halo.b])
            for tt in range(NT):
                self.norm_tile(s, tt, first, l, 4, stage=(self.xs[0] if tt % 2 == 0 else self.hout))
            hout2 = T(es, nc, "ffn_hout2", [128, KC * TT], F32)
            hs = [self.hout, hout2]

            def up(half, hooks):
                for g in range(NHC // 2):
                    wt = self.next_wb()
                    S.dma("pool", wt[:, :2 * 8 * 256].rearrange("p (g k m) -> p g k m", g=2, k=8),
                          self.din["w_up"][l, 2 * g:2 * g + 2].rearrange("g p k m -> p g k m"), writes=[wt.b])
                    for ci in range(2):
                        c = 2 * g + ci
                        S.op("act", lambda e, c=c: e.copy(out=gsb[:, 0:2], in_=halo[:, 2 * c:2 * c + 2]),
                             reads=[halo.b], writes=[gsb.b])
                        for k in range(NTH):
                            tt = half * NTH + k
                            pg = self.ps[(2 * k) % 4]
                            pu = self.ps[(2 * k + 1) % 4]
                            for which, pp in ((0, pg), (1, pu)):
                                for kc in range(KC):
                                    off = (ci * 8 + kc) * 256 + which * 128
                                    S.op("pe", lambda e, pp=pp, off=off, kc=kc, tt=tt: e.matmul(
                                        pp[:, :], lhsT=wt[:, off:off + 128], rhs=self.xn[tt][:, kc * TT:(kc + 1) * TT],
                                        start=(kc == 0), stop=(kc == KC - 1)),
                                        reads=[wt.b, self.xn[tt].b], writes=[pp.b])
                            S.op("act", lambda e, k=k, pg=pg: e.copy(out=gsb[:, 2 + k * TT:2 + (k + 1) * TT], in_=pg[:, :]),
                                 reads=[pg.b], writes=[gsb.b])
                            S.op("act", lambda e, k=k, pu=pu: e.copy(out=usb[:, k * TT:(k + 1) * TT], in_=pu[:, :]),
                                 reads=[pu.b], writes=[usb.b])
                        cw = lambda j, c=c: self.ffn_cw[:, (l * 4 + j) * NHC + c:(l * 4 + j) * NHC + c + 1]
                        S.op("dve", lambda e, cw=cw: e.tensor_scalar(out=a1[:], in0=gsb[:, 2:2 + HT], scalar1=cw(2), scalar2=cw(3),
                                                                      op0=ALU.mult, op1=ALU.add),
                             reads=[gsb.b, self.ffn_cw.b], writes=[a1.b])
                        S.op("dve", lambda e, cw=cw: e.scalar_tensor_tensor(out=a1[:], in0=gsb[:, 1:1 + HT], scalar=cw(1), in1=a1[:],
                                                                             op0=ALU.mult, op1=ALU.add),
                             reads=[gsb.b, a1.b, self.ffn_cw.b], writes=[a1.b])
                        S.op("dve", lambda e, cw=cw: e.scalar_tensor_tensor(out=a1[:], in0=gsb[:, 0:HT], scalar=cw(0), in1=a1[:],
                                                                             op0=ALU.mult, op1=ALU.add),
                             reads=[gsb.b, a1.b, self.ffn_cw.b], writes=[a1.b])
                        S.op("act", lambda e, c=c: e.copy(out=halo[:, 2 * c:2 * c + 2], in_=gsb[:, HT:HT + 2]),
                             reads=[gsb.b], writes=[halo.b])
                        S.op("act", lambda e: e.activation(out=a1[:], in_=a1[:], func=AF.Silu), reads=[a1.b], writes=[a1.b])
                        S.op("dve", lambda e, c=c: e.tensor_tensor(out=hT[:, c * HT:(c + 1) * HT], in0=a1[:], in1=usb[:], op=ALU.mult),
                             reads=[a1.b, usb.b], writes=[hT.b])
                    if g in hooks:
                        hooks[g]()

            def down(half):
                for o in range(KC):
                    wt = self.next_wb()
                    S.dma("pool", wt[:, :NHC * 128].rearrange("p (c m) -> p c m", c=NHC), self.din["w_down"][l, o], writes=[wt.b])
                    for k in range(NTH):
                        pp = self.ps[4 + (o * NTH + k) % 3]
                        for c in range(NHC):
                            S.op("pe", lambda e, pp=pp, c=c, k=k: e.matmul(
                                pp[:, :], lhsT=wt[:, c * 128:(c + 1) * 128], rhs=hT[:, c * HT + k * TT:c * HT + (k + 1) * TT],
                                start=(c == 0), stop=(c == NHC - 1)),
                                reads=[wt.b, hT.b], writes=[pp.b])
                        S.op("act", lambda e, pp=pp, o=o, k=k: e.copy(out=hs[k][:, o * TT:(o + 1) * TT], in_=pp[:, :]),
                             reads=[pp.b], writes=[hs[k].b])

            xs2 = T(es, nc, "ffn_xs2", [128, KC * TT], F32)
            hk = {g: self.pump for g in range(NHC // 2)}
            up(0, hk)
            self.drain()
            down(0)
            queue = [lambda: self.residual_gen(s, 0, first, l, 5, hout=hs[0]),
                     lambda: self.residual_gen(s, 1, first, l, 5, hout=hs[1], xs=xs2)]

            def hook():
                if getattr(self, "pending", []):
                    self.pump()
                elif queue:
                    self.pend(queue.pop(0)())
            up(1, {g: hook for g in range(NHC // 2)})
            self.drain()
            while queue:
                self.pend(queue.pop(0)())
                self.drain()
            down(1)
            self.residual_tile(s, 2, first, l, 5, hout=hs[0])
            self.residual_tile(s, 3, first, l, 5, hout=hs[1], xs=xs2)

    def cross_sublayer(self, s, l, first):
        S, nc = self.S, self.nc
        with contextlib.ExitStack() as es:
            memf = T(es, nc, "c_memf", [128, KC * NMEM], F32)
            memn = T(es, nc, "c_memn", [128, KC * NMEM], BF16)
            kT = T(es, nc, "c_kT", [128, 4 * NMEM], BF16)
            vtok = T(es, nc, "c_vtok", [128, 2 * 512], BF16)
            wq = T(es, nc, "c_wq", [128, 4 * 8 * 128], BF16)
            wo = T(es, nc, "c_wo", [128, 8 * 4 * 128], BF16)
            qT = T(es, nc, "c_qT", [128, 4 * TT], BF16)
            oT = T(es, nc, "c_oT", [128, 4 * TT], BF16)
            ex = [T(es, nc, "c_ex%d" % i, [128, TT], BF16) for i in range(4)]
            hout2 = T(es, nc, "c_hout2", [128, KC * TT], F32)
            houts = [self.hout, hout2]
            rden = T(es, nc, "c_rden", [128, TT], F32)
            S.dma("pool", wq[:].rearrange("p (o k m) -> p o k m", o=4, k=8), self.din["w_cq"][l].rearrange("o p k m -> p o k m"), writes=[wq.b])
            S.dma("pool", wo[:].rearrange("p (o h m) -> p o h m", o=8, h=4), self.din["w_co"][l].rearrange("o p h m -> p o h m"), writes=[wo.b])
            wk = self.next_wb()
            S.dma("pool", wk[:, :4096].rearrange("p (o k m) -> p o k m", o=4, k=8), self.din["w_ck"][l].rearrange("o p k m -> p o k m"), writes=[wk.b])
            wv = self.next_wb()
            S.dma("pool", wv[:, :4096].rearrange("p (k m) -> p k m", k=8), self.din["w_cv"][l], writes=[wv.b])
            S.dma("sp", memf[:].rearrange("p (c t) -> p c t", c=KC), self.din["memT"][s], writes=[memf.b])
            rs = self.rs[0]
            self.rms_T(memf, NMEM, KC, D, self.ps[7], rs)
            for c in range(KC):
                S.op("dve", lambda e, c=c: e.scalar_tensor_tensor(out=memn[:, c * NMEM:(c + 1) * NMEM], in0=memf[:, c * NMEM:(c + 1) * NMEM],
                                                                    scalar=self.gain_col(l, 6, c), in1=rs[:, :NMEM], op0=ALU.mult, op1=ALU.mult),
                     reads=[memf.b, rs.b, self.gains.b], writes=[memn.b])
            for h in range(4):
                pp = self.ps[h % 2]
                for kc in range(KC):
                    S.op("pe", lambda e, pp=pp, h=h, kc=kc: e.matmul(pp[:, :NMEM], lhsT=wk[:, (h * 8 + kc) * 128:(h * 8 + kc + 1) * 128],
                                                                      rhs=memn[:, kc * NMEM:(kc + 1) * NMEM], start=(kc == 0), stop=(kc == KC - 1)),
                         reads=[wk.b, memn.b], writes=[pp.b])
                S.op("act", lambda e, pp=pp, h=h: e.copy(out=kT[:, h * NMEM:(h + 1) * NMEM], in_=pp[:, :NMEM]), reads=[pp.b], writes=[kT.b])
            for mc in range(2):
                pp = self.ps[2 + mc]
                for kc in range(KC):
                    S.op("pe", lambda e, pp=pp, mc=mc, kc=kc: e.matmul(pp[:, :], lhsT=memn[:, kc * NMEM + mc * 128:kc * NMEM + (mc + 1) * 128],
                                                                        rhs=wv[:, kc * 512:(kc + 1) * 512], start=(kc == 0), stop=(kc == KC - 1)),
                         reads=[wv.b, memn.b], writes=[pp.b])
                S.op("act", lambda e, pp=pp, mc=mc: e.copy(out=vtok[:, mc * 512:(mc + 1) * 512], in_=pp[:, :]), reads=[pp.b], writes=[vtok.b])
            scale = 128 ** -0.5
            xs2 = T(es, nc, "c_xs2", [128, KC * TT], F32)
            self.norm_tile(s, 0, first, l, 2, stage=xs2)
            for tt in range(NT):
                xn = self.xn[tt]
                for h in range(4):
                    pp = self.ps[0]
                    for kc in range(KC):
                        S.op("pe", lambda e, pp=pp, h=h, kc=kc: e.matmul(pp[:, :], lhsT=wq[:, (h * 8 + kc) * 128:(h * 8 + kc + 1) * 128],
                                                                          rhs=xn[:, kc * TT:(kc + 1) * TT], start=(kc == 0), stop=(kc == KC - 1)),
                             reads=[wq.b, xn.b], writes=[pp.b])
                    S.op("act", lambda e, pp=pp, h=h: e.copy(out=qT[:, h * TT:(h + 1) * TT], in_=pp[:, :]), reads=[pp.b], writes=[qT.b])
                    if h % 2 == 1:
                        self.pump()
                if tt + 1 < NT:
                    self.norm_tile(s, tt + 1, first, l, 2, stage=xs2)
                units = [(h, mc) for h in range(4) for mc in range(2)]
                pob = [(self.ps[4], self.ps[5]), (self.ps[6], self.ps[1])]

                def c1(u):
                    h, mc = units[u]
                    pss = self.ps[2 + u % 2]
                    S.op("pe", lambda e: e.matmul(pss[:, :], lhsT=kT[:, h * NMEM + mc * 128:h * NMEM + (mc + 1) * 128],
                                                  rhs=qT[:, h * TT:(h + 1) * TT], start=True, stop=True),
                         reads=[kT.b, qT.b], writes=[pss.b])
                    S.op("act", lambda e: e.activation(out=ex[u % 4][:], in_=pss[:, :], func=AF.Exp, scale=scale),
                         reads=[pss.b], writes=[ex[u % 4].b])

                def c2(u):
                    h, mc = units[u]
                    po, pd = pob[h % 2]
                    S.op("pe", lambda e: e.matmul(po[:, :], lhsT=vtok[:, mc * 512 + h * 128:mc * 512 + (h + 1) * 128], rhs=ex[u % 4][:],
                                                  start=(mc == 0), stop=(mc == 1)),
                         reads=[vtok.b, ex[u % 4].b], writes=[po.b])
                    S.op("pe", lambda e: e.matmul(pd[:, :], lhsT=self.ones[:], rhs=ex[u % 4][:], start=(mc == 0), stop=(mc == 1)),
                         reads=[self.ones.b, ex[u % 4].b], writes=[pd.b])
                    if mc == 1:
                        S.op("act", lambda e: e.activation(out=rden[:], in_=pd[:, :], func=AF.Ln), reads=[pd.b], writes=[rden.b])
                        S.op("act", lambda e: e.activation(out=rden[:], in_=rden[:], func=AF.Exp, scale=-1.0), reads=[rden.b], writes=[rden.b])
                        S.op("dve", lambda e: e.tensor_tensor(out=oT[:, h * TT:(h + 1) * TT], in0=po[:, :], in1=rden[:], op=ALU.mult),
                             reads=[po.b, rden.b], writes=[oT.b])

                LA = 2
                for u in range(len(units) + LA):
                    if u < len(units):
                        c1(u)
                    if u - LA >= 0:
                        c2(u - LA)
                    if u % 3 == 2:
                        self.pump()
                self.drain()
                for o in range(KC):
                    pp = self.ps[2 + o % 2]
                    for h in range(4):
                        S.op("pe", lambda e, pp=pp, o=o, h=h: e.matmul(pp[:, :], lhsT=wo[:, (o * 4 + h) * 128:(o * 4 + h + 1) * 128],
                                                                        rhs=oT[:, h * TT:(h + 1) * TT], start=(h == 0), stop=(h == 3)),
                             reads=[wo.b, oT.b], writes=[pp.b])
                    S.op("act", lambda e, pp=pp, o=o: e.copy(out=houts[tt % 2][:, o * TT:(o + 1) * TT], in_=pp[:, :]), reads=[pp.b], writes=[houts[tt % 2].b])
                self.drain()
                self.pend(self.residual_gen(s, tt, first, l, 3, hout=houts[tt % 2], xs=(self.xs[0] if tt % 2 == 0 else xs2)))
            self.drain()

    def mixer_sublayer(self, s, l, first):
        raise NotImplementedError


OFF = dict(m_q=0, m_k=256, m_v=512, m_o=768, m_i=1024, m_f=1028, c_b=1032, c_c=1288, c_h=1544,
           a_q=1800, a_k=2056, a_v=2312, h_q=2568, h_f=2824, h_i=3080, h_g=3336)
LN8 = math.log(8.0)
BIGRAW = 240000.0


def rel_bucket_np(dist):
    n = np.maximum(dist, 0)
    exact = 16
    nf = np.maximum(n, 1).astype(np.float32)
    large = exact + (np.log(nf / exact) / math.log(128 / exact) * (32 - exact)).astype(np.int32)
    large = np.minimum(large, 31)
    return np.where(n < exact, n, large)


def host_prepare_mixer(inp, depth, sh):
    L = depth
    colsA, colsC, colsD = [], [], []
    for h in range(4):
        colsA += list(range(OFF["m_q"] + 64 * h, OFF["m_q"] + 64 * h + 64))
        colsA += list(range(OFF["m_k"] + 64 * h, OFF["m_k"] + 64 * h + 64))
        colsA += [OFF["m_i"] + h] * 64
        colsA += [OFF["m_f"] + h] * 64
        colsA += list(range(OFF["m_o"] + 64 * h, OFF["m_o"] + 64 * h + 64))
        colsC += list(range(OFF["a_q"] + 64 * h, OFF["a_q"] + 64 * h + 64))
        colsC += list(range(OFF["a_k"] + 64 * h, OFF["a_k"] + 64 * h + 64))
        colsD += list(range(OFF["h_q"] + 64 * h, OFF["h_q"] + 64 * h + 64))
        colsD += list(range(OFF["h_f"] + 64 * h, OFF["h_f"] + 64 * h + 64))
        colsD += list(range(OFF["h_g"] + 64 * h, OFF["h_g"] + 64 * h + 64))
    colsB = list(range(OFF["c_b"], OFF["c_b"] + 768))
    w_in, b_in = inp["w_in"], inp["b_in"]
    sh["wA_T"] = np.stack([arr_w(w_in[l][:, colsA], 64) for l in range(L)])
    sh["wC_T"] = np.stack([arr_w(w_in[l][:, colsC], 64) for l in range(L)])
    sh["wD_T"] = np.stack([arr_w(w_in[l][:, colsD], 64) for l in range(L)])
    sh["wB_T"] = np.stack([arr_w(w_in[l][:, colsB], 128) for l in range(L)])
    vcols = [OFF["m_v"], OFF["a_v"], OFF["h_i"]]
    sh["w_v"] = np.stack([np.stack([arr_w(w_in[l][:, o:o + 256], 256)[0] for o in vcols]) for l in range(L)])
    sh["b_v"] = np.stack([np.stack([b_in[l][o:o + 256] for o in vcols]) for l in range(L)])
    sh["pA"] = np.stack([colT(b_in[l][colsA], 64) for l in range(L)])
    sh["pC"] = np.stack([colT(b_in[l][colsC], 64) for l in range(L)])
    sh["pD"] = np.stack([colT(b_in[l][colsD], 64) for l in range(L)])
    sh["pB"] = np.stack([colT(b_in[l][colsB], 128) for l in range(L)])
    sh["scw"] = np.stack([np.stack([colT(inp["sconv_w"][l][j], 128) for j in range(3)], axis=1) for l in range(L)])
    sh["hn"] = np.stack([np.stack([colT(inp["mlstm_norm"][l], 64), colT(inp["hgrn_norm"][l], 64)], axis=1) for l in range(L)])
    lg = inp["hgrn_lb_logits"]
    sh["lbl"] = np.ascontiguousarray(lg.reshape(4, 4, 64).transpose(2, 1, 0))
    sh["w_mo"] = np.stack([arr_w(inp["w_mix_out"][l], 128) for l in range(L)])
    m8 = np.tile(np.triu(np.ones((64, 64), np.float32)), (1, 8))
    sh["mask8"] = m8
    x = np.arange(1152)
    dist = x - 511
    oh = np.zeros((33, 1152), np.float32)
    bk = rel_bucket_np(dist)
    for i in range(1152):
        if dist[i] >= 0:
            oh[bk[i], i] = 1.0
        else:
            oh[32, i] = 1.0
    sh["oh"] = oh
    ra = np.zeros((33, 4), np.float32)
    ra[:32] = inp["rel_bias"]
    ra[32] = NEG
    sh["rel_aug"] = ra
    sh["b31"] = np.ascontiguousarray(inp["rel_bias"][31:32, :])
    pairs = [(n, m) for n in range(8) for m in range(8) if m != n]
    Pm = np.zeros((8, 56), np.float32)
    Agg = np.zeros((56, 8), np.float32)
    for i, (n, m) in enumerate(pairs):
        Pm[m, i] += 1.0
        Pm[n, i] -= 1.0
        Agg[i, n] = 1.0
    sh["Pm"] = Pm
    sh["Agg"] = Agg
    seln = np.zeros((8, 8, 128), np.float32)
    for n in range(8):
        seln[n, n, :] = 1.0
    sh["seln"] = seln.reshape(8, 1024)
    pastm = np.zeros((8, 4, 2), np.float32)
    validc = np.zeros((8, 4, 2), np.float32)
    ownm1 = np.zeros((8, 4, 2), np.float32)
    for n in range(8):
        for tt in range(4):
            for j in range(2):
                b = 2 * tt + j
                pastm[n, tt, j] = 0.0 if n < b else -1e9
                validc[n, tt, j] = 1.0 if n < b else 0.0
                ownm1[n, tt, j] = (1.0 if n == b else 0.0) - 1.0
    sh["mobac"] = np.stack([pastm, validc, ownm1], axis=1).reshape(8, 24)
    kind = np.zeros((8, SEQ), np.float32)
    for n in range(8):
        kind[n, n * 256:(n + 1) * 256] = 1.0
    sh["kind"] = kind
    return sh


class V:
    def __init__(self, ap_fn):
        self.f = ap_fn
        self.b = Buf()

    def __getitem__(self, k):
        return self.f()[k]


class ProgM(Prog):
    def alloc_static(self):
        super().alloc_static()
        nc, es, L = self.nc, self.es, self.depth
        self.ident32 = T(es, nc, "ident32", [128, 128], F32)
        self.mask8 = T(es, nc, "mask8", [64, 512], F32)
        self.onesf = T(es, nc, "onesf", [64, 512], F32)
        self.oneb = T(es, nc, "oneb", [128, 1], F32)
        self.pA = T(es, nc, "pA", [64, L * 20], F32)
        self.pC = T(es, nc, "pC", [64, L * 8], F32)
        self.pD = T(es, nc, "pD", [64, L * 12], F32)
        self.pB = T(es, nc, "pB", [128, L * 6], F32)
        self.scw = T(es, nc, "scw", [128, L * 6], F32)
        self.hn = T(es, nc, "hn", [64, L * 8], F32)
        self.lbe = T(es, nc, "lbe", [64, 16], F32)
        self.lb = T(es, nc, "lb", [64, 16], F32)
        self.omlb = T(es, nc, "omlb", [64, 16], F32)
        self.lbs = T(es, nc, "lbs", [64, 4], F32)
        self.rel_aug = T(es, nc, "rel_aug", [33, 4], F32)
        self.b31 = T(es, nc, "b31", [128, 4], F32)
        self.Pm = T(es, nc, "Pm", [8, 56], F32)
        self.Agg = T(es, nc, "Agg", [56, 8], BF16)
        self.seln = T(es, nc, "seln", [8, 1024], BF16)
        self.mobac = T(es, nc, "mobac", [8, 24], F32)
        self.tbd = nc.dram_tensor("tbd", [4, 128, 1152], F32, kind="Internal")
        self.tbd_b = Buf()

    def load_consts(self):
        super().load_consts()
        S, L, din = self.S, self.depth, self.din
        S.dma("sp", self.ident32[:], din["ident"], writes=[self.ident32.b])
        S.dma("sp", self.mask8[:], din["mask8"], writes=[self.mask8.b])
        S.op("dve", lambda e: e.memset(self.onesf[:], 1.0), writes=[self.onesf.b])
        S.op("dve", lambda e: e.memset(self.oneb[:], 1.0), writes=[self.oneb.b])
        for nm, t, w in (("pA", self.pA, 20), ("pC", self.pC, 8), ("pD", self.pD, 12), ("pB", self.pB, 6)):
            S.dma("sp", t[:].rearrange("p (l f) -> p l f", l=L), din[nm].rearrange("l p f -> p l f"), writes=[t.b])
        S.dma("sp", self.scw[:].rearrange("p (l f) -> p l f", l=L), din["scw"].rearrange("l p a c -> p l (a c)"), writes=[self.scw.b])
        S.dma("sp", self.hn[:].rearrange("p (l f) -> p l f", l=L), din["hn"].rearrange("l p a c -> p l (a c)"), writes=[self.hn.b])
        S.dma("sp", self.lbe[:], din["lbl"].rearrange("p h l -> p (h l)"), writes=[self.lbe.b])
        S.dma("sp", self.rel_aug[:], din["rel_aug"], writes=[self.rel_aug.b])
        S.dma("sp", self.b31[:], din["b31"].partition_broadcast(128).rearrange("p a b -> p (a b)"), writes=[self.b31.b])
        S.dma("sp", self.Pm[:], din["Pm"], writes=[self.Pm.b])
        S.dma("pool", self.Agg[:], din["Agg"], writes=[self.Agg.b])
        S.dma("pool", self.seln[:], din["seln"], writes=[self.seln.b])
        S.dma("sp", self.mobac[:], din["mobac"], writes=[self.mobac.b])
        pa3 = self.pA[:].rearrange("p (g j) -> p g j", j=5)
        S.op("dve", lambda e: e.tensor_scalar(out=pa3[:, :, 2:3], in0=pa3[:, :, 2:3], scalar1=-LN8, scalar2=None, op0=ALU.add),
             reads=[self.pA.b], writes=[self.pA.b])
        S.op("dve", lambda e: e.tensor_scalar(out=pa3[:, :, 3:5], in0=pa3[:, :, 3:5], scalar1=-1.0, scalar2=None, op0=ALU.mult),
             reads=[self.pA.b], writes=[self.pA.b])
        lbe3 = self.lbe[:].rearrange("p (h l) -> p h l", l=4)
        S.op("act", lambda e: e.activation(out=self.lbe[:], in_=self.lbe[:], func=AF.Exp), reads=[self.lbe.b], writes=[self.lbe.b])
        S.op("dve", lambda e: e.reduce_sum(out=self.lbs[:], in_=lbe3, axis=AX.X), reads=[self.lbe.b], writes=[self.lbs.b])
        S.op("dve", lambda e: e.reciprocal(out=self.lbs[:], in_=self.lbs[:]), reads=[self.lbs.b], writes=[self.lbs.b])
        for h in range(4):
            S.op("dve", lambda e, h=h: e.tensor_scalar(out=self.lbe[:, h * 4:h * 4 + 4], in0=self.lbe[:, h * 4:h * 4 + 4],
                                                       scalar1=self.lbs[:, h:h + 1], scalar2=None, op0=ALU.mult),
                 reads=[self.lbe.b, self.lbs.b], writes=[self.lbe.b])
        lb3 = self.lb[:].rearrange("p (h l) -> p h l", l=4)
        S.op("dve", lambda e: e.memset(self.lb[:], 0.0), writes=[self.lb.b])
        for li in range(1, 4):
            S.op("dve", lambda e, li=li: e.tensor_tensor(out=lb3[:, :, li:li + 1], in0=lb3[:, :, li - 1:li], in1=lbe3[:, :, li:li + 1], op=ALU.add),
                 reads=[self.lb.b, self.lbe.b], writes=[self.lb.b])
        S.op("dve", lambda e: e.tensor_scalar(out=self.omlb[:], in0=self.lb[:], scalar1=-1.0, scalar2=1.0, op0=ALU.mult, op1=ALU.add),
             reads=[self.lb.b], writes=[self.omlb.b])
        nc = self.nc
        with contextlib.ExitStack() as es2:
            oh = T(es2, nc, "mboh", [33, 1152], F32)
            tbv = T(es2, nc, "mbtbv", [4, 1152], F32)
            S.dma("sp", oh[:], self.din["oh"], writes=[oh.b])
            for j in range(3):
                pp = self.ps[j % 2]
                S.op("pe", lambda e: e.matmul(pp[:4, :384], lhsT=self.rel_aug[:, :], rhs=oh[:, j * 384:(j + 1) * 384], start=True, stop=True),
                     reads=[self.rel_aug.b, oh.b], writes=[pp.b])
                S.op("act", lambda e: e.copy(out=tbv[:, j * 384:(j + 1) * 384], in_=pp[:4, :384]), reads=[pp.b], writes=[tbv.b])
            S.dma("sp", self.tbd.ap(), tbv[:].rearrange("p (a x) -> p a x", a=1).broadcast_to([4, 128, 1152]), reads=[tbv.b], writes=[self.tbd_b])

    def proj64(self, pp, W, woff, xn):
        S = self.S
        for kc in range(KC):
            S.op("pe", lambda e, kc=kc: e.matmul(pp[:64, :TT], lhsT=W[:, woff + kc * 64:woff + (kc + 1) * 64],
                                                 rhs=xn[:, kc * TT:(kc + 1) * TT], start=(kc == 0), stop=(kc == KC - 1)),
                 reads=[W.b, xn.b], writes=[pp.b])

    def y_store(self, yT, yb, chunk, h, tt):
        self.S.dma("sp", yT[(h % 2) * 64:(h % 2) * 64 + 64, chunk * SEQ + tt * TT:chunk * SEQ + (tt + 1) * TT], yb[:64, :TT],
                   reads=[yb.b], writes=[yT.b])

    def mixer_sublayer(self, s, l, first):
        S, nc, cfg = self.S, self.nc, self.cfg
        with contextlib.ExitStack() as es:
            yT = T(es, nc, "yT", [128, 8 * SEQ], BF16)
            self.yT = yT
            groups = cfg.get("groups", "ABCD")
            if groups != "ABCD":
                S.op("pool", lambda e: e.memset(yT[:], 0.0), writes=[yT.b])
            for tt in range(NT):
                self.norm_tile(s, tt, first, l, 0, stage=(self.xs[0] if tt % 2 == 0 else self.hout))
            if "B" in groups:
                self.group_B(s, l, yT)
            if "A" in groups:
                self.group_gla(s, l, yT, "A")
            if "D" in groups:
                self.group_gla(s, l, yT, "D")
            if "C" in groups:
                self.group_C(s, l, yT)
            if cfg.get("dbg_y", False) and s == 0 and l == 0:
                d = nc.dram_tensor("dbg_y", [128, 8 * SEQ], BF16, kind="ExternalOutput").ap()
                b = Buf()
                S.dma("sp", d, yT[:], reads=[yT.b], writes=[b])
                self.dbg_bufs.append(b)
            with contextlib.ExitStack() as es2:
                wo = T(es2, nc, "w_mo", [128, 8 * 8 * 128], BF16)
                S.dma("pool", wo[:].rearrange("p (o k m) -> p o k m", o=8, k=8), self.din["w_mo"][l].rearrange("o p k m -> p o k m"), writes=[wo.b])
                hout2 = T(es2, nc, "mo_hout2", [128, KC * TT], F32)
                houts = [self.hout, hout2]
                for tt in range(NT):
                    ho = houts[tt % 2]
                    for o in range(KC):
                        pp = self.ps[o % 4]
                        for kc in range(KC):
                            S.op("pe", lambda e, pp=pp, o=o, kc=kc, tt=tt: e.matmul(
                                pp[:, :], lhsT=wo[:, (o * 8 + kc) * 128:(o * 8 + kc + 1) * 128],
                                rhs=yT[:, kc * SEQ + tt * TT:kc * SEQ + (tt + 1) * TT], start=(kc == 0), stop=(kc == KC - 1)),
                                reads=[wo.b, yT.b], writes=[pp.b])
                        S.op("act", lambda e, pp=pp, o=o: e.copy(out=ho[:, o * TT:(o + 1) * TT], in_=pp[:, :]),
                             reads=[pp.b], writes=[ho.b])
                        if o % 2 == 1:
                            self.pump()
                    self.drain()
                    self.pend(self.residual_gen(s, tt, first, l, 1, hout=ho))
                self.drain()

    def group_B(self, s, l, yT):
        S, nc = self.S, self.nc
        with contextlib.ExitStack() as es:
            wB = T(es, nc, "wB", [128, 6 * 8 * 128], BF16)
            S.dma("pool", wB[:].rearrange("p (o k m) -> p o k m", o=6, k=8), self.din["wB_T"][l].rearrange("o p k m -> p o k m"), writes=[wB.b])
            u = T(es, nc, "scu", [128, 2 + SEQ], F32)
            cbs = T(es, nc, "sccb", [128, SEQ], F32)
            a = T(es, nc, "sca", [128, SEQ], F32)
            ccs = T(es, nc, "sccc", [128, TT], F32)
            bcol = lambda oc: self.pB[:, l * 6 + oc:l * 6 + oc + 1]
            wcol = lambda j, c: self.scw[:, l * 6 + j * 2 + c:l * 6 + j * 2 + c + 1]
            for j in range(2):
                S.op("dve", lambda e: e.memset(u[:, 0:2], 0.0), writes=[u.b])
                for tt in range(NT):
                    xn = self.xn[tt]
                    pcb, pcc, pch = self.ps[0], self.ps[1], self.ps[2]
                    for oc, pp in ((0 + j, pcb), (2 + j, pcc), (4 + j, pch)):
                        for kc in range(KC):
                            S.op("pe", lambda e, oc=oc, pp=pp, kc=kc: e.matmul(pp[:, :], lhsT=wB[:, (oc * 8 + kc) * 128:(oc * 8 + kc + 1) * 128],
                                                                                rhs=xn[:, kc * TT:(kc + 1) * TT], start=(kc == 0), stop=(kc == KC - 1)),
                                 reads=[wB.b, xn.b], writes=[pp.b])
                    S.op("act", lambda e: e.activation(out=ccs[:], in_=pcc[:, :], func=AF.Identity, bias=bcol(2 + j)),
                         reads=[pcc.b, self.pB.b], writes=[ccs.b])
                    S.op("dve", lambda e, tt=tt: e.scalar_tensor_tensor(out=u[:, 2 + tt * TT:2 + (tt + 1) * TT], in0=pch[:, :], scalar=bcol(4 + j),
                                                                          in1=ccs[:], op0=ALU.add, op1=ALU.mult),
                         reads=[pch.b, ccs.b, self.pB.b], writes=[u.b])
                    if self.cfg.get("dump"):
                        S.op("act", lambda e, tt=tt: e.copy(out=a[:, tt * TT:(tt + 1) * TT], in_=pcb[:, :]), reads=[pcb.b], writes=[a.b])
                    S.op("act", lambda e, tt=tt: e.activation(out=cbs[:, tt * TT:(tt + 1) * TT], in_=pcb[:, :], func=AF.Identity, bias=bcol(0 + j)),
                         reads=[pcb.b, self.pB.b], writes=[cbs.b])
                self.dump("cbs", cbs, cbs[:], [128, SEQ])
                self.dump("araw", a, a[:], [128, SEQ])
                self.dump("wB", wB, wB[:], [128, 6144], BF16)
                self.dump("u", u, u[:], [128, 2 + SEQ])
                self.dump("xn0", self.xn[0], self.xn[0][:], [128, KC * TT], BF16)
                S.op("dve", lambda e: e.tensor_scalar(out=a[:], in0=u[:, 2:2 + SEQ], scalar1=wcol(2, j), scalar2=None, op0=ALU.mult),
                     reads=[u.b, self.scw.b], writes=[a.b])
                S.op("dve", lambda e: e.scalar_tensor_tensor(out=a[:], in0=u[:, 1:1 + SEQ], scalar=wcol(1, j), in1=a[:], op0=ALU.mult, op1=ALU.add),
                     reads=[u.b, a.b, self.scw.b], writes=[a.b])
                S.op("dve", lambda e: e.scalar_tensor_tensor(out=a[:], in0=u[:, 0:SEQ], scalar=wcol(0, j), in1=a[:], op0=ALU.mult, op1=ALU.add),
                     reads=[u.b, a.b, self.scw.b], writes=[a.b])
                S.op("dve", lambda e: e.tensor_tensor(out=yT[:, (2 + j) * SEQ:(3 + j) * SEQ], in0=a[:], in1=cbs[:], op=ALU.mult),
                     reads=[a.b, cbs.b], writes=[yT.b])

    @staticmethod
    def run_interleaved(gens):
        gens = list(gens)
        while gens:
            nxt = []
            for g in gens:
                try:
                    next(g)
                    nxt.append(g)
                except StopIteration:
                    pass
            gens = nxt

    def group_gla(self, s, l, yT, mode):
        S, nc = self.S, self.nc
        isA = mode == "A"
        nT = 5 if isA else 3
        vidx = 0 if isA else 2
        vw = 128 if isA else 64
        ych0 = 0 if isA else 6
        pb = self.pA if isA else self.pD
        with contextlib.ExitStack() as es:
            W = T(es, nc, "glaW", [128, 4 * nT * 8 * 64], BF16)
            Wv = T(es, nc, "glaWv", [128, 8 * 256], BF16)
            bvb = T(es, nc, "glabv", [64, 256], F32)
            vt = T(es, nc, "glavt", [64, 8 * 4 * vw], BF16)
            Qs = [T(es, nc, "glaQs%d" % h, [64, TT], BF16) for h in range(4)]
            Am = [T(es, nc, "glaAm%d" % h, [64, TT], BF16) for h in range(4)]
            KeT = [T(es, nc, "glaKeT%d" % h, [64, TT], BF16) for h in range(4)]
            og = [T(es, nc, "glaog%d" % h, [64, TT], F32) for h in range(4)]
            dec = [T(es, nc, "gladec%d" % h, [64, 8], F32) for h in range(4)]
            Ks = [T(es, nc, "glaKs%d" % i, [64, TT], BF16) for i in range(2)]
            gcs = [T(es, nc, "glagc%d" % i, [64, TT + 8], F32) for i in range(2)]
            yb = [T(es, nc, "glayb%d" % i, [64, TT], BF16) for i in range(2)]
            S32 = [T(es, nc, "glaS32_%d" % h, [64, vw], F32) for h in range(4)]
            Sbf = [T(es, nc, "glaSbf_%d" % h, [64, vw], BF16) for h in range(4)]
            xs = self.xs[0]
            wbf = [self.wb[i] for i in range(2)]
            lane_slots = [
                [V(lambda i=i: xs[0:64, i * TT:(i + 1) * TT]) for i in range(7)],
                [V(lambda i=i: wbf[0][0:64, :].bitcast(F32)[:, i * TT:(i + 1) * TT]) for i in range(6)]
                + [V(lambda: wbf[1][0:64, :].bitcast(F32)[:, 0:TT])],
            ]
            sqv = [V(lambda h=h: self.sq[0:64, h * TT:(h + 1) * TT]) for h in range(4)]
            psU = [self.ps[i] for i in (6, 0, 1, 2)]
            wsrc = self.din["wA_T" if isA else "wD_T"][l]
            S.dma("pool", W[:].rearrange("p (o k m) -> p o k m", o=4 * nT, k=8), wsrc.rearrange("o p k m -> p o k m"), writes=[W.b])
            S.dma("pool", Wv[:].rearrange("p (k m) -> p k m", k=8), self.din["w_v"][l, vidx], writes=[Wv.b])
            S.dma("sp", bvb[:], self.din["b_v"][l, vidx:vidx + 1, :].partition_broadcast(64).rearrange("p a b -> p (a b)"), writes=[bvb.b])
            S.op("dve", lambda e: e.memset(gcs[0][:, 0:1], 0.0), reads=[xs.b], writes=[gcs[0].b] + [v.b for v in lane_slots[0]])
            S.op("dve", lambda e: e.memset(gcs[1][:, 0:1], 0.0), reads=[wbf[0].b, wbf[1].b], writes=[gcs[1].b] + [v.b for v in lane_slots[1]])
            S.op("dve", lambda e: e.memset(vt[:], 1.0), reads=[self.sq.b], writes=[vt.b] + [v.b for v in sqv])
            for h in range(4):
                S.op("dve", lambda e, h=h: e.memset(S32[h][:], 0.0), writes=[S32[h].b])
                S.op("dve", lambda e, h=h: e.memset(Sbf[h][:], 0.0), writes=[Sbf[h].b])
            vt4 = vt[:].rearrange("p (b h w) -> p b h w", b=8, h=4)
            g3 = lambda ap: ap.rearrange("p (b t) -> p b t", t=64)

            def vtok(tt):
                xn = self.xn[tt]
                for b in range(8):
                    pv = self.ps[2 + b % 2]
                    for kc in range(KC):
                        S.op("pe", lambda e, kc=kc: e.matmul(pv[:64, :256], lhsT=xn[:, kc * TT + b * 64:kc * TT + (b + 1) * 64],
                                                             rhs=Wv[:, kc * 256:(kc + 1) * 256], start=(kc == 0), stop=(kc == KC - 1)),
                             reads=[xn.b, Wv.b], writes=[pv.b])
                    S.op("dve", lambda e: e.tensor_tensor(out=vt4[:, b, :, 0:64], in0=pv[:64, :256].rearrange("p (h w) -> p h w", h=4),
                                                          in1=bvb[:].rearrange("p (h w) -> p h w", h=4), op=ALU.add),
                         reads=[pv.b, bvb.b], writes=[vt.b])

            def prep(tt, h, lane):
                xn = self.xn[tt]
                t_q, t_k, t_e, t_f, t_gn, t_x, t_ke = lane_slots[lane]
                gc = gcs[lane]
                ks = Ks[lane]
                p0, p1 = (self.ps[0], self.ps[1]) if lane == 0 else (self.ps[2], self.ps[3])
                pa = self.ps[4 + 2 * lane]
                ptr = self.ps[5 + 2 * lane]
                bc = lambda j: pb[:, (l * 4 + h) * nT + j:(l * 4 + h) * nT + j + 1]
                wo = lambda j: (h * nT + j) * 8 * 64
                if isA:
                    self.proj64(p0, W, wo(3), xn)
                    S.op("act", lambda e: e.activation(out=t_f[:, :], in_=p0[:64, :TT], func=AF.Exp, bias=bc(3), scale=-1.0), reads=[p0.b, pb.b], writes=[t_f.b])
                    yield
                    self.proj64(p1, W, wo(1), xn)
                    S.op("act", lambda e: e.activation(out=t_k[:, :], in_=p1[:64, :TT], func=AF.Identity, bias=bc(1)), reads=[p1.b, pb.b], writes=[t_k.b])
                    S.op("act", lambda e: e.activation(out=t_f[:, :], in_=t_f[:, :], func=AF.Ln, bias=self.oneb[0:64, 0:1]), reads=[t_f.b, self.oneb.b], writes=[t_f.b])
                    yield
                    self.proj64(p0, W, wo(2), xn)
                    S.op("act", lambda e: e.activation(out=t_e[:, :], in_=p0[:64, :TT], func=AF.Exp, bias=bc(2)), reads=[p0.b, pb.b], writes=[t_e.b])
                    S.op("dve", lambda e: e.tensor_tensor_scan(out=gc[:, 1:TT + 1], data0=self.onesf[:, :], data1=t_f[:, :], initial=0.0, op0=ALU.mult, op1=ALU.add),
                         reads=[self.onesf.b, t_f.b], writes=[gc.b])
                    yield
                    self.proj64(p1, W, wo(0), xn)
                    S.op("act", lambda e: e.activation(out=t_q[:, :], in_=p1[:64, :TT], func=AF.Identity, bias=bc(0)), reads=[p1.b, pb.b], writes=[t_q.b])
                    S.op("dve", lambda e: e.tensor_tensor(out=t_k[:, :], in0=t_k[:, :], in1=t_e[:, :], op=ALU.mult), reads=[t_k.b, t_e.b], writes=[t_k.b])
                    yield
                    self.proj64(p0, W, wo(4), xn)
                    S.op("act", lambda e: e.activation(out=t_e[:, :], in_=p0[:64, :TT], func=AF.Exp, bias=bc(4), scale=-1.0), reads=[p0.b, pb.b], writes=[t_e.b])
                    yield
                    S.op("act", lambda e: e.activation(out=t_e[:, :], in_=t_e[:, :], func=AF.Ln, bias=self.oneb[0:64, 0:1]), reads=[t_e.b, self.oneb.b], writes=[t_e.b])
                    yield
                    S.op("act", lambda e: e.activation(out=og[h][:], in_=t_e[:, :], func=AF.Exp, scale=-1.0), reads=[t_e.b], writes=[og[h].b])
                else:
                    lbi = h * 4 + l
                    self.proj64(p0, W, wo(1), xn)
                    S.op("act", lambda e: e.activation(out=t_f[:, :], in_=p0[:64, :TT], func=AF.Sigmoid, bias=bc(1)), reads=[p0.b, pb.b], writes=[t_f.b])
                    yield
                    self.proj64(p1, W, wo(0), xn)
                    S.op("act", lambda e: e.activation(out=t_q[:, :], in_=p1[:64, :TT], func=AF.Silu, bias=bc(0)), reads=[p1.b, pb.b], writes=[t_q.b])
                    S.op("dve", lambda e: e.tensor_scalar(out=t_f[:, :], in0=t_f[:, :], scalar1=self.omlb[:, lbi:lbi + 1], scalar2=self.lb[:, lbi:lbi + 1],
                                                          op0=ALU.mult, op1=ALU.add), reads=[t_f.b, self.omlb.b, self.lb.b], writes=[t_f.b])
                    yield
                    S.op("dve", lambda e: e.tensor_scalar(out=t_k[:, :], in0=t_f[:, :], scalar1=-1.0, scalar2=1.0, op0=ALU.mult, op1=ALU.add),
                         reads=[t_f.b], writes=[t_k.b])
                    S.op("act", lambda e: e.activation(out=t_e[:, :], in_=t_f[:, :], func=AF.Ln), reads=[t_f.b], writes=[t_e.b])
                    yield
                    self.proj64(p0, W, wo(2), xn)
                    S.op("act", lambda e: e.activation(out=og[h][:], in_=p0[:64, :TT], func=AF.Silu, bias=bc(2)), reads=[p0.b, pb.b], writes=[og[h].b])
                    S.op("dve", lambda e: e.tensor_tensor_scan(out=gc[:, 1:TT + 1], data0=self.onesf[:, :], data1=t_e[:, :], initial=0.0, op0=ALU.mult, op1=ALU.subtract),
                         reads=[self.onesf.b, t_e.b], writes=[gc.b])
                    yield
                gn3 = g3(t_gn[:, :])
                S.op("dve", lambda e: e.tensor_tensor(out=gn3, in0=g3(gc[:, 1:TT + 1]), in1=g3(gc[:, 0:TT])[:, :, 0:1].broadcast_to([64, 8, 64]), op=ALU.subtract),
                     reads=[gc.b], writes=[t_gn.b])
                yield
                S.op("act", lambda e: e.activation(out=t_x[:, :], in_=t_gn[:, :], func=AF.Exp, scale=-1.0), reads=[t_gn.b], writes=[t_x.b])
                S.op("dve", lambda e: e.tensor_tensor(out=g3(t_f[:, :]), in0=gn3, in1=gn3[:, :, 63:64].broadcast_to([64, 8, 64]), op=ALU.subtract),
                     reads=[t_gn.b], writes=[t_f.b])
                yield
                S.op("dve", lambda e: e.tensor_tensor(out=Qs[h][:], in0=t_q[:, :], in1=t_x[:, :], op=ALU.mult), reads=[t_q.b, t_x.b], writes=[Qs[h].b])
                S.op("act", lambda e: e.activation(out=t_e[:, :], in_=t_gn[:, :], func=AF.Exp), reads=[t_gn.b], writes=[t_e.b])
                yield
                S.op("dve", lambda e: e.tensor_tensor(out=ks[:], in0=t_k[:, :], in1=t_e[:, :], op=ALU.mult), reads=[t_k.b, t_e.b], writes=[ks.b])
                S.op("act", lambda e: e.activation(out=t_f[:, :], in_=t_f[:, :], func=AF.Exp), reads=[t_f.b], writes=[t_f.b])
                yield
                for b in range(8):
                    S.op("pe", lambda e, b=b: e.matmul(pa[:64, b * 64:(b + 1) * 64], lhsT=ks[:, b * 64:(b + 1) * 64], rhs=Qs[h][:, b * 64:(b + 1) * 64],
                                                       start=True, stop=True), reads=[ks.b, Qs[h].b], writes=[pa.b])
                S.op("act", lambda e: e.activation(out=dec[h][:, :], in_=t_gn[:, 63:TT:64], func=AF.Exp, scale=-1.0), reads=[t_gn.b], writes=[dec[h].b])
                S.op("dve", lambda e: e.tensor_tensor(out=t_ke[:, :], in0=t_k[:, :], in1=t_f[:, :], op=ALU.mult), reads=[t_k.b, t_f.b], writes=[t_ke.b])
                yield
                S.op("dve", lambda e: e.tensor_tensor(out=Am[h][:], in0=pa[:64, :TT], in1=self.mask8[:, :], op=ALU.mult),
                     reads=[pa.b, self.mask8.b], writes=[Am[h].b])
                for b in range(8):
                    S.op("pe", lambda e, b=b: e.transpose(out=ptr[:64, b * 64:(b + 1) * 64], in_=t_ke[:, b * 64:(b + 1) * 64], identity=self.ident32[0:64, 0:64]),
                         reads=[t_ke.b, self.ident32.b], writes=[ptr.b])
                yield
                S.op("act", lambda e: e.copy(out=KeT[h][:], in_=ptr[:64, :TT]), reads=[ptr.b], writes=[KeT[h].b])
                yield

            nd = self.hout

            def blocks(tt):
                for b in range(8):
                    pnd = self.ps[4 + b % 2]
                    for h in range(4):
                        vb = (b * 4 + h) * vw
                        bs = slice(b * 64, (b + 1) * 64)
                        S.op("pe", lambda e: e.matmul(pnd[:64, h * 64:(h + 1) * 64], lhsT=vt[:, vb:vb + 64], rhs=Am[h][:, bs], start=True, stop=False),
                             reads=[vt.b, Am[h].b], writes=[pnd.b])
                        S.op("pe", lambda e: e.matmul(pnd[:64, h * 64:(h + 1) * 64], lhsT=Sbf[h][:, 0:64], rhs=Qs[h][:, bs], start=False, stop=True),
                             reads=[Sbf[h].b, Qs[h].b], writes=[pnd.b])
                        if isA:
                            S.op("pe", lambda e: e.matmul(pnd[:64, 256 + h * 64:256 + (h + 1) * 64], lhsT=vt[:, vb + 64:vb + 128], rhs=Am[h][:, bs], start=True, stop=False),
                                 reads=[vt.b, Am[h].b], writes=[pnd.b])
                            S.op("pe", lambda e: e.matmul(pnd[:64, 256 + h * 64:256 + (h + 1) * 64], lhsT=Sbf[h][:, 64:128], rhs=Qs[h][:, bs], start=False, stop=True),
                                 reads=[Sbf[h].b, Qs[h].b], writes=[pnd.b])
                        S.op("pe", lambda e: e.matmul(psU[h][:64, 0:vw], lhsT=KeT[h][:, bs], rhs=vt[:, vb:vb + vw], start=True, stop=True),
                             reads=[KeT[h].b, vt.b], writes=[psU[h].b])
                        S.op("dve", lambda e: e.scalar_tensor_tensor(out=S32[h][:], in0=S32[h][:], scalar=dec[h][:, b:b + 1], in1=psU[h][:64, 0:vw],
                                                                      op0=ALU.mult, op1=ALU.add), reads=[S32[h].b, dec[h].b, psU[h].b], writes=[S32[h].b])
                        S.op("act", lambda e: e.copy(out=Sbf[h][:], in_=S32[h][:]), reads=[S32[h].b], writes=[Sbf[h].b])
                    ncl = TT if isA else 256
                    S.op("act", lambda e: e.copy(out=nd[0:64, b * TT:b * TT + ncl], in_=pnd[:64, :ncl]), reads=[pnd.b], writes=[nd.b])

            nd3 = nd[0:64, :].rearrange("p (b x) -> p b x", b=8)

            def outputs(tt, h):
                lane = h % 2
                t_hh = lane_slots[lane][(h // 2) * 2]
                rsh = lane_slots[lane][(h // 2) * 2 + 1]
                ybh = yb[lane]
                sq = sqv[h]
                pss = self.ps[h]
                numv = nd3[:, :, h * 64:(h + 1) * 64]
                hh3 = g3(t_hh[:, :])
                if isA:
                    denv = nd3[:, :, 256 + h * 64:256 + (h + 1) * 64]
                    S.op("dve", lambda e: e.scalar_tensor_tensor(out=hh3, in0=denv, scalar=-1.0, in1=denv, op0=ALU.mult, op1=ALU.max), reads=[nd.b], writes=[t_hh.b])
                    yield
                    S.op("dve", lambda e: e.tensor_scalar(out=t_hh[:, :], in0=t_hh[:, :], scalar1=1.0, scalar2=None, op0=ALU.max), reads=[t_hh.b], writes=[t_hh.b])
                    yield
                    S.op("act", lambda e: e.activation(out=t_hh[:, :], in_=t_hh[:, :], func=AF.Ln), reads=[t_hh.b], writes=[t_hh.b])
                    yield
                    S.op("act", lambda e: e.activation(out=t_hh[:, :], in_=t_hh[:, :], func=AF.Exp, scale=-1.0), reads=[t_hh.b], writes=[t_hh.b])
                    yield
                    S.op("dve", lambda e: e.tensor_tensor(out=hh3, in0=numv, in1=hh3, op=ALU.mult), reads=[nd.b, t_hh.b], writes=[t_hh.b])
                    yield
                else:
                    S.op("act", lambda e: e.copy(out=hh3, in_=numv), reads=[nd.b], writes=[t_hh.b])
                    yield
                S.op("act", lambda e: e.activation(out=sq[:, :], in_=t_hh[:, :], func=AF.Square), reads=[t_hh.b], writes=[sq.b])
                yield
                S.op("pe", lambda e: e.matmul(pss[:64, :TT], lhsT=self.ones[0:64, 0:64], rhs=sq[:, :], start=True, stop=True),
                     reads=[self.ones.b, sq.b], writes=[pss.b])
                yield
                S.op("act", lambda e: e.activation(out=rsh[:, :], in_=pss[:64, :TT], func=AF.Ln, scale=1.0 / 64, bias=self.epsb[0:64, 0:1]),
                     reads=[pss.b, self.epsb.b], writes=[rsh.b])
                yield
                S.op("act", lambda e: e.activation(out=rsh[:, :], in_=rsh[:, :], func=AF.Exp, scale=-0.5), reads=[rsh.b], writes=[rsh.b])
                yield
                gi = (l * 2 + (0 if isA else 1)) * 4 + h
                S.op("dve", lambda e: e.scalar_tensor_tensor(out=t_hh[:, :], in0=t_hh[:, :], scalar=self.hn[:, gi:gi + 1], in1=rsh[:, :], op0=ALU.mult, op1=ALU.mult),
                     reads=[t_hh.b, self.hn.b, rsh.b], writes=[t_hh.b])
                yield
                S.op("dve", lambda e: e.tensor_tensor(out=ybh[:], in0=t_hh[:, :], in1=og[h][:], op=ALU.mult), reads=[t_hh.b, og[h].b], writes=[ybh.b])
                self.y_store(yT, ybh, ych0 + h // 2, h, tt)
                yield

            vtok(0)
            for tt in range(NT):
                self.run_interleaved([prep(tt, 0, 0), prep(tt, 1, 1)])
                self.run_interleaved([prep(tt, 2, 0), prep(tt, 3, 1)])
                blocks(tt)
                if tt + 1 < NT:
                    vtok(tt + 1)
                self.run_interleaved([outputs(tt, h) for h in range(4)])
            S.op("dve", lambda e: e.memset(gcs[0][:, 0:1], 0.0), reads=[v.b for v in lane_slots[0]], writes=[xs.b, gcs[0].b])
            S.op("dve", lambda e: e.memset(gcs[1][:, 0:1], 0.0), reads=[v.b for v in lane_slots[1]], writes=[wbf[0].b, wbf[1].b, gcs[1].b])
            S.op("dve", lambda e: e.memset(gcs[0][:, 0:1], 0.0), reads=[v.b for v in sqv], writes=[self.sq.b, gcs[0].b])

    def group_C(self, s, l, yT):
        S, nc = self.S, self.nc
        scale = 0.125
        with contextlib.ExitStack() as es:
            W = T(es, nc, "mbW", [128, 8 * 8 * 64], BF16)
            Wv = T(es, nc, "mbWv", [128, 8 * 256], BF16)
            bvb = T(es, nc, "mbbv", [128, 256], F32)
            kT = [T(es, nc, "mbkT%d" % h, [128, SEQ], BF16) for h in range(4)]
            vtok = T(es, nc, "mbvtok", [128, 16 * 4 * 65], BF16)
            vt5 = vtok[:].rearrange("p (c h w) -> p c h w", c=16, h=4)
            rrowb = T(es, nc, "mbrrowb", [65, TT], BF16)
            TBr = [T(es, nc, "mbTB%d" % h, [128, 1024], F32) for h in range(2)]
            q32 = [T(es, nc, "mbq32_%d" % i, [64, TT], F32) for i in range(2)]
            qT = [T(es, nc, "mbqT%d" % i, [128, TT], BF16) for i in range(2)]
            k32 = q32[0]
            kmean = T(es, nc, "mbkmean", [64, 32], F32)
            gm0 = T(es, nc, "mbgm", [8, TT], F32)
            gm = [gm0, gm0]
            gt = [T(es, nc, "mbgt%d" % i, [56, TT], BF16) for i in range(2)]
            nm = [T(es, nc, "mbnm%d" % i, [8, TT], BF16) for i in range(2)]
            tmp80 = gm0
            tmp8 = [tmp80, tmp80]
            tmpS = [self.hout, self.xs[0]]
            ex = [T(es, nc, "mbex%d" % i, [128, TT], BF16) for i in range(3)]
            rden = T(es, nc, "mbrden", [65, TT], F32)
            rrow = rden
            yb = T(es, nc, "mbyb", [64, TT], BF16)
            S.dma("pool", W[:].rearrange("p (o k m) -> p o k m", o=8, k=8), self.din["wC_T"][l].rearrange("o p k m -> p o k m"), writes=[W.b])
            S.dma("pool", Wv[:].rearrange("p (k m) -> p k m", k=8), self.din["w_v"][l, 1], writes=[Wv.b])
            S.dma("sp", bvb[:], self.din["b_v"][l, 1:2, :].partition_broadcast(128).rearrange("p a b -> p (a b)"), writes=[bvb.b])
            S.op("dve", lambda e: e.memset(kmean[:], 0.0), writes=[kmean.b])
            S.op("pool", lambda e: e.memset(vtok[:], 1.0), writes=[vtok.b])
            for h in range(4):
                S.op("pool", lambda e, h=h: e.memset(kT[h][64:128, :], 0.0), writes=[kT[h].b])
                S.dma("pool", kT[h][64:72, :], self.din["kind"], reads=[kT[h].b], writes=[kT[h].b])
            for i in range(2):
                S.op("pool", lambda e, i=i: e.memset(qT[i][64:128, :], 0.0), writes=[qT[i].b])
            mc3 = lambda which, tt: self.mobac[:, (which * 4 + tt) * 2:(which * 4 + tt) * 2 + 2].rearrange("p (a b) -> p a b", b=1).broadcast_to([8, 2, 256])
            v3 = lambda ap: ap.rearrange("p (a b) -> p a b", b=256)

            def head_prep(tt, h):
                xn = self.xn[tt]
                i = h % 2
                pq = self.ps[1]
                self.proj64(pq, W, (h * 2) * 512, xn)
                S.op("act", lambda e: e.activation(out=q32[i][:], in_=pq[:64, :TT], func=AF.Identity, bias=self.pC[:, l * 8 + h * 2:l * 8 + h * 2 + 1]),
                     reads=[pq.b, self.pC.b], writes=[q32[i].b])
                S.op("act", lambda e: e.copy(out=qT[i][0:64, :], in_=q32[i][:]), reads=[q32[i].b], writes=[qT[i].b])
                pg, pdm = self.ps[4], self.ps[5]
                S.op("pe", lambda e: e.matmul(pg[:8, :TT], lhsT=kmean[:, h * 8:h * 8 + 8], rhs=q32[i][:], start=True, stop=True),
                     reads=[kmean.b, q32[i].b], writes=[pg.b])
                S.op("dve", lambda e: e.tensor_tensor(out=v3(gm[i][:]), in0=v3(pg[:8, :TT]), in1=mc3(0, tt), op=ALU.add),
                     reads=[pg.b, self.mobac.b], writes=[gm[i].b])
                S.op("pe", lambda e: e.matmul(pdm[:56, :TT], lhsT=self.Pm[:, :], rhs=gm[i][:], start=True, stop=True),
                     reads=[self.Pm.b, gm[i].b], writes=[pdm.b])
                S.op("dve", lambda e: e.tensor_single_scalar(out=gt[i][:], in_=pdm[:56, :TT], scalar=0.0, op=ALU.is_gt), reads=[pdm.b], writes=[gt[i].b])
                S.op("pe", lambda e: e.matmul(pg[:8, :TT], lhsT=self.Agg[:, :], rhs=gt[i][:], start=True, stop=True),
                     reads=[self.Agg.b, gt[i].b], writes=[pg.b])
                S.op("dve", lambda e: e.scalar_tensor_tensor(out=v3(tmp8[i][:]), in0=v3(pg[:8, :TT]), scalar=2.5, in1=mc3(1, tt), op0=ALU.is_lt, op1=ALU.mult),
                     reads=[pg.b, self.mobac.b], writes=[tmp8[i].b])
                S.op("dve", lambda e: e.tensor_tensor(out=v3(tmp8[i][:]), in0=v3(tmp8[i][:]), in1=mc3(2, tt), op=ALU.add),
                     reads=[tmp8[i].b, self.mobac.b], writes=[tmp8[i].b])
                S.op("dve", lambda e: e.tensor_scalar(out=nm[i][:], in0=tmp8[i][:], scalar1=BIGRAW, scalar2=None, op0=ALU.mult), reads=[tmp8[i].b], writes=[nm[i].b])
                S.dma("sp", qT[i][64:72, :], nm[i][:], reads=[nm[i].b, qT[i].b], writes=[qT[i].b])
                TBh = TBr[i]
                S.dma("sp", TBh[:], bass.AP(self.tbd, h * 128 * 1152 + 127, [[1151, 128], [1, 1024]]), reads=[self.tbd_b], writes=[TBh.b])

            def head_attn(tt, h):
                i = h % 2
                TBh = TBr[i]
                po, pd = self.ps[6], self.ps[7]
                nj = 4 * tt + 4
                pSr = [self.ps[2], self.ps[3], self.ps[0]]
                def st1(j):
                    pS = pSr[j % 3]
                    S.op("pe", lambda e: e.matmul(pS[:, :TT], lhsT=kT[h][:, j * 128:(j + 1) * 128], rhs=qT[i][:, :], start=True, stop=True),
                         reads=[kT[h].b, qT[i].b], writes=[pS.b])
                    o = tt * 512 - j * 128
                    exj = ex[j % 3]
                    if o <= 128:
                        tS = tmpS[j % 2]
                        S.op("dve", lambda e: e.scalar_tensor_tensor(out=tS[:, :TT], in0=pS[:, :TT], scalar=scale, in1=TBh[:, o + 384:o + 384 + 512],
                                                                      op0=ALU.mult, op1=ALU.add), reads=[pS.b, TBh.b], writes=[tS.b])
                        S.op("act", lambda e: e.activation(out=exj[:], in_=tS[:, :TT], func=AF.Exp), reads=[tS.b], writes=[exj.b])
                    else:
                        S.op("act", lambda e: e.activation(out=exj[:], in_=pS[:, :TT], func=AF.Exp, scale=scale, bias=self.b31[:, h:h + 1]),
                             reads=[pS.b, self.b31.b], writes=[exj.b])

                def st2(j):
                    exj = ex[j % 3]
                    S.op("pe", lambda e: e.matmul(po[:65, :TT], lhsT=vt5[:, j, h, :], rhs=exj[:], start=(j == 0), stop=(j == nj - 1)),
                         reads=[vtok.b, exj.b], writes=[po.b])

                LA = 2
                for j in range(nj + LA):
                    if j < nj:
                        st1(j)
                    if j - LA >= 0:
                        st2(j - LA)
                S.op("act", lambda e: e.activation(out=rrow[64:65, :], in_=po[64:65, :TT], func=AF.Ln), reads=[po.b], writes=[rrow.b])
                S.op("act", lambda e: e.activation(out=rrowb[64:65, :], in_=rrow[64:65, :], func=AF.Exp, scale=-1.0), reads=[rrow.b], writes=[rrowb.b])
                S.op("pe", lambda e: e.matmul(pd[:64, :TT], lhsT=self.ones[64:65, 0:64], rhs=rrowb[64:65, :], start=True, stop=True),
                     reads=[self.ones.b, rrowb.b], writes=[pd.b])
                S.op("act", lambda e: e.copy(out=rden[0:64, :], in_=pd[:64, :TT]), reads=[pd.b], writes=[rden.b])
                S.op("dve", lambda e: e.tensor_tensor(out=yb[:], in0=po[:64, :TT], in1=rden[0:64, :], op=ALU.mult), reads=[po.b, rden.b], writes=[yb.b])
                self.y_store(yT, yb, 4 + h // 2, h, tt)

            for tt in range(NT):
                xn = self.xn[tt]
                for h in range(4):
                    pp = self.ps[h % 2]
                    self.proj64(pp, W, (h * 2 + 1) * 512, xn)
                    S.op("act", lambda e: e.activation(out=k32[:], in_=pp[:64, :TT], func=AF.Identity, bias=self.pC[:, l * 8 + h * 2 + 1:l * 8 + h * 2 + 2]),
                         reads=[pp.b, self.pC.b], writes=[k32.b])
                    S.op("act", lambda e: e.copy(out=kT[h][0:64, tt * TT:(tt + 1) * TT], in_=k32[:]), reads=[k32.b], writes=[kT[h].b])
                    S.op("dve", lambda e: e.reduce_sum(out=kmean[:, h * 8 + 2 * tt:h * 8 + 2 * tt + 2], in_=v3(k32[:]), axis=AX.X),
                         reads=[k32.b], writes=[kmean.b])
                for j in range(4):
                    pv = self.ps[2 + j % 2]
                    for kc in range(KC):
                        S.op("pe", lambda e, kc=kc: e.matmul(pv[:, :256], lhsT=xn[:, kc * TT + j * 128:kc * TT + (j + 1) * 128],
                                                             rhs=Wv[:, kc * 256:(kc + 1) * 256], start=(kc == 0), stop=(kc == KC - 1)),
                             reads=[xn.b, Wv.b], writes=[pv.b])
                    S.op("dve", lambda e: e.tensor_tensor(out=vt5[:, tt * 4 + j, :, 0:64], in0=pv[:, :256].rearrange("p (h w) -> p h w", h=4),
                                                          in1=bvb[:].rearrange("p (h w) -> p h w", h=4), op=ALU.add),
                         reads=[pv.b, bvb.b], writes=[vtok.b])
                head_prep(tt, 0)
                head_prep(tt, 1)
                head_attn(tt, 0)
                head_prep(tt, 2)
                head_attn(tt, 1)
                head_prep(tt, 3)
                head_attn(tt, 2)
                head_attn(tt, 3)


N_CORES = 8


def kernel(**inputs):
    inp = {k: np.asarray(v) for k, v in inputs.items()}
    B = inp["x"].shape[0]
    nseq = B // N_CORES
    sh = host_prepare(inp, DEPTH)
    sh = host_prepare_mixer(inp, DEPTH, sh)
    in_maps = []
    for c in range(N_CORES):
        core = dict(sh)
        core["xT"] = np.stack([to_T(inp["x"][c * nseq + b]) for b in range(nseq)])
        core["memT"] = np.stack([np.ascontiguousarray(inp["mem"][c * nseq + b].reshape(NMEM, 8, 128).transpose(2, 1, 0))
                                 for b in range(nseq)])
        in_maps.append(core)
    P = ProgM(dict(depth=DEPTH, nseq=nseq))
    nc = P.build({k: v.shape for k, v in in_maps[0].items()})
    res = run_bass_kernel_spmd(nc, in_maps, core_ids=list(range(N_CORES)))
    out = np.empty((B, SEQ, D), np.float32)
    for c in range(N_CORES):
        o = np.asarray(res.results[c]["outT"])
        for b in range(nseq):
            out[c * nseq + b] = from_T(o[b])
    return out
```

```python
import contextlib
import math
import numpy as np
import concourse.bass as bass
import concourse.mybir as mybir
from concourse.bass_utils import run_bass_kernel_spmd

F32 = mybir.dt.float32
BF16 = mybir.dt.bfloat16
AF = mybir.ActivationFunctionType
ALU = mybir.AluOpType
AX = mybir.AxisListType

D = 1024
SEQ = 2048
NSEQ = 2
DEPTH = 4
TT = 512
NT = SEQ // TT
KC = 8
DFF = 2816
NHC = DFF // 128
NMEM = 256
EPS = 1e-6
NEG = -30000.0


class Buf:
    __slots__ = ("w", "r")

    def __init__(self):
        self.w = None
        self.r = []


class Sched:
    NDMA = 8

    def __init__(self, nc, es):
        self.nc = nc
        self.engs = {"pe": nc.tensor, "act": nc.scalar, "dve": nc.vector,
                     "pool": nc.gpsimd, "sp": nc.sync}
        self.sems = {}
        for k in ("pe", "act", "dve", "pool"):
            self.sems[("e", k)] = es.enter_context(nc.semaphore("prog_" + k))
        for k in ("sp", "pool", "act"):
            for i in range(self.NDMA):
                self.sems[("d", k, i)] = es.enter_context(nc.semaphore("dma_%s_%d" % (k, i)))
        self.cnt = {k: 0 for k in self.engs}
        self.dcnt = {k: 0 for k in self.engs}
        self.waited = {k: {} for k in self.engs}
        self.nops = 0

    def _deps(self, eng, reads, writes):
        need = {}

        def add(tok):
            if tok is None:
                return
            k, v = tok
            if need.get(k, 0) < v:
                need[k] = v
        for b in reads:
            add(b.w)
        for b in writes:
            add(b.w)
            for t in b.r:
                add(t)
        wd = self.waited[eng]
        e = self.engs[eng]
        for k, v in need.items():
            if k == ("e", "pe") and eng == "pe":
                continue
            if wd.get(k, 0) >= v:
                continue
            wd[k] = v
            e.wait_ge(self.sems[k], v)

    def _commit(self, tok, reads, writes):
        for b in reads:
            b.r.append(tok)
            if len(b.r) > 64:
                m = {}
                for k, v in b.r:
                    if m.get(k, 0) < v:
                        m[k] = v
                b.r = list(m.items())
        for b in writes:
            b.w = tok
            b.r = []

    def op(self, eng, fn, reads=(), writes=(), inc=True):
        self._deps(eng, reads, writes)
        k = ("e", eng)
        if inc:
            self.cnt[eng] += 1
            fn(self.engs[eng]).then_inc(self.sems[k], 1)
            tok = (k, self.cnt[eng])
        else:
            assert eng == "pe"
            fn(self.engs[eng])
            tok = (k, self.cnt[eng] + 1)
        self._commit(tok, reads, writes)
        self.nops += 1
        return tok

    def dma(self, eng, out, in_, reads=(), writes=(), **kw):
        j = self.dcnt[eng]
        self.dcnt[eng] += 1
        sk = ("d", eng, j % self.NDMA)
        val = 16 * (j // self.NDMA + 1)
        self._deps(eng, reads, writes)
        if j >= self.NDMA:
            prev = 16 * (j // self.NDMA)
            wd = self.waited[eng]
            if wd.get(sk, 0) < prev:
                wd[sk] = prev
                self.engs[eng].wait_ge(self.sems[sk], prev)
        self.engs[eng].dma_start(out=out, in_=in_, **kw).then_inc(self.sems[sk], 16)
        tok = (sk, val)
        self._commit(tok, reads, writes)
        self.nops += 1
        return tok

    def finish(self, eng, bufs):
        need = {}
        for b in bufs:
            if b.w is not None:
                k, v = b.w
                need[k] = max(need.get(k, 0), v)
        for k, v in need.items():
            self.engs[eng].wait_ge(self.sems[k], v)


class T:
    _n = [0]

    def __init__(self, es, nc, name, shape, dtype, psum=False):
        T._n[0] += 1
        name = "t%d_%s" % (T._n[0], name)
        if psum:
            self.t = es.enter_context(nc.psum_tensor(name, shape, dtype))
        else:
            self.t = es.enter_context(nc.sbuf_tensor(name, shape, dtype))
        self.b = Buf()
        self.b.r = list(T.grave.items())
        es.callback(self._retire)

    grave = {}

    def _retire(self):
        g = T.grave
        toks = list(self.b.r)
        if self.b.w is not None:
            toks.append(self.b.w)
        for k, v in toks:
            if g.get(k, 0) < v:
                g[k] = v

    def __getitem__(self, k):
        return self.t[k]


def arr_w(w, ocw):
    K, N = w.shape
    a = w.reshape(K // 128, 128, N // ocw, ocw)
    return np.ascontiguousarray(a.transpose(2, 1, 0, 3))


def to_T(x):
    a = x.reshape(x.shape[0] // TT, TT, KC, 128)
    return np.ascontiguousarray(a.transpose(0, 3, 2, 1))


def from_T(a):
    return np.ascontiguousarray(a.transpose(0, 3, 2, 1)).reshape(-1, KC * 128)


def colT(v, w=128):
    return np.ascontiguousarray(v.reshape(-1, w).T)


def host_prepare(inp, depth):
    sh = {}
    L = depth
    wup = []
    for l in range(L):
        w = inp["w_ffn_in"][l]
        g = arr_w(w[:, :DFF], 128)
        u = arr_w(w[:, DFF:], 128)
        wup.append(np.concatenate([g, u], axis=3))
    sh["w_up"] = np.stack(wup)
    wd = []
    for l in range(L):
        w = inp["w_ffn_out"][l]
        a = w.reshape(NHC, 128, 8, 128)
        wd.append(np.ascontiguousarray(a.transpose(2, 1, 0, 3)))
    sh["w_down"] = np.stack(wd)
    sh["ffn_cw"] = np.stack([np.stack([colT(inp["ffn_conv_w"][l][j]) for j in range(3)] + [colT(inp["ffn_conv_b"][l])], axis=1)
                             for l in range(L)])
    names = ["norm_mix_pre", "norm_mix_post", "norm_cross_pre", "norm_cross_post", "norm_ffn_pre", "norm_ffn_post", "mem_norm"]
    sh["gains"] = np.stack([np.stack([colT(inp[n][l]) for n in names], axis=1) for l in range(L)])
    sh["w_cq"] = np.stack([arr_w(inp["w_cq"][l], 128) for l in range(L)])
    sh["w_ck"] = np.stack([arr_w(inp["w_ck"][l], 128) for l in range(L)])
    sh["w_cv"] = np.stack([arr_w(inp["w_cv"][l], 512)[0] for l in range(L)])
    sh["w_co"] = np.stack([np.ascontiguousarray(inp["w_co"][l].reshape(4, 128, 8, 128).transpose(2, 1, 0, 3)) for l in range(L)])
    sh["ident"] = np.eye(128, dtype=np.float32)
    return sh


class Prog:
    def __init__(self, cfg):
        self.cfg = cfg
        self.depth = cfg.get("depth", DEPTH)
        self.nseq = cfg.get("nseq", NSEQ)

    def dram_in(self, name, shape, dt=F32):
        return self.nc.dram_tensor(name, list(shape), dt, kind="ExternalInput").ap()

    def build(self, shapes):
        cfg = self.cfg
        nc = self.nc = bass.Bass("TRN2", target_bir_lowering=False)
        L = self.depth
        self.din = {k: self.dram_in(k, v) for k, v in shapes.items()}
        self.outT = nc.dram_tensor("outT", [self.nseq, NT, 128, KC, TT], F32, kind="ExternalOutput").ap()
        self.dbg = {}
        es = self.es = contextlib.ExitStack()
        with es:
            S = self.S = Sched(nc, es)
            T.grave = {}
            self.x_buf = [[Buf() for _ in range(NT)] for _ in range(self.nseq)]
            self.alloc_static()
            self.load_consts()
            seq_list = []
            for l in range(L):
                if cfg.get("mix", True):
                    seq_list.append((l, 0, 1))
                if cfg.get("cross", True):
                    seq_list.append((l, 2, 3))
                if cfg.get("ffn", True):
                    seq_list.append((l, 4, 5))
            self.next_of = {}
            if cfg.get("chain_norm", True):
                for i in range(len(seq_list) - 1):
                    self.next_of[(seq_list[i][0], seq_list[i][2])] = (seq_list[i + 1][0], seq_list[i + 1][1])
            self.norm_done = set()
            for s in range(self.nseq):
                for l in range(L):
                    first = (l == 0)
                    src_first = first
                    if cfg.get("mix", True):
                        self.mixer_sublayer(s, l, src_first)
                        src_first = False
                    if cfg.get("cross", True):
                        self.cross_sublayer(s, l, src_first)
                        src_first = False
                    if cfg.get("ffn", True):
                        self.ffn_sublayer(s, l, src_first)
                        src_first = False
            S.finish("sp", [b for row in self.x_buf for b in row] + list(self.dbg_bufs))
        return nc

    def dump(self, name, t, ap, shape, dt=F32):
        if not self.cfg.get("dump", False) or name in self.dbg:
            return
        d = self.nc.dram_tensor("dbg_" + name, list(shape), dt, kind="ExternalOutput").ap()
        b = Buf()
        self.S.dma("sp", d, ap, reads=[t.b], writes=[b])
        self.dbg[name] = d
        self.dbg_bufs.append(b)

    def alloc_static(self):
        nc, es = self.nc, self.es
        L = self.depth
        self.dbg_bufs = []
        self.ones = T(es, nc, "ones", [128, 128], BF16)
        self.ident = T(es, nc, "ident", [128, 128], BF16)
        self.gains = T(es, nc, "gains", [128, L * 7 * 8], F32)
        self.ffn_cw = T(es, nc, "ffn_cw", [128, L * 4 * NHC], F32)
        self.ps = [T(es, nc, "ps%d" % i, [128, 512], F32, psum=True) for i in range(8)]
        self.wb = [T(es, nc, "wb%d" % i, [128, 6144], BF16) for i in range(2)]
        self.xs = [T(es, nc, "xs%d" % i, [128, KC * TT], F32) for i in range(1)]
        self.hout = T(es, nc, "hout", [128, KC * TT], F32)
        self.sq = T(es, nc, "sq", [128, KC * TT], BF16)
        self.rs = [T(es, nc, "rs%d" % i, [128, TT], F32) for i in range(2)]
        self.xn = [T(es, nc, "xn%d" % i, [128, KC * TT], BF16) for i in range(NT)]
        self.epsb = T(es, nc, "epsb", [128, 1], F32)
        self.wrot = 0

    def load_consts(self):
        S = self.S
        L = self.depth
        S.op("dve", lambda e: e.memset(self.ones[:], 1.0), writes=[self.ones.b])
        S.op("dve", lambda e: e.memset(self.epsb[:], EPS), writes=[self.epsb.b])
        S.dma("pool", self.ident[:], self.din["ident"], writes=[self.ident.b])
        S.dma("sp", self.gains[:].rearrange("p (l f) -> p l f", l=L), self.din["gains"].rearrange("l p a c -> p l (a c)"), writes=[self.gains.b])
        S.dma("sp", self.ffn_cw[:].rearrange("p (l f) -> p l f", l=L), self.din["ffn_cw"].rearrange("l p a c -> p l (a c)"), writes=[self.ffn_cw.b])

    def gain_col(self, l, which, c):
        i = (l * 7 + which) * 8 + c
        return self.gains[:, i:i + 1]

    def next_wb(self):
        w = self.wb[self.wrot % len(self.wb)]
        self.wrot += 1
        return w

    def x_src(self, s, tt, first):
        return self.din["xT"][s, tt] if first else self.outT[s, tt]

    def load_x(self, s, tt, first, dst):
        S = self.S
        S.dma("sp", dst[:].rearrange("p (c t) -> p c t", c=KC), self.x_src(s, tt, first),
              reads=[self.x_buf[s][tt]], writes=[dst.b])

    def rms_T(self, src, ncols, nchunks, dim, psA, rs_out):
        S = self.S
        n = nchunks * ncols
        S.op("act", lambda e: e.activation(out=self.sq[:, :n], in_=src[:, :n], func=AF.Square),
             reads=[src.b], writes=[self.sq.b])
        for c in range(nchunks):
            S.op("pe", lambda e, c=c: e.matmul(psA[:, :ncols], lhsT=self.ones[:], rhs=self.sq[:, c * ncols:(c + 1) * ncols],
                                               start=(c == 0), stop=(c == nchunks - 1)),
                 reads=[self.ones.b, self.sq.b], writes=[psA.b], inc=(c == nchunks - 1))
        S.op("act", lambda e: e.activation(out=rs_out[:, :ncols], in_=psA[:, :ncols], func=AF.Ln, scale=1.0 / dim, bias=self.epsb[:, 0:1]),
             reads=[psA.b, self.epsb.b], writes=[rs_out.b])
        S.op("act", lambda e: e.activation(out=rs_out[:, :ncols], in_=rs_out[:, :ncols], func=AF.Exp, scale=-0.5),
             reads=[rs_out.b], writes=[rs_out.b])

    def norm_tile(self, s, tt, first, l, which, stage=None):
        S = self.S
        if (s, l, which, tt) in self.norm_done:
            return
        xs = stage if stage is not None else self.xs[0]
        self.load_x(s, tt, first, xs)
        rs = self.rs[tt % 2]
        self.rms_T(xs, TT, KC, D, self.ps[7], rs)
        xn = self.xn[tt]
        for c in range(KC):
            S.op("dve", lambda e, c=c: e.scalar_tensor_tensor(out=xn[:, c * TT:(c + 1) * TT], in0=xs[:, c * TT:(c + 1) * TT],
                                                                scalar=self.gain_col(l, which, c), in1=rs[:, :TT],
                                                                op0=ALU.mult, op1=ALU.mult),
                 reads=[xs.b, rs.b, self.gains.b], writes=[xn.b])

    def residual_gen(self, s, tt, first, l, which, hout=None, xs=None):
        S = self.S
        hout = hout if hout is not None else self.hout
        rs = self.rs[tt % 2]
        xs = xs if xs is not None else self.xs[0]
        for _ in self.rms_gen(hout, TT, KC, D, self.ps[7], rs):
            yield
        self.load_x(s, tt, first, xs)
        for c in range(KC):
            sl = slice(c * TT, (c + 1) * TT)
            S.op("dve", lambda e, sl=sl, c=c: e.scalar_tensor_tensor(out=hout[:, sl], in0=hout[:, sl], scalar=self.gain_col(l, which, c),
                                                                      in1=rs[:, :TT], op0=ALU.mult, op1=ALU.mult),
                 reads=[hout.b, rs.b, self.gains.b], writes=[hout.b])
            S.op("dve", lambda e, sl=sl: e.tensor_tensor(out=xs[:, sl], in0=xs[:, sl], in1=hout[:, sl], op=ALU.add),
                 reads=[xs.b, hout.b], writes=[xs.b])
        S.dma("sp", self.outT[s, tt], xs[:].rearrange("p (c t) -> p c t", c=KC), reads=[xs.b], writes=[self.x_buf[s][tt]])
        nxt = self.next_of.get((l, which))
        if nxt is not None:
            l2, w2 = nxt
            rs2 = self.rs[(tt + 1) % 2]
            for _ in self.rms_gen(xs, TT, KC, D, self.ps[7], rs2):
                yield
            xn = self.xn[tt]
            for c in range(KC):
                S.op("dve", lambda e, c=c: e.scalar_tensor_tensor(out=xn[:, c * TT:(c + 1) * TT], in0=xs[:, c * TT:(c + 1) * TT],
                                                                    scalar=self.gain_col(l2, w2, c), in1=rs2[:, :TT],
                                                                    op0=ALU.mult, op1=ALU.mult),
                     reads=[xs.b, rs2.b, self.gains.b], writes=[xn.b])
            self.norm_done.add((s, l2, w2, tt))

    def residual_tile(self, *a, **kw):
        for _ in self.residual_gen(*a, **kw):
            pass

    def rms_gen(self, src, ncols, nchunks, dim, psA, rs_out):
        S = self.S
        n = nchunks * ncols
        S.op("act", lambda e: e.activation(out=self.sq[:, :n], in_=src[:, :n], func=AF.Square),
             reads=[src.b], writes=[self.sq.b])
        yield
        for c in range(nchunks):
            S.op("pe", lambda e, c=c: e.matmul(psA[:, :ncols], lhsT=self.ones[:], rhs=self.sq[:, c * ncols:(c + 1) * ncols],
                                               start=(c == 0), stop=(c == nchunks - 1)),
                 reads=[self.ones.b, self.sq.b], writes=[psA.b], inc=(c == nchunks - 1))
        S.op("act", lambda e: e.activation(out=rs_out[:, :ncols], in_=psA[:, :ncols], func=AF.Ln, scale=1.0 / dim, bias=self.epsb[:, 0:1]),
             reads=[psA.b, self.epsb.b], writes=[rs_out.b])
        S.op("act", lambda e: e.activation(out=rs_out[:, :ncols], in_=rs_out[:, :ncols], func=AF.Exp, scale=-0.5),
             reads=[rs_out.b], writes=[rs_out.b])

    def pend(self, gen):
        if not hasattr(self, "pending"):
            self.pending = []
        self.pending.append(gen)
        self.pump()

    def pump(self):
        for g in list(getattr(self, "pending", [])):
            try:
                next(g)
            except StopIteration:
                self.pending.remove(g)

    def drain(self):
        while getattr(self, "pending", []):
            self.pump()

    def ffn_sublayer(self, s, l, first):
        S, nc = self.S, self.nc
        HT = 1024
        NTH = HT // TT
        with contextlib.ExitStack() as es:
            hT = T(es, nc, "ffn_hT", [128, NHC * HT], BF16)
            gsb = T(es, nc, "ffn_g", [128, 2 + HT], F32)
            usb = T(es, nc, "ffn_u", [128, HT], F32)
            a1 = T(es, nc, "ffn_a1", [128, HT], F32)
            halo = T(es, nc, "ffn_halo", [128, NHC * 2], F32)
            S.op("dve", lambda e: e.memset(halo[:], 0.0), writes=[halo.b])
            for tt in range(NT):
                self.norm_tile(s, tt, first, l, 4, stage=(self.xs[0] if tt % 2 == 0 else self.hout))
            hout2 = T(es, nc, "ffn_hout2", [128, KC * TT], F32)
            hs = [self.hout, hout2]

            def up(half, hooks):
                for g in range(NHC // 2):
                    wt = self.next_wb()
                    S.dma("pool", wt[:, :2 * 8 * 256].rearrange("p (g k m) -> p g k m", g=2, k=8),
                          self.din["w_up"][l, 2 * g:2 * g + 2].rearrange("g p k m -> p g k m"), writes=[wt.b])
                    for ci in range(2):
                        c = 2 * g + ci
                        S.op("act", lambda e, c=c: e.copy(out=gsb[:, 0:2], in_=halo[:, 2 * c:2 * c + 2]),
                             reads=[halo.b], writes=[gsb.b])
                        for k in range(NTH):
                            tt = half * NTH + k
                            pg = self.ps[(2 * k) % 4]
                            pu = self.ps[(2 * k + 1) % 4]
                            for which, pp in ((0, pg), (1, pu)):
                                for kc in range(KC):
                                    off = (ci * 8 + kc) * 256 + which * 128
                                    S.op("pe", lambda e, pp=pp, off=off, kc=kc, tt=tt: e.matmul(
                                        pp[:, :], lhsT=wt[:, off:off + 128], rhs=self.xn[tt][:, kc * TT:(kc + 1) * TT],
                                        start=(kc == 0), stop=(kc == KC - 1)),
                                        reads=[wt.b, self.xn[tt].b], writes=[pp.b], inc=(kc == KC - 1))
                            S.op("act", lambda e, k=k, pg=pg: e.copy(out=gsb[:, 2 + k * TT:2 + (k + 1) * TT], in_=pg[:, :]),
                                 reads=[pg.b], writes=[gsb.b])
                            S.op("act", lambda e, k=k, pu=pu: e.copy(out=usb[:, k * TT:(k + 1) * TT], in_=pu[:, :]),
                                 reads=[pu.b], writes=[usb.b])
                        cw = lambda j, c=c: self.ffn_cw[:, (l * 4 + j) * NHC + c:(l * 4 + j) * NHC + c + 1]
                        S.op("dve", lambda e, cw=cw: e.tensor_scalar(out=a1[:], in0=gsb[:, 2:2 + HT], scalar1=cw(2), scalar2=cw(3),
                                                                      op0=ALU.mult, op1=ALU.add),
                             reads=[gsb.b, self.ffn_cw.b], writes=[a1.b])
                        S.op("dve", lambda e, cw=cw: e.scalar_tensor_tensor(out=a1[:], in0=gsb[:, 1:1 + HT], scalar=cw(1), in1=a1[:],
                                                                             op0=ALU.mult, op1=ALU.add),
                             reads=[gsb.b, a1.b, self.ffn_cw.b], writes=[a1.b])
                        S.op("dve", lambda e, cw=cw: e.scalar_tensor_tensor(out=a1[:], in0=gsb[:, 0:HT], scalar=cw(0), in1=a1[:],
                                                                             op0=ALU.mult, op1=ALU.add),
                             reads=[gsb.b, a1.b, self.ffn_cw.b], writes=[a1.b])
                        S.op("act", lambda e, c=c: e.copy(out=halo[:, 2 * c:2 * c + 2], in_=gsb[:, HT:HT + 2]),
                             reads=[gsb.b], writes=[halo.b])
                        S.op("act", lambda e: e.activation(out=a1[:], in_=a1[:], func=AF.Silu), reads=[a1.b], writes=[a1.b])
                        S.op("dve", lambda e, c=c: e.tensor_tensor(out=hT[:, c * HT:(c + 1) * HT], in0=a1[:], in1=usb[:], op=ALU.mult),
                             reads=[a1.b, usb.b], writes=[hT.b])
                    if g in hooks:
                        hooks[g]()

            def down(half):
                for o in range(KC):
                    wt = self.next_wb()
                    S.dma("pool", wt[:, :NHC * 128].rearrange("p (c m) -> p c m", c=NHC), self.din["w_down"][l, o], writes=[wt.b])
                    for k in range(NTH):
                        pp = self.ps[4 + (o * NTH + k) % 3]
                        for c in range(NHC):
                            S.op("pe", lambda e, pp=pp, c=c, k=k: e.matmul(
                                pp[:, :], lhsT=wt[:, c * 128:(c + 1) * 128], rhs=hT[:, c * HT + k * TT:c * HT + (k + 1) * TT],
                                start=(c == 0), stop=(c == NHC - 1)),
                                reads=[wt.b, hT.b], writes=[pp.b], inc=(c == NHC - 1))
                        S.op("act", lambda e, pp=pp, o=o, k=k: e.copy(out=hs[k][:, o * TT:(o + 1) * TT], in_=pp[:, :]),
                             reads=[pp.b], writes=[hs[k].b])

            xs2 = T(es, nc, "ffn_xs2", [128, KC * TT], F32)
            hk = {g: self.pump for g in range(NHC // 2)}
            up(0, hk)
            self.drain()
            down(0)
            queue = [lambda: self.residual_gen(s, 0, first, l, 5, hout=hs[0]),
                     lambda: self.residual_gen(s, 1, first, l, 5, hout=hs[1], xs=xs2)]

            def hook():
                if getattr(self, "pending", []):
                    self.pump()
                elif queue:
                    self.pend(queue.pop(0)())
            up(1, {g: hook for g in range(NHC // 2)})
            self.drain()
            while queue:
                self.pend(queue.pop(0)())
                self.drain()
            down(1)
            self.residual_tile(s, 2, first, l, 5, hout=hs[0])
            self.residual_tile(s, 3, first, l, 5, hout=hs[1], xs=xs2)

    def cross_sublayer(self, s, l, first):
        S, nc = self.S, self.nc
        with contextlib.ExitStack() as es:
            memf = T(es, nc, "c_memf", [128, KC * NMEM], F32)
            memn = T(es, nc, "c_memn", [128, KC * NMEM], BF16)
            kT = T(es, nc, "c_kT", [128, 4 * NMEM], BF16)
            vtok = T(es, nc, "c_vtok", [128, 2 * 512], BF16)
            wq = T(es, nc, "c_wq", [128, 4 * 8 * 128], BF16)
            wo = T(es, nc, "c_wo", [128, 8 * 4 * 128], BF16)
            qT = T(es, nc, "c_qT", [128, 4 * TT], BF16)
            oT = T(es, nc, "c_oT", [128, 4 * TT], BF16)
            ex = [T(es, nc, "c_ex%d" % i, [128, TT], BF16) for i in range(4)]
            hout2 = T(es, nc, "c_hout2", [128, KC * TT], F32)
            houts = [self.hout, hout2]
            rden = T(es, nc, "c_rden", [128, TT], F32)
            S.dma("pool", wq[:].rearrange("p (o k m) -> p o k m", o=4, k=8), self.din["w_cq"][l].rearrange("o p k m -> p o k m"), writes=[wq.b])
            S.dma("pool", wo[:].rearrange("p (o h m) -> p o h m", o=8, h=4), self.din["w_co"][l].rearrange("o p h m -> p o h m"), writes=[wo.b])
            wk = self.next_wb()
            S.dma("pool", wk[:, :4096].rearrange("p (o k m) -> p o k m", o=4, k=8), self.din["w_ck"][l].rearrange("o p k m -> p o k m"), writes=[wk.b])
            wv = self.next_wb()
            S.dma("pool", wv[:, :4096].rearrange("p (k m) -> p k m", k=8), self.din["w_cv"][l], writes=[wv.b])
            S.dma("sp", memf[:].rearrange("p (c t) -> p c t", c=KC), self.din["memT"][s], writes=[memf.b])
            rs = self.rs[0]
            self.rms_T(memf, NMEM, KC, D, self.ps[7], rs)
            for c in range(KC):
                S.op("dve", lambda e, c=c: e.scalar_tensor_tensor(out=memn[:, c * NMEM:(c + 1) * NMEM], in0=memf[:, c * NMEM:(c + 1) * NMEM],
                                                                    scalar=self.gain_col(l, 6, c), in1=rs[:, :NMEM], op0=ALU.mult, op1=ALU.mult),
                     reads=[memf.b, rs.b, self.gains.b], writes=[memn.b])
            for h in range(4):
                pp = self.ps[h % 2]
                for kc in range(KC):
                    S.op("pe", lambda e, pp=pp, h=h, kc=kc: e.matmul(pp[:, :NMEM], lhsT=wk[:, (h * 8 + kc) * 128:(h * 8 + kc + 1) * 128],
                                                                      rhs=memn[:, kc * NMEM:(kc + 1) * NMEM], start=(kc == 0), stop=(kc == KC - 1)),
                         reads=[wk.b, memn.b], writes=[pp.b], inc=(kc == KC - 1))
                S.op("act", lambda e, pp=pp, h=h: e.copy(out=kT[:, h * NMEM:(h + 1) * NMEM], in_=pp[:, :NMEM]), reads=[pp.b], writes=[kT.b])
            for mc in range(2):
                pp = self.ps[2 + mc]
                for kc in range(KC):
                    S.op("pe", lambda e, pp=pp, mc=mc, kc=kc: e.matmul(pp[:, :], lhsT=memn[:, kc * NMEM + mc * 128:kc * NMEM + (mc + 1) * 128],
                                                                        rhs=wv[:, kc * 512:(kc + 1) * 512], start=(kc == 0), stop=(kc == KC - 1)),
                         reads=[wv.b, memn.b], writes=[pp.b], inc=(kc == KC - 1))
                S.op("act", lambda e, pp=pp, mc=mc: e.copy(out=vtok[:, mc * 512:(mc + 1) * 512], in_=pp[:, :]), reads=[pp.b], writes=[vtok.b])
            scale = 128 ** -0.5
            xs2 = T(es, nc, "c_xs2", [128, KC * TT], F32)
            self.norm_tile(s, 0, first, l, 2, stage=xs2)
            for tt in range(NT):
                xn = self.xn[tt]
                for h in range(4):
                    pp = self.ps[0]
                    for kc in range(KC):
                        S.op("pe", lambda e, pp=pp, h=h, kc=kc: e.matmul(pp[:, :], lhsT=wq[:, (h * 8 + kc) * 128:(h * 8 + kc + 1) * 128],
                                                                          rhs=xn[:, kc * TT:(kc + 1) * TT], start=(kc == 0), stop=(kc == KC - 1)),
                             reads=[wq.b, xn.b], writes=[pp.b], inc=(kc == KC - 1))
                    S.op("act", lambda e, pp=pp, h=h: e.copy(out=qT[:, h * TT:(h + 1) * TT], in_=pp[:, :]), reads=[pp.b], writes=[qT.b])
                    if h % 2 == 1:
                        self.pump()
                if tt + 1 < NT:
                    self.norm_tile(s, tt + 1, first, l, 2, stage=xs2)
                units = [(h, mc) for h in range(4) for mc in range(2)]
                pob = [(self.ps[4], self.ps[5]), (self.ps[6], self.ps[1])]

                def c1(u):
                    h, mc = units[u]
                    pss = self.ps[2 + u % 2]
                    S.op("pe", lambda e: e.matmul(pss[:, :], lhsT=kT[:, h * NMEM + mc * 128:h * NMEM + (mc + 1) * 128],
                                                  rhs=qT[:, h * TT:(h + 1) * TT], start=True, stop=True),
                         reads=[kT.b, qT.b], writes=[pss.b])
                    S.op("act", lambda e: e.activation(out=ex[u % 4][:], in_=pss[:, :], func=AF.Exp, scale=scale),
                         reads=[pss.b], writes=[ex[u % 4].b])

                def c2(u):
                    h, mc = units[u]
                    po, pd = pob[h % 2]
                    S.op("pe", lambda e: e.matmul(po[:, :], lhsT=vtok[:, mc * 512 + h * 128:mc * 512 + (h + 1) * 128], rhs=ex[u % 4][:],
                                                  start=(mc == 0), stop=(mc == 1)),
                         reads=[vtok.b, ex[u % 4].b], writes=[po.b])
                    S.op("pe", lambda e: e.matmul(pd[:, :], lhsT=self.ones[:], rhs=ex[u % 4][:], start=(mc == 0), stop=(mc == 1)),
                         reads=[self.ones.b, ex[u % 4].b], writes=[pd.b])
                    if mc == 1:
                        S.op("act", lambda e: e.activation(out=rden[:], in_=pd[:, :], func=AF.Ln), reads=[pd.b], writes=[rden.b])
                        S.op("act", lambda e: e.activation(out=rden[:], in_=rden[:], func=AF.Exp, scale=-1.0), reads=[rden.b], writes=[rden.b])
                        S.op("dve", lambda e: e.tensor_tensor(out=oT[:, h * TT:(h + 1) * TT], in0=po[:, :], in1=rden[:], op=ALU.mult),
                             reads=[po.b, rden.b], writes=[oT.b])

                LA = 2
                for u in range(len(units) + LA):
                    if u < len(units):
                        c1(u)
                    if u - LA >= 0:
                        c2(u - LA)
                    if u % 3 == 2:
                        self.pump()
                self.drain()
                for o in range(KC):
                    pp = self.ps[2 + o % 2]
                    for h in range(4):
                        S.op("pe", lambda e, pp=pp, o=o, h=h: e.matmul(pp[:, :], lhsT=wo[:, (o * 4 + h) * 128:(o * 4 + h + 1) * 128],
                                                                        rhs=oT[:, h * TT:(h + 1) * TT], start=(h == 0), stop=(h == 3)),
                             reads=[wo.b, oT.b], writes=[pp.b], inc=(h == 3))
                    S.op("act", lambda e, pp=pp, o=o: e.copy(out=houts[tt % 2][:, o * TT:(o + 1) * TT], in_=pp[:, :]), reads=[pp.b], writes=[houts[tt % 2].b])
                self.drain()
                self.pend(self.residual_gen(s, tt, first, l, 3, hout=houts[tt % 2], xs=(self.xs[0] if tt % 2 == 0 else xs2)))
            self.drain()

    def mixer_sublayer(self, s, l, first):
        raise NotImplementedError


OFF = dict(m_q=0, m_k=256, m_v=512, m_o=768, m_i=1024, m_f=1028, c_b=1032, c_c=1288, c_h=1544,
           a_q=1800, a_k=2056, a_v=2312, h_q=2568, h_f=2824, h_i=3080, h_g=3336)
LN8 = math.log(8.0)
BIGRAW = 240000.0


def rel_bucket_np(dist):
    n = np.maximum(dist, 0)
    exact = 16
    nf = np.maximum(n, 1).astype(np.float32)
    large = exact + (np.log(nf / exact) / math.log(128 / exact) * (32 - exact)).astype(np.int32)
    large = np.minimum(large, 31)
    return np.where(n < exact, n, large)


def host_prepare_mixer(inp, depth, sh):
    L = depth
    colsA, colsC, colsD = [], [], []
    for h in range(4):
        colsA += list(range(OFF["m_q"] + 64 * h, OFF["m_q"] + 64 * h + 64))
        colsA += list(range(OFF["m_k"] + 64 * h, OFF["m_k"] + 64 * h + 64))
        colsA += [OFF["m_i"] + h] * 64
        colsA += [OFF["m_f"] + h] * 64
        colsA += list(range(OFF["m_o"] + 64 * h, OFF["m_o"] + 64 * h + 64))
        colsC += list(range(OFF["a_q"] + 64 * h, OFF["a_q"] + 64 * h + 64))
        colsC += list(range(OFF["a_k"] + 64 * h, OFF["a_k"] + 64 * h + 64))
        colsD += list(range(OFF["h_q"] + 64 * h, OFF["h_q"] + 64 * h + 64))
        colsD += list(range(OFF["h_f"] + 64 * h, OFF["h_f"] + 64 * h + 64))
        colsD += list(range(OFF["h_g"] + 64 * h, OFF["h_g"] + 64 * h + 64))
    colsB = list(range(OFF["c_b"], OFF["c_b"] + 768))
    w_in, b_in = inp["w_in"], inp["b_in"]
    sh["wA_T"] = np.stack([arr_w(w_in[l][:, colsA], 64) for l in range(L)])
    sh["wC_T"] = np.stack([arr_w(w_in[l][:, colsC], 64) for l in range(L)])
    sh["wD_T"] = np.stack([arr_w(w_in[l][:, colsD], 64) for l in range(L)])
    sh["wB_T"] = np.stack([arr_w(w_in[l][:, colsB], 128) for l in range(L)])
    vcols = [OFF["m_v"], OFF["a_v"], OFF["h_i"]]
    sh["w_v"] = np.stack([np.stack([arr_w(w_in[l][:, o:o + 256], 256)[0] for o in vcols]) for l in range(L)])
    sh["b_v"] = np.stack([np.stack([b_in[l][o:o + 256] for o in vcols]) for l in range(L)])
    sh["pA"] = np.stack([colT(b_in[l][colsA], 64) for l in range(L)])
    sh["pC"] = np.stack([colT(b_in[l][colsC], 64) for l in range(L)])
    sh["pD"] = np.stack([colT(b_in[l][colsD], 64) for l in range(L)])
    sh["pB"] = np.stack([colT(b_in[l][colsB], 128) for l in range(L)])
    sh["scw"] = np.stack([np.stack([colT(inp["sconv_w"][l][j], 128) for j in range(3)], axis=1) for l in range(L)])
    sh["hn"] = np.stack([np.stack([colT(inp["mlstm_norm"][l], 64), colT(inp["hgrn_norm"][l], 64)], axis=1) for l in range(L)])
    lg = inp["hgrn_lb_logits"]
    sh["lbl"] = np.ascontiguousarray(lg.reshape(4, 4, 64).transpose(2, 1, 0))
    sh["w_mo"] = np.stack([arr_w(inp["w_mix_out"][l], 128) for l in range(L)])
    m8 = np.tile(np.triu(np.ones((64, 64), np.float32)), (1, 8))
    sh["mask8"] = m8
    x = np.arange(1152)
    dist = x - 511
    oh = np.zeros((33, 1152), np.float32)
    bk = rel_bucket_np(dist)
    for i in range(1152):
        if dist[i] >= 0:
            oh[bk[i], i] = 1.0
        else:
            oh[32, i] = 1.0
    sh["oh"] = oh
    ra = np.zeros((33, 4), np.float32)
    ra[:32] = inp["rel_bias"]
    ra[32] = NEG
    sh["rel_aug"] = ra
    sh["b31"] = np.ascontiguousarray(inp["rel_bias"][31:32, :])
    pairs = [(n, m) for n in range(8) for m in range(8) if m != n]
    Pm = np.zeros((8, 56), np.float32)
    Agg = np.zeros((56, 8), np.float32)
    for i, (n, m) in enumerate(pairs):
        Pm[m, i] += 1.0
        Pm[n, i] -= 1.0
        Agg[i, n] = 1.0
    sh["Pm"] = Pm
    sh["Agg"] = Agg
    seln = np.zeros((8, 8, 128), np.float32)
    for n in range(8):
        seln[n, n, :] = 1.0
    sh["seln"] = seln.reshape(8, 1024)
    pastm = np.zeros((8, 4, 2), np.float32)
    validc = np.zeros((8, 4, 2), np.float32)
    ownm1 = np.zeros((8, 4, 2), np.float32)
    for n in range(8):
        for tt in range(4):
            for j in range(2):
                b = 2 * tt + j
                pastm[n, tt, j] = 0.0 if n < b else -1e9
                validc[n, tt, j] = 1.0 if n < b else 0.0
                ownm1[n, tt, j] = (1.0 if n == b else 0.0) - 1.0
    sh["mobac"] = np.stack([pastm, validc, ownm1], axis=1).reshape(8, 24)
    kind = np.zeros((8, SEQ), np.float32)
    for n in range(8):
        kind[n, n * 256:(n + 1) * 256] = 1.0
    sh["kind"] = kind
    return sh


class V:
    def __init__(self, ap_fn):
        self.f = ap_fn
        self.b = Buf()

    def __getitem__(self, k):
        return self.f()[k]


class ProgM(Prog):
    def alloc_static(self):
        super().alloc_static()
        nc, es, L = self.nc, self.es, self.depth
        self.ident32 = T(es, nc, "ident32", [128, 128], F32)
        self.mask8 = T(es, nc, "mask8", [64, 512], F32)
        self.onesf = T(es, nc, "onesf", [64, 512], F32)
        self.oneb = T(es, nc, "oneb", [128, 1], F32)
        self.pA = T(es, nc, "pA", [64, L * 20], F32)
        self.pC = T(es, nc, "pC", [64, L * 8], F32)
        self.pD = T(es, nc, "pD", [64, L * 12], F32)
        self.pB = T(es, nc, "pB", [128, L * 6], F32)
        self.scw = T(es, nc, "scw", [128, L * 6], F32)
        self.hn = T(es, nc, "hn", [64, L * 8], F32)
        self.lbe = T(es, nc, "lbe", [64, 16], F32)
        self.lb = T(es, nc, "lb", [64, 16], F32)
        self.omlb = T(es, nc, "omlb", [64, 16], F32)
        self.lbs = T(es, nc, "lbs", [64, 4], F32)
        self.rel_aug = T(es, nc, "rel_aug", [33, 4], F32)
        self.b31 = T(es, nc, "b31", [128, 4], F32)
        self.Pm = T(es, nc, "Pm", [8, 56], F32)
        self.Agg = T(es, nc, "Agg", [56, 8], BF16)
        self.seln = T(es, nc, "seln", [8, 1024], BF16)
        self.mobac = T(es, nc, "mobac", [8, 24], F32)
        self.tbd = nc.dram_tensor("tbd", [4, 128, 1152], F32, kind="Internal")
        self.tbd_b = Buf()

    def load_consts(self):
        super().load_consts()
        S, L, din = self.S, self.depth, self.din
        S.dma("sp", self.ident32[:], din["ident"], writes=[self.ident32.b])
        S.dma("sp", self.mask8[:], din["mask8"], writes=[self.mask8.b])
        S.op("dve", lambda e: e.memset(self.onesf[:], 1.0), writes=[self.onesf.b])
        S.op("dve", lambda e: e.memset(self.oneb[:], 1.0), writes=[self.oneb.b])
        for nm, t, w in (("pA", self.pA, 20), ("pC", self.pC, 8), ("pD", self.pD, 12), ("pB", self.pB, 6)):
            S.dma("sp", t[:].rearrange("p (l f) -> p l f", l=L), din[nm].rearrange("l p f -> p l f"), writes=[t.b])
        S.dma("sp", self.scw[:].rearrange("p (l f) -> p l f", l=L), din["scw"].rearrange("l p a c -> p l (a c)"), writes=[self.scw.b])
        S.dma("sp", self.hn[:].rearrange("p (l f) -> p l f", l=L), din["hn"].rearrange("l p a c -> p l (a c)"), writes=[self.hn.b])
        S.dma("sp", self.lbe[:], din["lbl"].rearrange("p h l -> p (h l)"), writes=[self.lbe.b])
        S.dma("sp", self.rel_aug[:], din["rel_aug"], writes=[self.rel_aug.b])
        S.dma("sp", self.b31[:], din["b31"].partition_broadcast(128).rearrange("p a b -> p (a b)"), writes=[self.b31.b])
        S.dma("sp", self.Pm[:], din["Pm"], writes=[self.Pm.b])
        S.dma("pool", self.Agg[:], din["Agg"], writes=[self.Agg.b])
        S.dma("pool", self.seln[:], din["seln"], writes=[self.seln.b])
        S.dma("sp", self.mobac[:], din["mobac"], writes=[self.mobac.b])
        pa3 = self.pA[:].rearrange("p (g j) -> p g j", j=5)
        S.op("dve", lambda e: e.tensor_scalar(out=pa3[:, :, 2:3], in0=pa3[:, :, 2:3], scalar1=-LN8, scalar2=None, op0=ALU.add),
             reads=[self.pA.b], writes=[self.pA.b])
        S.op("dve", lambda e: e.tensor_scalar(out=pa3[:, :, 3:5], in0=pa3[:, :, 3:5], scalar1=-1.0, scalar2=None, op0=ALU.mult),
             reads=[self.pA.b], writes=[self.pA.b])
        lbe3 = self.lbe[:].rearrange("p (h l) -> p h l", l=4)
        S.op("act", lambda e: e.activation(out=self.lbe[:], in_=self.lbe[:], func=AF.Exp), reads=[self.lbe.b], writes=[self.lbe.b])
        S.op("dve", lambda e: e.reduce_sum(out=self.lbs[:], in_=lbe3, axis=AX.X), reads=[self.lbe.b], writes=[self.lbs.b])
        S.op("dve", lambda e: e.reciprocal(out=self.lbs[:], in_=self.lbs[:]), reads=[self.lbs.b], writes=[self.lbs.b])
        for h in range(4):
            S.op("dve", lambda e, h=h: e.tensor_scalar(out=self.lbe[:, h * 4:h * 4 + 4], in0=self.lbe[:, h * 4:h * 4 + 4],
                                                       scalar1=self.lbs[:, h:h + 1], scalar2=None, op0=ALU.mult),
                 reads=[self.lbe.b, self.lbs.b], writes=[self.lbe.b])
        lb3 = self.lb[:].rearrange("p (h l) -> p h l", l=4)
        S.op("dve", lambda e: e.memset(self.lb[:], 0.0), writes=[self.lb.b])
        for li in range(1, 4):
            S.op("dve", lambda e, li=li: e.tensor_tensor(out=lb3[:, :, li:li + 1], in0=lb3[:, :, li - 1:li], in1=lbe3[:, :, li:li + 1], op=ALU.add),
                 reads=[self.lb.b, self.lbe.b], writes=[self.lb.b])
        S.op("dve", lambda e: e.tensor_scalar(out=self.omlb[:], in0=self.lb[:], scalar1=-1.0, scalar2=1.0, op0=ALU.mult, op1=ALU.add),
             reads=[self.lb.b], writes=[self.omlb.b])
        nc = self.nc
        with contextlib.ExitStack() as es2:
            oh = T(es2, nc, "mboh", [33, 1152], F32)
            tbv = T(es2, nc, "mbtbv", [4, 1152], F32)
            S.dma("sp", oh[:], self.din["oh"], writes=[oh.b])
            for j in range(3):
                pp = self.ps[j % 2]
                S.op("pe", lambda e: e.matmul(pp[:4, :384], lhsT=self.rel_aug[:, :], rhs=oh[:, j * 384:(j + 1) * 384], start=True, stop=True),
                     reads=[self.rel_aug.b, oh.b], writes=[pp.b])
                S.op("act", lambda e: e.copy(out=tbv[:, j * 384:(j + 1) * 384], in_=pp[:4, :384]), reads=[pp.b], writes=[tbv.b])
            S.dma("sp", self.tbd.ap(), tbv[:].rearrange("p (a x) -> p a x", a=1).broadcast_to([4, 128, 1152]), reads=[tbv.b], writes=[self.tbd_b])

    def proj64(self, pp, W, woff, xn):
        S = self.S
        for kc in range(KC):
            S.op("pe", lambda e, kc=kc: e.matmul(pp[:64, :TT], lhsT=W[:, woff + kc * 64:woff + (kc + 1) * 64],
                                                 rhs=xn[:, kc * TT:(kc + 1) * TT], start=(kc == 0), stop=(kc == KC - 1)),
                 reads=[W.b, xn.b], writes=[pp.b], inc=(kc == KC - 1))

    def y_store(self, yT, yb, chunk, h, tt):
        self.S.dma("sp", yT[(h % 2) * 64:(h % 2) * 64 + 64, chunk * SEQ + tt * TT:chunk * SEQ + (tt + 1) * TT], yb[:64, :TT],
                   reads=[yb.b], writes=[yT.b])

    def mixer_sublayer(self, s, l, first):
        S, nc, cfg = self.S, self.nc, self.cfg
        with contextlib.ExitStack() as es:
            yT = T(es, nc, "yT", [128, 8 * SEQ], BF16)
            self.yT = yT
            groups = cfg.get("groups", "ABCD")
            if groups != "ABCD":
                S.op("pool", lambda e: e.memset(yT[:], 0.0), writes=[yT.b])
            for tt in range(NT):
                self.norm_tile(s, tt, first, l, 0, stage=(self.xs[0] if tt % 2 == 0 else self.hout))
            if "B" in groups:
                self.group_B(s, l, yT)
            if "A" in groups:
                self.group_gla(s, l, yT, "A")
            if "D" in groups:
                self.group_gla(s, l, yT, "D")
            if "C" in groups:
                self.group_C(s, l, yT)
            if cfg.get("dbg_y", False) and s == 0 and l == 0:
                d = nc.dram_tensor("dbg_y", [128, 8 * SEQ], BF16, kind="ExternalOutput").ap()
                b = Buf()
                S.dma("sp", d, yT[:], reads=[yT.b], writes=[b])
                self.dbg_bufs.append(b)
            with contextlib.ExitStack() as es2:
                wo = T(es2, nc, "w_mo", [128, 8 * 8 * 128], BF16)
                S.dma("pool", wo[:].rearrange("p (o k m) -> p o k m", o=8, k=8), self.din["w_mo"][l].rearrange("o p k m -> p o k m"), writes=[wo.b])
                hout2 = T(es2, nc, "mo_hout2", [128, KC * TT], F32)
                houts = [self.hout, hout2]
                for tt in range(NT):
                    ho = houts[tt % 2]
                    for o in range(KC):
                        pp = self.ps[o % 4]
                        for kc in range(KC):
                            S.op("pe", lambda e, pp=pp, o=o, kc=kc, tt=tt: e.matmul(
                                pp[:, :], lhsT=wo[:, (o * 8 + kc) * 128:(o * 8 + kc + 1) * 128],
                                rhs=yT[:, kc * SEQ + tt * TT:kc * SEQ + (tt + 1) * TT], start=(kc == 0), stop=(kc == KC - 1)),
                                reads=[wo.b, yT.b], writes=[pp.b], inc=(kc == KC - 1))
                        S.op("act", lambda e, pp=pp, o=o: e.copy(out=ho[:, o * TT:(o + 1) * TT], in_=pp[:, :]),
                             reads=[pp.b], writes=[ho.b])
                        if o % 2 == 1:
                            self.pump()
                    self.drain()
                    self.pend(self.residual_gen(s, tt, first, l, 1, hout=ho))
                self.drain()

    def group_B(self, s, l, yT):
        S, nc = self.S, self.nc
        with contextlib.ExitStack() as es:
            wB = T(es, nc, "wB", [128, 6 * 8 * 128], BF16)
            S.dma("pool", wB[:].rearrange("p (o k m) -> p o k m", o=6, k=8), self.din["wB_T"][l].rearrange("o p k m -> p o k m"), writes=[wB.b])
            u = T(es, nc, "scu", [128, 2 + SEQ], F32)
            cbs = T(es, nc, "sccb", [128, SEQ], F32)
            a = T(es, nc, "sca", [128, SEQ], F32)
            ccs = T(es, nc, "sccc", [128, TT], F32)
            bcol = lambda oc: self.pB[:, l * 6 + oc:l * 6 + oc + 1]
            wcol = lambda j, c: self.scw[:, l * 6 + j * 2 + c:l * 6 + j * 2 + c + 1]
            for j in range(2):
                S.op("dve", lambda e: e.memset(u[:, 0:2], 0.0), writes=[u.b])
                for tt in range(NT):
                    xn = self.xn[tt]
                    pcb, pcc, pch = self.ps[0], self.ps[1], self.ps[2]
                    for oc, pp in ((0 + j, pcb), (2 + j, pcc), (4 + j, pch)):
                        for kc in range(KC):
                            S.op("pe", lambda e, oc=oc, pp=pp, kc=kc: e.matmul(pp[:, :], lhsT=wB[:, (oc * 8 + kc) * 128:(oc * 8 + kc + 1) * 128],
                                                                                rhs=xn[:, kc * TT:(kc + 1) * TT], start=(kc == 0), stop=(kc == KC - 1)),
                                 reads=[wB.b, xn.b], writes=[pp.b], inc=(kc == KC - 1))
                    S.op("act", lambda e: e.activation(out=ccs[:], in_=pcc[:, :], func=AF.Identity, bias=bcol(2 + j)),
                         reads=[pcc.b, self.pB.b], writes=[ccs.b])
                    S.op("dve", lambda e, tt=tt: e.scalar_tensor_tensor(out=u[:, 2 + tt * TT:2 + (tt + 1) * TT], in0=pch[:, :], scalar=bcol(4 + j),
                                                                          in1=ccs[:], op0=ALU.add, op1=ALU.mult),
                         reads=[pch.b, ccs.b, self.pB.b], writes=[u.b])
                    if self.cfg.get("dump"):
                        S.op("act", lambda e, tt=tt: e.copy(out=a[:, tt * TT:(tt + 1) * TT], in_=pcb[:, :]), reads=[pcb.b], writes=[a.b])
                    S.op("act", lambda e, tt=tt: e.activation(out=cbs[:, tt * TT:(tt + 1) * TT], in_=pcb[:, :], func=AF.Identity, bias=bcol(0 + j)),
                         reads=[pcb.b, self.pB.b], writes=[cbs.b])
                self.dump("cbs", cbs, cbs[:], [128, SEQ])
                self.dump("araw", a, a[:], [128, SEQ])
                self.dump("wB", wB, wB[:], [128, 6144], BF16)
                self.dump("u", u, u[:], [128, 2 + SEQ])
                self.dump("xn0", self.xn[0], self.xn[0][:], [128, KC * TT], BF16)
                S.op("dve", lambda e: e.tensor_scalar(out=a[:], in0=u[:, 2:2 + SEQ], scalar1=wcol(2, j), scalar2=None, op0=ALU.mult),
                     reads=[u.b, self.scw.b], writes=[a.b])
                S.op("dve", lambda e: e.scalar_tensor_tensor(out=a[:], in0=u[:, 1:1 + SEQ], scalar=wcol(1, j), in1=a[:], op0=ALU.mult, op1=ALU.add),
                     reads=[u.b, a.b, self.scw.b], writes=[a.b])
                S.op("dve", lambda e: e.scalar_tensor_tensor(out=a[:], in0=u[:, 0:SEQ], scalar=wcol(0, j), in1=a[:], op0=ALU.mult, op1=ALU.add),
                     reads=[u.b, a.b, self.scw.b], writes=[a.b])
                S.op("dve", lambda e: e.tensor_tensor(out=yT[:, (2 + j) * SEQ:(3 + j) * SEQ], in0=a[:], in1=cbs[:], op=ALU.mult),
                     reads=[a.b, cbs.b], writes=[yT.b])

    @staticmethod
    def run_interleaved(gens):
        gens = list(gens)
        while gens:
            nxt = []
            for g in gens:
                try:
                    next(g)
                    nxt.append(g)
                except StopIteration:
                    pass
            gens = nxt

    def group_gla(self, s, l, yT, mode):
        S, nc = self.S, self.nc
        isA = mode == "A"
        nT = 5 if isA else 3
        vidx = 0 if isA else 2
        vw = 128 if isA else 64
        ych0 = 0 if isA else 6
        pb = self.pA if isA else self.pD
        with contextlib.ExitStack() as es:
            W = T(es, nc, "glaW", [128, 4 * nT * 8 * 64], BF16)
            Wv = T(es, nc, "glaWv", [128, 8 * 256], BF16)
            bvb = T(es, nc, "glabv", [64, 256], F32)
            vt = T(es, nc, "glavt", [64, 8 * 4 * vw], BF16)
            Qs = [T(es, nc, "glaQs%d" % h, [64, TT], BF16) for h in range(4)]
            Am = [T(es, nc, "glaAm%d" % h, [64, TT], BF16) for h in range(4)]
            KeT = [T(es, nc, "glaKeT%d" % h, [64, TT], BF16) for h in range(4)]
            og = [T(es, nc, "glaog%d" % h, [64, TT], F32) for h in range(4)]
            dec = [T(es, nc, "gladec%d" % h, [64, 8], F32) for h in range(4)]
            Ks = [T(es, nc, "glaKs%d" % i, [64, TT], BF16) for i in range(2)]
            gcs = [T(es, nc, "glagc%d" % i, [64, TT + 8], F32) for i in range(2)]
            yb = [T(es, nc, "glayb%d" % i, [64, TT], BF16) for i in range(2)]
            S32 = [T(es, nc, "glaS32_%d" % h, [64, vw], F32) for h in range(4)]
            Sbf = [T(es, nc, "glaSbf_%d" % h, [64, vw], BF16) for h in range(4)]
            xs = self.xs[0]
            wbf = [self.wb[i] for i in range(2)]
            lane_slots = [
                [V(lambda i=i: xs[0:64, i * TT:(i + 1) * TT]) for i in range(7)],
                [V(lambda i=i: wbf[0][0:64, :].bitcast(F32)[:, i * TT:(i + 1) * TT]) for i in range(6)]
                + [V(lambda: wbf[1][0:64, :].bitcast(F32)[:, 0:TT])],
            ]
            sqv = [V(lambda h=h: self.sq[0:64, h * TT:(h + 1) * TT]) for h in range(4)]
            psU = [self.ps[i] for i in (6, 0, 1, 2)]
            wsrc = self.din["wA_T" if isA else "wD_T"][l]
            S.dma("pool", W[:].rearrange("p (o k m) -> p o k m", o=4 * nT, k=8), wsrc.rearrange("o p k m -> p o k m"), writes=[W.b])
            S.dma("pool", Wv[:].rearrange("p (k m) -> p k m", k=8), self.din["w_v"][l, vidx], writes=[Wv.b])
            S.dma("sp", bvb[:], self.din["b_v"][l, vidx:vidx + 1, :].partition_broadcast(64).rearrange("p a b -> p (a b)"), writes=[bvb.b])
            S.op("dve", lambda e: e.memset(gcs[0][:, 0:1], 0.0), writes=[xs.b, gcs[0].b] + [v.b for v in lane_slots[0]])
            S.op("dve", lambda e: e.memset(gcs[1][:, 0:1], 0.0), writes=[wbf[0].b, wbf[1].b, gcs[1].b] + [v.b for v in lane_slots[1]])
            S.op("dve", lambda e: e.memset(vt[:], 1.0), writes=[self.sq.b, vt.b] + [v.b for v in sqv])
            for h in range(4):
                S.op("dve", lambda e, h=h: e.memset(S32[h][:], 0.0), writes=[S32[h].b])
                S.op("dve", lambda e, h=h: e.memset(Sbf[h][:], 0.0), writes=[Sbf[h].b])
            vt4 = vt[:].rearrange("p (b h w) -> p b h w", b=8, h=4)
            g3 = lambda ap: ap.rearrange("p (b t) -> p b t", t=64)

            def vtok(tt):
                xn = self.xn[tt]
                for b in range(8):
                    pv = self.ps[2 + b % 2]
                    for kc in range(KC):
                        S.op("pe", lambda e, kc=kc: e.matmul(pv[:64, :256], lhsT=xn[:, kc * TT + b * 64:kc * TT + (b + 1) * 64],
                                                             rhs=Wv[:, kc * 256:(kc + 1) * 256], start=(kc == 0), stop=(kc == KC - 1)),
                             reads=[xn.b, Wv.b], writes=[pv.b], inc=(kc == KC - 1))
                    S.op("dve", lambda e: e.tensor_tensor(out=vt4[:, b, :, 0:64], in0=pv[:64, :256].rearrange("p (h w) -> p h w", h=4),
                                                          in1=bvb[:].rearrange("p (h w) -> p h w", h=4), op=ALU.add),
                         reads=[pv.b, bvb.b], writes=[vt.b])

            def prep(tt, h, lane):
                xn = self.xn[tt]
                t_q, t_k, t_e, t_f, t_gn, t_x, t_ke = lane_slots[lane]
                gc = gcs[lane]
                ks = Ks[lane]
                p0, p1 = (self.ps[0], self.ps[1]) if lane == 0 else (self.ps[2], self.ps[3])
                pa = self.ps[4 + 2 * lane]
                ptr = self.ps[5 + 2 * lane]
                bc = lambda j: pb[:, (l * 4 + h) * nT + j:(l * 4 + h) * nT + j + 1]
                wo = lambda j: (h * nT + j) * 8 * 64
                if isA:
                    self.proj64(p0, W, wo(3), xn)
                    S.op("act", lambda e: e.activation(out=t_f[:, :], in_=p0[:64, :TT], func=AF.Exp, bias=bc(3), scale=-1.0), reads=[p0.b, pb.b], writes=[t_f.b])
                    yield
                    self.proj64(p1, W, wo(1), xn)
                    S.op("act", lambda e: e.activation(out=t_k[:, :], in_=p1[:64, :TT], func=AF.Identity, bias=bc(1)), reads=[p1.b, pb.b], writes=[t_k.b])
                    S.op("act", lambda e: e.activation(out=t_f[:, :], in_=t_f[:, :], func=AF.Ln, bias=self.oneb[0:64, 0:1]), reads=[t_f.b, self.oneb.b], writes=[t_f.b])
                    yield
                    self.proj64(p0, W, wo(2), xn)
                    S.op("act", lambda e: e.activation(out=t_e[:, :], in_=p0[:64, :TT], func=AF.Exp, bias=bc(2)), reads=[p0.b, pb.b], writes=[t_e.b])
                    S.op("dve", lambda e: e.tensor_tensor_scan(out=gc[:, 1:TT + 1], data0=self.onesf[:, :], data1=t_f[:, :], initial=0.0, op0=ALU.mult, op1=ALU.add),
                         reads=[self.onesf.b, t_f.b], writes=[gc.b])
                    yield
                    self.proj64(p1, W, wo(0), xn)
                    S.op("act", lambda e: e.activation(out=t_q[:, :], in_=p1[:64, :TT], func=AF.Identity, bias=bc(0)), reads=[p1.b, pb.b], writes=[t_q.b])
                    S.op("dve", lambda e: e.tensor_tensor(out=t_k[:, :], in0=t_k[:, :], in1=t_e[:, :], op=ALU.mult), reads=[t_k.b, t_e.b], writes=[t_k.b])
                    yield
                    self.proj64(p0, W, wo(4), xn)
                    S.op("act", lambda e: e.activation(out=t_e[:, :], in_=p0[:64, :TT], func=AF.Exp, bias=bc(4), scale=-1.0), reads=[p0.b, pb.b], writes=[t_e.b])
                    yield
                    S.op("act", lambda e: e.activation(out=t_e[:, :], in_=t_e[:, :], func=AF.Ln, bias=self.oneb[0:64, 0:1]), reads=[t_e.b, self.oneb.b], writes=[t_e.b])
                    yield
                    S.op("act", lambda e: e.activation(out=og[h][:], in_=t_e[:, :], func=AF.Exp, scale=-1.0), reads=[t_e.b], writes=[og[h].b])
                else:
                    lbi = h * 4 + l
                    self.proj64(p0, W, wo(1), xn)
                    S.op("act", lambda e: e.activation(out=t_f[:, :], in_=p0[:64, :TT], func=AF.Sigmoid, bias=bc(1)), reads=[p0.b, pb.b], writes=[t_f.b])
                    yield
                    self.proj64(p1, W, wo(0), xn)
                    S.op("act", lambda e: e.activation(out=t_q[:, :], in_=p1[:64, :TT], func=AF.Silu, bias=bc(0)), reads=[p1.b, pb.b], writes=[t_q.b])
                    S.op("dve", lambda e: e.tensor_scalar(out=t_f[:, :], in0=t_f[:, :], scalar1=self.omlb[:, lbi:lbi + 1], scalar2=self.lb[:, lbi:lbi + 1],
                                                          op0=ALU.mult, op1=ALU.add), reads=[t_f.b, self.omlb.b, self.lb.b], writes=[t_f.b])
                    yield
                    S.op("dve", lambda e: e.tensor_scalar(out=t_k[:, :], in0=t_f[:, :], scalar1=-1.0, scalar2=1.0, op0=ALU.mult, op1=ALU.add),
                         reads=[t_f.b], writes=[t_k.b])
                    S.op("act", lambda e: e.activation(out=t_e[:, :], in_=t_f[:, :], func=AF.Ln), reads=[t_f.b], writes=[t_e.b])
                    yield
                    self.proj64(p0, W, wo(2), xn)
                    S.op("act", lambda e: e.activation(out=og[h][:], in_=p0[:64, :TT], func=AF.Silu, bias=bc(2)), reads=[p0.b, pb.b], writes=[og[h].b])
                    S.op("dve", lambda e: e.tensor_tensor_scan(out=gc[:, 1:TT + 1], data0=self.onesf[:, :], data1=t_e[:, :], initial=0.0, op0=ALU.mult, op1=ALU.subtract),
                         reads=[self.onesf.b, t_e.b], writes=[gc.b])
                    yield
                gn3 = g3(t_gn[:, :])
                S.op("dve", lambda e: e.tensor_tensor(out=gn3, in0=g3(gc[:, 1:TT + 1]), in1=g3(gc[:, 0:TT])[:, :, 0:1].broadcast_to([64, 8, 64]), op=ALU.subtract),
                     reads=[gc.b], writes=[t_gn.b])
                yield
                S.op("act", lambda e: e.activation(out=t_x[:, :], in_=t_gn[:, :], func=AF.Exp, scale=-1.0), reads=[t_gn.b], writes=[t_x.b])
                S.op("dve", lambda e: e.tensor_tensor(out=g3(t_f[:, :]), in0=gn3, in1=gn3[:, :, 63:64].broadcast_to([64, 8, 64]), op=ALU.subtract),
                     reads=[t_gn.b], writes=[t_f.b])
                yield
                S.op("dve", lambda e: e.tensor_tensor(out=Qs[h][:], in0=t_q[:, :], in1=t_x[:, :], op=ALU.mult), reads=[t_q.b, t_x.b], writes=[Qs[h].b])
                S.op("act", lambda e: e.activation(out=t_e[:, :], in_=t_gn[:, :], func=AF.Exp), reads=[t_gn.b], writes=[t_e.b])
                yield
                S.op("dve", lambda e: e.tensor_tensor(out=ks[:], in0=t_k[:, :], in1=t_e[:, :], op=ALU.mult), reads=[t_k.b, t_e.b], writes=[ks.b])
                S.op("act", lambda e: e.activation(out=t_f[:, :], in_=t_f[:, :], func=AF.Exp), reads=[t_f.b], writes=[t_f.b])
                yield
                for b in range(8):
                    S.op("pe", lambda e, b=b: e.matmul(pa[:64, b * 64:(b + 1) * 64], lhsT=ks[:, b * 64:(b + 1) * 64], rhs=Qs[h][:, b * 64:(b + 1) * 64],
                                                       start=True, stop=True), reads=[ks.b, Qs[h].b], writes=[pa.b])
                S.op("act", lambda e: e.activation(out=dec[h][:, :], in_=t_gn[:, 63:TT:64], func=AF.Exp, scale=-1.0), reads=[t_gn.b], writes=[dec[h].b])
                S.op("dve", lambda e: e.tensor_tensor(out=t_ke[:, :], in0=t_k[:, :], in1=t_f[:, :], op=ALU.mult), reads=[t_k.b, t_f.b], writes=[t_ke.b])
                yield
                S.op("dve", lambda e: e.tensor_tensor(out=Am[h][:], in0=pa[:64, :TT], in1=self.mask8[:, :], op=ALU.mult),
                     reads=[pa.b, self.mask8.b], writes=[Am[h].b])
                for b in range(8):
                    S.op("pe", lambda e, b=b: e.transpose(out=ptr[:64, b * 64:(b + 1) * 64], in_=t_ke[:, b * 64:(b + 1) * 64], identity=self.ident32[0:64, 0:64]),
                         reads=[t_ke.b, self.ident32.b], writes=[ptr.b])
                yield
                S.op("act", lambda e: e.copy(out=KeT[h][:], in_=ptr[:64, :TT]), reads=[ptr.b], writes=[KeT[h].b])
                yield

            nd = self.hout

            def blocks(tt):
                for b in range(8):
                    pnd = self.ps[4 + b % 2]
                    for h in range(4):
                        vb = (b * 4 + h) * vw
                        bs = slice(b * 64, (b + 1) * 64)
                        S.op("pe", lambda e: e.matmul(pnd[:64, h * 64:(h + 1) * 64], lhsT=vt[:, vb:vb + 64], rhs=Am[h][:, bs], start=True, stop=False),
                             reads=[vt.b, Am[h].b], writes=[pnd.b])
                        S.op("pe", lambda e: e.matmul(pnd[:64, h * 64:(h + 1) * 64], lhsT=Sbf[h][:, 0:64], rhs=Qs[h][:, bs], start=False, stop=True),
                             reads=[Sbf[h].b, Qs[h].b], writes=[pnd.b])
                        if isA:
                            S.op("pe", lambda e: e.matmul(pnd[:64, 256 + h * 64:256 + (h + 1) * 64], lhsT=vt[:, vb + 64:vb + 128], rhs=Am[h][:, bs], start=True, stop=False),
                                 reads=[vt.b, Am[h].b], writes=[pnd.b])
                            S.op("pe", lambda e: e.matmul(pnd[:64, 256 + h * 64:256 + (h + 1) * 64], lhsT=Sbf[h][:, 64:128], rhs=Qs[h][:, bs], start=False, stop=True),
                                 reads=[Sbf[h].b, Qs[h].b], writes=[pnd.b])
                        S.op("pe", lambda e: e.matmul(psU[h][:64, 0:vw], lhsT=KeT[h][:, bs], rhs=vt[:, vb:vb + vw], start=True, stop=True),
                             reads=[KeT[h].b, vt.b], writes=[psU[h].b])
                        S.op("dve", lambda e: e.scalar_tensor_tensor(out=S32[h][:], in0=S32[h][:], scalar=dec[h][:, b:b + 1], in1=psU[h][:64, 0:vw],
                                                                      op0=ALU.mult, op1=ALU.add), reads=[S32[h].b, dec[h].b, psU[h].b], writes=[S32[h].b])
                        S.op("act", lambda e: e.copy(out=Sbf[h][:], in_=S32[h][:]), reads=[S32[h].b], writes=[Sbf[h].b])
                    ncl = TT if isA else 256
                    S.op("act", lambda e: e.copy(out=nd[0:64, b * TT:b * TT + ncl], in_=pnd[:64, :ncl]), reads=[pnd.b], writes=[nd.b])

            nd3 = nd[0:64, :].rearrange("p (b x) -> p b x", b=8)

            def outputs(tt, h):
                lane = h % 2
                t_hh = lane_slots[lane][(h // 2) * 2]
                rsh = lane_slots[lane][(h // 2) * 2 + 1]
                ybh = yb[lane]
                sq = sqv[h]
                pss = self.ps[h]
                numv = nd3[:, :, h * 64:(h + 1) * 64]
                hh3 = g3(t_hh[:, :])
                if isA:
                    denv = nd3[:, :, 256 + h * 64:256 + (h + 1) * 64]
                    S.op("dve", lambda e: e.scalar_tensor_tensor(out=hh3, in0=denv, scalar=-1.0, in1=denv, op0=ALU.mult, op1=ALU.max), reads=[nd.b], writes=[t_hh.b])
                    yield
                    S.op("dve", lambda e: e.tensor_scalar(out=t_hh[:, :], in0=t_hh[:, :], scalar1=1.0, scalar2=None, op0=ALU.max), reads=[t_hh.b], writes=[t_hh.b])
                    yield
                    S.op("act", lambda e: e.activation(out=t_hh[:, :], in_=t_hh[:, :], func=AF.Ln), reads=[t_hh.b], writes=[t_hh.b])
                    yield
                    S.op("act", lambda e: e.activation(out=t_hh[:, :], in_=t_hh[:, :], func=AF.Exp, scale=-1.0), reads=[t_hh.b], writes=[t_hh.b])
                    yield
                    S.op("dve", lambda e: e.tensor_tensor(out=hh3, in0=numv, in1=hh3, op=ALU.mult), reads=[nd.b, t_hh.b], writes=[t_hh.b])
                    yield
                else:
                    S.op("act", lambda e: e.copy(out=hh3, in_=numv), reads=[nd.b], writes=[t_hh.b])
                    yield
                S.op("act", lambda e: e.activation(out=sq[:, :], in_=t_hh[:, :], func=AF.Square), reads=[t_hh.b], writes=[sq.b])
                yield
                S.op("pe", lambda e: e.matmul(pss[:64, :TT], lhsT=self.ones[0:64, 0:64], rhs=sq[:, :], start=True, stop=True),
                     reads=[self.ones.b, sq.b], writes=[pss.b])
                yield
                S.op("act", lambda e: e.activation(out=rsh[:, :], in_=pss[:64, :TT], func=AF.Ln, scale=1.0 / 64, bias=self.epsb[0:64, 0:1]),
                     reads=[pss.b, self.epsb.b], writes=[rsh.b])
                yield
                S.op("act", lambda e: e.activation(out=rsh[:, :], in_=rsh[:, :], func=AF.Exp, scale=-0.5), reads=[rsh.b], writes=[rsh.b])
                yield
                gi = (l * 2 + (0 if isA else 1)) * 4 + h
                S.op("dve", lambda e: e.scalar_tensor_tensor(out=t_hh[:, :], in0=t_hh[:, :], scalar=self.hn[:, gi:gi + 1], in1=rsh[:, :], op0=ALU.mult, op1=ALU.mult),
                     reads=[t_hh.b, self.hn.b, rsh.b], writes=[t_hh.b])
                yield
                S.op("dve", lambda e: e.tensor_tensor(out=ybh[:], in0=t_hh[:, :], in1=og[h][:], op=ALU.mult), reads=[t_hh.b, og[h].b], writes=[ybh.b])
                self.y_store(yT, ybh, ych0 + h // 2, h, tt)
                yield

            vtok(0)
            for tt in range(NT):
                self.run_interleaved([prep(tt, 0, 0), prep(tt, 1, 1)])
                self.run_interleaved([prep(tt, 2, 0), prep(tt, 3, 1)])
                blocks(tt)
                if tt + 1 < NT:
                    vtok(tt + 1)
                self.run_interleaved([outputs(tt, h) for h in range(4)])
            S.op("dve", lambda e: e.memset(gcs[0][:, 0:1], 0.0), reads=[v.b for v in lane_slots[0]], writes=[xs.b, gcs[0].b])
            S.op("dve", lambda e: e.memset(gcs[1][:, 0:1], 0.0), reads=[v.b for v in lane_slots[1]], writes=[wbf[0].b, wbf[1].b, gcs[1].b])
            S.op("dve", lambda e: e.memset(gcs[0][:, 0:1], 0.0), reads=[v.b for v in sqv], writes=[self.sq.b, gcs[0].b])

    def group_C(self, s, l, yT):
        S, nc = self.S, self.nc
        scale = 0.125
        with contextlib.ExitStack() as es:
            W = T(es, nc, "mbW", [128, 8 * 8 * 64], BF16)
            Wv = T(es, nc, "mbWv", [128, 8 * 256], BF16)
            bvb = T(es, nc, "mbbv", [128, 256], F32)
            kT = [T(es, nc, "mbkT%d" % h, [128, SEQ], BF16) for h in range(4)]
            vtok = T(es, nc, "mbvtok", [128, 16 * 4 * 65], BF16)
            vt5 = vtok[:].rearrange("p (c h w) -> p c h w", c=16, h=4)
            rrowb = T(es, nc, "mbrrowb", [65, TT], BF16)
            TBr = [T(es, nc, "mbTB%d" % h, [128, 1024], F32) for h in range(2)]
            q32 = [T(es, nc, "mbq32_%d" % i, [64, TT], F32) for i in range(2)]
            qT = [T(es, nc, "mbqT%d" % i, [128, TT], BF16) for i in range(2)]
            k32 = q32[0]
            kmean = T(es, nc, "mbkmean", [64, 32], F32)
            gm0 = T(es, nc, "mbgm", [8, TT], F32)
            gm = [gm0, gm0]
            gt = [T(es, nc, "mbgt%d" % i, [56, TT], BF16) for i in range(2)]
            nm = [T(es, nc, "mbnm%d" % i, [8, TT], BF16) for i in range(2)]
            tmp80 = gm0
            tmp8 = [tmp80, tmp80]
            tmpS = [self.hout, self.xs[0]]
            ex = [T(es, nc, "mbex%d" % i, [128, TT], BF16) for i in range(3)]
            rden = T(es, nc, "mbrden", [65, TT], F32)
            rrow = rden
            yb = T(es, nc, "mbyb", [64, TT], BF16)
            S.dma("pool", W[:].rearrange("p (o k m) -> p o k m", o=8, k=8), self.din["wC_T"][l].rearrange("o p k m -> p o k m"), writes=[W.b])
            S.dma("pool", Wv[:].rearrange("p (k m) -> p k m", k=8), self.din["w_v"][l, 1], writes=[Wv.b])
            S.dma("sp", bvb[:], self.din["b_v"][l, 1:2, :].partition_broadcast(128).rearrange("p a b -> p (a b)"), writes=[bvb.b])
            S.op("dve", lambda e: e.memset(kmean[:], 0.0), writes=[kmean.b])
            S.op("pool", lambda e: e.memset(vtok[:], 1.0), writes=[vtok.b])
            for h in range(4):
                S.op("pool", lambda e, h=h: e.memset(kT[h][64:128, :], 0.0), writes=[kT[h].b])
                S.dma("pool", kT[h][64:72, :], self.din["kind"], reads=[kT[h].b], writes=[kT[h].b])
            for i in range(2):
                S.op("pool", lambda e, i=i: e.memset(qT[i][64:128, :], 0.0), writes=[qT[i].b])
            mc3 = lambda which, tt: self.mobac[:, (which * 4 + tt) * 2:(which * 4 + tt) * 2 + 2].rearrange("p (a b) -> p a b", b=1).broadcast_to([8, 2, 256])
            v3 = lambda ap: ap.rearrange("p (a b) -> p a b", b=256)

            def head_prep(tt, h):
                xn = self.xn[tt]
                i = h % 2
                pq = self.ps[1]
                self.proj64(pq, W, (h * 2) * 512, xn)
                S.op("act", lambda e: e.activation(out=q32[i][:], in_=pq[:64, :TT], func=AF.Identity, bias=self.pC[:, l * 8 + h * 2:l * 8 + h * 2 + 1]),
                     reads=[pq.b, self.pC.b], writes=[q32[i].b])
                S.op("act", lambda e: e.copy(out=qT[i][0:64, :], in_=q32[i][:]), reads=[q32[i].b], writes=[qT[i].b])
                pg, pdm = self.ps[4], self.ps[5]
                S.op("pe", lambda e: e.matmul(pg[:8, :TT], lhsT=kmean[:, h * 8:h * 8 + 8], rhs=q32[i][:], start=True, stop=True),
                     reads=[kmean.b, q32[i].b], writes=[pg.b])
                S.op("dve", lambda e: e.tensor_tensor(out=v3(gm[i][:]), in0=v3(pg[:8, :TT]), in1=mc3(0, tt), op=ALU.add),
                     reads=[pg.b, self.mobac.b], writes=[gm[i].b])
                S.op("pe", lambda e: e.matmul(pdm[:56, :TT], lhsT=self.Pm[:, :], rhs=gm[i][:], start=True, stop=True),
                     reads=[self.Pm.b, gm[i].b], writes=[pdm.b])
                S.op("dve", lambda e: e.tensor_single_scalar(out=gt[i][:], in_=pdm[:56, :TT], scalar=0.0, op=ALU.is_gt), reads=[pdm.b], writes=[gt[i].b])
                S.op("pe", lambda e: e.matmul(pg[:8, :TT], lhsT=self.Agg[:, :], rhs=gt[i][:], start=True, stop=True),
                     reads=[self.Agg.b, gt[i].b], writes=[pg.b])
                S.op("dve", lambda e: e.scalar_tensor_tensor(out=v3(tmp8[i][:]), in0=v3(pg[:8, :TT]), scalar=2.5, in1=mc3(1, tt), op0=ALU.is_lt, op1=ALU.mult),
                     reads=[pg.b, self.mobac.b], writes=[tmp8[i].b])
                S.op("dve", lambda e: e.tensor_tensor(out=v3(tmp8[i][:]), in0=v3(tmp8[i][:]), in1=mc3(2, tt), op=ALU.add),
                     reads=[tmp8[i].b, self.mobac.b], writes=[tmp8[i].b])
                S.op("dve", lambda e: e.tensor_scalar(out=nm[i][:], in0=tmp8[i][:], scalar1=BIGRAW, scalar2=None, op0=ALU.mult), reads=[tmp8[i].b], writes=[nm[i].b])
                S.dma("sp", qT[i][64:72, :], nm[i][:], reads=[nm[i].b, qT[i].b], writes=[qT[i].b])
                TBh = TBr[i]
                S.dma("sp", TBh[:], bass.AP(self.tbd, h * 128 * 1152 + 127, [[1151, 128], [1, 1024]]), reads=[self.tbd_b], writes=[TBh.b])

            def head_attn(tt, h):
                i = h % 2
                TBh = TBr[i]
                po, pd = self.ps[6], self.ps[7]
                nj = 4 * tt + 4
                pSr = [self.ps[2], self.ps[3], self.ps[0]]
                def st1(j):
                    pS = pSr[j % 3]
                    S.op("pe", lambda e: e.matmul(pS[:, :TT], lhsT=kT[h][:, j * 128:(j + 1) * 128], rhs=qT[i][:, :], start=True, stop=True),
                         reads=[kT[h].b, qT[i].b], writes=[pS.b])
                    o = tt * 512 - j * 128
                    exj = ex[j % 3]
                    if o <= 128:
                        tS = tmpS[j % 2]
                        S.op("dve", lambda e: e.scalar_tensor_tensor(out=tS[:, :TT], in0=pS[:, :TT], scalar=scale, in1=TBh[:, o + 384:o + 384 + 512],
                                                                      op0=ALU.mult, op1=ALU.add), reads=[pS.b, TBh.b], writes=[tS.b])
                        S.op("act", lambda e: e.activation(out=exj[:], in_=tS[:, :TT], func=AF.Exp), reads=[tS.b], writes=[exj.b])
                    else:
                        S.op("act", lambda e: e.activation(out=exj[:], in_=pS[:, :TT], func=AF.Exp, scale=scale, bias=self.b31[:, h:h + 1]),
                             reads=[pS.b, self.b31.b], writes=[exj.b])

                def st2(j):
                    exj = ex[j % 3]
                    S.op("pe", lambda e: e.matmul(po[:65, :TT], lhsT=vt5[:, j, h, :], rhs=exj[:], start=(j == 0), stop=(j == nj - 1)),
                         reads=[vtok.b, exj.b], writes=[po.b])

                LA = 2
                for j in range(nj + LA):
                    if j < nj:
                        st1(j)
                    if j - LA >= 0:
                        st2(j - LA)
                S.op("act", lambda e: e.activation(out=rrow[64:65, :], in_=po[64:65, :TT], func=AF.Ln), reads=[po.b], writes=[rrow.b])
                S.op("act", lambda e: e.activation(out=rrowb[64:65, :], in_=rrow[64:65, :], func=AF.Exp, scale=-1.0), reads=[rrow.b], writes=[rrowb.b])
                S.op("pe", lambda e: e.matmul(pd[:64, :TT], lhsT=self.ones[64:65, 0:64], rhs=rrowb[64:65, :], start=True, stop=True),
                     reads=[self.ones.b, rrowb.b], writes=[pd.b])
                S.op("act", lambda e: e.copy(out=rden[0:64, :], in_=pd[:64, :TT]), reads=[pd.b], writes=[rden.b])
                S.op("dve", lambda e: e.tensor_tensor(out=yb[:], in0=po[:64, :TT], in1=rden[0:64, :], op=ALU.mult), reads=[po.b, rden.b], writes=[yb.b])
                self.y_store(yT, yb, 4 + h // 2, h, tt)

            for tt in range(NT):
                xn = self.xn[tt]
                for h in range(4):
                    pp = self.ps[h % 2]
                    self.proj64(pp, W, (h * 2 + 1) * 512, xn)
                    S.op("act", lambda e: e.activation(out=k32[:], in_=pp[:64, :TT], func=AF.Identity, bias=self.pC[:, l * 8 + h * 2 + 1:l * 8 + h * 2 + 2]),
                         reads=[pp.b, self.pC.b], writes=[k32.b])
                    S.op("act", lambda e: e.copy(out=kT[h][0:64, tt * TT:(tt + 1) * TT], in_=k32[:]), reads=[k32.b], writes=[kT[h].b])
                    S.op("dve", lambda e: e.reduce_sum(out=kmean[:, h * 8 + 2 * tt:h * 8 + 2 * tt + 2], in_=v3(k32[:]), axis=AX.X),
                         reads=[k32.b], writes=[kmean.b])
                for j in range(4):
                    pv = self.ps[2 + j % 2]
                    for kc in range(KC):
                        S.op("pe", lambda e, kc=kc: e.matmul(pv[:, :256], lhsT=xn[:, kc * TT + j * 128:kc * TT + (j + 1) * 128],
                                                             rhs=Wv[:, kc * 256:(kc + 1) * 256], start=(kc == 0), stop=(kc == KC - 1)),
                             reads=[xn.b, Wv.b], writes=[pv.b], inc=(kc == KC - 1))
                    S.op("dve", lambda e: e.tensor_tensor(out=vt5[:, tt * 4 + j, :, 0:64], in0=pv[:, :256].rearrange("p (h w) -> p h w", h=4),
                                                          in1=bvb[:].rearrange("p (h w) -> p h w", h=4), op=ALU.add),
                         reads=[pv.b, bvb.b], writes=[vtok.b])
                head_prep(tt, 0)
                head_prep(tt, 1)
                head_attn(tt, 0)
                head_prep(tt, 2)
                head_attn(tt, 1)
                head_prep(tt, 3)
                head_attn(tt, 2)
                head_attn(tt, 3)


N_CORES = 8


def kernel(**inputs):
    inp = {k: np.asarray(v) for k, v in inputs.items()}
    B = inp["x"].shape[0]
    nseq = B // N_CORES
    sh = host_prepare(inp, DEPTH)
    sh = host_prepare_mixer(inp, DEPTH, sh)
    in_maps = []
    for c in range(N_CORES):
        core = dict(sh)
        core["xT"] = np.stack([to_T(inp["x"][c * nseq + b]) for b in range(nseq)])
        core["memT"] = np.stack([np.ascontiguousarray(inp["mem"][c * nseq + b].reshape(NMEM, 8, 128).transpose(2, 1, 0))
                                 for b in range(nseq)])
        in_maps.append(core)
    P = ProgM(dict(depth=DEPTH, nseq=nseq))
    nc = P.build({k: v.shape for k, v in in_maps[0].items()})
    res = run_bass_kernel_spmd(nc, in_maps, core_ids=list(range(N_CORES)))
    out = np.empty((B, SEQ, D), np.float32)
    for c in range(N_CORES):
        o = np.asarray(res.results[c]["outT"])
        for b in range(nseq):
            out[c * nseq + b] = from_T(o[b])
    return out
```
